# Optimizing a Trainium2 kernel written in Bass

```python
import jax, jax.numpy as jnp
from jax import lax
import numpy as np

D_MODEL = 2048
BATCH = 2
SEQ = 4096
DEPTH = 1
DEC_BATCH = 128
DEC_SEQ = 1
PAST_LEN = 2048
PAGE_SIZE = 128

N_HEADS = 8
N_KV_HEADS = 2
HEAD_DIM = 128
ATTN_WIDTH = N_HEADS * HEAD_DIM
KV_WIDTH = N_KV_HEADS * HEAD_DIM
IDX_HEADS = 16
IDX_DIM = 64
TOPK_MAX = 256
Q_BLOCK = 128
ROPE_THETA = 10000.0
GLA_HEADS = 4
GLA_DK = 128
GLA_DV = 256
GLA_KEY_WIDTH = GLA_HEADS * GLA_DK
GLA_VAL_WIDTH = GLA_HEADS * GLA_DV
GLA_GATE_RANK = 16
GLA_GATE_TAU = 16.0
GLA_CHUNK = 64
D_FF = 5632
RMS_EPS = 1e-6
N_BRANCH = 2
D_IN = (ATTN_WIDTH + 2 * KV_WIDTH + IDX_HEADS * IDX_DIM + IDX_DIM + IDX_HEADS
        + 2 * GLA_KEY_WIDTH + 2 * GLA_VAL_WIDTH + GLA_GATE_RANK + N_BRANCH * D_MODEL)

kernel_name = "dsa_gla_macaron_sandwich_step"


def _split_points():
    sizes = (ATTN_WIDTH, KV_WIDTH, KV_WIDTH, IDX_HEADS * IDX_DIM, IDX_DIM, IDX_HEADS,
             GLA_KEY_WIDTH, GLA_KEY_WIDTH, GLA_VAL_WIDTH, GLA_GATE_RANK, GLA_VAL_WIDTH,
             D_MODEL, D_MODEL)
    pts, acc = [], 0
    for s in sizes[:-1]:
        acc += s
        pts.append(acc)
    return pts


def rmsnorm(x, w):
    xf = x.astype(jnp.float32)
    y = xf * lax.rsqrt(jnp.mean(xf * xf, axis=-1, keepdims=True) + RMS_EPS)
    return (y * w.astype(jnp.float32)).astype(x.dtype)


def rope(x, pos):
    d = x.shape[-1]
    inv = ROPE_THETA ** (-jnp.arange(0, d, 2, dtype=jnp.float32) / d)
    ang = pos.astype(jnp.float32)[:, None] * inv[None, :]
    cos = jnp.cos(ang)[:, None, :]
    sin = jnp.sin(ang)[:, None, :]
    x1, x2 = jnp.split(x.astype(jnp.float32), 2, axis=-1)
    out = jnp.concatenate([x1 * cos - x2 * sin, x2 * cos + x1 * sin], axis=-1)
    return out.astype(x.dtype)


def ffn_sublayer(h, pre_w, w_gate, w_up, w_down, post_w):
    z = rmsnorm(h, pre_w)
    f = (jax.nn.silu(z @ w_gate) * (z @ w_up)) @ w_down
    return h + 0.5 * rmsnorm(f, post_w)


def sparse_attend(q, qi, wi, qpos, k, v, ki, top):
    n, t = q.shape[:2]
    L = k.shape[1]
    kpos = jnp.arange(L, dtype=jnp.int32)
    s_idx = jnp.einsum('nthd,nld->nthl', qi.astype(jnp.float32), ki.astype(jnp.float32))
    w_sc = wi.astype(jnp.float32) * (IDX_HEADS ** -0.5 * IDX_DIM ** -0.5)
    score = jnp.einsum('nth,nthl->ntl', w_sc, jax.nn.relu(s_idx))
    causal = kpos[None, :] <= qpos[:, None]
    score = jnp.where(causal[None], score, -jnp.inf)
    _, sel = lax.top_k(score, top)
    valid = sel <= qpos[None, :, None]
    kg = jax.vmap(lambda kb, ib: kb[ib])(k, sel)
    vg = jax.vmap(lambda vb, ib: vb[ib])(v, sel)
    qg = q.reshape(n, t, N_KV_HEADS, N_HEADS // N_KV_HEADS, HEAD_DIM)
    logits = jnp.einsum('ntgrd,ntkgd->ntgrk', qg.astype(jnp.float32), kg.astype(jnp.float32))
    logits = logits * (HEAD_DIM ** -0.5)
    logits = jnp.where(valid[:, :, None, None, :], logits, -jnp.inf)
    p = jax.nn.softmax(logits, axis=-1)
    o = jnp.einsum('ntgrk,ntkgd->ntgrd', p.astype(v.dtype), vg)
    return o.reshape(n, t, ATTN_WIDTH).astype(q.dtype)


def gla_chunked(q, k, v, log_a, s0):
    n, t = q.shape[:2]
    c = GLA_CHUNK if t >= GLA_CHUNK else t
    pad = (-t) % c
    f32 = jnp.float32
    def prep(a):
        a = jnp.pad(a.astype(f32), ((0, 0), (0, pad), (0, 0), (0, 0)))
        nc = a.shape[1] // c
        return a.reshape(n, nc, c, *a.shape[2:]).swapaxes(0, 1)
    qs = prep(q * (GLA_DK ** -0.5))
    ks, vs, gs = prep(k), prep(v), prep(log_a)
    tri = jnp.tril(jnp.ones((c, c), dtype=bool))

    def step(S, inp):
        qc, kc, vc, gc = inp
        b = jnp.cumsum(gc, axis=1)
        o_inter = jnp.einsum('nchk,nhkv->nchv', qc * jnp.exp(b), S)
        diff = b[:, :, None] - b[:, None, :]
        decay = jnp.exp(jnp.where(tri[None, :, :, None, None], diff, -jnp.inf))
        A = jnp.einsum('nihk,njhk,nijhk->nhij', qc, kc, decay)
        o_intra = jnp.einsum('nhij,njhv->nihv', A, vc)
        b_last = b[:, -1]
        S_new = (jnp.exp(b_last)[..., None] * S
                 + jnp.einsum('nchk,nchv->nhkv', kc * jnp.exp(b_last[:, None] - b), vc))
        return S_new, o_inter + o_intra

    S_fin, o = lax.scan(step, s0.astype(f32), (qs, ks, vs, gs))
    o = o.swapaxes(0, 1).reshape(n, -1, GLA_HEADS, GLA_DV)[:, :t]
    return o.astype(q.dtype), S_fin.astype(q.dtype)


def decoder_layer(x, pos, attend_fn, s0, lw):
    (ffn1_pre_w, ffn1_w_gate, ffn1_w_up, ffn1_w_down, ffn1_post_w,
     mix_pre_w, w_in, gla_gate_w2, gla_gate_b, gla_norm_w,
     w_proj_attn, w_proj_gla, w_out, mix_post_w,
     ffn2_pre_w, ffn2_w_gate, ffn2_w_up, ffn2_w_down, ffn2_post_w) = lw
    n, t, _ = x.shape
    h = ffn_sublayer(x, ffn1_pre_w, ffn1_w_gate, ffn1_w_up, ffn1_w_down, ffn1_post_w)
    u = rmsnorm(h, mix_pre_w)
    z = u @ w_in
    (q, k, v, qi, ki, wi, gq, gk, gv, g_lr, g_r, g_attn, g_gla) = jnp.split(z, _split_points(), axis=-1)
    q = rope(q.reshape(n, t, N_HEADS, HEAD_DIM), pos)
    k = rope(k.reshape(n, t, N_KV_HEADS, HEAD_DIM), pos)
    v = v.reshape(n, t, N_KV_HEADS, HEAD_DIM)
    qi = rope(qi.reshape(n, t, IDX_HEADS, IDX_DIM), pos)
    ki = rope(ki.reshape(n, t, 1, IDX_DIM), pos)[:, :, 0]
    o_attn = attend_fn(q, qi, wi, k, v, ki)
    log_a = jax.nn.log_sigmoid((g_lr @ gla_gate_w2 + gla_gate_b).astype(jnp.float32)) / GLA_GATE_TAU
    o_g, s_fin = gla_chunked(gq.reshape(n, t, GLA_HEADS, GLA_DK),
                             gk.reshape(n, t, GLA_HEADS, GLA_DK),
                             gv.reshape(n, t, GLA_HEADS, GLA_DV),
                             log_a.reshape(n, t, GLA_HEADS, GLA_DK), s0)
    o_g = rmsnorm(o_g, gla_norm_w) * jax.nn.silu(g_r).reshape(n, t, GLA_HEADS, GLA_DV)
    o_g = o_g.reshape(n, t, GLA_VAL_WIDTH)
    merged = (jax.nn.sigmoid(g_attn) * (o_attn @ w_proj_attn)
              + jax.nn.sigmoid(g_gla) * (o_g @ w_proj_gla))
    h = h + rmsnorm(merged @ w_out, mix_post_w)
    h = ffn_sublayer(h, ffn2_pre_w, ffn2_w_gate, ffn2_w_up, ffn2_w_down, ffn2_post_w)
    return h, k, v, ki, s_fin


def setup_inputs(seed: int = 0) -> dict:
    key = jax.random.key(seed)
    ks = jax.random.split(key, 32)
    f32 = jnp.float32
    def nrm(kk, shape, scale=1.0):
        return jax.random.normal(kk, shape, f32) * scale
    def gain(kk, dim):
        return 1.0 + 0.01 * jax.random.normal(kk, (dim,), f32)
    n_pages = PAST_LEN // PAGE_SIZE
    n_used = DEC_BATCH * n_pages
    n_pool = n_used + max(1, n_used // 4)
    page_table = jax.random.permutation(ks[0], n_pool)[:n_used].reshape(DEC_BATCH, n_pages).astype(jnp.int32)
    return {
        "x_prompt": nrm(ks[1], (BATCH, SEQ, D_MODEL)),
        "x_sample": nrm(ks[2], (DEC_BATCH, DEC_SEQ, D_MODEL)),
        "cache_k": nrm(ks[3], (n_pool, PAGE_SIZE, N_KV_HEADS, HEAD_DIM)),
        "cache_v": nrm(ks[4], (n_pool, PAGE_SIZE, N_KV_HEADS, HEAD_DIM)),
        "cache_kidx": nrm(ks[5], (n_pool, PAGE_SIZE, IDX_DIM)),
        "page_table": page_table,
        "state_gla": nrm(ks[6], (DEC_BATCH, GLA_HEADS, GLA_DK, GLA_DV)),
        "ffn1_pre_w": gain(ks[7], D_MODEL),
        "ffn1_w_gate": nrm(ks[8], (D_MODEL, D_FF), D_MODEL ** -0.5),
        "ffn1_w_up": nrm(ks[9], (D_MODEL, D_FF), D_MODEL ** -0.5),
        "ffn1_w_down": nrm(ks[10], (D_FF, D_MODEL), D_FF ** -0.5),
        "ffn1_post_w": gain(ks[11], D_MODEL),
        "mix_pre_w": gain(ks[12], D_MODEL),
        "w_in": nrm(ks[13], (D_MODEL, D_IN), D_MODEL ** -0.5),
        "gla_gate_w2": nrm(ks[14], (GLA_GATE_RANK, GLA_KEY_WIDTH), GLA_GATE_RANK ** -0.5),
        "gla_gate_b": nrm(ks[15], (GLA_KEY_WIDTH,), 0.1),
        "gla_norm_w": gain(ks[16], GLA_DV),
        "w_proj_attn": nrm(ks[17], (ATTN_WIDTH, D_MODEL), ATTN_WIDTH ** -0.5),
        "w_proj_gla": nrm(ks[18], (GLA_VAL_WIDTH, D_MODEL), GLA_VAL_WIDTH ** -0.5),
        "w_out": nrm(ks[19], (D_MODEL, D_MODEL), D_MODEL ** -0.5),
        "mix_post_w": gain(ks[20], D_MODEL),
        "ffn2_pre_w": gain(ks[21], D_MODEL),
        "ffn2_w_gate": nrm(ks[22], (D_MODEL, D_FF), D_MODEL ** -0.5),
        "ffn2_w_up": nrm(ks[23], (D_MODEL, D_FF), D_MODEL ** -0.5),
        "ffn2_w_down": nrm(ks[24], (D_FF, D_MODEL), D_FF ** -0.5),
        "ffn2_post_w": gain(ks[25], D_MODEL),
    }


def reference(x_prompt, x_sample, cache_k, cache_v, cache_kidx, page_table, state_gla,
              ffn1_pre_w, ffn1_w_gate, ffn1_w_up, ffn1_w_down, ffn1_post_w,
              mix_pre_w, w_in, gla_gate_w2, gla_gate_b, gla_norm_w,
              w_proj_attn, w_proj_gla, w_out, mix_post_w,
              ffn2_pre_w, ffn2_w_gate, ffn2_w_up, ffn2_w_down, ffn2_post_w):
    lw = (ffn1_pre_w, ffn1_w_gate, ffn1_w_up, ffn1_w_down, ffn1_post_w,
          mix_pre_w, w_in, gla_gate_w2, gla_gate_b, gla_norm_w,
          w_proj_attn, w_proj_gla, w_out, mix_post_w,
          ffn2_pre_w, ffn2_w_gate, ffn2_w_up, ffn2_w_down, ffn2_post_w)

    b, s, _ = x_prompt.shape
    pos_p = jnp.arange(s, dtype=jnp.int32)
    top_p = min(TOPK_MAX, s // 4)
    nb = s // Q_BLOCK

    def prompt_attend(q, qi, wi, k, v, ki):
        def blk(a):
            return a.reshape(b, nb, Q_BLOCK, *a.shape[2:]).swapaxes(0, 1)
        pos_b = pos_p.reshape(nb, Q_BLOCK)
        out = lax.map(lambda xs: sparse_attend(xs[0], xs[1], xs[2], xs[3], k, v, ki, top_p),
                      (blk(q), blk(qi), blk(wi), pos_b))
        return out.swapaxes(0, 1).reshape(b, s, ATTN_WIDTH)

    s0_p = jnp.zeros((b, GLA_HEADS, GLA_DK, GLA_DV), jnp.float32)
    y_prompt, k_p, v_p, ki_p, gla_p = decoder_layer(x_prompt, pos_p, prompt_attend, s0_p, lw)

    nd, td, _ = x_sample.shape
    past = page_table.shape[1] * cache_k.shape[1]
    past_k = cache_k[page_table].reshape(nd, past, N_KV_HEADS, HEAD_DIM)
    past_v = cache_v[page_table].reshape(nd, past, N_KV_HEADS, HEAD_DIM)
    past_ki = cache_kidx[page_table].reshape(nd, past, IDX_DIM)
    pos_s = past + jnp.arange(td, dtype=jnp.int32)
    top_s = min(TOPK_MAX, (past + td) // 4)

    def sample_attend(q, qi, wi, k, v, ki):
        k_all = jnp.concatenate([past_k, k], axis=1)
        v_all = jnp.concatenate([past_v, v], axis=1)
        ki_all = jnp.concatenate([past_ki, ki], axis=1)
        return sparse_attend(q, qi, wi, pos_s, k_all, v_all, ki_all, top_s)

    y_sample, k_s, v_s, ki_s, gla_s = decoder_layer(x_sample, pos_s, sample_attend, state_gla, lw)

    return (y_prompt, y_sample, k_p, v_p, ki_p, gla_p, k_s, v_s, ki_s, gla_s)
```

```python
import numpy as np
from contextlib import ExitStack
import concourse.bass as bass
import concourse.mybir as mybir
from concourse.bass_utils import run_bass_kernel_spmd

F32 = mybir.dt.float32
BF16 = mybir.dt.bfloat16
I32 = mybir.dt.int32
ALU = mybir.AluOpType
AF = mybir.ActivationFunctionType
AX = mybir.AxisListType

NCORES = 8
NT = 9
TOK = NT * 128
D = 2048
DFF = 5632
DIN = 9824
EPS = 1e-6
TB = [(0, 512), (512, 512), (1024, 128)]


class Sem:
    def __init__(self, h, uid):
        self.h = h
        self.uid = uid
        self.cnt = 0


class Ev:
    __slots__ = ("sem", "val", "q", "idx")

    def __init__(self, sem, val, q=None, idx=0):
        self.sem = sem
        self.val = val
        self.q = q
        self.idx = idx


class Buf:
    def __init__(self, K=None, dma=False, persist=False):
        self.w = None
        self.r = {}
        self.dsem = K.new_sem("d", persist) if dma else None


class Q:
    def __init__(self, K, eng, name):
        self.K = K
        self.eng = eng
        self.name = name
        self.sem = K.new_sem(name, True)
        self.seen = {}
        self.nins = 0

    def wait(self, *evs):
        for ev in evs:
            if ev is None:
                continue
            if ev.q is self and self.nins - ev.idx >= 4:
                continue
            if self.seen.get(ev.sem.uid, -1) >= ev.val:
                continue
            self.seen[ev.sem.uid] = ev.val
            self.eng.wait_ge(ev.sem.h, ev.val)

    def done(self, ins):
        self.sem.cnt += 1
        self.nins += 1
        ins.then_inc(self.sem.h, 1)
        return Ev(self.sem, self.sem.cnt, self, self.nins)

    def tick(self, n=1):
        self.nins += n

    def pre(self, reads=(), writes=()):
        for b in reads:
            self.wait(b.w)
        for b in writes:
            self.wait(b.w)
            self.wait(*b.r.values())

    def post(self, ev, reads=(), writes=()):
        for b in reads:
            b.r[ev.sem.uid] = ev
        for b in writes:
            b.w = ev
            b.r = {}

    def op(self, fn, reads=(), writes=()):
        self.pre(reads, writes)
        ev = self.done(fn())
        self.post(ev, reads, writes)
        return ev

    def dma(self, out, in_, sem, reads=(), writes=(), **kw):
        for b in reads:
            self.wait(b.w)
        for b in writes:
            if not (b.w is not None and b.w.sem is sem):
                self.wait(b.w)
            self.wait(*b.r.values())
        ins = self.eng.dma_start(out=out, in_=in_, **kw)
        sem.cnt += 16
        ins.then_inc(sem.h, 16)
        self.nins += 1
        ev = Ev(sem, sem.cnt)
        self.post(ev, reads, writes)
        self.K.dma_sems[sem.uid] = sem
        return ev


class Kern:
    def __init__(self):
        self.nc = bass.Bass("TRN2", target_bir_lowering=False)
        self.es = ExitStack()
        self.nsem = 0
        self.dma_sems = {}
        self.free_sems = []
        self.phase_sems = []
        nc = self.nc
        self.pe = Q(self, nc.tensor, "pe")
        self.act = Q(self, nc.scalar, "act")
        self.dve = Q(self, nc.vector, "dve")
        self.pool = Q(self, nc.gpsimd, "pool")
        self.sp = Q(self, nc.sync, "sp")
        self.queues = [self.pe, self.act, self.dve, self.pool, self.sp]

    def new_sem(self, name, persist=False):
        if not persist and self.free_sems:
            s = self.free_sems.pop()
        else:
            self.nsem += 1
            h = self.es.enter_context(self.nc.semaphore(f"{name}{self.nsem}"))
            s = Sem(h, self.nsem)
        if not persist:
            self.phase_sems.append(s)
        return s

    def end_phase(self):
        self.barrier()
        self.free_sems.extend(self.phase_sems)
        self.phase_sems = []

    def barrier(self):
        evs = []
        for q in self.queues:
            if q.sem.cnt > 0:
                evs.append(Ev(q.sem, q.sem.cnt))
        for s in self.dma_sems.values():
            if s.cnt > 0:
                evs.append(Ev(s, s.cnt))
        for q in self.queues:
            q.wait(*evs)

    def dram(self, name, shape, dt, kind="Internal"):
        return self.nc.dram_tensor(name, list(shape), dt, kind=kind)


def ffn_phase(K, src, dst, pre_w, wg, wu, wd, post_w, ident, tag="f1"):
    nc = K.nc
    pe, act, dve, pool, sp = K.pe, K.act, K.dve, K.pool, K.sp
    NG = DFF // 256
    with ExitStack() as es:
        def sb(n, s, d):
            return es.enter_context(nc.sbuf_tensor(tag + n, s, d))

        def ps(n, s, d=F32):
            return es.enter_context(nc.psum_tensor(tag + n, s, d))

        acc = sb("f_acc", [128, NT, D], F32)
        zT = sb("f_zT", [128, 16, TOK], BF16)
        xst = sb("f_xst", [128, D], F32)
        zb = sb("f_zb", [128, D], BF16)
        wbc = sb("f_wbc", [128, D], F32)
        wgb = sb("f_wgb", [128, 2, 16, 256], BF16)
        wub = sb("f_wub", [128, 2, 16, 256], BF16)
        wdb = sb("f_wdb", [128, 3, 2, D], BF16)
        aT = sb("f_aT", [128, 2, 2, TOK], BF16)
        sg = sb("f_sg", [128, 2, 512], BF16)
        st = sb("f_st", [128, 4 * NT], F32)
        ptr = [ps(f"f_ptr{i}", [128, 1024], BF16) for i in range(2)]
        pg = [ps(f"f_pg{i}", [128, 512]) for i in range(2)]
        pu = [ps(f"f_pu{i}", [128, 512]) for i in range(2)]
        pd = [ps(f"f_pd{i}", [128, 512]) for i in range(2)]

        B_xst = Buf(K, dma=True)
        B_zb = Buf()
        B_wbc = Buf(K, dma=True)
        B_st = Buf()
        B_ptr = [Buf(), Buf()]
        B_zT = [Buf() for _ in range(NT)]
        B_wgu = [Buf(K, dma=True) for _ in range(2)]
        B_wd = [Buf(K, dma=True) for _ in range(3)]
        B_aT = [[[Buf() for _ in range(3)] for _ in range(2)] for _ in range(2)]
        B_pg = [Buf(), Buf()]
        B_pu = [Buf(), Buf()]
        B_sg = [Buf(), Buf()]
        B_pd = [Buf(), Buf()]
        B_acc = [Buf(K, dma=True) for _ in range(NT)]

        wg_v = wg.rearrange("(k p) f -> p k f", p=128)
        wu_v = wu.rearrange("(k p) f -> p k f", p=128)
        wd_v = wd.rearrange("(c p) n -> p c n", p=128)

        def load_group(gi):
            s2, s3 = gi % 2, gi % 3
            c0 = gi * 256
            pool.dma(wgb[:, s2], wg_v[:, :, c0:c0 + 256], B_wgu[s2].dsem, writes=[B_wgu[s2]])
            pool.dma(wub[:, s2], wu_v[:, :, c0:c0 + 256], B_wgu[s2].dsem, writes=[B_wgu[s2]])
            pool.dma(wdb[:, s3], wd_v[:, 2 * gi:2 * gi + 2, :], B_wd[s3].dsem, writes=[B_wd[s3]])

        sp.dma(wbc[:], pre_w.rearrange("(o d) -> o d", o=1).to_broadcast([128, D]), B_wbc.dsem, writes=[B_wbc])
        load_group(0)
        dve.op(lambda: nc.vector.memset(st[:], 0.0), writes=[B_st])

        for t in range(NT):
            sp.dma(xst[:], src[t * 128:(t + 1) * 128, :], B_xst.dsem, writes=[B_xst])
            act.op(lambda: nc.scalar.activation(out=zb[:], in_=xst[:], func=AF.Square,
                                                accum_out=st[:, t:t + 1]),
                   reads=[B_xst], writes=[B_zb, B_st])
            act.op(lambda: nc.scalar.activation(out=st[:, NT + t:NT + t + 1], in_=st[:, t:t + 1], func=AF.Sqrt,
                                                scale=1.0 / D, bias=EPS),
                   reads=[B_st], writes=[B_st])
            dve.op(lambda: nc.vector.reciprocal(out=st[:, NT + t:NT + t + 1], in_=st[:, NT + t:NT + t + 1]),
                   reads=[B_st], writes=[B_st])
            dve.op(lambda: nc.vector.scalar_tensor_tensor(out=zb[:], in0=xst[:], scalar=st[:, NT + t:NT + t + 1],
                                                          in1=wbc[:], op0=ALU.mult, op1=ALU.mult),
                   reads=[B_xst, B_st, B_wbc], writes=[B_zb])
            for q4 in range(4):
                sl = q4 % 2
                pe.pre(reads=[B_zb, K.B_ident], writes=[B_ptr[sl]])
                for i in range(4):
                    k = q4 * 4 + i
                    ins = nc.tensor.transpose(ptr[sl][:, i * 128:(i + 1) * 128], zb[:, k * 128:(k + 1) * 128], ident[:])
                    pe.tick()
                ev = pe.done(ins)
                pe.nins -= 1
                pe.post(ev, reads=[B_zb], writes=[B_ptr[sl]])
                src_ap = ptr[sl][:, 0:512].rearrange("p (a b) -> p a b", a=4)
                dst_ap = zT[:, q4 * 4:q4 * 4 + 4, t * 128:(t + 1) * 128]
                if q4 % 2 == 0:
                    act.op(lambda: nc.scalar.copy(out=dst_ap, in_=src_ap), reads=[B_ptr[sl]], writes=[B_zT[t]])
                else:
                    dve.op(lambda: nc.vector.tensor_copy(dst_ap, src_ap), reads=[B_ptr[sl]], writes=[B_zT[t]])

        sp.dma(wbc[:], post_w.rearrange("(o d) -> o d", o=1).to_broadcast([128, D]), B_wbc.dsem, writes=[B_wbc])

        tiles_of_tb = [[0, 1, 2, 3], [4, 5, 6, 7], [8]]
        down_units = []

        def emit_down(gi, t, nb, idx):
            s2, s3 = gi % 2, gi % 3
            tb = t // 4
            sl = idx % 2
            pe.pre(reads=[B_aT[s2][0][tb], B_aT[s2][1][tb], B_wd[s3]], writes=[B_pd[sl]])
            for ci in range(2):
                ins = nc.tensor.matmul(pd[sl][:], lhsT=aT[:, s2, ci, t * 128:(t + 1) * 128],
                                       rhs=wdb[:, s3, ci, nb * 512:(nb + 1) * 512],
                                       start=(ci == 0), stop=(ci == 1))
                pe.tick()
            ev = pe.done(ins)
            pe.nins -= 1
            pe.post(ev, reads=[B_aT[s2][0][tb], B_aT[s2][1][tb], B_wd[s3]], writes=[B_pd[sl]])
            a_ap = acc[:, t, nb * 512:(nb + 1) * 512]
            if gi == 0:
                dve.op(lambda: nc.vector.tensor_copy(a_ap, pd[sl][:]), reads=[B_pd[sl]], writes=[B_acc[t]])
            else:
                dve.op(lambda: nc.vector.tensor_tensor(out=a_ap, in0=a_ap, in1=pd[sl][:], op=ALU.add),
                       reads=[B_pd[sl]], writes=[B_acc[t]])

        didx = 0
        for gi in range(NG):
            s2 = gi % 2
            if gi + 1 < NG:
                load_group(gi + 1)
            step = 0
            for ci in range(2):
                for tbi, (t0, tn) in enumerate(TB):
                    sl = step % 2
                    zdeps = [B_zT[t] for t in tiles_of_tb[tbi]]
                    for (pp, Bp, wb) in ((pg, B_pg, wgb), (pu, B_pu, wub)):
                        pe.pre(reads=zdeps + [B_wgu[s2]], writes=[Bp[sl]])
                        for k in range(16):
                            ins = nc.tensor.matmul(pp[sl][:, 0:tn], lhsT=wb[:, s2, k, ci * 128:(ci + 1) * 128],
                                                   rhs=zT[:, k, t0:t0 + tn], start=(k == 0), stop=(k == 15))
                            pe.tick()
                        ev = pe.done(ins)
                        pe.nins -= 1
                        pe.post(ev, reads=zdeps + [B_wgu[s2]], writes=[Bp[sl]])
                    act.op(lambda: nc.scalar.activation(out=sg[:, sl, 0:tn], in_=pg[sl][:, 0:tn], func=AF.Silu),
                           reads=[B_pg[sl]], writes=[B_sg[sl]])
                    dve.op(lambda: nc.vector.tensor_tensor(out=aT[:, s2, ci, t0:t0 + tn], in0=sg[:, sl, 0:tn],
                                                           in1=pu[sl][:, 0:tn], op=ALU.mult),
                           reads=[B_sg[sl], B_pu[sl]], writes=[B_aT[s2][ci][tbi]])
                    step += 1
                    for _ in range(6):
                        if down_units:
                            g0, t, nb = down_units.pop(0)
                            emit_down(g0, t, nb, didx)
                            didx += 1
            down_units = [(gi, t, nb) for t in range(NT) for nb in range(4)]
        while down_units:
            g0, t, nb = down_units.pop(0)
            emit_down(g0, t, nb, didx)
            didx += 1

        for t in range(NT):
            sp.dma(xst[:], src[t * 128:(t + 1) * 128, :], B_xst.dsem, writes=[B_xst])
            act.op(lambda: nc.scalar.activation(out=zb[:], in_=acc[:, t, :], func=AF.Square,
                                                accum_out=st[:, 2 * NT + t:2 * NT + t + 1]),
                   reads=[B_acc[t]], writes=[B_zb, B_st])
            c = 3 * NT + t
            act.op(lambda: nc.scalar.activation(out=st[:, c:c + 1], in_=st[:, 2 * NT + t:2 * NT + t + 1], func=AF.Sqrt,
                                                scale=4.0 / D, bias=4.0 * EPS),
                   reads=[B_st], writes=[B_st])
            dve.op(lambda: nc.vector.reciprocal(out=st[:, c:c + 1], in_=st[:, c:c + 1]),
                   reads=[B_st], writes=[B_st])
            dve.op(lambda: nc.vector.scalar_tensor_tensor(out=acc[:, t, :], in0=acc[:, t, :], scalar=st[:, c:c + 1],
                                                          in1=wbc[:], op0=ALU.mult, op1=ALU.mult),
                   reads=[B_st, B_wbc, B_acc[t]], writes=[B_acc[t]])
            dve.op(lambda: nc.vector.tensor_tensor(out=acc[:, t, :], in0=acc[:, t, :], in1=xst[:], op=ALU.add),
                   reads=[B_xst, B_acc[t]], writes=[B_acc[t]])
            sp.dma(dst[t * 128:(t + 1) * 128, :], acc[:, t, :], B_acc[t].dsem, reads=[B_acc[t]])
        K.end_phase()


import math
LN_THETA = math.log(10000.0)
PI = math.pi
CQ, CK, CV, CQI, CKI, CWI, CGQ, CGK, CGV, CGLR, CGR, CGA, CGG = (
    0, 1024, 1280, 1536, 2560, 2624, 2640, 3152, 3664, 4688, 4704, 5728, 7776)
AG1_ROWS = 576
SQ = 1.0 / math.sqrt(128.0)


def all_gather(K, src, dst, rbufs, wbufs):
    pool = K.pool
    pool.pre(reads=rbufs, writes=wbufs)
    ins = K.nc.gpsimd.collective_compute("AllGather", ALU.bypass, replica_groups=[[0, 1, 2, 3], [4, 5, 6, 7]],
                                         ins=[src.opt()], outs=[dst.opt()])
    csem = K.new_sem("cc", True)
    ins.then_inc(csem.h)
    csem.cnt += 1
    pool.nins += 1
    ev = Ev(csem, 1)
    for b in rbufs:
        b.r[csem.uid] = ev
    for b in wbufs:
        b.r[csem.uid] = ev
    return ev


def norm_T(K, src, wvec, zT, B_zT, ident, ptr, B_ptr, tag):
    nc = K.nc
    pe, act, dve, pool, sp = K.pe, K.act, K.dve, K.pool, K.sp
    with ExitStack() as es:
        def sb(n, s, d):
            return es.enter_context(nc.sbuf_tensor(tag + n, s, d))
        xst = sb("xst", [128, D], F32)
        zb = sb("zb", [128, D], BF16)
        wbc = sb("wbc", [128, D], F32)
        st = sb("st", [128, 2 * NT], F32)
        B_xst = Buf(K, dma=True)
        B_zb = Buf()
        B_wbc = Buf(K, dma=True)
        B_st = Buf()
        sp.dma(wbc[:], wvec.rearrange("(o d) -> o d", o=1).to_broadcast([128, D]), B_wbc.dsem, writes=[B_wbc])
        dve.op(lambda: nc.vector.memset(st[:], 0.0), writes=[B_st])
        for t in range(NT):
            sp.dma(xst[:], src[t * 128:(t + 1) * 128, :], B_xst.dsem, writes=[B_xst])
            act.op(lambda: nc.scalar.activation(out=zb[:], in_=xst[:], func=AF.Square,
                                                accum_out=st[:, t:t + 1]),
                   reads=[B_xst], writes=[B_zb, B_st])
            act.op(lambda: nc.scalar.activation(out=st[:, NT + t:NT + t + 1], in_=st[:, t:t + 1], func=AF.Sqrt,
                                                scale=1.0 / D, bias=EPS),
                   reads=[B_st], writes=[B_st])
            dve.op(lambda: nc.vector.reciprocal(out=st[:, NT + t:NT + t + 1], in_=st[:, NT + t:NT + t + 1]),
                   reads=[B_st], writes=[B_st])
            dve.op(lambda: nc.vector.scalar_tensor_tensor(out=zb[:], in0=xst[:], scalar=st[:, NT + t:NT + t + 1],
                                                          in1=wbc[:], op0=ALU.mult, op1=ALU.mult),
                   reads=[B_xst, B_st, B_wbc], writes=[B_zb])
            for q4 in range(4):
                sl = q4 % 2
                pe.pre(reads=[B_zb, K.B_ident], writes=[B_ptr[sl]])
                for i in range(4):
                    k = q4 * 4 + i
                    ins = nc.tensor.transpose(ptr[sl][:, i * 128:(i + 1) * 128], zb[:, k * 128:(k + 1) * 128], ident[:])
                ev = pe.done(ins)
                pe.post(ev, reads=[B_zb], writes=[B_ptr[sl]])
                src_ap = ptr[sl][:, 0:512].rearrange("p (a b) -> p a b", a=4)
                dst_ap = zT[:, q4 * 4:q4 * 4 + 4, t * 128:(t + 1) * 128]
                if q4 % 2 == 0:
                    act.op(lambda: nc.scalar.copy(out=dst_ap, in_=src_ap), reads=[B_ptr[sl]], writes=[B_zT[t]])
                else:
                    dve.op(lambda: nc.vector.tensor_copy(dst_ap, src_ap), reads=[B_ptr[sl]], writes=[B_zT[t]])
        K.end_phase()


def phase_a(K, C):
    nc = K.nc
    pe, act, dve, pool, sp = K.pe, K.act, K.dve, K.pool, K.sp
    P = C["P"]
    ident = C["ident"]
    w_in = C["w_in"]
    win_v = w_in.rearrange("(k p) f -> p k f", p=128)
    ag_kT = C["agk_in"]
    ag_kiT = C["agki_in"]
    ag_v = C["agv_in"]
    with ExitStack() as es:
        def sb(n, s, d):
            return es.enter_context(nc.sbuf_tensor("a_" + n, s, d))

        def ps(n, s, d=F32):
            return es.enter_context(nc.psum_tensor("a_" + n, s, d))

        uT = sb("uT", [128, 16, TOK], BF16)
        B_uT = [Buf() for _ in range(NT)]
        ptr = [ps(f"ptr{i}", [128, 1024], BF16) for i in range(2)]
        B_ptr = [Buf(), Buf()]
        pp = [ps(f"pp{i}", [128, 512]) for i in range(2)]
        B_pp = [Buf(), Buf()]
        px = [ps(f"px{i}", [128, 512]) for i in range(2)]
        B_px = [Buf(), Buf()]
        norm_T(K, C["h1"], C["mix_pre_w"], uT, B_uT, ident, ptr, B_ptr, "a1_")

        tabs = sb("tabs", [128, NT, 384], F32)
        es_t = ExitStack()

        def sbt(n, s, d):
            return es_t.enter_context(nc.sbuf_tensor("a_" + n, s, d))
        posv = sbt("posv", [128, NT], F32)
        io = sbt("io", [128, 64], F32)
        inv = sbt("inv", [128, 96], F32)
        ang = sbt("ang", [128, NT, 96], F32)
        kf = sbt("kf", [128, NT, 96], F32)
        kint = sbt("kint", [128, NT, 96], I32)
        sn = sbt("sn", [128, NT, 96], F32)
        cs = sbt("cs", [128, NT, 96], F32)
        B_t = Buf(K, dma=True)
        sp.dma(posv[:], C["posv"], B_t.dsem, writes=[B_t])
        pool.op(lambda: nc.gpsimd.iota(io[:], pattern=[[1, 64]], base=0, channel_multiplier=0,
                                       allow_small_or_imprecise_dtypes=True), writes=[B_t])
        act.op(lambda: nc.scalar.activation(out=inv[:, 0:64], in_=io[:, 0:64], func=AF.Exp,
                                            scale=-2.0 * LN_THETA / 128.0), reads=[B_t], writes=[B_t])
        act.op(lambda: nc.scalar.activation(out=inv[:, 64:96], in_=io[:, 0:32], func=AF.Exp,
                                            scale=-2.0 * LN_THETA / 64.0), reads=[B_t], writes=[B_t])
        for s in range(NT):
            dve.op(lambda: nc.vector.tensor_scalar(out=ang[:, s, :], in0=inv[:], scalar1=posv[:, s:s + 1],
                                                   scalar2=None, op0=ALU.mult), reads=[B_t], writes=[B_t])

        def V(fn):
            return dve.op(fn, reads=[B_t], writes=[B_t])
        V(lambda: nc.vector.tensor_scalar(out=kf[:], in0=ang[:], scalar1=1.0 / (2 * PI), scalar2=None, op0=ALU.mult))
        V(lambda: nc.vector.tensor_copy(kint[:], kf[:]))
        V(lambda: nc.vector.tensor_copy(kf[:], kint[:]))
        V(lambda: nc.vector.scalar_tensor_tensor(out=ang[:], in0=kf[:], scalar=-2 * PI, in1=ang[:],
                                                 op0=ALU.mult, op1=ALU.add))
        act.op(lambda: nc.scalar.activation(out=sn[:], in_=ang[:], func=AF.Sin), reads=[B_t], writes=[B_t])
        V(lambda: nc.vector.tensor_scalar(out=ang[:], in0=ang[:], scalar1=PI / 2, scalar2=None, op0=ALU.add))
        V(lambda: nc.vector.tensor_scalar(out=kf[:], in0=ang[:], scalar1=PI, scalar2=-2 * PI,
                                          op0=ALU.is_gt, op1=ALU.mult))
        V(lambda: nc.vector.tensor_tensor(out=ang[:], in0=ang[:], in1=kf[:], op=ALU.add))
        act.op(lambda: nc.scalar.activation(out=cs[:], in_=ang[:], func=AF.Sin), reads=[B_t], writes=[B_t])
        V(lambda: nc.vector.tensor_copy(tabs[:, :, 0:64], cs[:, :, 0:64]))
        V(lambda: nc.vector.tensor_copy(tabs[:, :, 64:128], cs[:, :, 0:64]))
        V(lambda: nc.vector.tensor_scalar(out=tabs[:, :, 128:192], in0=sn[:, :, 0:64], scalar1=-1.0, scalar2=None,
                                          op0=ALU.mult))
        V(lambda: nc.vector.tensor_copy(tabs[:, :, 192:256], sn[:, :, 0:64]))
        V(lambda: nc.vector.tensor_copy(tabs[:, :, 256:288], cs[:, :, 64:96]))
        V(lambda: nc.vector.tensor_copy(tabs[:, :, 288:320], cs[:, :, 64:96]))
        V(lambda: nc.vector.tensor_scalar(out=tabs[:, :, 320:352], in0=sn[:, :, 64:96], scalar1=-1.0, scalar2=None,
                                          op0=ALU.mult))
        V(lambda: nc.vector.tensor_copy(tabs[:, :, 352:384], sn[:, :, 64:96]))
        B_tabs = B_t
        K.barrier()
        es_t.close()

        wbuf = sb("wbuf", [128, 2, 16, 512], BF16)
        B_w = [Buf(K, dma=True), Buf(K, dma=True)]
        xs = sb("xs", [128, 2, 512], F32)
        B_xs = [Buf(K, dma=True), Buf(K, dma=True)]
        t1 = sb("t1", [128, 2, 512], F32)
        B_t1 = [Buf(), Buf()]
        t2 = sb("t2", [128, 2, 512], F32)
        B_t2 = [Buf(), Buf()]
        ob = sb("ob", [128, 2, 512], BF16)
        B_ob = [Buf(K, dma=True), Buf(K, dma=True)]
        of = sb("of", [128, 2, 320], F32)
        B_of = [Buf(K, dma=True), Buf(K, dma=True)]
        tst = sb("tst", [128, 2, 256], BF16)
        B_tst = [Buf(K, dma=True), Buf(K, dma=True)]
        glrT = sb("glrT", [32, TOK], BF16)
        B_glr = Buf()
        w2b = sb("w2b", [16, 512], BF16)
        negb = sb("negb", [128, 4], F32)
        B_c = Buf(K, dma=True)
        ones = sb("ones", [128, 128], F32)
        eT = sb("eT", [128, 512], F32)
        lT = sb("lT", [128, 512], F32)
        cT = sb("cT", [128, 512], F32)
        B_e, B_l, B_cT = Buf(), Buf(), Buf()
        E1 = sb("E1", [128, TOK], F32)
        E2 = sb("E2", [128, TOK], F32)
        B_E = [Buf(), Buf(), Buf()]
        khT = sb("khT", [128, 512], BF16)
        B_khT = Buf()
        cnt = {"blk": 0, "pp": 0, "xs": 0, "tr": 0, "of": 0, "tst": 0, "px": 0}

        pool.dma(w2b[:], C["gla_gate_w2"], B_c.dsem, writes=[B_c])
        sp.dma(negb[:], C["gla_gate_b"].rearrange("(h p) -> p h", p=128), B_c.dsem, writes=[B_c],
               allow_slow_non_contiguous=True)
        dve.op(lambda: nc.vector.tensor_scalar(out=negb[:], in0=negb[:], scalar1=-1.0, scalar2=None, op0=ALU.mult),
               reads=[B_c], writes=[B_c])
        dve.op(lambda: nc.vector.memset(ones[:], 1.0), writes=[B_c])

        def load_w(pieces):
            slot = cnt["blk"] % 2
            cnt["blk"] += 1
            off = 0
            for (c0, w) in pieces:
                pool.dma(wbuf[:, slot, :, off:off + w], win_v[:, :, c0:c0 + w], B_w[slot].dsem, writes=[B_w[slot]])
                off += w
            return slot

        def mm_tok(slot, t, width):
            i = cnt["pp"] % 2
            cnt["pp"] += 1
            pe.pre(reads=[B_uT[t], B_w[slot]], writes=[B_pp[i]])
            for k in range(16):
                ins = nc.tensor.matmul(pp[i][:, 0:width], lhsT=uT[:, k, t * 128:(t + 1) * 128],
                                       rhs=wbuf[:, slot, k, 0:width], start=(k == 0), stop=(k == 15))
            ev = pe.done(ins)
            pe.post(ev, reads=[B_uT[t], B_w[slot]], writes=[B_pp[i]])
            return i

        def mm_feat(slot, off, m, tbi):
            t0, tn = TB[tbi]
            i = cnt["pp"] % 2
            cnt["pp"] += 1
            deps = [B_uT[t] for t in ([0, 1, 2, 3], [4, 5, 6, 7], [8])[tbi]]
            pe.pre(reads=deps + [B_w[slot]], writes=[B_pp[i]])
            for k in range(16):
                ins = nc.tensor.matmul(pp[i][0:m, 0:tn], lhsT=wbuf[:, slot, k, off:off + m],
                                       rhs=uT[:, k, t0:t0 + tn], start=(k == 0), stop=(k == 15))
            ev = pe.done(ins)
            pe.post(ev, reads=deps + [B_w[slot]], writes=[B_pp[i]])
            return i

        def evac(i, width):
            j = cnt["xs"] % 2
            cnt["xs"] += 1
            act.op(lambda: nc.scalar.copy(out=xs[:, j, 0:width], in_=pp[i][:, 0:width]),
                   reads=[B_pp[i]], writes=[B_xs[j]])
            return j

        def rope(j, c0, hs, nh, t, out_ap, B_out):
            w = nh * hs
            hh = hs // 2
            tb0 = 0 if hs == 128 else 256
            x3 = xs[:, j, c0:c0 + w].rearrange("p (h d) -> p h d", h=nh)
            cosb = tabs[:, t, tb0:tb0 + hs].unsqueeze(1).to_broadcast([128, nh, hs])
            sa = tabs[:, t, tb0 + hs:tb0 + hs + hh].unsqueeze(1).to_broadcast([128, nh, hh])
            sbb = tabs[:, t, tb0 + hs + hh:tb0 + 2 * hs].unsqueeze(1).to_broadcast([128, nh, hh])
            a3 = t1[:, j, 0:w].rearrange("p (h d) -> p h d", h=nh)
            b3 = t2[:, j, 0:w].rearrange("p (h d) -> p h d", h=nh)
            pool.op(lambda: nc.gpsimd.tensor_tensor(out=a3, in0=x3, in1=cosb, op=ALU.mult),
                    reads=[B_xs[j], B_tabs], writes=[B_t1[j]])
            dve.op(lambda: nc.vector.tensor_tensor(out=b3[:, :, 0:hh], in0=x3[:, :, hh:hs], in1=sa, op=ALU.mult),
                   reads=[B_xs[j], B_tabs], writes=[B_t2[j]])
            dve.op(lambda: nc.vector.tensor_tensor(out=b3[:, :, hh:hs], in0=x3[:, :, 0:hh], in1=sbb, op=ALU.mult),
                   reads=[B_xs[j], B_tabs], writes=[B_t2[j]])
            dve.op(lambda: nc.vector.tensor_tensor(out=out_ap, in0=t1[:, j, 0:w], in1=t2[:, j, 0:w], op=ALU.add),
                   reads=[B_t1[j], B_t2[j]], writes=[B_out])

        def transposes(j, nblk, rows, dst_fn, B_dst_fn):
            sl = cnt["tr"] % 2
            cnt["tr"] += 1
            pe.pre(reads=[B_ob[j], K.B_ident], writes=[B_ptr[sl]])
            for b in range(nblk):
                ins = nc.tensor.transpose(ptr[sl][0:rows, b * 128:(b + 1) * 128],
                                          ob[:, j, b * rows:(b + 1) * rows], ident[:])
            ev = pe.done(ins)
            pe.post(ev, reads=[B_ob[j]], writes=[B_ptr[sl]])
            return sl

        for blk in range(2):
            slot = load_w([(CQ + blk * 512, 512)])
            for t in range(NT):
                i = mm_tok(slot, t, 512)
                j = evac(i, 512)
                rope(j, 0, 128, 4, t, ob[:, j, :], B_ob[j])
                sl = transposes(j, 4, 128, None, None)
                act.op(lambda: nc.scalar.copy(out=P["qT"][:, blk * 4:blk * 4 + 4, t * 128:(t + 1) * 128],
                                              in_=ptr[sl][:, 0:512].rearrange("p (a b) -> p a b", a=4)),
                       reads=[B_ptr[sl]], writes=[C["B_qT"]])

        slot = load_w([(CK, 512)])
        for t in range(NT):
            i = mm_tok(slot, t, 512)
            j = evac(i, 512)
            o = cnt["of"] % 2
            cnt["of"] += 1
            rope(j, 0, 128, 2, t, of[:, o, 0:256], B_of[o])
            sp.dma(C["ko"][t * 128:(t + 1) * 128, :], of[:, o, 0:256], B_of[o].dsem, reads=[B_of[o]])
            sp.dma(C["vo"][t * 128:(t + 1) * 128, :], xs[:, j, 256:512], B_xs[j].dsem, reads=[B_xs[j]])
            pool.op(lambda: nc.gpsimd.tensor_copy(ob[:, j, 0:256], of[:, o, 0:256]), reads=[B_of[o]], writes=[B_ob[j]])
            pool.op(lambda: nc.gpsimd.tensor_copy(ob[:, j, 256:512], xs[:, j, 256:512]), reads=[B_xs[j]],
                    writes=[B_ob[j]])
            sl = transposes(j, 2, 128, None, None)
            if t < 8:
                q = cnt["tst"] % 2
                cnt["tst"] += 1
                act.op(lambda: nc.scalar.copy(out=tst[:, q, :], in_=ptr[sl][:, 0:256]), reads=[B_ptr[sl]],
                       writes=[B_tst[q]])
                for g in range(2):
                    sp.dma(ag_kT[g * 128:(g + 1) * 128, t * 64:(t + 1) * 64],
                           tst[:, q, g * 128:(g + 1) * 128].bitcast(F32),
                           B_tst[q].dsem, reads=[B_tst[q]], writes=[C["B_ag1"]])
                sp.dma(ag_v[t * 128:(t + 1) * 128, :], ob[:, j, 256:512].bitcast(F32), B_ob[j].dsem,
                       reads=[B_ob[j]], writes=[C["B_ag1"]])
            else:
                act.op(lambda: nc.scalar.copy(out=P["kT8"][:, :], in_=ptr[sl][:, 0:256]), reads=[B_ptr[sl]],
                       writes=[C["B_s8"]])
                pool.op(lambda: nc.gpsimd.tensor_copy(P["v8"][:, :], ob[:, j, 256:512]), reads=[B_ob[j]],
                        writes=[C["B_s8"]])

        for blk in range(2):
            slot = load_w([(CQI + blk * 512, 512)])
            for t in range(NT):
                i = mm_tok(slot, t, 512)
                j = evac(i, 512)
                rope(j, 0, 64, 8, t, ob[:, j, :], B_ob[j])
                sl = transposes(j, 4, 128, None, None)
                act.op(lambda: nc.scalar.copy(out=P["qiT"][:, blk * 4:blk * 4 + 4, t * 128:(t + 1) * 128],
                                              in_=ptr[sl][:, 0:512].rearrange("p (a b) -> p a b", a=4)),
                       reads=[B_ptr[sl]], writes=[C["B_qiT"]])

        slot = load_w([(CKI, 80)])
        for t in range(NT):
            i = mm_tok(slot, t, 80)
            j = evac(i, 80)
            o = cnt["of"] % 2
            cnt["of"] += 1
            rope(j, 0, 64, 1, t, of[:, o, 256:320], B_of[o])
            sp.dma(C["kio"][t * 128:(t + 1) * 128, :], of[:, o, 256:320], B_of[o].dsem, reads=[B_of[o]])
            dve.op(lambda: nc.vector.tensor_scalar(out=P["wi"][:, t, :], in0=xs[:, j, 64:80], scalar1=1.0 / 32.0,
                                                   scalar2=None, op0=ALU.mult), reads=[B_xs[j]], writes=[C["B_wi"]])
            pool.op(lambda: nc.gpsimd.tensor_copy(ob[:, j, 0:64], of[:, o, 256:320]), reads=[B_of[o]], writes=[B_ob[j]])
            pool.op(lambda: nc.gpsimd.tensor_copy(ob[:, j, 64:128], of[:, o, 256:320]), reads=[B_of[o]],
                    writes=[B_ob[j]])
            sl = transposes(j, 1, 128, None, None)
            if t < 8:
                q = cnt["tst"] % 2
                cnt["tst"] += 1
                act.op(lambda: nc.scalar.copy(out=tst[0:64, q, 0:128], in_=ptr[sl][0:64, 0:128]), reads=[B_ptr[sl]],
                       writes=[B_tst[q]])
                sp.dma(ag_kiT[:, t * 64:(t + 1) * 64], tst[0:64, q, 0:128].bitcast(F32), B_tst[q].dsem,
                       reads=[B_tst[q]], writes=[C["B_ag1"]])
            else:
                act.op(lambda: nc.scalar.copy(out=P["kiT8"][:, :], in_=ptr[sl][:, 0:128]), reads=[B_ptr[sl]],
                       writes=[C["B_s8"]])

        for nm in ("agk", "agki", "agv"):
            all_gather(K, C[nm + "_in"], C[nm + "_out"], [C["B_ag1"]], [C["B_ag1o"]])

        for blk in range(2):
            slot = load_w([(CGV + blk * 512, 512)])
            for t in range(NT):
                i = mm_tok(slot, t, 512)
                act.op(lambda: nc.scalar.copy(out=P["gv"][:, t, blk * 512:(blk + 1) * 512], in_=pp[i][:, :]),
                       reads=[B_pp[i]], writes=[C["B_gv"]])

        slot = load_w([(CGLR, 16)])
        for tbi in range(3):
            t0, tn = TB[tbi]
            i = mm_feat(slot, 0, 16, tbi)
            act.op(lambda: nc.scalar.copy(out=glrT[0:16, t0:t0 + tn], in_=pp[i][0:16, 0:tn]), reads=[B_pp[i]],
                   writes=[B_glr])

        for h in range(4):
            slot = load_w([(CGQ + h * 128, 128), (CGK + h * 128, 128)])
            for tbi in range(3):
                t0, tn = TB[tbi]
                x = cnt["px"] % 2
                cnt["px"] += 1
                pe.op(lambda: nc.tensor.matmul(px[x][:, 0:tn], lhsT=w2b[:, h * 128:(h + 1) * 128],
                                               rhs=glrT[0:16, t0:t0 + tn], start=True, stop=True),
                      reads=[B_glr, B_c], writes=[B_px[x]])
                act.op(lambda: nc.scalar.activation(out=eT[:, 0:tn], in_=px[x][:, 0:tn], func=AF.Exp, scale=-1.0,
                                                    bias=negb[:, h:h + 1]), reads=[B_px[x], B_c], writes=[B_e])
                act.op(lambda: nc.scalar.activation(out=lT[:, 0:tn], in_=eT[:, 0:tn], func=AF.Ln, bias=1.0),
                       reads=[B_e], writes=[B_l])
                if tbi < 2:
                    for q4 in range(4):
                        dve.op(lambda: nc.vector.tensor_tensor_scan(out=cT[:, q4 * 128:(q4 + 1) * 128], data0=ones[:],
                                                                    data1=lT[:, q4 * 128:(q4 + 1) * 128], initial=0.0,
                                                                    op0=ALU.mult, op1=ALU.add),
                               reads=[B_l, B_c], writes=[B_cT])
                    act.op(lambda: nc.scalar.activation(out=E1[:, t0:t0 + tn], in_=cT[:, 0:tn], func=AF.Exp,
                                                        scale=-1.0 / 16.0), reads=[B_cT], writes=[B_E[tbi]])
                    act.op(lambda: nc.scalar.activation(out=E2[:, t0:t0 + tn], in_=cT[:, 0:tn], func=AF.Exp,
                                                        scale=1.0 / 16.0), reads=[B_cT], writes=[B_E[tbi]])
                    dve.op(lambda: nc.vector.tensor_copy(
                        P["dec"][:, h, tbi * 4:tbi * 4 + 4],
                        E1[:, t0:t0 + tn].rearrange("p (a b) -> p a b", a=4)[:, :, 127]),
                        reads=[B_E[tbi]], writes=[C["B_dec"]])
                else:
                    act.op(lambda: nc.scalar.activation(out=E1[:, t0:t0 + tn], in_=lT[:, 0:tn], func=AF.Exp,
                                                        scale=-1.0 / 16.0), reads=[B_l], writes=[B_E[tbi]])
                    dve.op(lambda: nc.vector.tensor_copy(P["dec8"][:, h, :], E1[:, t0:t0 + tn]),
                           reads=[B_E[tbi]], writes=[C["B_dec"]])
            for tbi in range(3):
                t0, tn = TB[tbi]
                i = mm_feat(slot, 0, 128, tbi)
                if tbi < 2:
                    dve.op(lambda: nc.vector.scalar_tensor_tensor(out=P["qgT"][:, h, t0:t0 + tn], in0=pp[i][:, 0:tn],
                                                                  scalar=SQ, in1=E1[:, t0:t0 + tn],
                                                                  op0=ALU.mult, op1=ALU.mult),
                           reads=[B_pp[i], B_E[tbi]], writes=[C["B_qgT"]])
                else:
                    dve.op(lambda: nc.vector.tensor_scalar(out=P["qgT"][:, h, t0:t0 + tn], in0=pp[i][:, 0:tn],
                                                           scalar1=SQ, scalar2=None, op0=ALU.mult),
                           reads=[B_pp[i]], writes=[C["B_qgT"]])
                i = mm_feat(slot, 128, 128, tbi)
                if tbi < 2:
                    dve.op(lambda: nc.vector.tensor_tensor(out=P["kgT"][:, h, t0:t0 + tn], in0=pp[i][:, 0:tn],
                                                           in1=E2[:, t0:t0 + tn], op=ALU.mult),
                           reads=[B_pp[i], B_E[tbi]], writes=[C["B_kgT"]])
                    dve.op(lambda: nc.vector.tensor_tensor(
                        out=khT[:, :].rearrange("p (a b) -> p a b", a=4),
                        in0=P["kgT"][:, h, t0:t0 + tn].rearrange("p (a b) -> p a b", a=4),
                        in1=P["dec"][:, h, tbi * 4:tbi * 4 + 4].unsqueeze(2).to_broadcast([128, 4, 128]),
                        op=ALU.mult), reads=[C["B_kgT"], C["B_dec"]], writes=[B_khT])
                    sl = cnt["tr"] % 2
                    cnt["tr"] += 1
                    pe.pre(reads=[B_khT, K.B_ident], writes=[B_ptr[sl]])
                    for b in range(4):
                        ins = nc.tensor.transpose(ptr[sl][:, b * 128:(b + 1) * 128], khT[:, b * 128:(b + 1) * 128],
                                                  ident[:])
                    ev = pe.done(ins)
                    pe.post(ev, reads=[B_khT], writes=[B_ptr[sl]])
                    act.op(lambda: nc.scalar.copy(out=P["khat"][:, tbi * 4:tbi * 4 + 4, h * 128:(h + 1) * 128],
                                                  in_=ptr[sl][:, 0:512].rearrange("p (a b) -> p a b", a=4)),
                           reads=[B_ptr[sl]], writes=[C["B_khat"]])
                else:
                    act.op(lambda: nc.scalar.copy(out=P["kgT"][:, h, t0:t0 + tn], in_=pp[i][:, 0:tn]),
                           reads=[B_pp[i]], writes=[C["B_kgT"]])
                    dve.op(lambda: nc.vector.tensor_copy(P["kg8f"][:, h, :], pp[i][:, 0:tn]),
                           reads=[B_pp[i]], writes=[C["B_kgT"]])
        K.end_phase()
NIT = 14
TOPK = 256
NEG = -1.0e4


def topk_threshold(K, S3, np_, junk3, st, B_S, B_junk, B_st, pw2, B_c):
    nc = K.nc
    dve = K.dve

    def V(fn, r=(), w=()):
        return dve.op(fn, reads=list(r), writes=list(w))
    V(lambda: nc.vector.tensor_scalar(out=st[0:np_, 8:8 + NIT], in0=pw2[0:np_, 0:NIT], scalar1=st[0:np_, 0:1],
                                      scalar2=None, op0=ALU.mult), r=[B_st, B_c], w=[B_st])
    V(lambda: nc.vector.tensor_scalar(out=st[0:np_, 1:2], in0=st[0:np_, 0:1], scalar1=-1.0, scalar2=None,
                                      op0=ALU.mult), r=[B_st], w=[B_st])
    V(lambda: nc.vector.memset(st[0:np_, 40:40 + NIT], 0.0), r=[B_st], w=[B_st])
    for k in range(NIT):
        V(lambda: nc.vector.tensor_tensor(out=st[0:np_, 2:3], in0=st[0:np_, 1:2], in1=st[0:np_, 8 + k:9 + k],
                                          op=ALU.add), r=[B_st], w=[B_st])
        V(lambda: nc.vector.tensor_scalar(out=junk3, in0=S3, scalar1=st[0:np_, 2:3], scalar2=0.0, op0=ALU.is_ge,
                                          op1=ALU.add, accum_out=st[0:np_, 40 + k:41 + k]),
          r=[B_S, B_st], w=[B_junk, B_st])
        V(lambda: nc.vector.tensor_scalar(out=st[0:np_, 4:5], in0=st[0:np_, 40 + k:41 + k], scalar1=TOPK - 0.5,
                                          scalar2=None, op0=ALU.is_ge), r=[B_st], w=[B_st])
        V(lambda: nc.vector.scalar_tensor_tensor(out=st[0:np_, 1:2], in0=st[0:np_, 4:5], scalar=st[0:np_, 8 + k:9 + k],
                                                 in1=st[0:np_, 1:2], op0=ALU.mult, op1=ALU.add), r=[B_st], w=[B_st])


def phase_b(K, C):
    nc = K.nc
    pe, act, dve, pool, sp = K.pe, K.act, K.dve, K.pool, K.sp
    P = C["P"]
    ident = C["ident"]
    with ExitStack() as es:
        def sb(n, s, d):
            return es.enter_context(nc.sbuf_tensor("b_" + n, s, d))

        def ps(n, s, d=F32):
            return es.enter_context(nc.psum_tensor("b_" + n, s, d))

        kT_all = sb("kT", [128, 2, 4, 1024], BF16)
        kiT2 = sb("kiT2", [128, 4, 1024], BF16)
        V1 = sb("V1", [128, 4, 8, 2, 130], BF16)
        S = sb("S", [128, 4096], F32)
        selm = sb("selm", [128, 4096], BF16)
        selT = sb("selT", [128, 4, 8, 128], BF16)
        diagw = sb("diagw", [128, 16, 128], BF16)
        rh = sb("rh", [128, 3, 512], BF16)
        pex = sb("pex", [128, 2, 512], BF16)
        pm = sb("pm", [128, 2, 512], BF16)
        cmask = sb("cmask", [128, 512], F32)
        pw2 = sb("pw2", [128, 32], F32)
        st = sb("st", [128, 64], F32)
        rec = sb("rec", [128, 8], F32)
        psh = [ps(f"psh{i}", [128, 512]) for i in range(3)]
        psc = [ps(f"psc{i}", [128, 512]) for i in range(2)]
        ptr = [ps(f"ptr{i}", [128, 1024], BF16) for i in range(2)]
        B_kv = Buf(K, dma=True)
        B_S, B_selm, B_selT, B_diagw, B_st, B_c, B_rec = Buf(), Buf(), Buf(), Buf(), Buf(), Buf(K, dma=True), Buf()
        B_rh = [Buf(), Buf(), Buf()]
        B_pex = [Buf(), Buf()]
        B_pm = [Buf(), Buf()]
        B_psh = [Buf(), Buf(), Buf()]
        B_psc = [Buf(), Buf()]
        B_ptr = [Buf(), Buf()]
        cnt = {"h": 0, "c": 0, "r": 0, "e": 0, "t": 0}

        agk, agki, agv = C["agk_out"], C["agki_out"], C["agv_out"]
        for r in range(4):
            for g in range(2):
                sp.dma(kT_all[:, g, r, :].bitcast(F32), agk[r * 256 + g * 128:r * 256 + (g + 1) * 128, :], B_kv.dsem,
                       reads=[C["B_ag1o"]], writes=[B_kv])
            for hf in range(2):
                sp.dma(kiT2[hf * 64:(hf + 1) * 64, r, :].bitcast(F32), agki[r * 64:(r + 1) * 64, :], B_kv.dsem,
                       reads=[C["B_ag1o"]], writes=[B_kv])
            for g in range(2):
                sp.dma(V1[:, r, :, g, 0:128],
                       agv.bitcast(BF16)[r * 1024:(r + 1) * 1024, g * 128:(g + 1) * 128].rearrange(
                           "(s p) c -> p s c", p=128),
                       B_kv.dsem, reads=[C["B_ag1o"]], writes=[B_kv])
        pool.op(lambda: nc.gpsimd.memset(V1[:, :, :, :, 128:130], 1.0), writes=[B_kv])
        sp.dma(cmask[:], C["cmask"], B_c.dsem, writes=[B_c])
        for k in range(NIT):
            dve.op(lambda: nc.vector.memset(pw2[:, k:k + 1], 2.0 ** (-k)), writes=[B_c])

        for s in range(8):
            Lr = (s + 1) * 128
            qs = slice(s * 128, (s + 1) * 128)
            for h in range(16):
                dve.op(lambda: nc.vector.tensor_scalar(out=diagw[:, h, :], in0=ident[:], scalar1=P["wi"][:, s, h:h + 1],
                                                       scalar2=None, op0=ALU.mult),
                       reads=[C["B_wi"], K.B_ident], writes=[B_diagw])
            for r in range(4):
                for c0 in range(0, Lr, 512):
                    cw = min(512, Lr - c0)
                    ci = cnt["c"] % 2
                    cnt["c"] += 1
                    for h in range(16):
                        hi = cnt["h"] % 3
                        cnt["h"] += 1
                        p0 = (h % 2) * 64
                        pe.op(lambda: nc.tensor.matmul(psh[hi][:, 0:cw], lhsT=P["qiT"][p0:p0 + 64, h // 2, qs],
                                                       rhs=kiT2[p0:p0 + 64, r, c0:c0 + cw], start=True, stop=True),
                              reads=[C["B_qiT"], B_kv], writes=[B_psh[hi]])
                        act.op(lambda: nc.scalar.activation(out=rh[:, hi, 0:cw], in_=psh[hi][:, 0:cw], func=AF.Relu),
                               reads=[B_psh[hi]], writes=[B_rh[hi]])
                        pe.op(lambda: nc.tensor.matmul(psc[ci][:, 0:cw], lhsT=diagw[:, h, :], rhs=rh[:, hi, 0:cw],
                                                       start=(h == 0), stop=(h == 15)),
                              reads=[B_diagw, B_rh[hi]], writes=[B_psc[ci]])
                    dve.op(lambda: nc.vector.tensor_copy(S[:, r * Lr + c0:r * Lr + c0 + cw], psc[ci][:, 0:cw]),
                           reads=[B_psc[ci]], writes=[B_S])
            S3 = S[:, 0:4 * Lr]
            dve.op(lambda: nc.vector.tensor_reduce(out=st[:, 32:33], in_=S3, axis=AX.X, op=ALU.max),
                   reads=[B_S], writes=[B_st])
            dve.op(lambda: nc.vector.tensor_reduce(out=st[:, 33:34], in_=S3, axis=AX.X, op=ALU.min),
                   reads=[B_S], writes=[B_st])
            dve.op(lambda: nc.vector.tensor_scalar(out=st[:, 33:34], in0=st[:, 33:34], scalar1=-1.0, scalar2=None,
                                                   op0=ALU.mult), reads=[B_st], writes=[B_st])
            dve.op(lambda: nc.vector.tensor_tensor(out=st[:, 0:1], in0=st[:, 32:33], in1=st[:, 33:34], op=ALU.max),
                   reads=[B_st], writes=[B_st])
            Sl = S3.rearrange("p (r l) -> p r l", r=4)[:, :, s * 128:(s + 1) * 128]
            dve.op(lambda: nc.vector.tensor_tensor(out=Sl, in0=Sl,
                                                   in1=cmask[:, :].rearrange("p (a b) -> p a b", a=4), op=ALU.add),
                   reads=[B_S, B_c], writes=[B_S])
            topk_threshold(K, S3, 128, selm[:, 0:4 * Lr], st, B_S, B_selm, B_st, pw2, B_c)
            dve.op(lambda: nc.vector.tensor_scalar(out=selm[:, 0:4 * Lr], in0=S3, scalar1=st[:, 1:2], scalar2=None,
                                                   op0=ALU.is_ge), reads=[B_S, B_st], writes=[B_selm])
            if s == 7 and "dbg2" in C:
                B_S.dsem = K.new_sem("d")
                B_st.dsem = B_S.dsem
                sp.dma(C["dbg2"][:, 0:4096], S[:], B_S.dsem, reads=[B_S])
                sp.dma(C["dbg2"][:, 4096:4136], st[:, 0:40], B_S.dsem, reads=[B_st])
            for r in range(4):
                for s0 in range(0, s + 1, 4):
                    nb = min(4, s + 1 - s0)
                    ti = cnt["t"] % 2
                    cnt["t"] += 1
                    pe.pre(reads=[B_selm, K.B_ident], writes=[B_ptr[ti]])
                    for b in range(nb):
                        ins = nc.tensor.transpose(ptr[ti][:, b * 128:(b + 1) * 128],
                                                  selm[:, r * Lr + (s0 + b) * 128:r * Lr + (s0 + b + 1) * 128], ident[:])
                    ev = pe.done(ins)
                    pe.post(ev, reads=[B_selm], writes=[B_ptr[ti]])
                    act.op(lambda: nc.scalar.copy(out=selT[:, r, s0:s0 + nb, :],
                                                  in_=ptr[ti][:, 0:nb * 128].rearrange("p (a b) -> p a b", a=nb)),
                           reads=[B_ptr[ti]], writes=[B_selT])
            for g in range(2):
                tiles = [(r, s1) for r in range(4) for s1 in range(s + 1)]
                for n, (r, s1) in enumerate(tiles):
                    hi = cnt["h"] % 3
                    cnt["h"] += 1
                    ei = cnt["e"] % 2
                    cnt["e"] += 1
                    pe.op(lambda: nc.tensor.matmul(psh[hi][:, :], lhsT=kT_all[:, g, r, s1 * 128:(s1 + 1) * 128],
                                                   rhs=P["qT"][:, g * 4:(g + 1) * 4, qs], start=True, stop=True),
                          reads=[B_kv, C["B_qT"]], writes=[B_psh[hi]])
                    act.op(lambda: nc.scalar.activation(out=pex[:, ei, :], in_=psh[hi][:, :], func=AF.Exp, scale=SQ),
                           reads=[B_psh[hi]], writes=[B_pex[ei]])
                    eng = dve if n % 2 == 0 else pool
                    veng = nc.vector if n % 2 == 0 else nc.gpsimd
                    eng.op(lambda: veng.tensor_tensor(
                        out=pm[:, ei, :].rearrange("p (a b) -> p a b", a=4),
                        in0=pex[:, ei, :].rearrange("p (a b) -> p a b", a=4),
                        in1=selT[:, r, s1, :].unsqueeze(1).to_broadcast([128, 4, 128]), op=ALU.mult),
                        reads=[B_pex[ei], B_selT], writes=[B_pm[ei]])
                    pe.pre(reads=[B_pm[ei], B_kv], writes=[B_psc[0], B_psc[1]])
                    for hh in range(4):
                        po = psc[hh // 3][:, (hh % 3) * 129:(hh % 3) * 129 + 129]
                        ins = nc.tensor.matmul(po, lhsT=pm[:, ei, hh * 128:(hh + 1) * 128], rhs=V1[:, r, s1, g, 0:129],
                                               start=(n == 0 and hh % 3 == 0), stop=(n == len(tiles) - 1),
                                               skip_group_check=True)
                    ev = pe.done(ins)
                    pe.post(ev, reads=[B_pm[ei], B_kv], writes=[B_psc[0], B_psc[1]])
                for hh in range(4):
                    po = psc[hh // 3][:, (hh % 3) * 129:(hh % 3) * 129 + 129]
                    hcol = g * 4 + hh
                    dve.op(lambda: nc.vector.reciprocal(out=rec[:, hcol:hcol + 1], in_=po[:, 128:129]),
                           reads=[B_psc[hh // 3]], writes=[B_rec])
                    dve.op(lambda: nc.vector.tensor_scalar(out=P["oat"][:, s, hcol * 128:(hcol + 1) * 128],
                                                           in0=po[:, 0:128], scalar1=rec[:, hcol:hcol + 1],
                                                           scalar2=None, op0=ALU.mult),
                           reads=[B_psc[hh // 3], B_rec], writes=[C["B_oat"]])
        K.end_phase()
def phase_bs(K, C):
    nc = K.nc
    pe, act, dve, pool, sp = K.pe, K.act, K.dve, K.pool, K.sp
    P = C["P"]
    ident = C["ident"]
    NS = 16
    LK = 2049
    with ExitStack() as es:
        def sb(n, s, d):
            return es.enter_context(nc.sbuf_tensor("s_" + n, s, d))
        ptb = sb("ptb", [128, 256], I32)
        iop = sb("iop", [128, 1], I32)
        idx = sb("idx", [128, 256], I32)
        B_idx = Buf(K, dma=True)
        sp.dma(ptb[:], C["pt"].rearrange("i p -> (i p)").rearrange("(o n) -> o n", o=1).to_broadcast([128, 256]),
               B_idx.dsem, writes=[B_idx])
        pool.op(lambda: nc.gpsimd.iota(iop[:], pattern=[[0, 1]], base=0, channel_multiplier=1), writes=[B_idx])
        pool.op(lambda: nc.gpsimd.tensor_scalar(out=idx[:], in0=ptb[:], scalar1=128, scalar2=None, op0=ALU.mult),
                reads=[B_idx], writes=[B_idx])
        pool.op(lambda: nc.gpsimd.tensor_tensor(out=idx[:], in0=idx[:], in1=iop[:].to_broadcast([128, 256]), op=ALU.add),
                reads=[B_idx], writes=[B_idx])

        wperm = sb("wperm", [128, 64], BF16)
        wTp = sb("wTp", [32, 2, 128], BF16)
        B_w = Buf()
        Ssmp = sb("Ssmp", [NS, 2176], F32)
        B_Ss = Buf(K, dma=True)
        selms = sb("selms", [NS, 2176], BF16)
        B_selms = Buf()
        selTs = sb("selTs", [128, 16, 16], BF16)
        selfs = sb("selfs", [1, 16], F32)
        B_selT = Buf()
        st = sb("st", [NS, 64], F32)
        B_st = Buf()
        pw2 = sb("pw2", [NS, 32], F32)
        B_c = Buf()
        for k in range(NIT):
            dve.op(lambda: nc.vector.memset(pw2[:, k:k + 1], 2.0 ** (-k)), writes=[B_c])

        with ExitStack() as es1:
            def sb1(n, s, d):
                return es1.enter_context(nc.sbuf_tensor("s1_" + n, s, d))

            def ps1(n, s, d=F32):
                return es1.enter_context(nc.psum_tensor("s1_" + n, s, d))
            kig = sb1("kig", [128, 2, 16, 128], BF16)
            B_kig = [Buf(K, dma=True), Buf(K, dma=True)]
            kiTs = sb1("kiTs", [128, 2, 2048], BF16)
            B_kiTs = [Buf(), Buf()]
            rh = sb1("rh", [32, 2, 2, 512], BF16)
            B_rh = [Buf(), Buf()]
            srow = sb1("srow", [1, 1, 2176], F32)
            B_srow = [Buf(K, dma=True)] * 2
            ptr = [ps1(f"ptr{i}", [128, 1024], BF16) for i in range(2)]
            B_ptr = [Buf(), Buf()]
            psh = [ps1(f"psh{i}", [128, 512]) for i in range(2)]
            pso = [ps1(f"pso{i}", [128, 512]) for i in range(2)]
            B_psh = [Buf(), Buf()]
            pss = [ps1(f"pss{i}", [128, 512]) for i in range(2)]
            B_pss = [Buf(), Buf()]
            dve.op(lambda: nc.vector.memset(wperm[:], 0.0), writes=[B_w])
            wi8 = P["wi"][:, 8, :].rearrange("p (a b) -> p a b", b=2)
            dve.op(lambda: nc.vector.tensor_copy(wperm[:, 0:8], wi8[:, :, 0]), reads=[C["B_wi"]], writes=[B_w])
            dve.op(lambda: nc.vector.tensor_copy(wperm[:, 32:40], wi8[:, :, 1]), reads=[C["B_wi"]], writes=[B_w])
            for e in range(2):
                pe.op(lambda: nc.tensor.transpose(ptr[e][0:32, 0:128], wperm[:, e * 32:(e + 1) * 32], ident[:]),
                      reads=[B_w, K.B_ident], writes=[B_ptr[e]])
                act.op(lambda: nc.scalar.copy(out=wTp[:, e, :], in_=ptr[e][0:32, 0:128]), reads=[B_ptr[e]], writes=[B_w])
            nt = 1
            nh = 0
            for i in range(NS):
                o = i % 2
                tok = 1024 + i
                pool.pre(reads=[B_idx], writes=[B_kig[o]])
                for pg in range(16):
                    ins = nc.gpsimd.indirect_dma_start(
                        out=kig[:, o, pg, 0:64], out_offset=None, in_=C["cache_kidx"],
                        in_offset=bass.IndirectOffsetOnAxis(ap=idx[:, i * 16 + pg:i * 16 + pg + 1], axis=0))
                    B_kig[o].dsem.cnt += 16
                    ins.then_inc(B_kig[o].dsem.h, 16)
                    pool.nins += 1
                K.dma_sems[B_kig[o].dsem.uid] = B_kig[o].dsem
                ev = Ev(B_kig[o].dsem, B_kig[o].dsem.cnt)
                pool.post(ev, writes=[B_kig[o]])
                dve.op(lambda: nc.vector.tensor_copy(kig[:, o, :, 64:128], kig[:, o, :, 0:64]), reads=[B_kig[o]],
                       writes=[B_kig[o]])
                for q4 in range(4):
                    x = nt % 2
                    nt += 1
                    pe.pre(reads=[B_kig[o], K.B_ident], writes=[B_ptr[x]])
                    for b in range(4):
                        ins = nc.tensor.transpose(ptr[x][:, b * 128:(b + 1) * 128], kig[:, o, q4 * 4 + b, :], ident[:])
                    ev = pe.done(ins)
                    pe.post(ev, reads=[B_kig[o]], writes=[B_ptr[x]])
                    act.op(lambda: nc.scalar.copy(out=kiTs[:, o, q4 * 512:(q4 + 1) * 512], in_=ptr[x][:, 0:512]),
                           reads=[B_ptr[x]], writes=[B_kiTs[o]])
                for c in range(5):
                    c0 = c * 512
                    cw = 512 if c < 4 else 1
                    x = nh % 2
                    nh += 1
                    if c < 4:
                        rhs_e, rhs_o = kiTs[0:64, o, c0:c0 + cw], kiTs[64:128, o, c0:c0 + cw]
                        deps = [B_kiTs[o]]
                    else:
                        rhs_e, rhs_o = P["kiT8"][0:64, i:i + 1], P["kiT8"][64:128, i:i + 1]
                        deps = [C["B_s8"]]
                    pe.pre(reads=deps + [C["B_qiT"]], writes=[B_psh[x]])
                    nc.tensor.matmul(psh[x][0:8, 0:cw], lhsT=P["qiT"][0:64, :, tok], rhs=rhs_e, start=True, stop=True)
                    ins = nc.tensor.matmul(pso[x][0:8, 0:cw], lhsT=P["qiT"][64:128, :, tok], rhs=rhs_o, start=True,
                                           stop=True)
                    ev = pe.done(ins)
                    pe.post(ev, reads=deps + [C["B_qiT"]], writes=[B_psh[x]])
                    act.op(lambda: nc.scalar.activation(out=rh[0:8, x, 0, 0:cw], in_=psh[x][0:8, 0:cw], func=AF.Relu),
                           reads=[B_psh[x]], writes=[B_rh[x]])
                    act.op(lambda: nc.scalar.activation(out=rh[0:8, x, 1, 0:cw], in_=pso[x][0:8, 0:cw], func=AF.Relu),
                           reads=[B_psh[x]], writes=[B_rh[x]])
                    pe.pre(reads=[B_rh[x], B_w], writes=[B_pss[x]])
                    nc.tensor.matmul(pss[x][0:1, 0:cw], lhsT=wTp[0:8, 0, i:i + 1], rhs=rh[0:8, x, 0, 0:cw], start=True,
                                     stop=False)
                    ins = nc.tensor.matmul(pss[x][0:1, 0:cw], lhsT=wTp[0:8, 1, i:i + 1], rhs=rh[0:8, x, 1, 0:cw],
                                           start=False, stop=True)
                    ev = pe.done(ins)
                    pe.post(ev, reads=[B_rh[x], B_w], writes=[B_pss[x]])
                    dve.op(lambda: nc.vector.tensor_copy(srow[0:1, 0, c0:c0 + cw], pss[x][0:1, 0:cw]), reads=[B_pss[x]],
                           writes=[B_srow[o]])
                sp.dma(C["sscr"][i:i + 1, 0:LK], srow[0:1, 0, 0:LK], B_srow[o].dsem, reads=[B_srow[o]],
                       writes=[C["B_sscr"]])
            K.barrier()
        sp.dma(Ssmp[:, 0:LK], C["sscr"][:, 0:LK], B_Ss.dsem, reads=[C["B_sscr"]], writes=[B_Ss])
        S3 = Ssmp[:, 0:LK]
        dve.op(lambda: nc.vector.tensor_reduce(out=st[:, 32:33], in_=S3, axis=AX.X, op=ALU.max), reads=[B_Ss],
               writes=[B_st])
        dve.op(lambda: nc.vector.tensor_reduce(out=st[:, 33:34], in_=S3, axis=AX.X, op=ALU.min), reads=[B_Ss],
               writes=[B_st])
        dve.op(lambda: nc.vector.tensor_scalar(out=st[:, 33:34], in0=st[:, 33:34], scalar1=-1.0, scalar2=None,
                                               op0=ALU.mult), reads=[B_st], writes=[B_st])
        dve.op(lambda: nc.vector.tensor_tensor(out=st[:, 0:1], in0=st[:, 32:33], in1=st[:, 33:34], op=ALU.max),
               reads=[B_st], writes=[B_st])
        topk_threshold(K, S3, NS, selms[:, 0:LK], st, B_Ss, B_selms, B_st, pw2, B_c)
        dve.op(lambda: nc.vector.tensor_scalar(out=selms[:, 0:LK], in0=S3, scalar1=st[:, 1:2], scalar2=None,
                                               op0=ALU.is_ge), reads=[B_Ss, B_st], writes=[B_selms])

        with ExitStack() as es2:
            def sb2(n, s, d):
                return es2.enter_context(nc.sbuf_tensor("s2_" + n, s, d))

            def ps2(n, s, d=F32):
                return es2.enter_context(nc.psum_tensor("s2_" + n, s, d))
            Kg = sb2("Kg", [128, 2, 16, 256], BF16)
            B_Kg = [Buf(K, dma=True), Buf(K, dma=True)]
            Vc = sb2("Vc", [128, 1, 16, 256], BF16)
            B_Vc = [Buf(K, dma=True)] * 2
            Vg = sb2("Vg", [128, 2, 16, 2, 130], BF16)
            B_Vg = [Buf(), Buf()]
            kTs = sb2("kTs", [128, 2, 2, 2048], BF16)
            B_kTs = [Buf(), Buf()]
            vself = sb2("vself", [1, 16, 2, 130], BF16)
            B_vs = Buf(K, dma=True)
            pTs = sb2("pTs", [128, 2, 128], BF16)
            B_pTs = [Buf(), Buf()]
            pms = sb2("pms", [128, 2, 128], BF16)
            B_pms = [Buf(), Buf()]
            pself = sb2("pself", [1, 2, 8], BF16)
            pselfm = sb2("pselfm", [1, 2, 8], BF16)
            B_pself = [Buf(), Buf()]
            osm = sb2("osm", [4, 2, 2, 128], F32)
            B_osm = [Buf(K, dma=True), Buf(K, dma=True)]
            rec = sb2("rec", [4, 4], F32)
            B_rec = Buf()
            ptr = [ps2(f"ptr{i}", [128, 1024], BF16) for i in range(2)]
            B_ptr = [Buf(), Buf()]
            pl = [ps2(f"pl{i}", [128, 512]) for i in range(2)]
            B_pl = [Buf(), Buf()]
            psf = ps2("psf", [128, 512])
            B_psf = Buf()
            pos = [ps2(f"pos{i}", [128, 512]) for i in range(2)]
            B_pos = [Buf(), Buf()]
            pe.pre(reads=[B_selms, K.B_ident], writes=[B_ptr[0]])
            for pg in range(16):
                ins = nc.tensor.transpose(ptr[0][:, pg * 16:(pg + 1) * 16], selms[0:NS, pg * 128:(pg + 1) * 128],
                                          ident[0:NS, 0:NS])
            ev = pe.done(ins)
            pe.post(ev, reads=[B_selms], writes=[B_ptr[0]])
            act.op(lambda: nc.scalar.copy(out=selTs[:].rearrange("p a b -> p (a b)"), in_=ptr[0][:, 0:256]),
                   reads=[B_ptr[0]], writes=[B_selT])
            pe.op(lambda: nc.tensor.transpose(ptr[1][0:1, 0:NS], selms[0:NS, 2048:2049], ident[0:NS, 0:NS]),
                  reads=[B_selms, K.B_ident], writes=[B_ptr[1]])
            act.op(lambda: nc.scalar.copy(out=selfs[0:1, :], in_=ptr[1][0:1, 0:NS]), reads=[B_ptr[1]], writes=[B_selT])
            pool.op(lambda: nc.gpsimd.memset(vself[:], 1.0), writes=[B_vs])
            pool.dma(vself[0:1, :, :, 0:128], C["vo"][1024:1040, :].rearrange("(o i) (g d) -> o i g d", o=1, g=2),
                     B_vs.dsem, writes=[B_vs])
            for o in range(2):
                pool.op(lambda: nc.gpsimd.memset(Vg[:, o, :, :, 128:130], 1.0), writes=[B_Vg[o]])
            nt = 0
            for i in range(NS):
                o = i % 2
                tok = 1024 + i
                for (dst, Bd, srcc, oo) in ((Kg, B_Kg, C["cache_k"], o), (Vc, B_Vc, C["cache_v"], 0)):
                    pool.pre(reads=[B_idx], writes=[Bd[o]])
                    for pg in range(16):
                        ins = nc.gpsimd.indirect_dma_start(
                            out=dst[:, oo, pg, :], out_offset=None, in_=srcc,
                            in_offset=bass.IndirectOffsetOnAxis(ap=idx[:, i * 16 + pg:i * 16 + pg + 1], axis=0))
                        Bd[o].dsem.cnt += 16
                        ins.then_inc(Bd[o].dsem.h, 16)
                        pool.nins += 1
                    K.dma_sems[Bd[o].dsem.uid] = Bd[o].dsem
                    ev = Ev(Bd[o].dsem, Bd[o].dsem.cnt)
                    pool.post(ev, writes=[Bd[o]])
                act.op(lambda: nc.scalar.copy(out=Vg[:, o, :, :, 0:128],
                                              in_=Vc[:, 0, :, :].rearrange("p a (g d) -> p a g d", g=2)),
                       reads=[B_Vc[o]], writes=[B_Vg[o]])
                for pg4 in range(8):
                    x = nt % 2
                    nt += 1
                    pe.pre(reads=[B_Kg[o], K.B_ident], writes=[B_ptr[x]])
                    for b in range(4):
                        pg, g = (pg4 * 4 + b) // 2, (pg4 * 4 + b) % 2
                        ins = nc.tensor.transpose(ptr[x][:, b * 128:(b + 1) * 128], Kg[:, o, pg, g * 128:(g + 1) * 128],
                                                  ident[:])
                    ev = pe.done(ins)
                    pe.post(ev, reads=[B_Kg[o]], writes=[B_ptr[x]])
                    dstv = kTs[:, o, :, pg4 * 256:(pg4 + 1) * 256].rearrange("p g (a l) -> p a g l", a=2)
                    srcv = ptr[x][:, 0:512].rearrange("p (a g l) -> p a g l", a=2, g=2)
                    eng, veng = (act, None) if pg4 % 2 == 0 else (dve, None)
                    if pg4 % 2 == 0:
                        act.op(lambda: nc.scalar.copy(out=dstv, in_=srcv), reads=[B_ptr[x]], writes=[B_kTs[o]])
                    else:
                        dve.op(lambda: nc.vector.tensor_copy(dstv, srcv), reads=[B_ptr[x]], writes=[B_kTs[o]])
                pe.pre(reads=[B_kTs[o], C["B_qT"]], writes=[B_pl[o]])
                for pg in range(16):
                    for g in range(2):
                        ins = nc.tensor.matmul(pl[o][:, (pg * 2 + g) * 4:(pg * 2 + g) * 4 + 4],
                                               lhsT=kTs[:, o, g, pg * 128:(pg + 1) * 128],
                                               rhs=P["qT"][:, g * 4:(g + 1) * 4, tok], start=True, stop=True,
                                               skip_group_check=True)
                ev = pe.done(ins)
                pe.post(ev, reads=[B_kTs[o], C["B_qT"]], writes=[B_pl[o]])
                pe.pre(reads=[C["B_s8"], C["B_qT"]], writes=[B_psf])
                for g in range(2):
                    ins = nc.tensor.matmul(psf[0:1, g * 4:(g + 1) * 4], lhsT=P["kT8"][:, g * 128 + i:g * 128 + i + 1],
                                           rhs=P["qT"][:, g * 4:(g + 1) * 4, tok], start=True, stop=True,
                                           skip_group_check=True)
                ev = pe.done(ins)
                pe.post(ev, reads=[C["B_s8"], C["B_qT"]], writes=[B_psf])
                act.op(lambda: nc.scalar.activation(out=pTs[:, o, :], in_=pl[o][:, 0:128], func=AF.Exp, scale=SQ),
                       reads=[B_pl[o]], writes=[B_pTs[o]])
                act.op(lambda: nc.scalar.activation(out=pself[0:1, o, :], in_=psf[0:1, 0:8], func=AF.Exp, scale=SQ),
                       reads=[B_psf], writes=[B_pself[o]])
                dve.op(lambda: nc.vector.tensor_tensor(
                    out=pms[:, o, :].rearrange("p (a b) -> p a b", a=16),
                    in0=pTs[:, o, :].rearrange("p (a b) -> p a b", a=16),
                    in1=selTs[:, :, i].unsqueeze(2).to_broadcast([128, 16, 8]), op=ALU.mult),
                    reads=[B_pTs[o], B_selT], writes=[B_pms[o]])
                dve.op(lambda: nc.vector.tensor_scalar(out=pselfm[0:1, o, :], in0=pself[0:1, o, :],
                                                       scalar1=selfs[0:1, i:i + 1], scalar2=None, op0=ALU.mult),
                       reads=[B_pself[o], B_selT], writes=[B_pself[o]])
                for g in range(2):
                    pe.pre(reads=[B_pms[o], B_Vg[o], B_pself[o], B_vs], writes=[B_pos[g]])
                    for pg in range(16):
                        nc.tensor.matmul(pos[g][0:4, 0:129], lhsT=pms[:, o, (pg * 2 + g) * 4:(pg * 2 + g) * 4 + 4],
                                         rhs=Vg[:, o, pg, g, 0:129], start=(pg == 0), stop=False)
                    ins = nc.tensor.matmul(pos[g][0:4, 0:129], lhsT=pselfm[0:1, o, g * 4:(g + 1) * 4],
                                           rhs=vself[0:1, i, g, 0:129], start=False, stop=True)
                    ev = pe.done(ins)
                    pe.post(ev, reads=[B_pms[o], B_Vg[o], B_pself[o], B_vs], writes=[B_pos[g]])
                    dve.op(lambda: nc.vector.reciprocal(out=rec[0:4, g:g + 1], in_=pos[g][0:4, 128:129]),
                           reads=[B_pos[g]], writes=[B_rec])
                    dve.op(lambda: nc.vector.tensor_scalar(out=osm[0:4, o, g, :], in0=pos[g][0:4, 0:128],
                                                           scalar1=rec[0:4, g:g + 1], scalar2=None, op0=ALU.mult),
                           reads=[B_pos[g], B_rec], writes=[B_osm[o]])
                    sp.dma(C["oscr"][i, g * 512:(g + 1) * 512].rearrange("(h d) -> h d", h=4), osm[0:4, o, g, :],
                           B_osm[o].dsem, reads=[B_osm[o]], writes=[C["B_oscr"]])
            K.barrier()
        pool.dma(P["oat"][0:NS, 8, :], C["oscr"][:, :], C["B_oat"].dsem, reads=[C["B_oscr"]], writes=[C["B_oat"]])
        K.end_phase()
def phase_g1(K, C):
    nc = K.nc
    pe, act, dve, pool, sp = K.pe, K.act, K.dve, K.pool, K.sp
    P = C["P"]
    with ExitStack() as es:
        def sb(n, s, d):
            return es.enter_context(nc.sbuf_tensor("g1_" + n, s, d))

        def ps(n, s, d=F32):
            return es.enter_context(nc.psum_tensor("g1_" + n, s, d))
        triu = sb("triu", [128, 128], F32)
        B_tri = Buf()
        sst = sb("sst", [128, 2, 4, 256], F32)
        B_sst = [Buf(K, dma=True), Buf(K, dma=True)]
        pa = [ps(f"pa{i}", [128, 512]) for i in range(2)]
        B_pa = [Buf(), Buf()]
        pl = [ps(f"pl{i}", [128, 512]) for i in range(2)]
        B_pl = [Buf(), Buf()]
        pool.op(lambda: nc.gpsimd.memset(triu[:], 1.0), writes=[B_tri])
        pool.op(lambda: nc.gpsimd.affine_select(out=triu[:], in_=triu[:], pattern=[[1, 128]], compare_op=ALU.is_ge,
                                                fill=0.0, base=0, channel_multiplier=-1),
                reads=[B_tri], writes=[B_tri])
        sp.dma(C["dec_in"], P["dec"][:].rearrange("p h t -> p (h t)"), C["B_dec"].dsem, reads=[C["B_dec"]],
               writes=[C["B_decin"]])
        all_gather(K, C["dec_in"], C["dec_out"], [C["B_decin"]], [C["B_deco"]])
        n = 0
        for t in range(8):
            ts = slice(t * 128, (t + 1) * 128)
            o = t % 2
            for h in range(4):
                i = n % 2
                n += 1
                pe.op(lambda: nc.tensor.matmul(pa[i][:, 0:128], lhsT=P["kgT"][:, h, ts], rhs=P["qgT"][:, h, ts],
                                               start=True, stop=True),
                      reads=[C["B_kgT"], C["B_qgT"]], writes=[B_pa[i]])
                dve.op(lambda: nc.vector.tensor_tensor(out=P["AT"][:, t, h, :], in0=pa[i][:, 0:128], in1=triu[:],
                                                       op=ALU.mult), reads=[B_pa[i], B_tri], writes=[C["B_AT"]])
                pe.op(lambda: nc.tensor.matmul(pl[i][:, 0:256], lhsT=P["khat"][:, t, h * 128:(h + 1) * 128],
                                               rhs=P["gv"][:, t, h * 256:(h + 1) * 256], start=True, stop=True),
                      reads=[C["B_khat"], C["B_gv"]], writes=[B_pl[i]])
                act.op(lambda: nc.scalar.copy(out=sst[:, o, h, :], in_=pl[i][:, 0:256]), reads=[B_pl[i]],
                       writes=[B_sst[o]])
            sp.dma(C["gst_in"][t].rearrange("(h p) v -> p h v", p=128), sst[:, o], B_sst[o].dsem, reads=[B_sst[o]],
                   writes=[C["B_gin"][t]])
            all_gather(K, C["gst_in"][t], C["gst_out"][t], [C["B_gin"][t]], [C["B_gout"][t]])
        K.end_phase()


def phase_g2(K, C):
    nc = K.nc
    pe, act, dve, pool, sp = K.pe, K.act, K.dve, K.pool, K.sp
    P = C["P"]
    ident = C["ident"]
    with ExitStack() as es:
        def sb(n, s, d):
            return es.enter_context(nc.sbuf_tensor("g2_" + n, s, d))

        def ps(n, s, d=F32):
            return es.enter_context(nc.psum_tensor("g2_" + n, s, d))
        Sg = sb("Sg", [128, 4, 4, 256], F32)
        B_Sg = Buf(K, dma=True)
        dg = sb("dg", [128, 4, 32], F32)
        B_dg = Buf(K, dma=True)
        oh = sb("oh", [128, 4], F32)
        gnw = sb("gnw", [128, 256], F32)
        B_c = Buf(K, dma=True)
        Srun = sb("Srun", [128, 4, 256], F32)
        B_run = Buf(K, dma=True)
        Sin = sb("Sin", [128, 4, 256], F32)
        B_in = Buf()
        Sinb = sb("Sinb", [128, 4, 256], BF16)
        B_inb = Buf()
        st = sb("st", [128, 16], F32)
        B_st = Buf()
        junk = sb("junk", [128, 256], BF16)
        B_junk = Buf()
        po = [ps(f"po{i}", [128, 512]) for i in range(2)]
        B_po = [Buf(), Buf()]

        sp.dma(oh[:], C["onehot"], B_c.dsem, writes=[B_c])
        sp.dma(gnw[:], C["gla_norm_w"].rearrange("(o d) -> o d", o=1).to_broadcast([128, 256]), B_c.dsem, writes=[B_c])
        sp.dma(dg[:], C["dec_out"].rearrange("(r p) c -> p r c", p=128), B_dg.dsem, reads=[C["B_deco"]], writes=[B_dg])
        dve.op(lambda: nc.vector.memset(Srun[:], 0.0), writes=[B_run])

        def head_norm(pz, B_pz, np_, out_ap, B_out):
            dve.op(lambda: nc.vector.memset(st[0:np_, 0:1], 0.0), writes=[B_st])
            act.op(lambda: nc.scalar.activation(out=junk[0:np_, :], in_=pz, func=AF.Square,
                                                accum_out=st[0:np_, 0:1]), reads=[B_pz, B_st], writes=[B_junk, B_st])
            act.op(lambda: nc.scalar.activation(out=st[0:np_, 1:2], in_=st[0:np_, 0:1], func=AF.Sqrt,
                                                scale=1.0 / 256.0, bias=EPS), reads=[B_st], writes=[B_st])
            dve.op(lambda: nc.vector.reciprocal(out=st[0:np_, 1:2], in_=st[0:np_, 1:2]), reads=[B_st], writes=[B_st])
            dve.op(lambda: nc.vector.scalar_tensor_tensor(out=out_ap, in0=pz, scalar=st[0:np_, 1:2],
                                                          in1=gnw[0:np_, :], op0=ALU.mult, op1=ALU.mult),
                   reads=[B_pz, B_st, B_c], writes=[B_out])

        n = 0
        for s in range(8):
            ts = slice(s * 128, (s + 1) * 128)
            sp.dma(Sg[:], C["gst_out"][s].rearrange("(r h p) v -> p r h v", p=128, h=4), B_Sg.dsem,
                   reads=[C["B_gout"][s]], writes=[B_Sg])
            dve.op(lambda: nc.vector.memset(Sin[:], 0.0), reads=[], writes=[B_in])
            for r in range(4):
                dve.op(lambda: nc.vector.scalar_tensor_tensor(out=Sin[:], in0=Srun[:], scalar=oh[:, r:r + 1],
                                                              in1=Sin[:], op0=ALU.mult, op1=ALU.add),
                       reads=[B_run, B_c, B_in], writes=[B_in])
                for h in range(4):
                    dve.op(lambda: nc.vector.scalar_tensor_tensor(out=Srun[:, h, :], in0=Srun[:, h, :],
                                                                  scalar=dg[:, r, h * 8 + s:h * 8 + s + 1],
                                                                  in1=Sg[:, r, h, :], op0=ALU.mult, op1=ALU.add),
                           reads=[B_run, B_dg, B_Sg], writes=[B_run])
            act.op(lambda: nc.scalar.copy(out=Sinb[:], in_=Sin[:]), reads=[B_in], writes=[B_inb])
            for h in range(4):
                i = n % 2
                n += 1
                pe.pre(reads=[C["B_AT"], C["B_gv"], C["B_qgT"], B_inb], writes=[B_po[i]])
                nc.tensor.matmul(po[i][:, 0:256], lhsT=P["AT"][:, s, h, :], rhs=P["gv"][:, s, h * 256:(h + 1) * 256],
                                 start=True, stop=False)
                ins = nc.tensor.matmul(po[i][:, 0:256], lhsT=P["qgT"][:, h, ts], rhs=Sinb[:, h, :],
                                       start=False, stop=True)
                ev = pe.done(ins)
                pe.post(ev, reads=[C["B_AT"], C["B_gv"], C["B_qgT"], B_inb], writes=[B_po[i]])
                head_norm(po[i][:, 0:256], B_po[i], 128, P["og"][:, s, h * 256:(h + 1) * 256], C["B_og"])
        sp.dma(C["gla_p"].rearrange("(h p) v -> p h v", p=128), Srun[:], B_run.dsem, reads=[B_run])

        S0 = sb("S0", [128, 2, 4, 256], F32)
        B_S0 = [Buf(K, dma=True), Buf(K, dma=True)]
        Sn = sb("Sn", [128, 2, 4, 256], F32)
        B_Sn = [Buf(K, dma=True), Buf(K, dma=True)]
        Snb = sb("Snb", [128, 2, 4, 256], BF16)
        B_Snb = [Buf(), Buf()]
        tmp = sb("tmp", [128, 2, 256], F32)
        B_tmp = [Buf(), Buf()]
        Bsel = sb("Bsel", [128, 16, 128], BF16)
        I16 = sb("I16", [128, 16, 16], BF16)
        Qpad = sb("Qpad", [128, 4, 16, 16], BF16)
        B_q = Buf()
        pb = [ps(f"pb{i}", [128, 512]) for i in range(2)]
        B_pb = [Buf(), Buf()]
        pso = [ps(f"pso{i}", [128, 512]) for i in range(2)]
        B_pso = Buf()
        dve.op(lambda: nc.vector.tensor_copy(Bsel[:], ident[:, 0:16].unsqueeze(2).to_broadcast([128, 16, 128])),
               reads=[K.B_ident], writes=[B_q])
        dve.op(lambda: nc.vector.memset(I16[:], 0.0), writes=[B_q])
        for i in range(16):
            dve.op(lambda: nc.vector.memset(I16[:, i, i:i + 1], 1.0), writes=[B_q])
        for h in range(4):
            dve.op(lambda: nc.vector.tensor_tensor(out=Qpad[:, h], in0=P["qgT"][:, h, 1024:1040].unsqueeze(2).to_broadcast(
                [128, 16, 16]), in1=I16[:], op=ALU.mult), reads=[C["B_qgT"], B_q], writes=[B_q])
        stin = C["state_in"].rearrange("(i h p) v -> i p h v", p=128, h=4)
        stout = C["gla_s"].rearrange("(i h p) v -> i p h v", p=128, h=4)
        pe.pre(writes=[B_pso])
        for i in range(16):
            o = i % 2
            sp.dma(S0[:, o], stin[i], B_S0[o].dsem, writes=[B_S0[o]])
            for h in range(4):
                x = n % 2
                n += 1
                pe.op(lambda: nc.tensor.matmul(pb[x][:, 0:256], lhsT=Bsel[:, i, :], rhs=P["gv"][:, 8, h * 256:(h + 1) * 256],
                                               start=True, stop=True), reads=[B_q, C["B_gv"]], writes=[B_pb[x]])
                dve.op(lambda: nc.vector.tensor_scalar(out=tmp[:, x, :], in0=pb[x][:, 0:256],
                                                       scalar1=P["kg8f"][:, h, i:i + 1], scalar2=None, op0=ALU.mult),
                       reads=[B_pb[x], C["B_kgT"]], writes=[B_tmp[x]])
                dve.op(lambda: nc.vector.scalar_tensor_tensor(out=Sn[:, o, h, :], in0=S0[:, o, h, :],
                                                              scalar=P["dec8"][:, h, i:i + 1], in1=tmp[:, x, :],
                                                              op0=ALU.mult, op1=ALU.add),
                       reads=[B_S0[o], B_tmp[x], C["B_dec"]], writes=[B_Sn[o]])
            sp.dma(stout[i], Sn[:, o], B_Sn[o].dsem, reads=[B_Sn[o]])
            act.op(lambda: nc.scalar.copy(out=Snb[:, o], in_=Sn[:, o]), reads=[B_Sn[o]], writes=[B_Snb[o]])
            pe.pre(reads=[B_Snb[o], B_q])
            for h in range(4):
                ins = nc.tensor.matmul(pso[h // 2][0:16, (h % 2) * 256:(h % 2) * 256 + 256], lhsT=Qpad[:, h, i, :],
                                       rhs=Snb[:, o, h, :], start=(i == 0 and h % 2 == 0), stop=(i == 15),
                                       skip_group_check=True)
            ev = pe.done(ins)
            pe.post(ev, reads=[B_Snb[o], B_q], writes=[B_pso])
        for h in range(4):
            head_norm(pso[h // 2][0:16, (h % 2) * 256:(h % 2) * 256 + 256], B_pso, 16,
                      P["og"][0:16, 8, h * 256:(h + 1) * 256], C["B_og"])
        K.end_phase()
def phase_c(K, C):
    nc = K.nc
    pe, act, dve, pool, sp = K.pe, K.act, K.dve, K.pool, K.sp
    ident = C["ident"]
    win_v = C["w_in"].rearrange("(k p) f -> p k f", p=128)
    wpa_v = C["w_proj_attn"].rearrange("(k p) f -> p k f", p=128)
    wpg_v = C["w_proj_gla"].rearrange("(k p) f -> p k f", p=128)
    wo_v = C["w_out"].rearrange("(k p) f -> p k f", p=128)
    with ExitStack() as esm:
        def sbm(n, s, d):
            return esm.enter_context(nc.sbuf_tensor("c_" + n, s, d))
        mT = sbm("mT", [128, 16, TOK], BF16)
        B_mT = [Buf() for _ in range(NT)]
        with ExitStack() as esg:
            merged = esg.enter_context(nc.sbuf_tensor("c_merged", [128, NT, D], BF16))
            B_mg = [Buf() for _ in range(NT)]
            with ExitStack() as es:
                def sb(n, s, d):
                    return es.enter_context(nc.sbuf_tensor("c_" + n, s, d))

                def ps(n, s, d=F32):
                    return es.enter_context(nc.psum_tensor("c_" + n, s, d))
                uT = sb("uT", [128, 16, TOK], BF16)
                B_uT = [Buf() for _ in range(NT)]
                oatT = sb("oatT", [128, 8, TOK], BF16)
                ogT = sb("ogT", [128, 8, TOK], BF16)
                B_oatT = [Buf() for _ in range(NT)]
                B_ogT = [Buf() for _ in range(NT)]
                ptr = [ps(f"ptr{i}", [128, 1024], BF16) for i in range(2)]
                B_ptr = [Buf(), Buf()]
                norm_T(K, C["h1"], C["mix_pre_w"], uT, B_uT, ident, ptr, B_ptr, "c1_")
                pp = [ps(f"pp{i}", [128, 512]) for i in range(4)]
                B_pp = [Buf() for _ in range(4)]
                wg = sb("wg", [128, 2, 16, 512], BF16)
                B_wg = [Buf(K, dma=True), Buf(K, dma=True)]
                wp = sb("wp", [128, 2, 8, 512], BF16)
                B_wp = [Buf(K, dma=True), Buf(K, dma=True)]
                ost = sb("ost", [128, 1, 1024], BF16)
                B_ost = [Buf(K, dma=True)] * 2
                gst = sb("gst", [128, 1, 1024], BF16)
                B_gst = [Buf(K, dma=True)] * 2
                sg = sb("sg", [128, 4, 512], BF16)
                B_sg = [Buf() for _ in range(4)]
                tm = sb("tm", [128, 2, 512], F32)
                B_tm = [Buf(), Buf()]
                npp = [0]
                ntr = [0]

                def tr8(src_slot_ap, B_src, dstT, B_dst, t):
                    for half in range(2):
                        x = ntr[0] % 2
                        ntr[0] += 1
                        pe.pre(reads=[B_src, K.B_ident], writes=[B_ptr[x]])
                        for b in range(4):
                            k = half * 4 + b
                            ins = nc.tensor.transpose(ptr[x][:, b * 128:(b + 1) * 128],
                                                      src_slot_ap[:, k * 128:(k + 1) * 128], ident[:])
                        ev = pe.done(ins)
                        pe.post(ev, reads=[B_src], writes=[B_ptr[x]])
                        act.op(lambda: nc.scalar.copy(out=dstT[:, half * 4:half * 4 + 4, t * 128:(t + 1) * 128],
                                                      in_=ptr[x][:, 0:512].rearrange("p (a b) -> p a b", a=4)),
                               reads=[B_ptr[x]], writes=[B_dst[t]])

                oat_d = C["oat_d"].bitcast(BF16)
                og_d = C["og_d"].bitcast(BF16)
                for t in range(NT):
                    o = t % 2
                    sp.dma(ost[:, 0, :], oat_d[t * 128:(t + 1) * 128, :], B_ost[o].dsem, writes=[B_ost[o]])
                    tr8(ost[:, 0, :], B_ost[o], oatT, B_oatT, t)
                for blk in range(2):
                    pool.dma(wg[:, blk, :, :], win_v[:, :, CGR + blk * 512:CGR + (blk + 1) * 512], B_wg[blk].dsem,
                             writes=[B_wg[blk]])
                for t in range(NT):
                    o = t % 2
                    sp.dma(gst[:, 0, :], og_d[t * 128:(t + 1) * 128, :], B_gst[o].dsem, writes=[B_gst[o]])
                    for blk in range(2):
                        i = npp[0] % 4
                        npp[0] += 1
                        pe.pre(reads=[B_uT[t], B_wg[blk]], writes=[B_pp[i]])
                        for k in range(16):
                            ins = nc.tensor.matmul(pp[i][:, :], lhsT=uT[:, k, t * 128:(t + 1) * 128], rhs=wg[:, blk, k, :],
                                                   start=(k == 0), stop=(k == 15))
                        ev = pe.done(ins)
                        pe.post(ev, reads=[B_uT[t], B_wg[blk]], writes=[B_pp[i]])
                        act.op(lambda: nc.scalar.activation(out=sg[:, i, :], in_=pp[i][:, :], func=AF.Silu),
                               reads=[B_pp[i]], writes=[B_sg[i]])
                        dve.op(lambda: nc.vector.tensor_tensor(out=gst[:, 0, blk * 512:(blk + 1) * 512],
                                                               in0=gst[:, 0, blk * 512:(blk + 1) * 512], in1=sg[:, i, :],
                                                               op=ALU.mult), reads=[B_sg[i], B_gst[o]], writes=[B_gst[o]])
                    tr8(gst[:, 0, :], B_gst[o], ogT, B_ogT, t)
                for nb in range(4):
                    cs = slice(nb * 512, (nb + 1) * 512)
                    pool.dma(wg[:, 0, :, :], win_v[:, :, CGA + nb * 512:CGA + (nb + 1) * 512], B_wg[0].dsem,
                             writes=[B_wg[0]])
                    pool.dma(wg[:, 1, :, :], win_v[:, :, CGG + nb * 512:CGG + (nb + 1) * 512], B_wg[1].dsem,
                             writes=[B_wg[1]])
                    pool.dma(wp[:, 0, :, :], wpa_v[:, :, cs], B_wp[0].dsem, writes=[B_wp[0]])
                    pool.dma(wp[:, 1, :, :], wpg_v[:, :, cs], B_wp[1].dsem, writes=[B_wp[1]])
                    for t in range(NT):
                        ts = slice(t * 128, (t + 1) * 128)
                        ids = []
                        for which in range(4):
                            i = npp[0] % 4
                            npp[0] += 1
                            ids.append(i)
                            if which < 2:
                                srcT, Bs, w, Bw, nk = uT, B_uT, wg[:, which], B_wg[which], 16
                            elif which == 2:
                                srcT, Bs, w, Bw, nk = oatT, B_oatT, wp[:, 0], B_wp[0], 8
                            else:
                                srcT, Bs, w, Bw, nk = ogT, B_ogT, wp[:, 1], B_wp[1], 8
                            pe.pre(reads=[Bs[t], Bw], writes=[B_pp[i]])
                            for k in range(nk):
                                ins = nc.tensor.matmul(pp[i][:, :], lhsT=srcT[:, k, ts], rhs=w[:, k, :],
                                                       start=(k == 0), stop=(k == nk - 1))
                            ev = pe.done(ins)
                            pe.post(ev, reads=[Bs[t], Bw], writes=[B_pp[i]])
                            if which < 2:
                                act.op(lambda: nc.scalar.activation(out=sg[:, i, :], in_=pp[i][:, :], func=AF.Sigmoid),
                                       reads=[B_pp[i]], writes=[B_sg[i]])
                        ia, ig, ipa, ipg = ids
                        x = t % 2
                        dve.op(lambda: nc.vector.tensor_tensor(out=tm[:, x, :], in0=sg[:, ia, :], in1=pp[ipa][:, :],
                                                               op=ALU.mult), reads=[B_sg[ia], B_pp[ipa]], writes=[B_tm[x]])
                        dve.op(lambda: nc.vector.tensor_tensor(out=sg[:, ig, :], in0=sg[:, ig, :], in1=pp[ipg][:, :],
                                                               op=ALU.mult), reads=[B_sg[ig], B_pp[ipg]], writes=[B_sg[ig]])
                        pool.op(lambda: nc.gpsimd.tensor_tensor(out=merged[:, t, cs], in0=tm[:, x, :], in1=sg[:, ig, :],
                                                                op=ALU.add), reads=[B_tm[x], B_sg[ig]], writes=[B_mg[t]])
                K.barrier()
            with ExitStack() as es:
                ptr = [es.enter_context(nc.psum_tensor(f"c2_ptr{i}", [128, 1024], BF16)) for i in range(2)]
                B_ptr = [Buf(), Buf()]
                n = 0
                for t in range(NT):
                    for q4 in range(4):
                        x = n % 2
                        n += 1
                        pe.pre(reads=[B_mg[t], K.B_ident], writes=[B_ptr[x]])
                        for b in range(4):
                            k = q4 * 4 + b
                            ins = nc.tensor.transpose(ptr[x][:, b * 128:(b + 1) * 128], merged[:, t, k * 128:(k + 1) * 128],
                                                      ident[:])
                        ev = pe.done(ins)
                        pe.post(ev, reads=[B_mg[t]], writes=[B_ptr[x]])
                        if q4 % 2 == 0:
                            act.op(lambda: nc.scalar.copy(out=mT[:, q4 * 4:q4 * 4 + 4, t * 128:(t + 1) * 128],
                                                          in_=ptr[x][:, 0:512].rearrange("p (a b) -> p a b", a=4)),
                                   reads=[B_ptr[x]], writes=[B_mT[t]])
                        else:
                            dve.op(lambda: nc.vector.tensor_copy(mT[:, q4 * 4:q4 * 4 + 4, t * 128:(t + 1) * 128],
                                                                 ptr[x][:, 0:512].rearrange("p (a b) -> p a b", a=4)),
                                   reads=[B_ptr[x]], writes=[B_mT[t]])
                K.barrier()
        with ExitStack() as es:
            def sb(n, s, d):
                return es.enter_context(nc.sbuf_tensor("c3_" + n, s, d))
            wo = sb("wo", [128, 16, D], BF16)
            B_wo = Buf(K, dma=True)
            wbc = sb("wbc", [128, D], F32)
            hst = sb("hst", [128, 2, D], F32)
            B_hst = [Buf(K, dma=True), Buf(K, dma=True)]
            ot = sb("ot", [128, 2, D], F32)
            B_ot = [Buf(K, dma=True), Buf(K, dma=True)]
            junk = sb("junk", [128, 512], BF16)
            B_junk = Buf()
            st = sb("st", [128, 8 * NT], F32)
            B_st = Buf()
            po = [es.enter_context(nc.psum_tensor(f"c3_po{i}", [128, 512])) for i in range(8)]
            B_po = [Buf() for _ in range(8)]
            for nb in range(4):
                pool.dma(wo[:, :, nb * 512:(nb + 1) * 512], wo_v[:, :, nb * 512:(nb + 1) * 512], B_wo.dsem, writes=[B_wo])
            sp.dma(wbc[:], C["mix_post_w"].rearrange("(o d) -> o d", o=1).to_broadcast([128, D]), B_wo.dsem,
                   writes=[B_wo])
            dve.op(lambda: nc.vector.memset(st[:], 0.0), writes=[B_st])
            for t in range(NT):
                o = t % 2
                ts = slice(t * 128, (t + 1) * 128)
                sp.dma(hst[:, o, :], C["h1"][ts, :], B_hst[o].dsem, writes=[B_hst[o]])
                for nb in range(4):
                    i = o * 4 + nb
                    pe.pre(reads=[B_mT[t], B_wo], writes=[B_po[i]])
                    for k in range(16):
                        ins = nc.tensor.matmul(po[i][:, :], lhsT=mT[:, k, ts], rhs=wo[:, k, nb * 512:(nb + 1) * 512],
                                               start=(k == 0), stop=(k == 15))
                    ev = pe.done(ins)
                    pe.post(ev, reads=[B_mT[t], B_wo], writes=[B_po[i]])
                    act.op(lambda: nc.scalar.activation(out=junk[:], in_=po[i][:, :], func=AF.Square,
                                                        accum_out=st[:, t * 8 + nb:t * 8 + nb + 1]),
                           reads=[B_po[i], B_st], writes=[B_junk, B_st])
                c = t * 8
                dve.op(lambda: nc.vector.tensor_reduce(out=st[:, c + 4:c + 5], in_=st[:, c:c + 4], axis=AX.X, op=ALU.add),
                       reads=[B_st], writes=[B_st])
                act.op(lambda: nc.scalar.activation(out=st[:, c + 5:c + 6], in_=st[:, c + 4:c + 5], func=AF.Sqrt,
                                                    scale=1.0 / D, bias=EPS), reads=[B_st], writes=[B_st])
                dve.op(lambda: nc.vector.reciprocal(out=st[:, c + 5:c + 6], in_=st[:, c + 5:c + 6]), reads=[B_st],
                       writes=[B_st])
                for nb in range(4):
                    i = o * 4 + nb
                    cs = slice(nb * 512, (nb + 1) * 512)
                    dve.op(lambda: nc.vector.scalar_tensor_tensor(out=ot[:, o, cs], in0=po[i][:, :],
                                                                  scalar=st[:, c + 5:c + 6], in1=wbc[:, cs],
                                                                  op0=ALU.mult, op1=ALU.mult),
                           reads=[B_po[i], B_st, B_wo], writes=[B_ot[o]])
                pool.op(lambda: nc.gpsimd.tensor_tensor(out=ot[:, o, :], in0=ot[:, o, :], in1=hst[:, o, :], op=ALU.add),
                        reads=[B_hst[o], B_ot[o]], writes=[B_ot[o]])
                sp.dma(C["h2"][ts, :], ot[:, o, :], B_ot[o].dsem, reads=[B_ot[o]])
            K.barrier()


WNAMES = [("ffn1_pre_w", [D]), ("ffn1_w_gate", [D, DFF]), ("ffn1_w_up", [D, DFF]), ("ffn1_w_down", [DFF, D]),
          ("ffn1_post_w", [D]), ("mix_pre_w", [D]), ("w_in", [D, DIN]), ("gla_gate_w2", [16, 512]),
          ("gla_gate_b", [512]), ("gla_norm_w", [256]), ("w_proj_attn", [1024, D]), ("w_proj_gla", [1024, D]),
          ("w_out", [D, D]), ("mix_post_w", [D]), ("ffn2_pre_w", [D]), ("ffn2_w_gate", [D, DFF]),
          ("ffn2_w_up", [D, DFF]), ("ffn2_w_down", [DFF, D]), ("ffn2_post_w", [D])]
NPOOL_ROWS = 2560 * 128


def build(stage=99, debug=False):
    K = Kern()
    nc = K.nc
    C = {}
    full = stage >= 4
    x = K.dram("x", [TOK, D], F32, "ExternalInput").ap()
    C["posv"] = K.dram("posv", [128, NT], F32, "ExternalInput").ap()
    K.used = WNAMES[:5] if stage == 1 else (WNAMES[:9] if stage in (2, 3) else WNAMES)
    for name, shape in K.used:
        C[name] = K.dram(name, shape, F32, "ExternalInput").ap()
    y = K.dram("y", [TOK, D], F32, "ExternalOutput").ap()
    C["ko"] = K.dram("ko", [TOK, 256], F32, "ExternalOutput").ap()
    C["vo"] = K.dram("vo", [TOK, 256], F32, "ExternalOutput").ap()
    C["kio"] = K.dram("kio", [TOK, 64], F32, "ExternalOutput").ap()
    dk = "ExternalOutput" if debug else "Internal"
    C["h1"] = K.dram("h1s", [TOK, D], F32, dk).ap()
    C["h2"] = K.dram("h2s", [TOK, D], F32, dk).ap()
    C["cmask"] = K.dram("cmask", [128, 512], F32, "ExternalInput").ap()
    if stage == 3:
        C["dbg"] = K.dram("dbg", [TOK, 1024], F32, "ExternalOutput").ap()
        C["dbg2"] = K.dram("dbg2", [128, 4136], F32, "ExternalOutput").ap()
    for nm, r, c in (("agk", 256, 512), ("agki", 64, 512), ("agv", 1024, 128), ("dec", 128, 32)):
        C[nm + "_in"] = K.dram(nm + "_in", [r, c], F32).ap()
        C[nm + "_out"] = K.dram(nm + "_out", [4 * r, c], F32).ap()
    if full:
        C["onehot"] = K.dram("onehot", [128, 4], F32, "ExternalInput").ap()
        C["pt"] = K.dram("pt", [16, 16], I32, "ExternalInput").ap()
        C["state_in"] = K.dram("state_in", [16 * 512, 256], F32, "ExternalInput").ap()
        C["cache_k"] = K.dram("cache_k", [NPOOL_ROWS, 256], F32, "ExternalInput").ap()
        C["cache_v"] = K.dram("cache_v", [NPOOL_ROWS, 256], F32, "ExternalInput").ap()
        C["cache_kidx"] = K.dram("cache_kidx", [NPOOL_ROWS, 64], F32, "ExternalInput").ap()
        C["gla_p"] = K.dram("gla_p", [512, 256], F32, "ExternalOutput").ap()
        C["gla_s"] = K.dram("gla_s", [16 * 512, 256], F32, "ExternalOutput").ap()
        C["gst_in"] = [K.dram(f"gst_in{t}", [512, 256], F32).ap() for t in range(8)]
        C["gst_out"] = [K.dram(f"gst_out{t}", [2048, 256], F32).ap() for t in range(8)]
        C["sscr"] = K.dram("sscr", [16, 2176], F32).ap()
        C["oscr"] = K.dram("oscr", [16, 1024], F32).ap()
        C["oat_d"] = K.dram("oat_d", [TOK, 512], F32, dk).ap()
        C["og_d"] = K.dram("og_d", [TOK, 512], F32, dk).ap()

    with ExitStack() as es0:
        ident = es0.enter_context(nc.sbuf_tensor("ident", [128, 128], BF16))
        C["ident"] = ident
        K.B_ident = Buf()
        K.pool.op(lambda: nc.gpsimd.memset(ident[:], 1.0), writes=[K.B_ident])
        K.pool.op(lambda: nc.gpsimd.affine_select(out=ident[:], in_=ident[:], pattern=[[-1, 128]],
                                                  compare_op=ALU.is_equal, fill=0.0, base=0, channel_multiplier=1),
                  reads=[K.B_ident], writes=[K.B_ident])
        K.barrier()

        ffn_phase(K, x, (y if stage == 1 else C["h1"]), C["ffn1_pre_w"], C["ffn1_w_gate"], C["ffn1_w_up"],
                  C["ffn1_w_down"], C["ffn1_post_w"], ident)
        if stage >= 2:
            with ExitStack() as es:
                def sb(n, s, d):
                    return es.enter_context(nc.sbuf_tensor(n, s, d))
                P = {}
                P["qT"] = sb("p_qT", [128, 8, TOK], BF16)
                P["qiT"] = sb("p_qiT", [128, 8, TOK], BF16)
                P["wi"] = sb("p_wi", [128, NT, 16], F32)
                P["qgT"] = sb("p_qgT", [128, 4, TOK], BF16)
                P["kgT"] = sb("p_kgT", [128, 4, TOK], BF16)
                P["khat"] = sb("p_khat", [128, NT, 512], BF16)
                P["gv"] = sb("p_gv", [128, NT, 1024], BF16)
                P["dec"] = sb("p_dec", [128, 4, 8], F32)
                P["dec8"] = sb("p_dec8", [128, 4, 128], F32)
                P["kg8f"] = sb("p_kg8f", [128, 4, 128], F32)
                P["kT8"] = sb("p_kT8", [128, 256], BF16)
                P["v8"] = sb("p_v8", [128, 256], BF16)
                P["kiT8"] = sb("p_kiT8", [128, 128], BF16)
                C["P"] = P
                for n in ("B_qT", "B_qiT", "B_wi", "B_qgT", "B_kgT", "B_khat", "B_gv", "B_s8", "B_AT", "B_og",
                          "B_ag1", "B_ag1o", "B_decin", "B_deco", "B_sscr", "B_oscr"):
                    C[n] = Buf()
                C["B_dec"] = Buf(K, dma=True, persist=True)
                C["B_gin"] = [Buf() for _ in range(8)]
                C["B_gout"] = [Buf() for _ in range(8)]
                phase_a(K, C)
                if full:
                    P["AT"] = sb("p_AT", [128, 8, 4, 128], BF16)
                    phase_g1(K, C)
                if stage >= 3:
                    P["oat"] = sb("p_oat", [128, NT, 1024], BF16)
                    C["B_oat"] = Buf(K, dma=True, persist=True)
                    K.dve.op(lambda: nc.vector.memset(P["oat"][:, 8, :], 0.0), writes=[C["B_oat"]])
                    phase_b(K, C)
                if stage == 3:
                    K.pool.dma(C["dbg"].rearrange("(t p) c -> p t c", p=128), P["oat"][:], C["B_oat"].dsem,
                               reads=[C["B_oat"]])
                if full:
                    phase_bs(K, C)
                    K.sp.dma(C["oat_d"].bitcast(BF16).rearrange("(t p) c -> p t c", p=128), P["oat"][:],
                             C["B_oat"].dsem, reads=[C["B_oat"]])
                    P["og"] = sb("p_og", [128, NT, 1024], BF16)
                    C["B_og"] = Buf(K, dma=True, persist=True)
                    K.dve.op(lambda: nc.vector.memset(P["og"][:, 8, :], 0.0), writes=[C["B_og"]])
                    phase_g2(K, C)
                    K.sp.dma(C["og_d"].bitcast(BF16).rearrange("(t p) c -> p t c", p=128), P["og"][:],
                             C["B_og"].dsem, reads=[C["B_og"]])
                K.barrier()
            if full:
                phase_c(K, C)
                ffn_phase(K, C["h2"], y, C["ffn2_pre_w"], C["ffn2_w_gate"], C["ffn2_w_up"], C["ffn2_w_down"],
                          C["ffn2_post_w"], ident, tag="f2")
    K.barrier()
    K.es.close()
    return K


def core_rows(x_prompt, x_sample, c):
    b, j = c // 4, c % 4
    rows = [x_prompt[b, (4 * s + j) * 128:(4 * s + j + 1) * 128] for s in range(8)]
    pad = np.zeros((128, x_sample.shape[-1]), np.float32)
    pad[:16] = x_sample[16 * c:16 * c + 16, 0]
    rows.append(pad)
    return np.ascontiguousarray(np.concatenate(rows, 0))


def make_in_maps(inputs, used=WNAMES, full=True):
    in_maps = []
    shared = {k: np.ascontiguousarray(inputs[k], dtype=np.float32) for k, _ in used}
    if full:
        shared["cache_k"] = np.ascontiguousarray(inputs["cache_k"]).reshape(NPOOL_ROWS, 256)
        shared["cache_v"] = np.ascontiguousarray(inputs["cache_v"]).reshape(NPOOL_ROWS, 256)
        shared["cache_kidx"] = np.ascontiguousarray(inputs["cache_kidx"]).reshape(NPOOL_ROWS, 64)
    for c in range(NCORES):
        j = c % 4
        m = {"x": core_rows(inputs["x_prompt"], inputs["x_sample"], c)}
        posv = np.zeros((128, NT), np.float32)
        for s in range(8):
            posv[:, s] = (4 * s + j) * 128 + np.arange(128)
        posv[:, 8] = 2048.0
        m["posv"] = posv
        cm = np.zeros((128, 4, 128), np.float32)
        for r in range(4):
            if r > j:
                cm[:, r, :] = -1.0e4
            elif r == j:
                cm[:, r, :] = np.where(np.arange(128)[None, :] > np.arange(128)[:, None], -1.0e4, 0.0)
        m["cmask"] = cm.reshape(128, 512)
        if full:
            oh = np.zeros((128, 4), np.float32)
            oh[:, j] = 1.0
            m["onehot"] = oh
            m["pt"] = np.ascontiguousarray(inputs["page_table"][16 * c:16 * c + 16], dtype=np.int32)
            m["state_in"] = np.ascontiguousarray(inputs["state_gla"][16 * c:16 * c + 16], dtype=np.float32).reshape(
                16 * 512, 256)
        m.update(shared)
        in_maps.append(m)
    return in_maps


def assemble(results):
    y_p = np.zeros((2, 4096, D), np.float32)
    y_s = np.zeros((128, 1, D), np.float32)
    k_p = np.zeros((2, 4096, 2, 128), np.float32)
    v_p = np.zeros((2, 4096, 2, 128), np.float32)
    ki_p = np.zeros((2, 4096, 64), np.float32)
    k_s = np.zeros((128, 1, 2, 128), np.float32)
    v_s = np.zeros((128, 1, 2, 128), np.float32)
    ki_s = np.zeros((128, 1, 64), np.float32)
    gla_p = np.zeros((2, 4, 128, 256), np.float32)
    gla_s = np.zeros((128, 4, 128, 256), np.float32)
    for c, r in enumerate(results):
        b, j = c // 4, c % 4
        for s in range(8):
            sl = slice((4 * s + j) * 128, (4 * s + j + 1) * 128)
            rs = slice(s * 128, (s + 1) * 128)
            y_p[b, sl] = r["y"][rs]
            k_p[b, sl] = r["ko"][rs].reshape(128, 2, 128)
            v_p[b, sl] = r["vo"][rs].reshape(128, 2, 128)
            ki_p[b, sl] = r["kio"][rs]
        ss = slice(16 * c, 16 * c + 16)
        y_s[ss, 0] = r["y"][1024:1040]
        k_s[ss, 0] = r["ko"][1024:1040].reshape(16, 2, 128)
        v_s[ss, 0] = r["vo"][1024:1040].reshape(16, 2, 128)
        ki_s[ss, 0] = r["kio"][1024:1040]
        gla_s[ss] = r["gla_s"].reshape(16, 4, 128, 256)
        if j == 0:
            gla_p[b] = r["gla_p"].reshape(4, 128, 256)
    return (y_p, y_s, k_p, v_p, ki_p, gla_p, k_s, v_s, ki_s, gla_s)


def kernel(**inputs):
    K = build()
    in_maps = make_in_maps(inputs, K.used, True)
    res = run_bass_kernel_spmd(K.nc, in_maps, core_ids=list(range(NCORES)))
    return assemble(res.results)
```

```python
import numpy as np
from contextlib import ExitStack
import concourse.bass as bass
import concourse.mybir as mybir
from concourse.bass_utils import run_bass_kernel_spmd

F32 = mybir.dt.float32
BF16 = mybir.dt.bfloat16
I32 = mybir.dt.int32
ALU = mybir.AluOpType
AF = mybir.ActivationFunctionType
AX = mybir.AxisListType

NCORES = 8
NT = 9
TOK = NT * 128
D = 2048
DFF = 5632
DIN = 9824
EPS = 1e-6
TB = [(0, 512), (512, 512), (1024, 128)]


class Sem:
    def __init__(self, h, uid):
        self.h = h
        self.uid = uid
        self.cnt = 0


class Ev:
    __slots__ = ("sem", "val", "q", "idx")

    def __init__(self, sem, val, q=None, idx=0):
        self.sem = sem
        self.val = val
        self.q = q
        self.idx = idx


class Buf:
    def __init__(self, K=None, dma=False, persist=False):
        self.w = None
        self.r = {}
        self.dsem = K.new_sem("d", persist) if dma else None


class Q:
    def __init__(self, K, eng, name):
        self.K = K
        self.eng = eng
        self.name = name
        self.sem = K.new_sem(name, True)
        self.seen = {}
        self.nins = 0

    def wait(self, *evs):
        for ev in evs:
            if ev is None:
                continue
            if ev.q is self and self.nins - ev.idx >= 4:
                continue
            if self.seen.get(ev.sem.uid, -1) >= ev.val:
                continue
            self.seen[ev.sem.uid] = ev.val
            self.eng.wait_ge(ev.sem.h, ev.val)

    def done(self, ins):
        self.sem.cnt += 1
        self.nins += 1
        ins.then_inc(self.sem.h, 1)
        return Ev(self.sem, self.sem.cnt, self, self.nins)

    def tick(self, n=1):
        self.nins += n

    def pre(self, reads=(), writes=()):
        for b in reads:
            self.wait(b.w)
        for b in writes:
            self.wait(b.w)
            self.wait(*b.r.values())

    def post(self, ev, reads=(), writes=()):
        for b in reads:
            b.r[ev.sem.uid] = ev
        for b in writes:
            b.w = ev
            b.r = {}

    def op(self, fn, reads=(), writes=()):
        self.pre(reads, writes)
        ev = self.done(fn())
        self.post(ev, reads, writes)
        return ev

    def dma(self, out, in_, sem, reads=(), writes=(), **kw):
        for b in reads:
            self.wait(b.w)
        for b in writes:
            if not (b.w is not None and b.w.sem is sem):
                self.wait(b.w)
            self.wait(*b.r.values())
        ins = self.eng.dma_start(out=out, in_=in_, **kw)
        sem.cnt += 16
        ins.then_inc(sem.h, 16)
        self.nins += 1
        ev = Ev(sem, sem.cnt)
        self.post(ev, reads, writes)
        self.K.dma_sems[sem.uid] = sem
        return ev


class Kern:
    def __init__(self):
        self.nc = bass.Bass("TRN2", target_bir_lowering=False)
        self.es = ExitStack()
        self.nsem = 0
        self.dma_sems = {}
        self.free_sems = []
        self.phase_sems = []
        nc = self.nc
        self.pe = Q(self, nc.tensor, "pe")
        self.act = Q(self, nc.scalar, "act")
        self.dve = Q(self, nc.vector, "dve")
        self.pool = Q(self, nc.gpsimd, "pool")
        self.sp = Q(self, nc.sync, "sp")
        self.queues = [self.pe, self.act, self.dve, self.pool, self.sp]

    def new_sem(self, name, persist=False):
        if not persist and self.free_sems:
            s = self.free_sems.pop()
        else:
            self.nsem += 1
            h = self.es.enter_context(self.nc.semaphore(f"{name}{self.nsem}"))
            s = Sem(h, self.nsem)
        if not persist:
            self.phase_sems.append(s)
        return s

    def end_phase(self):
        self.barrier()
        self.free_sems.extend(self.phase_sems)
        self.phase_sems = []

    def barrier(self):
        evs = []
        for q in self.queues:
            if q.sem.cnt > 0:
                evs.append(Ev(q.sem, q.sem.cnt))
        for s in self.dma_sems.values():
            if s.cnt > 0:
                evs.append(Ev(s, s.cnt))
        for q in self.queues:
            q.wait(*evs)

    def dram(self, name, shape, dt, kind="Internal"):
        return self.nc.dram_tensor(name, list(shape), dt, kind=kind)


def ffn_phase(K, src, dst, pre_w, wg, wu, wd, post_w, ident, tag="f1"):
    nc = K.nc
    pe, act, dve, pool, sp = K.pe, K.act, K.dve, K.pool, K.sp
    NG = DFF // 256
    with ExitStack() as es:
        def sb(n, s, d):
            return es.enter_context(nc.sbuf_tensor(tag + n, s, d))

        def ps(n, s, d=F32):
            return es.enter_context(nc.psum_tensor(tag + n, s, d))

        acc = sb("f_acc", [128, NT, D], F32)
        zT = sb("f_zT", [128, 16, TOK], BF16)
        xst = sb("f_xst", [128, D], F32)
        zb = sb("f_zb", [128, D], BF16)
        wbc = sb("f_wbc", [128, D], F32)
        wgb = sb("f_wgb", [128, 2, 16, 256], BF16)
        wub = sb("f_wub", [128, 2, 16, 256], BF16)
        wdb = sb("f_wdb", [128, 3, 2, D], BF16)
        aT = sb("f_aT", [128, 2, 2, TOK], BF16)
        sg = sb("f_sg", [128, 2, 512], BF16)
        st = sb("f_st", [128, 4 * NT], F32)
        ptr = [ps(f"f_ptr{i}", [128, 1024], BF16) for i in range(2)]
        pg = [ps(f"f_pg{i}", [128, 512]) for i in range(2)]
        pu = [ps(f"f_pu{i}", [128, 512]) for i in range(2)]
        pd = [ps(f"f_pd{i}", [128, 512]) for i in range(2)]

        B_xst = Buf(K, dma=True)
        B_zb = Buf()
        B_wbc = Buf(K, dma=True)
        B_st = Buf()
        B_ptr = [Buf(), Buf()]
        B_zT = [Buf() for _ in range(NT)]
        B_wgu = [Buf(K, dma=True) for _ in range(2)]
        B_wd = [Buf(K, dma=True) for _ in range(3)]
        B_aT = [[[Buf() for _ in range(3)] for _ in range(2)] for _ in range(2)]
        B_pg = [Buf(), Buf()]
        B_pu = [Buf(), Buf()]
        B_sg = [Buf(), Buf()]
        B_pd = [Buf(), Buf()]
        B_acc = [Buf(K, dma=True) for _ in range(NT)]

        wg_v = wg.rearrange("(k p) f -> p k f", p=128)
        wu_v = wu.rearrange("(k p) f -> p k f", p=128)
        wd_v = wd.rearrange("(c p) n -> p c n", p=128)

        def load_group(gi):
            s2, s3 = gi % 2, gi % 3
            c0 = gi * 256
            pool.dma(wgb[:, s2], wg_v[:, :, c0:c0 + 256], B_wgu[s2].dsem, writes=[B_wgu[s2]])
            pool.dma(wub[:, s2], wu_v[:, :, c0:c0 + 256], B_wgu[s2].dsem, writes=[B_wgu[s2]])
            pool.dma(wdb[:, s3], wd_v[:, 2 * gi:2 * gi + 2, :], B_wd[s3].dsem, writes=[B_wd[s3]])

        sp.dma(wbc[:], pre_w.rearrange("(o d) -> o d", o=1).to_broadcast([128, D]), B_wbc.dsem, writes=[B_wbc])
        load_group(0)
        dve.op(lambda: nc.vector.memset(st[:], 0.0), writes=[B_st])

        for t in range(NT):
            sp.dma(xst[:], src[t * 128:(t + 1) * 128, :], B_xst.dsem, writes=[B_xst])
            act.op(lambda: nc.scalar.activation(out=zb[:], in_=xst[:], func=AF.Square,
                                                accum_out=st[:, t:t + 1]),
                   reads=[B_xst], writes=[B_zb, B_st])
            act.op(lambda: nc.scalar.activation(out=st[:, NT + t:NT + t + 1], in_=st[:, t:t + 1], func=AF.Sqrt,
                                                scale=1.0 / D, bias=EPS),
                   reads=[B_st], writes=[B_st])
            dve.op(lambda: nc.vector.reciprocal(out=st[:, NT + t:NT + t + 1], in_=st[:, NT + t:NT + t + 1]),
                   reads=[B_st], writes=[B_st])
            dve.op(lambda: nc.vector.scalar_tensor_tensor(out=zb[:], in0=xst[:], scalar=st[:, NT + t:NT + t + 1],
                                                          in1=wbc[:], op0=ALU.mult, op1=ALU.mult),
                   reads=[B_xst, B_st, B_wbc], writes=[B_zb])
            for q4 in range(4):
                sl = q4 % 2
                pe.pre(reads=[B_zb, K.B_ident], writes=[B_ptr[sl]])
                for i in range(4):
                    k = q4 * 4 + i
                    ins = nc.tensor.transpose(ptr[sl][:, i * 128:(i + 1) * 128], zb[:, k * 128:(k + 1) * 128], ident[:])
                    pe.tick()
                ev = pe.done(ins)
                pe.nins -= 1
                pe.post(ev, reads=[B_zb], writes=[B_ptr[sl]])
                src_ap = ptr[sl][:, 0:512].rearrange("p (a b) -> p a b", a=4)
                dst_ap = zT[:, q4 * 4:q4 * 4 + 4, t * 128:(t + 1) * 128]
                if q4 % 2 == 0:
                    act.op(lambda: nc.scalar.copy(out=dst_ap, in_=src_ap), reads=[B_ptr[sl]], writes=[B_zT[t]])
                else:
                    dve.op(lambda: nc.vector.tensor_copy(dst_ap, src_ap), reads=[B_ptr[sl]], writes=[B_zT[t]])

        sp.dma(wbc[:], post_w.rearrange("(o d) -> o d", o=1).to_broadcast([128, D]), B_wbc.dsem, writes=[B_wbc])

        tiles_of_tb = [[0, 1, 2, 3], [4, 5, 6, 7], [8]]
        down_units = []

        def emit_down(gi, t, nb, idx):
            s2, s3 = gi % 2, gi % 3
            tb = t // 4
            sl = idx % 2
            pe.pre(reads=[B_aT[s2][0][tb], B_aT[s2][1][tb], B_wd[s3]], writes=[B_pd[sl]])
            for ci in range(2):
                ins = nc.tensor.matmul(pd[sl][:], lhsT=aT[:, s2, ci, t * 128:(t + 1) * 128],
                                       rhs=wdb[:, s3, ci, nb * 512:(nb + 1) * 512],
                                       start=(ci == 0), stop=(ci == 1))
                pe.tick()
            ev = pe.done(ins)
            pe.nins -= 1
            pe.post(ev, reads=[B_aT[s2][0][tb], B_aT[s2][1][tb], B_wd[s3]], writes=[B_pd[sl]])
            a_ap = acc[:, t, nb * 512:(nb + 1) * 512]
            if gi == 0:
                dve.op(lambda: nc.vector.tensor_copy(a_ap, pd[sl][:]), reads=[B_pd[sl]], writes=[B_acc[t]])
            else:
                dve.op(lambda: nc.vector.tensor_tensor(out=a_ap, in0=a_ap, in1=pd[sl][:], op=ALU.add),
                       reads=[B_pd[sl]], writes=[B_acc[t]])

        didx = 0
        for gi in range(NG):
            s2 = gi % 2
            if gi + 1 < NG:
                load_group(gi + 1)
            step = 0
            for ci in range(2):
                for tbi, (t0, tn) in enumerate(TB):
                    sl = step % 2
                    zdeps = [B_zT[t] for t in tiles_of_tb[tbi]]
                    for (pp, Bp, wb) in ((pg, B_pg, wgb), (pu, B_pu, wub)):
                        pe.pre(reads=zdeps + [B_wgu[s2]], writes=[Bp[sl]])
                        for k in range(16):
                            ins = nc.tensor.matmul(pp[sl][:, 0:tn], lhsT=wb[:, s2, k, ci * 128:(ci + 1) * 128],
                                                   rhs=zT[:, k, t0:t0 + tn], start=(k == 0), stop=(k == 15))
                            pe.tick()
                        ev = pe.done(ins)
                        pe.nins -= 1
                        pe.post(ev, reads=zdeps + [B_wgu[s2]], writes=[Bp[sl]])
                    act.op(lambda: nc.scalar.activation(out=sg[:, sl, 0:tn], in_=pg[sl][:, 0:tn], func=AF.Silu),
                           reads=[B_pg[sl]], writes=[B_sg[sl]])
                    dve.op(lambda: nc.vector.tensor_tensor(out=aT[:, s2, ci, t0:t0 + tn], in0=sg[:, sl, 0:tn],
                                                           in1=pu[sl][:, 0:tn], op=ALU.mult),
                           reads=[B_sg[sl], B_pu[sl]], writes=[B_aT[s2][ci][tbi]])
                    step += 1
                    for _ in range(6):
                        if down_units:
                            g0, t, nb = down_units.pop(0)
                            emit_down(g0, t, nb, didx)
                            didx += 1
            down_units = [(gi, t, nb) for t in range(NT) for nb in range(4)]
        while down_units:
            g0, t, nb = down_units.pop(0)
            emit_down(g0, t, nb, didx)
            didx += 1

        for t in range(NT):
            sp.dma(xst[:], src[t * 128:(t + 1) * 128, :], B_xst.dsem, writes=[B_xst])
            act.op(lambda: nc.scalar.activation(out=zb[:], in_=acc[:, t, :], func=AF.Square,
                                                accum_out=st[:, 2 * NT + t:2 * NT + t + 1]),
                   reads=[B_acc[t]], writes=[B_zb, B_st])
            c = 3 * NT + t
            act.op(lambda: nc.scalar.activation(out=st[:, c:c + 1], in_=st[:, 2 * NT + t:2 * NT + t + 1], func=AF.Sqrt,
                                                scale=4.0 / D, bias=4.0 * EPS),
                   reads=[B_st], writes=[B_st])
            dve.op(lambda: nc.vector.reciprocal(out=st[:, c:c + 1], in_=st[:, c:c + 1]),
                   reads=[B_st], writes=[B_st])
            dve.op(lambda: nc.vector.scalar_tensor_tensor(out=acc[:, t, :], in0=acc[:, t, :], scalar=st[:, c:c + 1],
                                                          in1=wbc[:], op0=ALU.mult, op1=ALU.mult),
                   reads=[B_st, B_wbc, B_acc[t]], writes=[B_acc[t]])
            dve.op(lambda: nc.vector.tensor_tensor(out=acc[:, t, :], in0=acc[:, t, :], in1=xst[:], op=ALU.add),
                   reads=[B_xst, B_acc[t]], writes=[B_acc[t]])
            sp.dma(dst[t * 128:(t + 1) * 128, :], acc[:, t, :], B_acc[t].dsem, reads=[B_acc[t]])
        K.end_phase()


import math
LN_THETA = math.log(10000.0)
PI = math.pi
CQ, CK, CV, CQI, CKI, CWI, CGQ, CGK, CGV, CGLR, CGR, CGA, CGG = (
    0, 1024, 1280, 1536, 2560, 2624, 2640, 3152, 3664, 4688, 4704, 5728, 7776)
AG1_ROWS = 576
SQ = 1.0 / math.sqrt(128.0)


def all_gather(K, src, dst, rbufs, wbufs):
    pool = K.pool
    pool.pre(reads=rbufs, writes=wbufs)
    ins = K.nc.gpsimd.collective_compute("AllGather", ALU.bypass, replica_groups=[[0, 1, 2, 3], [4, 5, 6, 7]],
                                         ins=[src.opt()], outs=[dst.opt()])
    csem = K.new_sem("cc", True)
    ins.then_inc(csem.h)
    csem.cnt += 1
    pool.nins += 1
    ev = Ev(csem, 1)
    for b in rbufs:
        b.r[csem.uid] = ev
    for b in wbufs:
        b.r[csem.uid] = ev
    return ev


def norm_T(K, src, wvec, zT, B_zT, ident, ptr, B_ptr, tag):
    nc = K.nc
    pe, act, dve, pool, sp = K.pe, K.act, K.dve, K.pool, K.sp
    with ExitStack() as es:
        def sb(n, s, d):
            return es.enter_context(nc.sbuf_tensor(tag + n, s, d))
        xst = sb("xst", [128, D], F32)
        zb = sb("zb", [128, D], BF16)
        wbc = sb("wbc", [128, D], F32)
        st = sb("st", [128, 2 * NT], F32)
        B_xst = Buf(K, dma=True)
        B_zb = Buf()
        B_wbc = Buf(K, dma=True)
        B_st = Buf()
        sp.dma(wbc[:], wvec.rearrange("(o d) -> o d", o=1).to_broadcast([128, D]), B_wbc.dsem, writes=[B_wbc])
        dve.op(lambda: nc.vector.memset(st[:], 0.0), writes=[B_st])
        for t in range(NT):
            sp.dma(xst[:], src[t * 128:(t + 1) * 128, :], B_xst.dsem, writes=[B_xst])
            act.op(lambda: nc.scalar.activation(out=zb[:], in_=xst[:], func=AF.Square,
                                                accum_out=st[:, t:t + 1]),
                   reads=[B_xst], writes=[B_zb, B_st])
            act.op(lambda: nc.scalar.activation(out=st[:, NT + t:NT + t + 1], in_=st[:, t:t + 1], func=AF.Sqrt,
                                                scale=1.0 / D, bias=EPS),
                   reads=[B_st], writes=[B_st])
            dve.op(lambda: nc.vector.reciprocal(out=st[:, NT + t:NT + t + 1], in_=st[:, NT + t:NT + t + 1]),
                   reads=[B_st], writes=[B_st])
            dve.op(lambda: nc.vector.scalar_tensor_tensor(out=zb[:], in0=xst[:], scalar=st[:, NT + t:NT + t + 1],
                                                          in1=wbc[:], op0=ALU.mult, op1=ALU.mult),
                   reads=[B_xst, B_st, B_wbc], writes=[B_zb])
            for q4 in range(4):
                sl = q4 % 2
                pe.pre(reads=[B_zb, K.B_ident], writes=[B_ptr[sl]])
                for i in range(4):
                    k = q4 * 4 + i
                    ins = nc.tensor.transpose(ptr[sl][:, i * 128:(i + 1) * 128], zb[:, k * 128:(k + 1) * 128], ident[:])
                ev = pe.done(ins)
                pe.post(ev, reads=[B_zb], writes=[B_ptr[sl]])
                src_ap = ptr[sl][:, 0:512].rearrange("p (a b) -> p a b", a=4)
                dst_ap = zT[:, q4 * 4:q4 * 4 + 4, t * 128:(t + 1) * 128]
                if q4 % 2 == 0:
                    act.op(lambda: nc.scalar.copy(out=dst_ap, in_=src_ap), reads=[B_ptr[sl]], writes=[B_zT[t]])
                else:
                    dve.op(lambda: nc.vector.tensor_copy(dst_ap, src_ap), reads=[B_ptr[sl]], writes=[B_zT[t]])
        K.end_phase()


def phase_a(K, C):
    nc = K.nc
    pe, act, dve, pool, sp = K.pe, K.act, K.dve, K.pool, K.sp
    P = C["P"]
    ident = C["ident"]
    w_in = C["w_in"]
    win_v = w_in.rearrange("(k p) f -> p k f", p=128)
    ag_kT = C["agk_in"]
    ag_kiT = C["agki_in"]
    ag_v = C["agv_in"]
    with ExitStack() as es:
        def sb(n, s, d):
            return es.enter_context(nc.sbuf_tensor("a_" + n, s, d))

        def ps(n, s, d=F32):
            return es.enter_context(nc.psum_tensor("a_" + n, s, d))

        uT = sb("uT", [128, 16, TOK], BF16)
        B_uT = [Buf() for _ in range(NT)]
        ptr = [ps(f"ptr{i}", [128, 1024], BF16) for i in range(2)]
        B_ptr = [Buf(), Buf()]
        pp = [ps(f"pp{i}", [128, 512]) for i in range(2)]
        B_pp = [Buf(), Buf()]
        px = [ps(f"px{i}", [128, 512]) for i in range(2)]
        B_px = [Buf(), Buf()]
        norm_T(K, C["h1"], C["mix_pre_w"], uT, B_uT, ident, ptr, B_ptr, "a1_")

        tabs = sb("tabs", [128, NT, 384], F32)
        es_t = ExitStack()

        def sbt(n, s, d):
            return es_t.enter_context(nc.sbuf_tensor("a_" + n, s, d))
        posv = sbt("posv", [128, NT], F32)
        io = sbt("io", [128, 64], F32)
        inv = sbt("inv", [128, 96], F32)
        ang = sbt("ang", [128, NT, 96], F32)
        kf = sbt("kf", [128, NT, 96], F32)
        kint = sbt("kint", [128, NT, 96], I32)
        sn = sbt("sn", [128, NT, 96], F32)
        cs = sbt("cs", [128, NT, 96], F32)
        B_t = Buf(K, dma=True)
        sp.dma(posv[:], C["posv"], B_t.dsem, writes=[B_t])
        pool.op(lambda: nc.gpsimd.iota(io[:], pattern=[[1, 64]], base=0, channel_multiplier=0,
                                       allow_small_or_imprecise_dtypes=True), writes=[B_t])
        act.op(lambda: nc.scalar.activation(out=inv[:, 0:64], in_=io[:, 0:64], func=AF.Exp,
                                            scale=-2.0 * LN_THETA / 128.0), reads=[B_t], writes=[B_t])
        act.op(lambda: nc.scalar.activation(out=inv[:, 64:96], in_=io[:, 0:32], func=AF.Exp,
                                            scale=-2.0 * LN_THETA / 64.0), reads=[B_t], writes=[B_t])
        for s in range(NT):
            dve.op(lambda: nc.vector.tensor_scalar(out=ang[:, s, :], in0=inv[:], scalar1=posv[:, s:s + 1],
                                                   scalar2=None, op0=ALU.mult), reads=[B_t], writes=[B_t])

        def V(fn):
            return dve.op(fn, reads=[B_t], writes=[B_t])
        V(lambda: nc.vector.tensor_scalar(out=kf[:], in0=ang[:], scalar1=1.0 / (2 * PI), scalar2=None, op0=ALU.mult))
        V(lambda: nc.vector.tensor_copy(kint[:], kf[:]))
        V(lambda: nc.vector.tensor_copy(kf[:], kint[:]))
        V(lambda: nc.vector.scalar_tensor_tensor(out=ang[:], in0=kf[:], scalar=-2 * PI, in1=ang[:],
                                                 op0=ALU.mult, op1=ALU.add))
        act.op(lambda: nc.scalar.activation(out=sn[:], in_=ang[:], func=AF.Sin), reads=[B_t], writes=[B_t])
        V(lambda: nc.vector.tensor_scalar(out=ang[:], in0=ang[:], scalar1=PI / 2, scalar2=None, op0=ALU.add))
        V(lambda: nc.vector.tensor_scalar(out=kf[:], in0=ang[:], scalar1=PI, scalar2=-2 * PI,
                                          op0=ALU.is_gt, op1=ALU.mult))
        V(lambda: nc.vector.tensor_tensor(out=ang[:], in0=ang[:], in1=kf[:], op=ALU.add))
        act.op(lambda: nc.scalar.activation(out=cs[:], in_=ang[:], func=AF.Sin), reads=[B_t], writes=[B_t])
        V(lambda: nc.vector.tensor_copy(tabs[:, :, 0:64], cs[:, :, 0:64]))
        V(lambda: nc.vector.tensor_copy(tabs[:, :, 64:128], cs[:, :, 0:64]))
        V(lambda: nc.vector.tensor_scalar(out=tabs[:, :, 128:192], in0=sn[:, :, 0:64], scalar1=-1.0, scalar2=None,
                                          op0=ALU.mult))
        V(lambda: nc.vector.tensor_copy(tabs[:, :, 192:256], sn[:, :, 0:64]))
        V(lambda: nc.vector.tensor_copy(tabs[:, :, 256:288], cs[:, :, 64:96]))
        V(lambda: nc.vector.tensor_copy(tabs[:, :, 288:320], cs[:, :, 64:96]))
        V(lambda: nc.vector.tensor_scalar(out=tabs[:, :, 320:352], in0=sn[:, :, 64:96], scalar1=-1.0, scalar2=None,
                                          op0=ALU.mult))
        V(lambda: nc.vector.tensor_copy(tabs[:, :, 352:384], sn[:, :, 64:96]))
        B_tabs = B_t
        K.barrier()
        es_t.close()

        wbuf = sb("wbuf", [128, 2, 16, 512], BF16)
        B_w = [Buf(K, dma=True), Buf(K, dma=True)]
        xs = sb("xs", [128, 2, 512], F32)
        B_xs = [Buf(K, dma=True), Buf(K, dma=True)]
        t1 = sb("t1", [128, 2, 512], F32)
        B_t1 = [Buf(), Buf()]
        t2 = sb("t2", [128, 2, 512], F32)
        B_t2 = [Buf(), Buf()]
        ob = sb("ob", [128, 2, 512], BF16)
        B_ob = [Buf(K, dma=True), Buf(K, dma=True)]
        of = sb("of", [128, 2, 320], F32)
        B_of = [Buf(K, dma=True), Buf(K, dma=True)]
        tst = sb("tst", [128, 2, 256], BF16)
        B_tst = [Buf(K, dma=True), Buf(K, dma=True)]
        glrT = sb("glrT", [32, TOK], BF16)
        B_glr = Buf()
        w2b = sb("w2b", [16, 512], BF16)
        negb = sb("negb", [128, 4], F32)
        B_c = Buf(K, dma=True)
        ones = sb("ones", [128, 128], F32)
        eT = sb("eT", [128, 512], F32)
        lT = sb("lT", [128, 512], F32)
        cT = sb("cT", [128, 512], F32)
        B_e, B_l, B_cT = Buf(), Buf(), Buf()
        E1 = sb("E1", [128, TOK], F32)
        E2 = sb("E2", [128, TOK], F32)
        B_E = [Buf(), Buf(), Buf()]
        khT = sb("khT", [128, 512], BF16)
        B_khT = Buf()
        cnt = {"blk": 0, "pp": 0, "xs": 0, "tr": 0, "of": 0, "tst": 0, "px": 0}

        pool.dma(w2b[:], C["gla_gate_w2"], B_c.dsem, writes=[B_c])
        sp.dma(negb[:], C["gla_gate_b"].rearrange("(h p) -> p h", p=128), B_c.dsem, writes=[B_c],
               allow_slow_non_contiguous=True)
        dve.op(lambda: nc.vector.tensor_scalar(out=negb[:], in0=negb[:], scalar1=-1.0, scalar2=None, op0=ALU.mult),
               reads=[B_c], writes=[B_c])
        dve.op(lambda: nc.vector.memset(ones[:], 1.0), writes=[B_c])

        def load_w(pieces):
            slot = cnt["blk"] % 2
            cnt["blk"] += 1
            off = 0
            for (c0, w) in pieces:
                pool.dma(wbuf[:, slot, :, off:off + w], win_v[:, :, c0:c0 + w], B_w[slot].dsem, writes=[B_w[slot]])
                off += w
            return slot

        def mm_tok(slot, t, width):
            i = cnt["pp"] % 2
            cnt["pp"] += 1
            pe.pre(reads=[B_uT[t], B_w[slot]], writes=[B_pp[i]])
            for k in range(16):
                ins = nc.tensor.matmul(pp[i][:, 0:width], lhsT=uT[:, k, t * 128:(t + 1) * 128],
                                       rhs=wbuf[:, slot, k, 0:width], start=(k == 0), stop=(k == 15))
            ev = pe.done(ins)
            pe.post(ev, reads=[B_uT[t], B_w[slot]], writes=[B_pp[i]])
            return i

        def mm_feat(slot, off, m, tbi):
            t0, tn = TB[tbi]
            i = cnt["pp"] % 2
            cnt["pp"] += 1
            deps = [B_uT[t] for t in ([0, 1, 2, 3], [4, 5, 6, 7], [8])[tbi]]
            pe.pre(reads=deps + [B_w[slot]], writes=[B_pp[i]])
            for k in range(16):
                ins = nc.tensor.matmul(pp[i][0:m, 0:tn], lhsT=wbuf[:, slot, k, off:off + m],
                                       rhs=uT[:, k, t0:t0 + tn], start=(k == 0), stop=(k == 15))
            ev = pe.done(ins)
            pe.post(ev, reads=deps + [B_w[slot]], writes=[B_pp[i]])
            return i

        def evac(i, width):
            j = cnt["xs"] % 2
            cnt["xs"] += 1
            act.op(lambda: nc.scalar.copy(out=xs[:, j, 0:width], in_=pp[i][:, 0:width]),
                   reads=[B_pp[i]], writes=[B_xs[j]])
            return j

        def rope(j, c0, hs, nh, t, out_ap, B_out):
            w = nh * hs
            hh = hs // 2
            tb0 = 0 if hs == 128 else 256
            x3 = xs[:, j, c0:c0 + w].rearrange("p (h d) -> p h d", h=nh)
            cosb = tabs[:, t, tb0:tb0 + hs].unsqueeze(1).to_broadcast([128, nh, hs])
            sa = tabs[:, t, tb0 + hs:tb0 + hs + hh].unsqueeze(1).to_broadcast([128, nh, hh])
            sbb = tabs[:, t, tb0 + hs + hh:tb0 + 2 * hs].unsqueeze(1).to_broadcast([128, nh, hh])
            a3 = t1[:, j, 0:w].rearrange("p (h d) -> p h d", h=nh)
            b3 = t2[:, j, 0:w].rearrange("p (h d) -> p h d", h=nh)
            pool.op(lambda: nc.gpsimd.tensor_tensor(out=a3, in0=x3, in1=cosb, op=ALU.mult),
                    reads=[B_xs[j], B_tabs], writes=[B_t1[j]])
            dve.op(lambda: nc.vector.tensor_tensor(out=b3[:, :, 0:hh], in0=x3[:, :, hh:hs], in1=sa, op=ALU.mult),
                   reads=[B_xs[j], B_tabs], writes=[B_t2[j]])
            dve.op(lambda: nc.vector.tensor_tensor(out=b3[:, :, hh:hs], in0=x3[:, :, 0:hh], in1=sbb, op=ALU.mult),
                   reads=[B_xs[j], B_tabs], writes=[B_t2[j]])
            dve.op(lambda: nc.vector.tensor_tensor(out=out_ap, in0=t1[:, j, 0:w], in1=t2[:, j, 0:w], op=ALU.add),
                   reads=[B_t1[j], B_t2[j]], writes=[B_out])

        def transposes(j, nblk, rows, dst_fn, B_dst_fn):
            sl = cnt["tr"] % 2
            cnt["tr"] += 1
            pe.pre(reads=[B_ob[j], K.B_ident], writes=[B_ptr[sl]])
            for b in range(nblk):
                ins = nc.tensor.transpose(ptr[sl][0:rows, b * 128:(b + 1) * 128],
                                          ob[:, j, b * rows:(b + 1) * rows], ident[:])
            ev = pe.done(ins)
            pe.post(ev, reads=[B_ob[j]], writes=[B_ptr[sl]])
            return sl

        for blk in range(2):
            slot = load_w([(CQ + blk * 512, 512)])
            for t in range(NT):
                i = mm_tok(slot, t, 512)
                j = evac(i, 512)
                rope(j, 0, 128, 4, t, ob[:, j, :], B_ob[j])
                sl = transposes(j, 4, 128, None, None)
                act.op(lambda: nc.scalar.copy(out=P["qT"][:, blk * 4:blk * 4 + 4, t * 128:(t + 1) * 128],
                                              in_=ptr[sl][:, 0:512].rearrange("p (a b) -> p a b", a=4)),
                       reads=[B_ptr[sl]], writes=[C["B_qT"]])

        slot = load_w([(CK, 512)])
        for t in range(NT):
            i = mm_tok(slot, t, 512)
            j = evac(i, 512)
            o = cnt["of"] % 2
            cnt["of"] += 1
            rope(j, 0, 128, 2, t, of[:, o, 0:256], B_of[o])
            sp.dma(C["ko"][t * 128:(t + 1) * 128, :], of[:, o, 0:256], B_of[o].dsem, reads=[B_of[o]])
            sp.dma(C["vo"][t * 128:(t + 1) * 128, :], xs[:, j, 256:512], B_xs[j].dsem, reads=[B_xs[j]])
            pool.op(lambda: nc.gpsimd.tensor_copy(ob[:, j, 0:256], of[:, o, 0:256]), reads=[B_of[o]], writes=[B_ob[j]])
            pool.op(lambda: nc.gpsimd.tensor_copy(ob[:, j, 256:512], xs[:, j, 256:512]), reads=[B_xs[j]],
                    writes=[B_ob[j]])
            sl = transposes(j, 2, 128, None, None)
            if t < 8:
                q = cnt["tst"] % 2
                cnt["tst"] += 1
                act.op(lambda: nc.scalar.copy(out=tst[:, q, :], in_=ptr[sl][:, 0:256]), reads=[B_ptr[sl]],
                       writes=[B_tst[q]])
                for g in range(2):
                    sp.dma(ag_kT[g * 128:(g + 1) * 128, t * 64:(t + 1) * 64],
                           tst[:, q, g * 128:(g + 1) * 128].bitcast(F32),
                           B_tst[q].dsem, reads=[B_tst[q]], writes=[C["B_ag1"]])
                sp.dma(ag_v[t * 128:(t + 1) * 128, :], ob[:, j, 256:512].bitcast(F32), B_ob[j].dsem,
                       reads=[B_ob[j]], writes=[C["B_ag1"]])
            else:
                act.op(lambda: nc.scalar.copy(out=P["kT8"][:, :], in_=ptr[sl][:, 0:256]), reads=[B_ptr[sl]],
                       writes=[C["B_s8"]])
                pool.op(lambda: nc.gpsimd.tensor_copy(P["v8"][:, :], ob[:, j, 256:512]), reads=[B_ob[j]],
                        writes=[C["B_s8"]])

        for blk in range(2):
            slot = load_w([(CQI + blk * 512, 512)])
            for t in range(NT):
                i = mm_tok(slot, t, 512)
                j = evac(i, 512)
                rope(j, 0, 64, 8, t, ob[:, j, :], B_ob[j])
                sl = transposes(j, 4, 128, None, None)
                act.op(lambda: nc.scalar.copy(out=P["qiT"][:, blk * 4:blk * 4 + 4, t * 128:(t + 1) * 128],
                                              in_=ptr[sl][:, 0:512].rearrange("p (a b) -> p a b", a=4)),
                       reads=[B_ptr[sl]], writes=[C["B_qiT"]])

        slot = load_w([(CKI, 80)])
        for t in range(NT):
            i = mm_tok(slot, t, 80)
            j = evac(i, 80)
            o = cnt["of"] % 2
            cnt["of"] += 1
            rope(j, 0, 64, 1, t, of[:, o, 256:320], B_of[o])
            sp.dma(C["kio"][t * 128:(t + 1) * 128, :], of[:, o, 256:320], B_of[o].dsem, reads=[B_of[o]])
            dve.op(lambda: nc.vector.tensor_scalar(out=P["wi"][:, t, :], in0=xs[:, j, 64:80], scalar1=1.0 / 32.0,
                                                   scalar2=None, op0=ALU.mult), reads=[B_xs[j]], writes=[C["B_wi"]])
            pool.op(lambda: nc.gpsimd.tensor_copy(ob[:, j, 0:64], of[:, o, 256:320]), reads=[B_of[o]], writes=[B_ob[j]])
            pool.op(lambda: nc.gpsimd.tensor_copy(ob[:, j, 64:128], of[:, o, 256:320]), reads=[B_of[o]],
                    writes=[B_ob[j]])
            sl = transposes(j, 1, 128, None, None)
            if t < 8:
                q = cnt["tst"] % 2
                cnt["tst"] += 1
                act.op(lambda: nc.scalar.copy(out=tst[0:64, q, 0:128], in_=ptr[sl][0:64, 0:128]), reads=[B_ptr[sl]],
                       writes=[B_tst[q]])
                sp.dma(ag_kiT[:, t * 64:(t + 1) * 64], tst[0:64, q, 0:128].bitcast(F32), B_tst[q].dsem,
                       reads=[B_tst[q]], writes=[C["B_ag1"]])
            else:
                act.op(lambda: nc.scalar.copy(out=P["kiT8"][:, :], in_=ptr[sl][:, 0:128]), reads=[B_ptr[sl]],
                       writes=[C["B_s8"]])

        for nm in ("agk", "agki", "agv"):
            all_gather(K, C[nm + "_in"], C[nm + "_out"], [C["B_ag1"]], [C["B_ag1o"]])

        for blk in range(2):
            slot = load_w([(CGV + blk * 512, 512)])
            for t in range(NT):
                i = mm_tok(slot, t, 512)
                act.op(lambda: nc.scalar.copy(out=P["gv"][:, t, blk * 512:(blk + 1) * 512], in_=pp[i][:, :]),
                       reads=[B_pp[i]], writes=[C["B_gv"]])

        slot = load_w([(CGLR, 16)])
        for tbi in range(3):
            t0, tn = TB[tbi]
            i = mm_feat(slot, 0, 16, tbi)
            act.op(lambda: nc.scalar.copy(out=glrT[0:16, t0:t0 + tn], in_=pp[i][0:16, 0:tn]), reads=[B_pp[i]],
                   writes=[B_glr])

        for h in range(4):
            slot = load_w([(CGQ + h * 128, 128), (CGK + h * 128, 128)])
            for tbi in range(3):
                t0, tn = TB[tbi]
                x = cnt["px"] % 2
                cnt["px"] += 1
                pe.op(lambda: nc.tensor.matmul(px[x][:, 0:tn], lhsT=w2b[:, h * 128:(h + 1) * 128],
                                               rhs=glrT[0:16, t0:t0 + tn], start=True, stop=True),
                      reads=[B_glr, B_c], writes=[B_px[x]])
                act.op(lambda: nc.scalar.activation(out=eT[:, 0:tn], in_=px[x][:, 0:tn], func=AF.Exp, scale=-1.0,
                                                    bias=negb[:, h:h + 1]), reads=[B_px[x], B_c], writes=[B_e])
                act.op(lambda: nc.scalar.activation(out=lT[:, 0:tn], in_=eT[:, 0:tn], func=AF.Ln, bias=1.0),
                       reads=[B_e], writes=[B_l])
                if tbi < 2:
                    for q4 in range(4):
                        dve.op(lambda: nc.vector.tensor_tensor_scan(out=cT[:, q4 * 128:(q4 + 1) * 128], data0=ones[:],
                                                                    data1=lT[:, q4 * 128:(q4 + 1) * 128], initial=0.0,
                                                                    op0=ALU.mult, op1=ALU.add),
                               reads=[B_l, B_c], writes=[B_cT])
                    act.op(lambda: nc.scalar.activation(out=E1[:, t0:t0 + tn], in_=cT[:, 0:tn], func=AF.Exp,
                                                        scale=-1.0 / 16.0), reads=[B_cT], writes=[B_E[tbi]])
                    act.op(lambda: nc.scalar.activation(out=E2[:, t0:t0 + tn], in_=cT[:, 0:tn], func=AF.Exp,
                                                        scale=1.0 / 16.0), reads=[B_cT], writes=[B_E[tbi]])
                    dve.op(lambda: nc.vector.tensor_copy(
                        P["dec"][:, h, tbi * 4:tbi * 4 + 4],
                        E1[:, t0:t0 + tn].rearrange("p (a b) -> p a b", a=4)[:, :, 127]),
                        reads=[B_E[tbi]], writes=[C["B_dec"]])
                else:
                    act.op(lambda: nc.scalar.activation(out=E1[:, t0:t0 + tn], in_=lT[:, 0:tn], func=AF.Exp,
                                                        scale=-1.0 / 16.0), reads=[B_l], writes=[B_E[tbi]])
                    dve.op(lambda: nc.vector.tensor_copy(P["dec8"][:, h, :], E1[:, t0:t0 + tn]),
                           reads=[B_E[tbi]], writes=[C["B_dec"]])
            for tbi in range(3):
                t0, tn = TB[tbi]
                i = mm_feat(slot, 0, 128, tbi)
                if tbi < 2:
                    dve.op(lambda: nc.vector.scalar_tensor_tensor(out=P["qgT"][:, h, t0:t0 + tn], in0=pp[i][:, 0:tn],
                                                                  scalar=SQ, in1=E1[:, t0:t0 + tn],
                                                                  op0=ALU.mult, op1=ALU.mult),
                           reads=[B_pp[i], B_E[tbi]], writes=[C["B_qgT"]])
                else:
                    dve.op(lambda: nc.vector.tensor_scalar(out=P["qgT"][:, h, t0:t0 + tn], in0=pp[i][:, 0:tn],
                                                           scalar1=SQ, scalar2=None, op0=ALU.mult),
                           reads=[B_pp[i]], writes=[C["B_qgT"]])
                i = mm_feat(slot, 128, 128, tbi)
                if tbi < 2:
                    dve.op(lambda: nc.vector.tensor_tensor(out=P["kgT"][:, h, t0:t0 + tn], in0=pp[i][:, 0:tn],
                                                           in1=E2[:, t0:t0 + tn], op=ALU.mult),
                           reads=[B_pp[i], B_E[tbi]], writes=[C["B_kgT"]])
                    dve.op(lambda: nc.vector.tensor_tensor(
                        out=khT[:, :].rearrange("p (a b) -> p a b", a=4),
                        in0=P["kgT"][:, h, t0:t0 + tn].rearrange("p (a b) -> p a b", a=4),
                        in1=P["dec"][:, h, tbi * 4:tbi * 4 + 4].unsqueeze(2).to_broadcast([128, 4, 128]),
                        op=ALU.mult), reads=[C["B_kgT"], C["B_dec"]], writes=[B_khT])
                    sl = cnt["tr"] % 2
                    cnt["tr"] += 1
                    pe.pre(reads=[B_khT, K.B_ident], writes=[B_ptr[sl]])
                    for b in range(4):
                        ins = nc.tensor.transpose(ptr[sl][:, b * 128:(b + 1) * 128], khT[:, b * 128:(b + 1) * 128],
                                                  ident[:])
                    ev = pe.done(ins)
                    pe.post(ev, reads=[B_khT], writes=[B_ptr[sl]])
                    act.op(lambda: nc.scalar.copy(out=P["khat"][:, tbi * 4:tbi * 4 + 4, h * 128:(h + 1) * 128],
                                                  in_=ptr[sl][:, 0:512].rearrange("p (a b) -> p a b", a=4)),
                           reads=[B_ptr[sl]], writes=[C["B_khat"]])
                else:
                    act.op(lambda: nc.scalar.copy(out=P["kgT"][:, h, t0:t0 + tn], in_=pp[i][:, 0:tn]),
                           reads=[B_pp[i]], writes=[C["B_kgT"]])
                    dve.op(lambda: nc.vector.tensor_copy(P["kg8f"][:, h, :], pp[i][:, 0:tn]),
                           reads=[B_pp[i]], writes=[C["B_kgT"]])
        K.end_phase()
NIT = 14
TOPK = 256
NEG = -1.0e4


def topk_threshold(K, S3, np_, junk3, st, B_S, B_junk, B_st, pw2, B_c):
    nc = K.nc
    dve = K.dve

    def V(fn, r=(), w=()):
        return dve.op(fn, reads=list(r), writes=list(w))
    V(lambda: nc.vector.tensor_scalar(out=st[0:np_, 8:8 + NIT], in0=pw2[0:np_, 0:NIT], scalar1=st[0:np_, 0:1],
                                      scalar2=None, op0=ALU.mult), r=[B_st, B_c], w=[B_st])
    V(lambda: nc.vector.tensor_scalar(out=st[0:np_, 1:2], in0=st[0:np_, 0:1], scalar1=-1.0, scalar2=None,
                                      op0=ALU.mult), r=[B_st], w=[B_st])
    V(lambda: nc.vector.memset(st[0:np_, 40:40 + NIT], 0.0), r=[B_st], w=[B_st])
    for k in range(NIT):
        V(lambda: nc.vector.tensor_tensor(out=st[0:np_, 2:3], in0=st[0:np_, 1:2], in1=st[0:np_, 8 + k:9 + k],
                                          op=ALU.add), r=[B_st], w=[B_st])
        V(lambda: nc.vector.tensor_scalar(out=junk3, in0=S3, scalar1=st[0:np_, 2:3], scalar2=0.0, op0=ALU.is_ge,
                                          op1=ALU.add, accum_out=st[0:np_, 40 + k:41 + k]),
          r=[B_S, B_st], w=[B_junk, B_st])
        V(lambda: nc.vector.tensor_scalar(out=st[0:np_, 4:5], in0=st[0:np_, 40 + k:41 + k], scalar1=TOPK - 0.5,
                                          scalar2=None, op0=ALU.is_ge), r=[B_st], w=[B_st])
        V(lambda: nc.vector.scalar_tensor_tensor(out=st[0:np_, 1:2], in0=st[0:np_, 4:5], scalar=st[0:np_, 8 + k:9 + k],
                                                 in1=st[0:np_, 1:2], op0=ALU.mult, op1=ALU.add), r=[B_st], w=[B_st])


def phase_b(K, C):
    nc = K.nc
    pe, act, dve, pool, sp = K.pe, K.act, K.dve, K.pool, K.sp
    P = C["P"]
    ident = C["ident"]
    with ExitStack() as es:
        def sb(n, s, d):
            return es.enter_context(nc.sbuf_tensor("b_" + n, s, d))

        def ps(n, s, d=F32):
            return es.enter_context(nc.psum_tensor("b_" + n, s, d))

        kT_all = sb("kT", [128, 2, 4, 1024], BF16)
        kiT2 = sb("kiT2", [128, 4, 1024], BF16)
        V1 = sb("V1", [128, 4, 8, 2, 130], BF16)
        S = sb("S", [128, 4096], F32)
        selm = sb("selm", [128, 4096], BF16)
        selT = sb("selT", [128, 4, 8, 128], BF16)
        diagw = sb("diagw", [128, 16, 128], BF16)
        rh = sb("rh", [128, 3, 512], BF16)
        pex = sb("pex", [128, 3, 512], BF16)
        pm = sb("pm", [128, 3, 512], BF16)
        cmask = sb("cmask", [128, 512], F32)
        pw2 = sb("pw2", [128, 32], F32)
        st = sb("st", [128, 64], F32)
        rec = sb("rec", [128, 8], F32)
        psh = [ps(f"psh{i}", [128, 512]) for i in range(3)]
        psc = [ps(f"psc{i}", [128, 512]) for i in range(2)]
        ptr = [ps(f"ptr{i}", [128, 1024], BF16) for i in range(2)]
        B_kv = Buf(K, dma=True)
        B_S, B_selm, B_selT, B_diagw, B_st, B_c, B_rec = Buf(), Buf(), Buf(), Buf(), Buf(), Buf(K, dma=True), Buf()
        B_rh = [Buf(), Buf(), Buf()]
        B_pex = [Buf(), Buf(), Buf()]
        B_pm = [Buf(), Buf(), Buf()]
        B_psh = [Buf(), Buf(), Buf()]
        B_psc = [Buf(), Buf()]
        B_ptr = [Buf(), Buf()]
        cnt = {"h": 0, "c": 0, "r": 0, "e": 0, "t": 0}

        agk, agki, agv = C["agk_out"], C["agki_out"], C["agv_out"]
        for r in range(4):
            for g in range(2):
                sp.dma(kT_all[:, g, r, :].bitcast(F32), agk[r * 256 + g * 128:r * 256 + (g + 1) * 128, :], B_kv.dsem,
                       reads=[C["B_ag1o"]], writes=[B_kv])
            for hf in range(2):
                sp.dma(kiT2[hf * 64:(hf + 1) * 64, r, :].bitcast(F32), agki[r * 64:(r + 1) * 64, :], B_kv.dsem,
                       reads=[C["B_ag1o"]], writes=[B_kv])
            for g in range(2):
                sp.dma(V1[:, r, :, g, 0:128],
                       agv.bitcast(BF16)[r * 1024:(r + 1) * 1024, g * 128:(g + 1) * 128].rearrange(
                           "(s p) c -> p s c", p=128),
                       B_kv.dsem, reads=[C["B_ag1o"]], writes=[B_kv])
        pool.op(lambda: nc.gpsimd.memset(V1[:, :, :, :, 128:130], 1.0), writes=[B_kv])
        sp.dma(cmask[:], C["cmask"], B_c.dsem, writes=[B_c])
        for k in range(NIT):
            dve.op(lambda: nc.vector.memset(pw2[:, k:k + 1], 2.0 ** (-k)), writes=[B_c])

        for s in range(8):
            Lr = (s + 1) * 128
            qs = slice(s * 128, (s + 1) * 128)
            for h in range(16):
                dve.op(lambda: nc.vector.tensor_scalar(out=diagw[:, h, :], in0=ident[:], scalar1=P["wi"][:, s, h:h + 1],
                                                       scalar2=None, op0=ALU.mult),
                       reads=[C["B_wi"], K.B_ident], writes=[B_diagw])
            LA = 2
            units = [(r, c0, min(512, Lr - c0), h) for r in range(4) for c0 in range(0, Lr, 512) for h in range(16)]
            hi_of = {}
            ci_of = {}

            def sc_front(u):
                r, c0, cw, h = units[u]
                hi = cnt["h"] % 3
                cnt["h"] += 1
                hi_of[u] = hi
                p0 = (h % 2) * 64
                pe.op(lambda: nc.tensor.matmul(psh[hi][:, 0:cw], lhsT=P["qiT"][p0:p0 + 64, h // 2, qs],
                                               rhs=kiT2[p0:p0 + 64, r, c0:c0 + cw], start=True, stop=True),
                      reads=[C["B_qiT"], B_kv], writes=[B_psh[hi]])
                if u % 2 == 0:
                    act.op(lambda: nc.scalar.activation(out=rh[:, hi, 0:cw], in_=psh[hi][:, 0:cw], func=AF.Relu),
                           reads=[B_psh[hi]], writes=[B_rh[hi]])
                else:
                    dve.op(lambda: nc.vector.tensor_scalar(out=rh[:, hi, 0:cw], in0=psh[hi][:, 0:cw], scalar1=0.0,
                                                           scalar2=None, op0=ALU.max),
                           reads=[B_psh[hi]], writes=[B_rh[hi]])

            def sc_back(u):
                r, c0, cw, h = units[u]
                hi = hi_of[u]
                if h == 0:
                    ci_of[(r, c0)] = cnt["c"] % 2
                    cnt["c"] += 1
                ci = ci_of[(r, c0)]
                pe.op(lambda: nc.tensor.matmul(psc[ci][:, 0:cw], lhsT=diagw[:, h, :], rhs=rh[:, hi, 0:cw],
                                               start=(h == 0), stop=(h == 15)),
                      reads=[B_diagw, B_rh[hi]], writes=[B_psc[ci]])
                if h == 15:
                    dve.op(lambda: nc.vector.tensor_copy(S[:, r * Lr + c0:r * Lr + c0 + cw], psc[ci][:, 0:cw]),
                           reads=[B_psc[ci]], writes=[B_S])
            for u in range(min(LA, len(units))):
                sc_front(u)
            for u in range(len(units)):
                if u + LA < len(units):
                    sc_front(u + LA)
                sc_back(u)
            S3 = S[:, 0:4 * Lr]
            dve.op(lambda: nc.vector.tensor_reduce(out=st[:, 32:33], in_=S3, axis=AX.X, op=ALU.max),
                   reads=[B_S], writes=[B_st])
            dve.op(lambda: nc.vector.tensor_reduce(out=st[:, 33:34], in_=S3, axis=AX.X, op=ALU.min),
                   reads=[B_S], writes=[B_st])
            dve.op(lambda: nc.vector.tensor_scalar(out=st[:, 33:34], in0=st[:, 33:34], scalar1=-1.0, scalar2=None,
                                                   op0=ALU.mult), reads=[B_st], writes=[B_st])
            dve.op(lambda: nc.vector.tensor_tensor(out=st[:, 0:1], in0=st[:, 32:33], in1=st[:, 33:34], op=ALU.max),
                   reads=[B_st], writes=[B_st])
            Sl = S3.rearrange("p (r l) -> p r l", r=4)[:, :, s * 128:(s + 1) * 128]
            dve.op(lambda: nc.vector.tensor_tensor(out=Sl, in0=Sl,
                                                   in1=cmask[:, :].rearrange("p (a b) -> p a b", a=4), op=ALU.add),
                   reads=[B_S, B_c], writes=[B_S])
            topk_threshold(K, S3, 128, selm[:, 0:4 * Lr], st, B_S, B_selm, B_st, pw2, B_c)
            dve.op(lambda: nc.vector.tensor_scalar(out=selm[:, 0:4 * Lr], in0=S3, scalar1=st[:, 1:2], scalar2=None,
                                                   op0=ALU.is_ge), reads=[B_S, B_st], writes=[B_selm])
            if s == 7 and "dbg2" in C:
                B_S.dsem = K.new_sem("d")
                B_st.dsem = B_S.dsem
                sp.dma(C["dbg2"][:, 0:4096], S[:], B_S.dsem, reads=[B_S])
                sp.dma(C["dbg2"][:, 4096:4136], st[:, 0:40], B_S.dsem, reads=[B_st])
            for r in range(4):
                for s0 in range(0, s + 1, 4):
                    nb = min(4, s + 1 - s0)
                    ti = cnt["t"] % 2
                    cnt["t"] += 1
                    pe.pre(reads=[B_selm, K.B_ident], writes=[B_ptr[ti]])
                    for b in range(nb):
                        ins = nc.tensor.transpose(ptr[ti][:, b * 128:(b + 1) * 128],
                                                  selm[:, r * Lr + (s0 + b) * 128:r * Lr + (s0 + b + 1) * 128], ident[:])
                    ev = pe.done(ins)
                    pe.post(ev, reads=[B_selm], writes=[B_ptr[ti]])
                    act.op(lambda: nc.scalar.copy(out=selT[:, r, s0:s0 + nb, :],
                                                  in_=ptr[ti][:, 0:nb * 128].rearrange("p (a b) -> p a b", a=nb)),
                           reads=[B_ptr[ti]], writes=[B_selT])
            tiles = [(r, s1) for r in range(4) for s1 in range(s + 1)]
            nt_ = len(tiles)
            aunits = [(g, n, r, s1) for g in range(2) for n, (r, s1) in enumerate(tiles)]
            bi_of = {}

            def at_front(u):
                g, n, r, s1 = aunits[u]
                hi = cnt["h"] % 3
                cnt["h"] += 1
                ei = cnt["e"] % 3
                cnt["e"] += 1
                bi_of[u] = ei
                pe.op(lambda: nc.tensor.matmul(psh[hi][:, :], lhsT=kT_all[:, g, r, s1 * 128:(s1 + 1) * 128],
                                               rhs=P["qT"][:, g * 4:(g + 1) * 4, qs], start=True, stop=True),
                      reads=[B_kv, C["B_qT"]], writes=[B_psh[hi]])
                act.op(lambda: nc.scalar.activation(out=pex[:, ei, :], in_=psh[hi][:, :], func=AF.Exp, scale=SQ),
                       reads=[B_psh[hi]], writes=[B_pex[ei]])
                eng = dve if u % 2 == 0 else pool
                veng = nc.vector if u % 2 == 0 else nc.gpsimd
                eng.op(lambda: veng.tensor_tensor(
                    out=pm[:, ei, :].rearrange("p (a b) -> p a b", a=4),
                    in0=pex[:, ei, :].rearrange("p (a b) -> p a b", a=4),
                    in1=selT[:, r, s1, :].unsqueeze(1).to_broadcast([128, 4, 128]), op=ALU.mult),
                    reads=[B_pex[ei], B_selT], writes=[B_pm[ei]])

            def at_back(u):
                g, n, r, s1 = aunits[u]
                ei = bi_of[u]
                pe.pre(reads=[B_pm[ei], B_kv], writes=[B_psc[0], B_psc[1]])
                for hh in range(4):
                    po = psc[hh // 3][:, (hh % 3) * 129:(hh % 3) * 129 + 129]
                    ins = nc.tensor.matmul(po, lhsT=pm[:, ei, hh * 128:(hh + 1) * 128], rhs=V1[:, r, s1, g, 0:129],
                                           start=(n == 0 and hh % 3 == 0), stop=(n == nt_ - 1),
                                           skip_group_check=True)
                ev = pe.done(ins)
                pe.post(ev, reads=[B_pm[ei], B_kv], writes=[B_psc[0], B_psc[1]])
                if n == nt_ - 1:
                    for hh in range(4):
                        po = psc[hh // 3][:, (hh % 3) * 129:(hh % 3) * 129 + 129]
                        hcol = g * 4 + hh
                        dve.op(lambda: nc.vector.reciprocal(out=rec[:, hcol:hcol + 1], in_=po[:, 128:129]),
                               reads=[B_psc[hh // 3]], writes=[B_rec])
                        dve.op(lambda: nc.vector.tensor_scalar(out=P["oat"][:, s, hcol * 128:(hcol + 1) * 128],
                                                               in0=po[:, 0:128], scalar1=rec[:, hcol:hcol + 1],
                                                               scalar2=None, op0=ALU.mult),
                               reads=[B_psc[hh // 3], B_rec], writes=[C["B_oat"]])
            for u in range(min(LA, len(aunits))):
                at_front(u)
            for u in range(len(aunits)):
                if u + LA < len(aunits):
                    at_front(u + LA)
                at_back(u)
        K.end_phase()
def phase_bs(K, C):
    nc = K.nc
    pe, act, dve, pool, sp = K.pe, K.act, K.dve, K.pool, K.sp
    P = C["P"]
    ident = C["ident"]
    NS = 16
    LK = 2049
    with ExitStack() as es:
        def sb(n, s, d):
            return es.enter_context(nc.sbuf_tensor("s_" + n, s, d))
        ptb = sb("ptb", [128, 256], I32)
        iop = sb("iop", [128, 1], I32)
        idx = sb("idx", [128, 256], I32)
        B_idx = Buf(K, dma=True)
        sp.dma(ptb[:], C["pt"].rearrange("i p -> (i p)").rearrange("(o n) -> o n", o=1).to_broadcast([128, 256]),
               B_idx.dsem, writes=[B_idx])
        pool.op(lambda: nc.gpsimd.iota(iop[:], pattern=[[0, 1]], base=0, channel_multiplier=1), writes=[B_idx])
        pool.op(lambda: nc.gpsimd.tensor_scalar(out=idx[:], in0=ptb[:], scalar1=128, scalar2=None, op0=ALU.mult),
                reads=[B_idx], writes=[B_idx])
        pool.op(lambda: nc.gpsimd.tensor_tensor(out=idx[:], in0=idx[:], in1=iop[:].to_broadcast([128, 256]), op=ALU.add),
                reads=[B_idx], writes=[B_idx])

        wperm = sb("wperm", [128, 64], BF16)
        wTp = sb("wTp", [32, 2, 128], BF16)
        B_w = Buf()
        Ssmp = sb("Ssmp", [NS, 2176], F32)
        B_Ss = Buf(K, dma=True)
        selms = sb("selms", [NS, 2176], BF16)
        B_selms = Buf()
        selTs = sb("selTs", [128, 16, 16], BF16)
        selfs = sb("selfs", [1, 16], F32)
        B_selT = Buf()
        st = sb("st", [NS, 64], F32)
        B_st = Buf()
        pw2 = sb("pw2", [NS, 32], F32)
        B_c = Buf()
        for k in range(NIT):
            dve.op(lambda: nc.vector.memset(pw2[:, k:k + 1], 2.0 ** (-k)), writes=[B_c])

        with ExitStack() as es1:
            def sb1(n, s, d):
                return es1.enter_context(nc.sbuf_tensor("s1_" + n, s, d))

            def ps1(n, s, d=F32):
                return es1.enter_context(nc.psum_tensor("s1_" + n, s, d))
            kig = sb1("kig", [128, 2, 16, 128], BF16)
            B_kig = [Buf(K, dma=True), Buf(K, dma=True)]
            kiTs = sb1("kiTs", [128, 2, 2048], BF16)
            B_kiTs = [Buf(), Buf()]
            rh = sb1("rh", [32, 2, 2, 512], BF16)
            B_rh = [Buf(), Buf()]
            srow = sb1("srow", [1, 1, 2176], F32)
            B_srow = [Buf(K, dma=True)] * 2
            ptr = [ps1(f"ptr{i}", [128, 1024], BF16) for i in range(2)]
            B_ptr = [Buf(), Buf()]
            psh = [ps1(f"psh{i}", [128, 512]) for i in range(2)]
            pso = [ps1(f"pso{i}", [128, 512]) for i in range(2)]
            B_psh = [Buf(), Buf()]
            pss = [ps1(f"pss{i}", [128, 512]) for i in range(2)]
            B_pss = [Buf(), Buf()]
            dve.op(lambda: nc.vector.memset(wperm[:], 0.0), writes=[B_w])
            wi8 = P["wi"][:, 8, :].rearrange("p (a b) -> p a b", b=2)
            dve.op(lambda: nc.vector.tensor_copy(wperm[:, 0:8], wi8[:, :, 0]), reads=[C["B_wi"]], writes=[B_w])
            dve.op(lambda: nc.vector.tensor_copy(wperm[:, 32:40], wi8[:, :, 1]), reads=[C["B_wi"]], writes=[B_w])
            for e in range(2):
                pe.op(lambda: nc.tensor.transpose(ptr[e][0:32, 0:128], wperm[:, e * 32:(e + 1) * 32], ident[:]),
                      reads=[B_w, K.B_ident], writes=[B_ptr[e]])
                act.op(lambda: nc.scalar.copy(out=wTp[:, e, :], in_=ptr[e][0:32, 0:128]), reads=[B_ptr[e]], writes=[B_w])
            nt = 1
            nh = 0
            for i in range(NS):
                o = i % 2
                tok = 1024 + i
                pool.pre(reads=[B_idx], writes=[B_kig[o]])
                for pg in range(16):
                    ins = nc.gpsimd.indirect_dma_start(
                        out=kig[:, o, pg, 0:64], out_offset=None, in_=C["cache_kidx"],
                        in_offset=bass.IndirectOffsetOnAxis(ap=idx[:, i * 16 + pg:i * 16 + pg + 1], axis=0))
                    B_kig[o].dsem.cnt += 16
                    ins.then_inc(B_kig[o].dsem.h, 16)
                    pool.nins += 1
                K.dma_sems[B_kig[o].dsem.uid] = B_kig[o].dsem
                ev = Ev(B_kig[o].dsem, B_kig[o].dsem.cnt)
                pool.post(ev, writes=[B_kig[o]])
                dve.op(lambda: nc.vector.tensor_copy(kig[:, o, :, 64:128], kig[:, o, :, 0:64]), reads=[B_kig[o]],
                       writes=[B_kig[o]])
                for q4 in range(4):
                    x = nt % 2
                    nt += 1
                    pe.pre(reads=[B_kig[o], K.B_ident], writes=[B_ptr[x]])
                    for b in range(4):
                        ins = nc.tensor.transpose(ptr[x][:, b * 128:(b + 1) * 128], kig[:, o, q4 * 4 + b, :], ident[:])
                    ev = pe.done(ins)
                    pe.post(ev, reads=[B_kig[o]], writes=[B_ptr[x]])
                    act.op(lambda: nc.scalar.copy(out=kiTs[:, o, q4 * 512:(q4 + 1) * 512], in_=ptr[x][:, 0:512]),
                           reads=[B_ptr[x]], writes=[B_kiTs[o]])
                for c in range(5):
                    c0 = c * 512
                    cw = 512 if c < 4 else 1
                    x = nh % 2
                    nh += 1
                    if c < 4:
                        rhs_e, rhs_o = kiTs[0:64, o, c0:c0 + cw], kiTs[64:128, o, c0:c0 + cw]
                        deps = [B_kiTs[o]]
                    else:
                        rhs_e, rhs_o = P["kiT8"][0:64, i:i + 1], P["kiT8"][64:128, i:i + 1]
                        deps = [C["B_s8"]]
                    pe.pre(reads=deps + [C["B_qiT"]], writes=[B_psh[x]])
                    nc.tensor.matmul(psh[x][0:8, 0:cw], lhsT=P["qiT"][0:64, :, tok], rhs=rhs_e, start=True, stop=True)
                    ins = nc.tensor.matmul(pso[x][0:8, 0:cw], lhsT=P["qiT"][64:128, :, tok], rhs=rhs_o, start=True,
                                           stop=True)
                    ev = pe.done(ins)
                    pe.post(ev, reads=deps + [C["B_qiT"]], writes=[B_psh[x]])
                    act.op(lambda: nc.scalar.activation(out=rh[0:8, x, 0, 0:cw], in_=psh[x][0:8, 0:cw], func=AF.Relu),
                           reads=[B_psh[x]], writes=[B_rh[x]])
                    act.op(lambda: nc.scalar.activation(out=rh[0:8, x, 1, 0:cw], in_=pso[x][0:8, 0:cw], func=AF.Relu),
                           reads=[B_psh[x]], writes=[B_rh[x]])
                    pe.pre(reads=[B_rh[x], B_w], writes=[B_pss[x]])
                    nc.tensor.matmul(pss[x][0:1, 0:cw], lhsT=wTp[0:8, 0, i:i + 1], rhs=rh[0:8, x, 0, 0:cw], start=True,
                                     stop=False)
                    ins = nc.tensor.matmul(pss[x][0:1, 0:cw], lhsT=wTp[0:8, 1, i:i + 1], rhs=rh[0:8, x, 1, 0:cw],
                                           start=False, stop=True)
                    ev = pe.done(ins)
                    pe.post(ev, reads=[B_rh[x], B_w], writes=[B_pss[x]])
                    dve.op(lambda: nc.vector.tensor_copy(srow[0:1, 0, c0:c0 + cw], pss[x][0:1, 0:cw]), reads=[B_pss[x]],
                           writes=[B_srow[o]])
                sp.dma(C["sscr"][i:i + 1, 0:LK], srow[0:1, 0, 0:LK], B_srow[o].dsem, reads=[B_srow[o]],
                       writes=[C["B_sscr"]])
            K.barrier()
        sp.dma(Ssmp[:, 0:LK], C["sscr"][:, 0:LK], B_Ss.dsem, reads=[C["B_sscr"]], writes=[B_Ss])
        S3 = Ssmp[:, 0:LK]
        dve.op(lambda: nc.vector.tensor_reduce(out=st[:, 32:33], in_=S3, axis=AX.X, op=ALU.max), reads=[B_Ss],
               writes=[B_st])
        dve.op(lambda: nc.vector.tensor_reduce(out=st[:, 33:34], in_=S3, axis=AX.X, op=ALU.min), reads=[B_Ss],
               writes=[B_st])
        dve.op(lambda: nc.vector.tensor_scalar(out=st[:, 33:34], in0=st[:, 33:34], scalar1=-1.0, scalar2=None,
                                               op0=ALU.mult), reads=[B_st], writes=[B_st])
        dve.op(lambda: nc.vector.tensor_tensor(out=st[:, 0:1], in0=st[:, 32:33], in1=st[:, 33:34], op=ALU.max),
               reads=[B_st], writes=[B_st])
        topk_threshold(K, S3, NS, selms[:, 0:LK], st, B_Ss, B_selms, B_st, pw2, B_c)
        dve.op(lambda: nc.vector.tensor_scalar(out=selms[:, 0:LK], in0=S3, scalar1=st[:, 1:2], scalar2=None,
                                               op0=ALU.is_ge), reads=[B_Ss, B_st], writes=[B_selms])

        with ExitStack() as es2:
            def sb2(n, s, d):
                return es2.enter_context(nc.sbuf_tensor("s2_" + n, s, d))

            def ps2(n, s, d=F32):
                return es2.enter_context(nc.psum_tensor("s2_" + n, s, d))
            Kg = sb2("Kg", [128, 2, 16, 256], BF16)
            B_Kg = [Buf(K, dma=True), Buf(K, dma=True)]
            Vc = sb2("Vc", [128, 1, 16, 256], BF16)
            B_Vc = [Buf(K, dma=True)] * 2
            Vg = sb2("Vg", [128, 2, 16, 2, 130], BF16)
            B_Vg = [Buf(), Buf()]
            kTs = sb2("kTs", [128, 2, 2, 2048], BF16)
            B_kTs = [Buf(), Buf()]
            vself = sb2("vself", [1, 16, 2, 130], BF16)
            B_vs = Buf(K, dma=True)
            pTs = sb2("pTs", [128, 2, 128], BF16)
            B_pTs = [Buf(), Buf()]
            pms = sb2("pms", [128, 2, 128], BF16)
            B_pms = [Buf(), Buf()]
            pself = sb2("pself", [1, 2, 8], BF16)
            pselfm = sb2("pselfm", [1, 2, 8], BF16)
            B_pself = [Buf(), Buf()]
            osm = sb2("osm", [4, 2, 2, 128], F32)
            B_osm = [Buf(K, dma=True), Buf(K, dma=True)]
            rec = sb2("rec", [4, 4], F32)
            B_rec = Buf()
            ptr = [ps2(f"ptr{i}", [128, 1024], BF16) for i in range(2)]
            B_ptr = [Buf(), Buf()]
            pl = [ps2(f"pl{i}", [128, 512]) for i in range(2)]
            B_pl = [Buf(), Buf()]
            psf = ps2("psf", [128, 512])
            B_psf = Buf()
            pos = [ps2(f"pos{i}", [128, 512]) for i in range(2)]
            B_pos = [Buf(), Buf()]
            pe.pre(reads=[B_selms, K.B_ident], writes=[B_ptr[0]])
            for pg in range(16):
                ins = nc.tensor.transpose(ptr[0][:, pg * 16:(pg + 1) * 16], selms[0:NS, pg * 128:(pg + 1) * 128],
                                          ident[0:NS, 0:NS])
            ev = pe.done(ins)
            pe.post(ev, reads=[B_selms], writes=[B_ptr[0]])
            act.op(lambda: nc.scalar.copy(out=selTs[:].rearrange("p a b -> p (a b)"), in_=ptr[0][:, 0:256]),
                   reads=[B_ptr[0]], writes=[B_selT])
            pe.op(lambda: nc.tensor.transpose(ptr[1][0:1, 0:NS], selms[0:NS, 2048:2049], ident[0:NS, 0:NS]),
                  reads=[B_selms, K.B_ident], writes=[B_ptr[1]])
            act.op(lambda: nc.scalar.copy(out=selfs[0:1, :], in_=ptr[1][0:1, 0:NS]), reads=[B_ptr[1]], writes=[B_selT])
            pool.op(lambda: nc.gpsimd.memset(vself[:], 1.0), writes=[B_vs])
            pool.dma(vself[0:1, :, :, 0:128], C["vo"][1024:1040, :].rearrange("(o i) (g d) -> o i g d", o=1, g=2),
                     B_vs.dsem, writes=[B_vs])
            for o in range(2):
                pool.op(lambda: nc.gpsimd.memset(Vg[:, o, :, :, 128:130], 1.0), writes=[B_Vg[o]])
            nt = 0
            for i in range(NS):
                o = i % 2
                tok = 1024 + i
                for (dst, Bd, srcc, oo) in ((Kg, B_Kg, C["cache_k"], o), (Vc, B_Vc, C["cache_v"], 0)):
                    pool.pre(reads=[B_idx], writes=[Bd[o]])
                    for pg in range(16):
                        ins = nc.gpsimd.indirect_dma_start(
                            out=dst[:, oo, pg, :], out_offset=None, in_=srcc,
                            in_offset=bass.IndirectOffsetOnAxis(ap=idx[:, i * 16 + pg:i * 16 + pg + 1], axis=0))
                        Bd[o].dsem.cnt += 16
                        ins.then_inc(Bd[o].dsem.h, 16)
                        pool.nins += 1
                    K.dma_sems[Bd[o].dsem.uid] = Bd[o].dsem
                    ev = Ev(Bd[o].dsem, Bd[o].dsem.cnt)
                    pool.post(ev, writes=[Bd[o]])
                act.op(lambda: nc.scalar.copy(out=Vg[:, o, :, :, 0:128],
                                              in_=Vc[:, 0, :, :].rearrange("p a (g d) -> p a g d", g=2)),
                       reads=[B_Vc[o]], writes=[B_Vg[o]])
                for pg4 in range(8):
                    x = nt % 2
                    nt += 1
                    pe.pre(reads=[B_Kg[o], K.B_ident], writes=[B_ptr[x]])
                    for b in range(4):
                        pg, g = (pg4 * 4 + b) // 2, (pg4 * 4 + b) % 2
                        ins = nc.tensor.transpose(ptr[x][:, b * 128:(b + 1) * 128], Kg[:, o, pg, g * 128:(g + 1) * 128],
                                                  ident[:])
                    ev = pe.done(ins)
                    pe.post(ev, reads=[B_Kg[o]], writes=[B_ptr[x]])
                    dstv = kTs[:, o, :, pg4 * 256:(pg4 + 1) * 256].rearrange("p g (a l) -> p a g l", a=2)
                    srcv = ptr[x][:, 0:512].rearrange("p (a g l) -> p a g l", a=2, g=2)
                    eng, veng = (act, None) if pg4 % 2 == 0 else (dve, None)
                    if pg4 % 2 == 0:
                        act.op(lambda: nc.scalar.copy(out=dstv, in_=srcv), reads=[B_ptr[x]], writes=[B_kTs[o]])
                    else:
                        dve.op(lambda: nc.vector.tensor_copy(dstv, srcv), reads=[B_ptr[x]], writes=[B_kTs[o]])
                pe.pre(reads=[B_kTs[o], C["B_qT"]], writes=[B_pl[o]])
                for pg in range(16):
                    for g in range(2):
                        ins = nc.tensor.matmul(pl[o][:, (pg * 2 + g) * 4:(pg * 2 + g) * 4 + 4],
                                               lhsT=kTs[:, o, g, pg * 128:(pg + 1) * 128],
                                               rhs=P["qT"][:, g * 4:(g + 1) * 4, tok], start=True, stop=True,
                                               skip_group_check=True)
                ev = pe.done(ins)
                pe.post(ev, reads=[B_kTs[o], C["B_qT"]], writes=[B_pl[o]])
                pe.pre(reads=[C["B_s8"], C["B_qT"]], writes=[B_psf])
                for g in range(2):
                    ins = nc.tensor.matmul(psf[0:1, g * 4:(g + 1) * 4], lhsT=P["kT8"][:, g * 128 + i:g * 128 + i + 1],
                                           rhs=P["qT"][:, g * 4:(g + 1) * 4, tok], start=True, stop=True,
                                           skip_group_check=True)
                ev = pe.done(ins)
                pe.post(ev, reads=[C["B_s8"], C["B_qT"]], writes=[B_psf])
                act.op(lambda: nc.scalar.activation(out=pTs[:, o, :], in_=pl[o][:, 0:128], func=AF.Exp, scale=SQ),
                       reads=[B_pl[o]], writes=[B_pTs[o]])
                act.op(lambda: nc.scalar.activation(out=pself[0:1, o, :], in_=psf[0:1, 0:8], func=AF.Exp, scale=SQ),
                       reads=[B_psf], writes=[B_pself[o]])
                dve.op(lambda: nc.vector.tensor_tensor(
                    out=pms[:, o, :].rearrange("p (a b) -> p a b", a=16),
                    in0=pTs[:, o, :].rearrange("p (a b) -> p a b", a=16),
                    in1=selTs[:, :, i].unsqueeze(2).to_broadcast([128, 16, 8]), op=ALU.mult),
                    reads=[B_pTs[o], B_selT], writes=[B_pms[o]])
                dve.op(lambda: nc.vector.tensor_scalar(out=pselfm[0:1, o, :], in0=pself[0:1, o, :],
                                                       scalar1=selfs[0:1, i:i + 1], scalar2=None, op0=ALU.mult),
                       reads=[B_pself[o], B_selT], writes=[B_pself[o]])
                for g in range(2):
                    pe.pre(reads=[B_pms[o], B_Vg[o], B_pself[o], B_vs], writes=[B_pos[g]])
                    for pg in range(16):
                        nc.tensor.matmul(pos[g][0:4, 0:129], lhsT=pms[:, o, (pg * 2 + g) * 4:(pg * 2 + g) * 4 + 4],
                                         rhs=Vg[:, o, pg, g, 0:129], start=(pg == 0), stop=False)
                    ins = nc.tensor.matmul(pos[g][0:4, 0:129], lhsT=pselfm[0:1, o, g * 4:(g + 1) * 4],
                                           rhs=vself[0:1, i, g, 0:129], start=False, stop=True)
                    ev = pe.done(ins)
                    pe.post(ev, reads=[B_pms[o], B_Vg[o], B_pself[o], B_vs], writes=[B_pos[g]])
                    dve.op(lambda: nc.vector.reciprocal(out=rec[0:4, g:g + 1], in_=pos[g][0:4, 128:129]),
                           reads=[B_pos[g]], writes=[B_rec])
                    dve.op(lambda: nc.vector.tensor_scalar(out=osm[0:4, o, g, :], in0=pos[g][0:4, 0:128],
                                                           scalar1=rec[0:4, g:g + 1], scalar2=None, op0=ALU.mult),
                           reads=[B_pos[g], B_rec], writes=[B_osm[o]])
                    sp.dma(C["oscr"][i, g * 512:(g + 1) * 512].rearrange("(h d) -> h d", h=4), osm[0:4, o, g, :],
                           B_osm[o].dsem, reads=[B_osm[o]], writes=[C["B_oscr"]])
            K.barrier()
        pool.dma(P["oat"][0:NS, 8, :], C["oscr"][:, :], C["B_oat"].dsem, reads=[C["B_oscr"]], writes=[C["B_oat"]])
        K.end_phase()
def phase_g1(K, C):
    nc = K.nc
    pe, act, dve, pool, sp = K.pe, K.act, K.dve, K.pool, K.sp
    P = C["P"]
    with ExitStack() as es:
        def sb(n, s, d):
            return es.enter_context(nc.sbuf_tensor("g1_" + n, s, d))

        def ps(n, s, d=F32):
            return es.enter_context(nc.psum_tensor("g1_" + n, s, d))
        triu = sb("triu", [128, 128], F32)
        B_tri = Buf()
        sst = sb("sst", [128, 2, 4, 256], F32)
        B_sst = [Buf(K, dma=True), Buf(K, dma=True)]
        pa = [ps(f"pa{i}", [128, 512]) for i in range(2)]
        B_pa = [Buf(), Buf()]
        pl = [ps(f"pl{i}", [128, 512]) for i in range(2)]
        B_pl = [Buf(), Buf()]
        pool.op(lambda: nc.gpsimd.memset(triu[:], 1.0), writes=[B_tri])
        pool.op(lambda: nc.gpsimd.affine_select(out=triu[:], in_=triu[:], pattern=[[1, 128]], compare_op=ALU.is_ge,
                                                fill=0.0, base=0, channel_multiplier=-1),
                reads=[B_tri], writes=[B_tri])
        sp.dma(C["dec_in"], P["dec"][:].rearrange("p h t -> p (h t)"), C["B_dec"].dsem, reads=[C["B_dec"]],
               writes=[C["B_decin"]])
        all_gather(K, C["dec_in"], C["dec_out"], [C["B_decin"]], [C["B_deco"]])
        n = 0
        for t in range(8):
            ts = slice(t * 128, (t + 1) * 128)
            o = t % 2
            for h in range(4):
                i = n % 2
                n += 1
                pe.op(lambda: nc.tensor.matmul(pa[i][:, 0:128], lhsT=P["kgT"][:, h, ts], rhs=P["qgT"][:, h, ts],
                                               start=True, stop=True),
                      reads=[C["B_kgT"], C["B_qgT"]], writes=[B_pa[i]])
                dve.op(lambda: nc.vector.tensor_tensor(out=P["AT"][:, t, h, :], in0=pa[i][:, 0:128], in1=triu[:],
                                                       op=ALU.mult), reads=[B_pa[i], B_tri], writes=[C["B_AT"]])
                pe.op(lambda: nc.tensor.matmul(pl[i][:, 0:256], lhsT=P["khat"][:, t, h * 128:(h + 1) * 128],
                                               rhs=P["gv"][:, t, h * 256:(h + 1) * 256], start=True, stop=True),
                      reads=[C["B_khat"], C["B_gv"]], writes=[B_pl[i]])
                act.op(lambda: nc.scalar.copy(out=sst[:, o, h, :], in_=pl[i][:, 0:256]), reads=[B_pl[i]],
                       writes=[B_sst[o]])
            sp.dma(C["gst_in"][t].rearrange("(h p) v -> p h v", p=128), sst[:, o], B_sst[o].dsem, reads=[B_sst[o]],
                   writes=[C["B_gin"][t]])
            all_gather(K, C["gst_in"][t], C["gst_out"][t], [C["B_gin"][t]], [C["B_gout"][t]])
        K.end_phase()


def phase_g2(K, C):
    nc = K.nc
    pe, act, dve, pool, sp = K.pe, K.act, K.dve, K.pool, K.sp
    P = C["P"]
    ident = C["ident"]
    with ExitStack() as es:
        def sb(n, s, d):
            return es.enter_context(nc.sbuf_tensor("g2_" + n, s, d))

        def ps(n, s, d=F32):
            return es.enter_context(nc.psum_tensor("g2_" + n, s, d))
        Sg = sb("Sg", [128, 4, 4, 256], F32)
        B_Sg = Buf(K, dma=True)
        dg = sb("dg", [128, 4, 32], F32)
        B_dg = Buf(K, dma=True)
        oh = sb("oh", [128, 4], F32)
        gnw = sb("gnw", [128, 256], F32)
        B_c = Buf(K, dma=True)
        Srun = sb("Srun", [128, 4, 256], F32)
        B_run = Buf(K, dma=True)
        Sin = sb("Sin", [128, 4, 256], F32)
        B_in = Buf()
        Sinb = sb("Sinb", [128, 4, 256], BF16)
        B_inb = Buf()
        st = sb("st", [128, 16], F32)
        B_st = Buf()
        junk = sb("junk", [128, 256], BF16)
        B_junk = Buf()
        po = [ps(f"po{i}", [128, 512]) for i in range(2)]
        B_po = [Buf(), Buf()]

        sp.dma(oh[:], C["onehot"], B_c.dsem, writes=[B_c])
        sp.dma(gnw[:], C["gla_norm_w"].rearrange("(o d) -> o d", o=1).to_broadcast([128, 256]), B_c.dsem, writes=[B_c])
        sp.dma(dg[:], C["dec_out"].rearrange("(r p) c -> p r c", p=128), B_dg.dsem, reads=[C["B_deco"]], writes=[B_dg])
        dve.op(lambda: nc.vector.memset(Srun[:], 0.0), writes=[B_run])

        def head_norm(pz, B_pz, np_, out_ap, B_out):
            dve.op(lambda: nc.vector.memset(st[0:np_, 0:1], 0.0), writes=[B_st])
            act.op(lambda: nc.scalar.activation(out=junk[0:np_, :], in_=pz, func=AF.Square,
                                                accum_out=st[0:np_, 0:1]), reads=[B_pz, B_st], writes=[B_junk, B_st])
            act.op(lambda: nc.scalar.activation(out=st[0:np_, 1:2], in_=st[0:np_, 0:1], func=AF.Sqrt,
                                                scale=1.0 / 256.0, bias=EPS), reads=[B_st], writes=[B_st])
            dve.op(lambda: nc.vector.reciprocal(out=st[0:np_, 1:2], in_=st[0:np_, 1:2]), reads=[B_st], writes=[B_st])
            dve.op(lambda: nc.vector.scalar_tensor_tensor(out=out_ap, in0=pz, scalar=st[0:np_, 1:2],
                                                          in1=gnw[0:np_, :], op0=ALU.mult, op1=ALU.mult),
                   reads=[B_pz, B_st, B_c], writes=[B_out])

        n = 0
        for s in range(8):
            ts = slice(s * 128, (s + 1) * 128)
            sp.dma(Sg[:], C["gst_out"][s].rearrange("(r h p) v -> p r h v", p=128, h=4), B_Sg.dsem,
                   reads=[C["B_gout"][s]], writes=[B_Sg])
            dve.op(lambda: nc.vector.memset(Sin[:], 0.0), reads=[], writes=[B_in])
            for r in range(4):
                dve.op(lambda: nc.vector.scalar_tensor_tensor(out=Sin[:], in0=Srun[:], scalar=oh[:, r:r + 1],
                                                              in1=Sin[:], op0=ALU.mult, op1=ALU.add),
                       reads=[B_run, B_c, B_in], writes=[B_in])
                for h in range(4):
                    dve.op(lambda: nc.vector.scalar_tensor_tensor(out=Srun[:, h, :], in0=Srun[:, h, :],
                                                                  scalar=dg[:, r, h * 8 + s:h * 8 + s + 1],
                                                                  in1=Sg[:, r, h, :], op0=ALU.mult, op1=ALU.add),
                           reads=[B_run, B_dg, B_Sg], writes=[B_run])
            act.op(lambda: nc.scalar.copy(out=Sinb[:], in_=Sin[:]), reads=[B_in], writes=[B_inb])
            for h in range(4):
                i = n % 2
                n += 1
                pe.pre(reads=[C["B_AT"], C["B_gv"], C["B_qgT"], B_inb], writes=[B_po[i]])
                nc.tensor.matmul(po[i][:, 0:256], lhsT=P["AT"][:, s, h, :], rhs=P["gv"][:, s, h * 256:(h + 1) * 256],
                                 start=True, stop=False)
                ins = nc.tensor.matmul(po[i][:, 0:256], lhsT=P["qgT"][:, h, ts], rhs=Sinb[:, h, :],
                                       start=False, stop=True)
                ev = pe.done(ins)
                pe.post(ev, reads=[C["B_AT"], C["B_gv"], C["B_qgT"], B_inb], writes=[B_po[i]])
                head_norm(po[i][:, 0:256], B_po[i], 128, P["og"][:, s, h * 256:(h + 1) * 256], C["B_og"])
        sp.dma(C["gla_p"].rearrange("(h p) v -> p h v", p=128), Srun[:], B_run.dsem, reads=[B_run])

        S0 = sb("S0", [128, 2, 4, 256], F32)
        B_S0 = [Buf(K, dma=True), Buf(K, dma=True)]
        Sn = sb("Sn", [128, 2, 4, 256], F32)
        B_Sn = [Buf(K, dma=True), Buf(K, dma=True)]
        Snb = sb("Snb", [128, 2, 4, 256], BF16)
        B_Snb = [Buf(), Buf()]
        tmp = sb("tmp", [128, 2, 256], F32)
        B_tmp = [Buf(), Buf()]
        Bsel = sb("Bsel", [128, 16, 128], BF16)
        I16 = sb("I16", [128, 16, 16], BF16)
        Qpad = sb("Qpad", [128, 4, 16, 16], BF16)
        B_q = Buf()
        pb = [ps(f"pb{i}", [128, 512]) for i in range(2)]
        B_pb = [Buf(), Buf()]
        pso = [ps(f"pso{i}", [128, 512]) for i in range(2)]
        B_pso = Buf()
        dve.op(lambda: nc.vector.tensor_copy(Bsel[:], ident[:, 0:16].unsqueeze(2).to_broadcast([128, 16, 128])),
               reads=[K.B_ident], writes=[B_q])
        dve.op(lambda: nc.vector.memset(I16[:], 0.0), writes=[B_q])
        for i in range(16):
            dve.op(lambda: nc.vector.memset(I16[:, i, i:i + 1], 1.0), writes=[B_q])
        for h in range(4):
            dve.op(lambda: nc.vector.tensor_tensor(out=Qpad[:, h], in0=P["qgT"][:, h, 1024:1040].unsqueeze(2).to_broadcast(
                [128, 16, 16]), in1=I16[:], op=ALU.mult), reads=[C["B_qgT"], B_q], writes=[B_q])
        stin = C["state_in"].rearrange("(i h p) v -> i p h v", p=128, h=4)
        stout = C["gla_s"].rearrange("(i h p) v -> i p h v", p=128, h=4)
        pe.pre(writes=[B_pso])
        for i in range(16):
            o = i % 2
            sp.dma(S0[:, o], stin[i], B_S0[o].dsem, writes=[B_S0[o]])
            for h in range(4):
                x = n % 2
                n += 1
                pe.op(lambda: nc.tensor.matmul(pb[x][:, 0:256], lhsT=Bsel[:, i, :], rhs=P["gv"][:, 8, h * 256:(h + 1) * 256],
                                               start=True, stop=True), reads=[B_q, C["B_gv"]], writes=[B_pb[x]])
                dve.op(lambda: nc.vector.tensor_scalar(out=tmp[:, x, :], in0=pb[x][:, 0:256],
                                                       scalar1=P["kg8f"][:, h, i:i + 1], scalar2=None, op0=ALU.mult),
                       reads=[B_pb[x], C["B_kgT"]], writes=[B_tmp[x]])
                dve.op(lambda: nc.vector.scalar_tensor_tensor(out=Sn[:, o, h, :], in0=S0[:, o, h, :],
                                                              scalar=P["dec8"][:, h, i:i + 1], in1=tmp[:, x, :],
                                                              op0=ALU.mult, op1=ALU.add),
                       reads=[B_S0[o], B_tmp[x], C["B_dec"]], writes=[B_Sn[o]])
            sp.dma(stout[i], Sn[:, o], B_Sn[o].dsem, reads=[B_Sn[o]])
            act.op(lambda: nc.scalar.copy(out=Snb[:, o], in_=Sn[:, o]), reads=[B_Sn[o]], writes=[B_Snb[o]])
            pe.pre(reads=[B_Snb[o], B_q])
            for h in range(4):
                ins = nc.tensor.matmul(pso[h // 2][0:16, (h % 2) * 256:(h % 2) * 256 + 256], lhsT=Qpad[:, h, i, :],
                                       rhs=Snb[:, o, h, :], start=(i == 0 and h % 2 == 0), stop=(i == 15),
                                       skip_group_check=True)
            ev = pe.done(ins)
            pe.post(ev, reads=[B_Snb[o], B_q], writes=[B_pso])
        for h in range(4):
            head_norm(pso[h // 2][0:16, (h % 2) * 256:(h % 2) * 256 + 256], B_pso, 16,
                      P["og"][0:16, 8, h * 256:(h + 1) * 256], C["B_og"])
        K.end_phase()
def phase_c(K, C):
    nc = K.nc
    pe, act, dve, pool, sp = K.pe, K.act, K.dve, K.pool, K.sp
    ident = C["ident"]
    win_v = C["w_in"].rearrange("(k p) f -> p k f", p=128)
    wpa_v = C["w_proj_attn"].rearrange("(k p) f -> p k f", p=128)
    wpg_v = C["w_proj_gla"].rearrange("(k p) f -> p k f", p=128)
    wo_v = C["w_out"].rearrange("(k p) f -> p k f", p=128)
    with ExitStack() as esm:
        def sbm(n, s, d):
            return esm.enter_context(nc.sbuf_tensor("c_" + n, s, d))
        mT = sbm("mT", [128, 16, TOK], BF16)
        B_mT = [Buf() for _ in range(NT)]
        with ExitStack() as esg:
            merged = esg.enter_context(nc.sbuf_tensor("c_merged", [128, NT, D], BF16))
            B_mg = [Buf() for _ in range(NT)]
            with ExitStack() as es:
                def sb(n, s, d):
                    return es.enter_context(nc.sbuf_tensor("c_" + n, s, d))

                def ps(n, s, d=F32):
                    return es.enter_context(nc.psum_tensor("c_" + n, s, d))
                uT = sb("uT", [128, 16, TOK], BF16)
                B_uT = [Buf() for _ in range(NT)]
                oatT = sb("oatT", [128, 8, TOK], BF16)
                ogT = sb("ogT", [128, 8, TOK], BF16)
                B_oatT = [Buf() for _ in range(NT)]
                B_ogT = [Buf() for _ in range(NT)]
                ptr = [ps(f"ptr{i}", [128, 1024], BF16) for i in range(2)]
                B_ptr = [Buf(), Buf()]
                norm_T(K, C["h1"], C["mix_pre_w"], uT, B_uT, ident, ptr, B_ptr, "c1_")
                pp = [ps(f"pp{i}", [128, 512]) for i in range(4)]
                B_pp = [Buf() for _ in range(4)]
                wg = sb("wg", [128, 2, 16, 512], BF16)
                B_wg = [Buf(K, dma=True), Buf(K, dma=True)]
                wp = sb("wp", [128, 2, 8, 512], BF16)
                B_wp = [Buf(K, dma=True), Buf(K, dma=True)]
                ost = sb("ost", [128, 1, 1024], BF16)
                B_ost = [Buf(K, dma=True)] * 2
                gst = sb("gst", [128, 1, 1024], BF16)
                B_gst = [Buf(K, dma=True)] * 2
                sg = sb("sg", [128, 4, 512], BF16)
                B_sg = [Buf() for _ in range(4)]
                tm = sb("tm", [128, 2, 512], F32)
                B_tm = [Buf(), Buf()]
                npp = [0]
                ntr = [0]

                def tr8(src_slot_ap, B_src, dstT, B_dst, t):
                    for half in range(2):
                        x = ntr[0] % 2
                        ntr[0] += 1
                        pe.pre(reads=[B_src, K.B_ident], writes=[B_ptr[x]])
                        for b in range(4):
                            k = half * 4 + b
                            ins = nc.tensor.transpose(ptr[x][:, b * 128:(b + 1) * 128],
                                                      src_slot_ap[:, k * 128:(k + 1) * 128], ident[:])
                        ev = pe.done(ins)
                        pe.post(ev, reads=[B_src], writes=[B_ptr[x]])
                        act.op(lambda: nc.scalar.copy(out=dstT[:, half * 4:half * 4 + 4, t * 128:(t + 1) * 128],
                                                      in_=ptr[x][:, 0:512].rearrange("p (a b) -> p a b", a=4)),
                               reads=[B_ptr[x]], writes=[B_dst[t]])

                oat_d = C["oat_d"].bitcast(BF16)
                og_d = C["og_d"].bitcast(BF16)
                for t in range(NT):
                    o = t % 2
                    sp.dma(ost[:, 0, :], oat_d[t * 128:(t + 1) * 128, :], B_ost[o].dsem, writes=[B_ost[o]])
                    tr8(ost[:, 0, :], B_ost[o], oatT, B_oatT, t)
                for blk in range(2):
                    pool.dma(wg[:, blk, :, :], win_v[:, :, CGR + blk * 512:CGR + (blk + 1) * 512], B_wg[blk].dsem,
                             writes=[B_wg[blk]])
                for t in range(NT):
                    o = t % 2
                    sp.dma(gst[:, 0, :], og_d[t * 128:(t + 1) * 128, :], B_gst[o].dsem, writes=[B_gst[o]])
                    for blk in range(2):
                        i = npp[0] % 4
                        npp[0] += 1
                        pe.pre(reads=[B_uT[t], B_wg[blk]], writes=[B_pp[i]])
                        for k in range(16):
                            ins = nc.tensor.matmul(pp[i][:, :], lhsT=uT[:, k, t * 128:(t + 1) * 128], rhs=wg[:, blk, k, :],
                                                   start=(k == 0), stop=(k == 15))
                        ev = pe.done(ins)
                        pe.post(ev, reads=[B_uT[t], B_wg[blk]], writes=[B_pp[i]])
                        act.op(lambda: nc.scalar.activation(out=sg[:, i, :], in_=pp[i][:, :], func=AF.Silu),
                               reads=[B_pp[i]], writes=[B_sg[i]])
                        dve.op(lambda: nc.vector.tensor_tensor(out=gst[:, 0, blk * 512:(blk + 1) * 512],
                                                               in0=gst[:, 0, blk * 512:(blk + 1) * 512], in1=sg[:, i, :],
                                                               op=ALU.mult), reads=[B_sg[i], B_gst[o]], writes=[B_gst[o]])
                    tr8(gst[:, 0, :], B_gst[o], ogT, B_ogT, t)
                for nb in range(4):
                    cs = slice(nb * 512, (nb + 1) * 512)
                    pool.dma(wg[:, 0, :, :], win_v[:, :, CGA + nb * 512:CGA + (nb + 1) * 512], B_wg[0].dsem,
                             writes=[B_wg[0]])
                    pool.dma(wg[:, 1, :, :], win_v[:, :, CGG + nb * 512:CGG + (nb + 1) * 512], B_wg[1].dsem,
                             writes=[B_wg[1]])
                    pool.dma(wp[:, 0, :, :], wpa_v[:, :, cs], B_wp[0].dsem, writes=[B_wp[0]])
                    pool.dma(wp[:, 1, :, :], wpg_v[:, :, cs], B_wp[1].dsem, writes=[B_wp[1]])
                    for t in range(NT):
                        ts = slice(t * 128, (t + 1) * 128)
                        ids = []
                        for which in range(4):
                            i = npp[0] % 4
                            npp[0] += 1
                            ids.append(i)
                            if which < 2:
                                srcT, Bs, w, Bw, nk = uT, B_uT, wg[:, which], B_wg[which], 16
                            elif which == 2:
                                srcT, Bs, w, Bw, nk = oatT, B_oatT, wp[:, 0], B_wp[0], 8
                            else:
                                srcT, Bs, w, Bw, nk = ogT, B_ogT, wp[:, 1], B_wp[1], 8
                            pe.pre(reads=[Bs[t], Bw], writes=[B_pp[i]])
                            for k in range(nk):
                                ins = nc.tensor.matmul(pp[i][:, :], lhsT=srcT[:, k, ts], rhs=w[:, k, :],
                                                       start=(k == 0), stop=(k == nk - 1))
                            ev = pe.done(ins)
                            pe.post(ev, reads=[Bs[t], Bw], writes=[B_pp[i]])
                            if which < 2:
                                act.op(lambda: nc.scalar.activation(out=sg[:, i, :], in_=pp[i][:, :], func=AF.Sigmoid),
                                       reads=[B_pp[i]], writes=[B_sg[i]])
                        ia, ig, ipa, ipg = ids
                        x = t % 2
                        dve.op(lambda: nc.vector.tensor_tensor(out=tm[:, x, :], in0=sg[:, ia, :], in1=pp[ipa][:, :],
                                                               op=ALU.mult), reads=[B_sg[ia], B_pp[ipa]], writes=[B_tm[x]])
                        dve.op(lambda: nc.vector.tensor_tensor(out=sg[:, ig, :], in0=sg[:, ig, :], in1=pp[ipg][:, :],
                                                               op=ALU.mult), reads=[B_sg[ig], B_pp[ipg]], writes=[B_sg[ig]])
                        pool.op(lambda: nc.gpsimd.tensor_tensor(out=merged[:, t, cs], in0=tm[:, x, :], in1=sg[:, ig, :],
                                                                op=ALU.add), reads=[B_tm[x], B_sg[ig]], writes=[B_mg[t]])
                K.barrier()
            with ExitStack() as es:
                ptr = [es.enter_context(nc.psum_tensor(f"c2_ptr{i}", [128, 1024], BF16)) for i in range(2)]
                B_ptr = [Buf(), Buf()]
                n = 0
                for t in range(NT):
                    for q4 in range(4):
                        x = n % 2
                        n += 1
                        pe.pre(reads=[B_mg[t], K.B_ident], writes=[B_ptr[x]])
                        for b in range(4):
                            k = q4 * 4 + b
                            ins = nc.tensor.transpose(ptr[x][:, b * 128:(b + 1) * 128], merged[:, t, k * 128:(k + 1) * 128],
                                                      ident[:])
                        ev = pe.done(ins)
                        pe.post(ev, reads=[B_mg[t]], writes=[B_ptr[x]])
                        if q4 % 2 == 0:
                            act.op(lambda: nc.scalar.copy(out=mT[:, q4 * 4:q4 * 4 + 4, t * 128:(t + 1) * 128],
                                                          in_=ptr[x][:, 0:512].rearrange("p (a b) -> p a b", a=4)),
                                   reads=[B_ptr[x]], writes=[B_mT[t]])
                        else:
                            dve.op(lambda: nc.vector.tensor_copy(mT[:, q4 * 4:q4 * 4 + 4, t * 128:(t + 1) * 128],
                                                                 ptr[x][:, 0:512].rearrange("p (a b) -> p a b", a=4)),
                                   reads=[B_ptr[x]], writes=[B_mT[t]])
                K.barrier()
        with ExitStack() as es:
            def sb(n, s, d):
                return es.enter_context(nc.sbuf_tensor("c3_" + n, s, d))
            wo = sb("wo", [128, 16, D], BF16)
            B_wo = Buf(K, dma=True)
            wbc = sb("wbc", [128, D], F32)
            hst = sb("hst", [128, 2, D], F32)
            B_hst = [Buf(K, dma=True), Buf(K, dma=True)]
            ot = sb("ot", [128, 2, D], F32)
            B_ot = [Buf(K, dma=True), Buf(K, dma=True)]
            junk = sb("junk", [128, 512], BF16)
            B_junk = Buf()
            st = sb("st", [128, 8 * NT], F32)
            B_st = Buf()
            po = [es.enter_context(nc.psum_tensor(f"c3_po{i}", [128, 512])) for i in range(8)]
            B_po = [Buf() for _ in range(8)]
            for nb in range(4):
                pool.dma(wo[:, :, nb * 512:(nb + 1) * 512], wo_v[:, :, nb * 512:(nb + 1) * 512], B_wo.dsem, writes=[B_wo])
            sp.dma(wbc[:], C["mix_post_w"].rearrange("(o d) -> o d", o=1).to_broadcast([128, D]), B_wo.dsem,
                   writes=[B_wo])
            dve.op(lambda: nc.vector.memset(st[:], 0.0), writes=[B_st])
            for t in range(NT):
                o = t % 2
                ts = slice(t * 128, (t + 1) * 128)
                sp.dma(hst[:, o, :], C["h1"][ts, :], B_hst[o].dsem, writes=[B_hst[o]])
                for nb in range(4):
                    i = o * 4 + nb
                    pe.pre(reads=[B_mT[t], B_wo], writes=[B_po[i]])
                    for k in range(16):
                        ins = nc.tensor.matmul(po[i][:, :], lhsT=mT[:, k, ts], rhs=wo[:, k, nb * 512:(nb + 1) * 512],
                                               start=(k == 0), stop=(k == 15))
                    ev = pe.done(ins)
                    pe.post(ev, reads=[B_mT[t], B_wo], writes=[B_po[i]])
                    act.op(lambda: nc.scalar.activation(out=junk[:], in_=po[i][:, :], func=AF.Square,
                                                        accum_out=st[:, t * 8 + nb:t * 8 + nb + 1]),
                           reads=[B_po[i], B_st], writes=[B_junk, B_st])
                c = t * 8
                dve.op(lambda: nc.vector.tensor_reduce(out=st[:, c + 4:c + 5], in_=st[:, c:c + 4], axis=AX.X, op=ALU.add),
                       reads=[B_st], writes=[B_st])
                act.op(lambda: nc.scalar.activation(out=st[:, c + 5:c + 6], in_=st[:, c + 4:c + 5], func=AF.Sqrt,
                                                    scale=1.0 / D, bias=EPS), reads=[B_st], writes=[B_st])
                dve.op(lambda: nc.vector.reciprocal(out=st[:, c + 5:c + 6], in_=st[:, c + 5:c + 6]), reads=[B_st],
                       writes=[B_st])
                for nb in range(4):
                    i = o * 4 + nb
                    cs = slice(nb * 512, (nb + 1) * 512)
                    dve.op(lambda: nc.vector.scalar_tensor_tensor(out=ot[:, o, cs], in0=po[i][:, :],
                                                                  scalar=st[:, c + 5:c + 6], in1=wbc[:, cs],
                                                                  op0=ALU.mult, op1=ALU.mult),
                           reads=[B_po[i], B_st, B_wo], writes=[B_ot[o]])
                pool.op(lambda: nc.gpsimd.tensor_tensor(out=ot[:, o, :], in0=ot[:, o, :], in1=hst[:, o, :], op=ALU.add),
                        reads=[B_hst[o], B_ot[o]], writes=[B_ot[o]])
                sp.dma(C["h2"][ts, :], ot[:, o, :], B_ot[o].dsem, reads=[B_ot[o]])
            K.barrier()


WNAMES = [("ffn1_pre_w", [D]), ("ffn1_w_gate", [D, DFF]), ("ffn1_w_up", [D, DFF]), ("ffn1_w_down", [DFF, D]),
          ("ffn1_post_w", [D]), ("mix_pre_w", [D]), ("w_in", [D, DIN]), ("gla_gate_w2", [16, 512]),
          ("gla_gate_b", [512]), ("gla_norm_w", [256]), ("w_proj_attn", [1024, D]), ("w_proj_gla", [1024, D]),
          ("w_out", [D, D]), ("mix_post_w", [D]), ("ffn2_pre_w", [D]), ("ffn2_w_gate", [D, DFF]),
          ("ffn2_w_up", [D, DFF]), ("ffn2_w_down", [DFF, D]), ("ffn2_post_w", [D])]
NPOOL_ROWS = 2560 * 128


def build(stage=99, debug=False):
    K = Kern()
    nc = K.nc
    C = {}
    full = stage >= 4
    x = K.dram("x", [TOK, D], F32, "ExternalInput").ap()
    C["posv"] = K.dram("posv", [128, NT], F32, "ExternalInput").ap()
    K.used = WNAMES[:5] if stage == 1 else (WNAMES[:9] if stage in (2, 3) else WNAMES)
    for name, shape in K.used:
        C[name] = K.dram(name, shape, F32, "ExternalInput").ap()
    y = K.dram("y", [TOK, D], F32, "ExternalOutput").ap()
    C["ko"] = K.dram("ko", [TOK, 256], F32, "ExternalOutput").ap()
    C["vo"] = K.dram("vo", [TOK, 256], F32, "ExternalOutput").ap()
    C["kio"] = K.dram("kio", [TOK, 64], F32, "ExternalOutput").ap()
    dk = "ExternalOutput" if debug else "Internal"
    C["h1"] = K.dram("h1s", [TOK, D], F32, dk).ap()
    C["h2"] = K.dram("h2s", [TOK, D], F32, dk).ap()
    C["cmask"] = K.dram("cmask", [128, 512], F32, "ExternalInput").ap()
    if stage == 3:
        C["dbg"] = K.dram("dbg", [TOK, 1024], F32, "ExternalOutput").ap()
        C["dbg2"] = K.dram("dbg2", [128, 4136], F32, "ExternalOutput").ap()
    for nm, r, c in (("agk", 256, 512), ("agki", 64, 512), ("agv", 1024, 128), ("dec", 128, 32)):
        C[nm + "_in"] = K.dram(nm + "_in", [r, c], F32).ap()
        C[nm + "_out"] = K.dram(nm + "_out", [4 * r, c], F32).ap()
    if full:
        C["onehot"] = K.dram("onehot", [128, 4], F32, "ExternalInput").ap()
        C["pt"] = K.dram("pt", [16, 16], I32, "ExternalInput").ap()
        C["state_in"] = K.dram("state_in", [16 * 512, 256], F32, "ExternalInput").ap()
        C["cache_k"] = K.dram("cache_k", [NPOOL_ROWS, 256], F32, "ExternalInput").ap()
        C["cache_v"] = K.dram("cache_v", [NPOOL_ROWS, 256], F32, "ExternalInput").ap()
        C["cache_kidx"] = K.dram("cache_kidx", [NPOOL_ROWS, 64], F32, "ExternalInput").ap()
        C["gla_p"] = K.dram("gla_p", [512, 256], F32, "ExternalOutput").ap()
        C["gla_s"] = K.dram("gla_s", [16 * 512, 256], F32, "ExternalOutput").ap()
        C["gst_in"] = [K.dram(f"gst_in{t}", [512, 256], F32).ap() for t in range(8)]
        C["gst_out"] = [K.dram(f"gst_out{t}", [2048, 256], F32).ap() for t in range(8)]
        C["sscr"] = K.dram("sscr", [16, 2176], F32).ap()
        C["oscr"] = K.dram("oscr", [16, 1024], F32).ap()
        C["oat_d"] = K.dram("oat_d", [TOK, 512], F32, dk).ap()
        C["og_d"] = K.dram("og_d", [TOK, 512], F32, dk).ap()

    with ExitStack() as es0:
        ident = es0.enter_context(nc.sbuf_tensor("ident", [128, 128], BF16))
        C["ident"] = ident
        K.B_ident = Buf()
        K.pool.op(lambda: nc.gpsimd.memset(ident[:], 1.0), writes=[K.B_ident])
        K.pool.op(lambda: nc.gpsimd.affine_select(out=ident[:], in_=ident[:], pattern=[[-1, 128]],
                                                  compare_op=ALU.is_equal, fill=0.0, base=0, channel_multiplier=1),
                  reads=[K.B_ident], writes=[K.B_ident])
        K.barrier()

        ffn_phase(K, x, (y if stage == 1 else C["h1"]), C["ffn1_pre_w"], C["ffn1_w_gate"], C["ffn1_w_up"],
                  C["ffn1_w_down"], C["ffn1_post_w"], ident)
        if stage >= 2:
            with ExitStack() as es:
                def sb(n, s, d):
                    return es.enter_context(nc.sbuf_tensor(n, s, d))
                P = {}
                P["qT"] = sb("p_qT", [128, 8, TOK], BF16)
                P["qiT"] = sb("p_qiT", [128, 8, TOK], BF16)
                P["wi"] = sb("p_wi", [128, NT, 16], F32)
                P["qgT"] = sb("p_qgT", [128, 4, TOK], BF16)
                P["kgT"] = sb("p_kgT", [128, 4, TOK], BF16)
                P["khat"] = sb("p_khat", [128, NT, 512], BF16)
                P["gv"] = sb("p_gv", [128, NT, 1024], BF16)
                P["dec"] = sb("p_dec", [128, 4, 8], F32)
                P["dec8"] = sb("p_dec8", [128, 4, 128], F32)
                P["kg8f"] = sb("p_kg8f", [128, 4, 128], F32)
                P["kT8"] = sb("p_kT8", [128, 256], BF16)
                P["v8"] = sb("p_v8", [128, 256], BF16)
                P["kiT8"] = sb("p_kiT8", [128, 128], BF16)
                C["P"] = P
                for n in ("B_qT", "B_qiT", "B_wi", "B_qgT", "B_kgT", "B_khat", "B_gv", "B_s8", "B_AT", "B_og",
                          "B_ag1", "B_ag1o", "B_decin", "B_deco", "B_sscr", "B_oscr"):
                    C[n] = Buf()
                C["B_dec"] = Buf(K, dma=True, persist=True)
                C["B_gin"] = [Buf() for _ in range(8)]
                C["B_gout"] = [Buf() for _ in range(8)]
                phase_a(K, C)
                if full:
                    P["AT"] = sb("p_AT", [128, 8, 4, 128], BF16)
                    phase_g1(K, C)
                if stage >= 3:
                    P["oat"] = sb("p_oat", [128, NT, 1024], BF16)
                    C["B_oat"] = Buf(K, dma=True, persist=True)
                    K.dve.op(lambda: nc.vector.memset(P["oat"][:, 8, :], 0.0), writes=[C["B_oat"]])
                    phase_b(K, C)
                if stage == 3:
                    K.pool.dma(C["dbg"].rearrange("(t p) c -> p t c", p=128), P["oat"][:], C["B_oat"].dsem,
                               reads=[C["B_oat"]])
                if full:
                    phase_bs(K, C)
                    K.sp.dma(C["oat_d"].bitcast(BF16).rearrange("(t p) c -> p t c", p=128), P["oat"][:],
                             C["B_oat"].dsem, reads=[C["B_oat"]])
                    P["og"] = sb("p_og", [128, NT, 1024], BF16)
                    C["B_og"] = Buf(K, dma=True, persist=True)
                    K.dve.op(lambda: nc.vector.memset(P["og"][:, 8, :], 0.0), writes=[C["B_og"]])
                    phase_g2(K, C)
                    K.sp.dma(C["og_d"].bitcast(BF16).rearrange("(t p) c -> p t c", p=128), P["og"][:],
                             C["B_og"].dsem, reads=[C["B_og"]])
                K.barrier()
            if full:
                phase_c(K, C)
                ffn_phase(K, C["h2"], y, C["ffn2_pre_w"], C["ffn2_w_gate"], C["ffn2_w_up"], C["ffn2_w_down"],
                          C["ffn2_post_w"], ident, tag="f2")
    K.barrier()
    K.es.close()
    return K


def core_rows(x_prompt, x_sample, c):
    b, j = c // 4, c % 4
    rows = [x_prompt[b, (4 * s + j) * 128:(4 * s + j + 1) * 128] for s in range(8)]
    pad = np.zeros((128, x_sample.shape[-1]), np.float32)
    pad[:16] = x_sample[16 * c:16 * c + 16, 0]
    rows.append(pad)
    return np.ascontiguousarray(np.concatenate(rows, 0))


def make_in_maps(inputs, used=WNAMES, full=True):
    in_maps = []
    shared = {k: np.ascontiguousarray(inputs[k], dtype=np.float32) for k, _ in used}
    if full:
        shared["cache_k"] = np.ascontiguousarray(inputs["cache_k"]).reshape(NPOOL_ROWS, 256)
        shared["cache_v"] = np.ascontiguousarray(inputs["cache_v"]).reshape(NPOOL_ROWS, 256)
        shared["cache_kidx"] = np.ascontiguousarray(inputs["cache_kidx"]).reshape(NPOOL_ROWS, 64)
    for c in range(NCORES):
        j = c % 4
        m = {"x": core_rows(inputs["x_prompt"], inputs["x_sample"], c)}
        posv = np.zeros((128, NT), np.float32)
        for s in range(8):
            posv[:, s] = (4 * s + j) * 128 + np.arange(128)
        posv[:, 8] = 2048.0
        m["posv"] = posv
        cm = np.zeros((128, 4, 128), np.float32)
        for r in range(4):
            if r > j:
                cm[:, r, :] = -1.0e4
            elif r == j:
                cm[:, r, :] = np.where(np.arange(128)[None, :] > np.arange(128)[:, None], -1.0e4, 0.0)
        m["cmask"] = cm.reshape(128, 512)
        if full:
            oh = np.zeros((128, 4), np.float32)
            oh[:, j] = 1.0
            m["onehot"] = oh
            m["pt"] = np.ascontiguousarray(inputs["page_table"][16 * c:16 * c + 16], dtype=np.int32)
            m["state_in"] = np.ascontiguousarray(inputs["state_gla"][16 * c:16 * c + 16], dtype=np.float32).reshape(
                16 * 512, 256)
        m.update(shared)
        in_maps.append(m)
    return in_maps


def assemble(results):
    y_p = np.zeros((2, 4096, D), np.float32)
    y_s = np.zeros((128, 1, D), np.float32)
    k_p = np.zeros((2, 4096, 2, 128), np.float32)
    v_p = np.zeros((2, 4096, 2, 128), np.float32)
    ki_p = np.zeros((2, 4096, 64), np.float32)
    k_s = np.zeros((128, 1, 2, 128), np.float32)
    v_s = np.zeros((128, 1, 2, 128), np.float32)
    ki_s = np.zeros((128, 1, 64), np.float32)
    gla_p = np.zeros((2, 4, 128, 256), np.float32)
    gla_s = np.zeros((128, 4, 128, 256), np.float32)
    for c, r in enumerate(results):
        b, j = c // 4, c % 4
        for s in range(8):
            sl = slice((4 * s + j) * 128, (4 * s + j + 1) * 128)
            rs = slice(s * 128, (s + 1) * 128)
            y_p[b, sl] = r["y"][rs]
            k_p[b, sl] = r["ko"][rs].reshape(128, 2, 128)
            v_p[b, sl] = r["vo"][rs].reshape(128, 2, 128)
            ki_p[b, sl] = r["kio"][rs]
        ss = slice(16 * c, 16 * c + 16)
        y_s[ss, 0] = r["y"][1024:1040]
        k_s[ss, 0] = r["ko"][1024:1040].reshape(16, 2, 128)
        v_s[ss, 0] = r["vo"][1024:1040].reshape(16, 2, 128)
        ki_s[ss, 0] = r["kio"][1024:1040]
        gla_s[ss] = r["gla_s"].reshape(16, 4, 128, 256)
        if j == 0:
            gla_p[b] = r["gla_p"].reshape(4, 128, 256)
    return (y_p, y_s, k_p, v_p, ki_p, gla_p, k_s, v_s, ki_s, gla_s)


def kernel(**inputs):
    K = build()
    in_maps = make_in_maps(inputs, K.used, True)
    res = run_bass_kernel_spmd(K.nc, in_maps, core_ids=list(range(NCORES)))
    return assemble(res.results)
```

```python
import numpy as np
from contextlib import ExitStack
import concourse.bass as bass
import concourse.mybir as mybir
from concourse.bass_utils import run_bass_kernel_spmd

F32 = mybir.dt.float32
BF16 = mybir.dt.bfloat16
I32 = mybir.dt.int32
ALU = mybir.AluOpType
AF = mybir.ActivationFunctionType
AX = mybir.AxisListType

NCORES = 8
NT = 9
TOK = NT * 128
D = 2048
DFF = 5632
DIN = 9824
EPS = 1e-6
TB = [(0, 512), (512, 512), (1024, 128)]


class Sem:
    def __init__(self, h, uid):
        self.h = h
        self.uid = uid
        self.cnt = 0


class Ev:
    __slots__ = ("sem", "val", "q", "idx")

    def __init__(self, sem, val, q=None, idx=0):
        self.sem = sem
        self.val = val
        self.q = q
        self.idx = idx


class Buf:
    def __init__(self, K=None, dma=False, persist=False):
        self.w = None
        self.r = {}
        self.dsem = K.new_sem("d", persist) if dma else None


class Q:
    def __init__(self, K, eng, name):
        self.K = K
        self.eng = eng
        self.name = name
        self.sem = K.new_sem(name, True)
        self.seen = {}
        self.nins = 0

    def wait(self, *evs):
        for ev in evs:
            if ev is None:
                continue
            if ev.q is self and self.nins - ev.idx >= 4:
                continue
            if self.seen.get(ev.sem.uid, -1) >= ev.val:
                continue
            self.seen[ev.sem.uid] = ev.val
            self.eng.wait_ge(ev.sem.h, ev.val)

    def done(self, ins):
        self.sem.cnt += 1
        self.nins += 1
        ins.then_inc(self.sem.h, 1)
        return Ev(self.sem, self.sem.cnt, self, self.nins)

    def tick(self, n=1):
        self.nins += n

    def pre(self, reads=(), writes=()):
        for b in reads:
            self.wait(b.w)
        for b in writes:
            self.wait(b.w)
            self.wait(*b.r.values())

    def post(self, ev, reads=(), writes=()):
        for b in reads:
            b.r[ev.sem.uid] = ev
        for b in writes:
            b.w = ev
            b.r = {}

    def op(self, fn, reads=(), writes=()):
        self.pre(reads, writes)
        ev = self.done(fn())
        self.post(ev, reads, writes)
        return ev

    def dma(self, out, in_, sem, reads=(), writes=(), **kw):
        for b in reads:
            self.wait(b.w)
        for b in writes:
            if not (b.w is not None and b.w.sem is sem):
                self.wait(b.w)
            self.wait(*b.r.values())
        ins = self.eng.dma_start(out=out, in_=in_, **kw)
        sem.cnt += 16
        ins.then_inc(sem.h, 16)
        self.nins += 1
        ev = Ev(sem, sem.cnt)
        self.post(ev, reads, writes)
        self.K.dma_sems[sem.uid] = sem
        return ev


class Kern:
    def __init__(self):
        self.nc = bass.Bass("TRN2", target_bir_lowering=False)
        self.es = ExitStack()
        self.nsem = 0
        self.dma_sems = {}
        self.free_sems = []
        self.phase_sems = []
        nc = self.nc
        self.pe = Q(self, nc.tensor, "pe")
        self.act = Q(self, nc.scalar, "act")
        self.dve = Q(self, nc.vector, "dve")
        self.pool = Q(self, nc.gpsimd, "pool")
        self.sp = Q(self, nc.sync, "sp")
        self.queues = [self.pe, self.act, self.dve, self.pool, self.sp]

    def new_sem(self, name, persist=False):
        if not persist and self.free_sems:
            s = self.free_sems.pop()
        else:
            self.nsem += 1
            h = self.es.enter_context(self.nc.semaphore(f"{name}{self.nsem}"))
            s = Sem(h, self.nsem)
        if not persist:
            self.phase_sems.append(s)
        return s

    def end_phase(self):
        self.barrier()
        self.free_sems.extend(self.phase_sems)
        self.phase_sems = []

    def barrier(self):
        evs = []
        for q in self.queues:
            if q.sem.cnt > 0:
                evs.append(Ev(q.sem, q.sem.cnt))
        for s in self.dma_sems.values():
            if s.cnt > 0:
                evs.append(Ev(s, s.cnt))
        for q in self.queues:
            q.wait(*evs)

    def dram(self, name, shape, dt, kind="Internal"):
        return self.nc.dram_tensor(name, list(shape), dt, kind=kind)


def ffn_phase(K, src, dst, pre_w, wg, wu, wd, post_w, ident, tag="f1"):
    nc = K.nc
    pe, act, dve, pool, sp = K.pe, K.act, K.dve, K.pool, K.sp
    NG = DFF // 256
    with ExitStack() as es:
        def sb(n, s, d):
            return es.enter_context(nc.sbuf_tensor(tag + n, s, d))

        def ps(n, s, d=F32):
            return es.enter_context(nc.psum_tensor(tag + n, s, d))

        acc = sb("f_acc", [128, NT, D], F32)
        zT = sb("f_zT", [128, 16, TOK], BF16)
        xst = sb("f_xst", [128, D], F32)
        zb = sb("f_zb", [128, D], BF16)
        wbc = sb("f_wbc", [128, D], F32)
        wgb = sb("f_wgb", [128, 2, 16, 256], BF16)
        wub = sb("f_wub", [128, 2, 16, 256], BF16)
        wdb = sb("f_wdb", [128, 3, 2, D], BF16)
        aT = sb("f_aT", [128, 2, 2, TOK], BF16)
        sg = sb("f_sg", [128, 2, 512], BF16)
        st = sb("f_st", [128, 4 * NT], F32)
        ptr = [ps(f"f_ptr{i}", [128, 1024], BF16) for i in range(2)]
        pg = [ps(f"f_pg{i}", [128, 512]) for i in range(2)]
        pu = [ps(f"f_pu{i}", [128, 512]) for i in range(2)]
        pd = [ps(f"f_pd{i}", [128, 512]) for i in range(2)]

        B_xst = Buf(K, dma=True)
        B_zb = Buf()
        B_wbc = Buf(K, dma=True)
        B_st = Buf()
        B_ptr = [Buf(), Buf()]
        B_zT = [Buf() for _ in range(NT)]
        B_wgu = [Buf(K, dma=True) for _ in range(2)]
        B_wd = [Buf(K, dma=True) for _ in range(3)]
        B_aT = [[[Buf() for _ in range(3)] for _ in range(2)] for _ in range(2)]
        B_pg = [Buf(), Buf()]
        B_pu = [Buf(), Buf()]
        B_sg = [Buf(), Buf()]
        B_pd = [Buf(), Buf()]
        B_acc = [Buf(K, dma=True) for _ in range(NT)]

        wg_v = wg.rearrange("(k p) f -> p k f", p=128)
        wu_v = wu.rearrange("(k p) f -> p k f", p=128)
        wd_v = wd.rearrange("(c p) n -> p c n", p=128)

        def load_group(gi):
            s2, s3 = gi % 2, gi % 3
            c0 = gi * 256
            pool.dma(wgb[:, s2], wg_v[:, :, c0:c0 + 256], B_wgu[s2].dsem, writes=[B_wgu[s2]])
            pool.dma(wub[:, s2], wu_v[:, :, c0:c0 + 256], B_wgu[s2].dsem, writes=[B_wgu[s2]])
            pool.dma(wdb[:, s3], wd_v[:, 2 * gi:2 * gi + 2, :], B_wd[s3].dsem, writes=[B_wd[s3]])

        sp.dma(wbc[:], pre_w.rearrange("(o d) -> o d", o=1).to_broadcast([128, D]), B_wbc.dsem, writes=[B_wbc])
        load_group(0)
        dve.op(lambda: nc.vector.memset(st[:], 0.0), writes=[B_st])

        for t in range(NT):
            sp.dma(xst[:], src[t * 128:(t + 1) * 128, :], B_xst.dsem, writes=[B_xst])
            act.op(lambda: nc.scalar.activation(out=zb[:], in_=xst[:], func=AF.Square,
                                                accum_out=st[:, t:t + 1]),
                   reads=[B_xst], writes=[B_zb, B_st])
            act.op(lambda: nc.scalar.activation(out=st[:, NT + t:NT + t + 1], in_=st[:, t:t + 1], func=AF.Sqrt,
                                                scale=1.0 / D, bias=EPS),
                   reads=[B_st], writes=[B_st])
            dve.op(lambda: nc.vector.reciprocal(out=st[:, NT + t:NT + t + 1], in_=st[:, NT + t:NT + t + 1]),
                   reads=[B_st], writes=[B_st])
            dve.op(lambda: nc.vector.scalar_tensor_tensor(out=zb[:], in0=xst[:], scalar=st[:, NT + t:NT + t + 1],
                                                          in1=wbc[:], op0=ALU.mult, op1=ALU.mult),
                   reads=[B_xst, B_st, B_wbc], writes=[B_zb])
            for q4 in range(4):
                sl = q4 % 2
                pe.pre(reads=[B_zb, K.B_ident], writes=[B_ptr[sl]])
                for i in range(4):
                    k = q4 * 4 + i
                    ins = nc.tensor.transpose(ptr[sl][:, i * 128:(i + 1) * 128], zb[:, k * 128:(k + 1) * 128], ident[:])
                    pe.tick()
                ev = pe.done(ins)
                pe.nins -= 1
                pe.post(ev, reads=[B_zb], writes=[B_ptr[sl]])
                src_ap = ptr[sl][:, 0:512].rearrange("p (a b) -> p a b", a=4)
                dst_ap = zT[:, q4 * 4:q4 * 4 + 4, t * 128:(t + 1) * 128]
                if q4 % 2 == 0:
                    act.op(lambda: nc.scalar.copy(out=dst_ap, in_=src_ap), reads=[B_ptr[sl]], writes=[B_zT[t]])
                else:
                    dve.op(lambda: nc.vector.tensor_copy(dst_ap, src_ap), reads=[B_ptr[sl]], writes=[B_zT[t]])

        sp.dma(wbc[:], post_w.rearrange("(o d) -> o d", o=1).to_broadcast([128, D]), B_wbc.dsem, writes=[B_wbc])

        tiles_of_tb = [[0, 1, 2, 3], [4, 5, 6, 7], [8]]
        down_units = []

        def emit_down(gi, t, nb, idx):
            s2, s3 = gi % 2, gi % 3
            tb = t // 4
            sl = idx % 2
            pe.pre(reads=[B_aT[s2][0][tb], B_aT[s2][1][tb], B_wd[s3]], writes=[B_pd[sl]])
            for ci in range(2):
                ins = nc.tensor.matmul(pd[sl][:], lhsT=aT[:, s2, ci, t * 128:(t + 1) * 128],
                                       rhs=wdb[:, s3, ci, nb * 512:(nb + 1) * 512],
                                       start=(ci == 0), stop=(ci == 1))
                pe.tick()
            ev = pe.done(ins)
            pe.nins -= 1
            pe.post(ev, reads=[B_aT[s2][0][tb], B_aT[s2][1][tb], B_wd[s3]], writes=[B_pd[sl]])
            a_ap = acc[:, t, nb * 512:(nb + 1) * 512]
            if gi == 0:
                dve.op(lambda: nc.vector.tensor_copy(a_ap, pd[sl][:]), reads=[B_pd[sl]], writes=[B_acc[t]])
            else:
                dve.op(lambda: nc.vector.tensor_tensor(out=a_ap, in0=a_ap, in1=pd[sl][:], op=ALU.add),
                       reads=[B_pd[sl]], writes=[B_acc[t]])

        didx = 0
        for gi in range(NG):
            s2 = gi % 2
            if gi + 1 < NG:
                load_group(gi + 1)
            step = 0
            for ci in range(2):
                for tbi, (t0, tn) in enumerate(TB):
                    sl = step % 2
                    zdeps = [B_zT[t] for t in tiles_of_tb[tbi]]
                    for (pp, Bp, wb) in ((pg, B_pg, wgb), (pu, B_pu, wub)):
                        pe.pre(reads=zdeps + [B_wgu[s2]], writes=[Bp[sl]])
                        for k in range(16):
                            ins = nc.tensor.matmul(pp[sl][:, 0:tn], lhsT=wb[:, s2, k, ci * 128:(ci + 1) * 128],
                                                   rhs=zT[:, k, t0:t0 + tn], start=(k == 0), stop=(k == 15))
                            pe.tick()
                        ev = pe.done(ins)
                        pe.nins -= 1
                        pe.post(ev, reads=zdeps + [B_wgu[s2]], writes=[Bp[sl]])
                    act.op(lambda: nc.scalar.activation(out=sg[:, sl, 0:tn], in_=pg[sl][:, 0:tn], func=AF.Silu),
                           reads=[B_pg[sl]], writes=[B_sg[sl]])
                    dve.op(lambda: nc.vector.tensor_tensor(out=aT[:, s2, ci, t0:t0 + tn], in0=sg[:, sl, 0:tn],
                                                           in1=pu[sl][:, 0:tn], op=ALU.mult),
                           reads=[B_sg[sl], B_pu[sl]], writes=[B_aT[s2][ci][tbi]])
                    step += 1
                    for _ in range(6):
                        if down_units:
                            g0, t, nb = down_units.pop(0)
                            emit_down(g0, t, nb, didx)
                            didx += 1
            down_units = [(gi, t, nb) for t in range(NT) for nb in range(4)]
        while down_units:
            g0, t, nb = down_units.pop(0)
            emit_down(g0, t, nb, didx)
            didx += 1

        for t in range(NT):
            sp.dma(xst[:], src[t * 128:(t + 1) * 128, :], B_xst.dsem, writes=[B_xst])
            act.op(lambda: nc.scalar.activation(out=zb[:], in_=acc[:, t, :], func=AF.Square,
                                                accum_out=st[:, 2 * NT + t:2 * NT + t + 1]),
                   reads=[B_acc[t]], writes=[B_zb, B_st])
            c = 3 * NT + t
            act.op(lambda: nc.scalar.activation(out=st[:, c:c + 1], in_=st[:, 2 * NT + t:2 * NT + t + 1], func=AF.Sqrt,
                                                scale=4.0 / D, bias=4.0 * EPS),
                   reads=[B_st], writes=[B_st])
            dve.op(lambda: nc.vector.reciprocal(out=st[:, c:c + 1], in_=st[:, c:c + 1]),
                   reads=[B_st], writes=[B_st])
            dve.op(lambda: nc.vector.scalar_tensor_tensor(out=acc[:, t, :], in0=acc[:, t, :], scalar=st[:, c:c + 1],
                                                          in1=wbc[:], op0=ALU.mult, op1=ALU.mult),
                   reads=[B_st, B_wbc, B_acc[t]], writes=[B_acc[t]])
            dve.op(lambda: nc.vector.tensor_tensor(out=acc[:, t, :], in0=acc[:, t, :], in1=xst[:], op=ALU.add),
                   reads=[B_xst, B_acc[t]], writes=[B_acc[t]])
            sp.dma(dst[t * 128:(t + 1) * 128, :], acc[:, t, :], B_acc[t].dsem, reads=[B_acc[t]])
        K.end_phase()


import math
LN_THETA = math.log(10000.0)
PI = math.pi
CQ, CK, CV, CQI, CKI, CWI, CGQ, CGK, CGV, CGLR, CGR, CGA, CGG = (
    0, 1024, 1280, 1536, 2560, 2624, 2640, 3152, 3664, 4688, 4704, 5728, 7776)
AG1_ROWS = 576
SQ = 1.0 / math.sqrt(128.0)


def all_gather(K, src, dst, rbufs, wbufs):
    pool = K.pool
    pool.pre(reads=rbufs, writes=wbufs)
    ins = K.nc.gpsimd.collective_compute("AllGather", ALU.bypass, replica_groups=[[0, 1, 2, 3], [4, 5, 6, 7]],
                                         ins=[src.opt()], outs=[dst.opt()])
    csem = K.new_sem("cc", True)
    ins.then_inc(csem.h)
    csem.cnt += 1
    pool.nins += 1
    ev = Ev(csem, 1)
    for b in rbufs:
        b.r[csem.uid] = ev
    for b in wbufs:
        b.r[csem.uid] = ev
    return ev


def norm_T(K, src, wvec, zT, B_zT, ident, ptr, B_ptr, tag):
    nc = K.nc
    pe, act, dve, pool, sp = K.pe, K.act, K.dve, K.pool, K.sp
    with ExitStack() as es:
        def sb(n, s, d):
            return es.enter_context(nc.sbuf_tensor(tag + n, s, d))
        xst = sb("xst", [128, D], F32)
        zb = sb("zb", [128, D], BF16)
        wbc = sb("wbc", [128, D], F32)
        st = sb("st", [128, 2 * NT], F32)
        B_xst = Buf(K, dma=True)
        B_zb = Buf()
        B_wbc = Buf(K, dma=True)
        B_st = Buf()
        sp.dma(wbc[:], wvec.rearrange("(o d) -> o d", o=1).to_broadcast([128, D]), B_wbc.dsem, writes=[B_wbc])
        dve.op(lambda: nc.vector.memset(st[:], 0.0), writes=[B_st])
        for t in range(NT):
            sp.dma(xst[:], src[t * 128:(t + 1) * 128, :], B_xst.dsem, writes=[B_xst])
            act.op(lambda: nc.scalar.activation(out=zb[:], in_=xst[:], func=AF.Square,
                                                accum_out=st[:, t:t + 1]),
                   reads=[B_xst], writes=[B_zb, B_st])
            act.op(lambda: nc.scalar.activation(out=st[:, NT + t:NT + t + 1], in_=st[:, t:t + 1], func=AF.Sqrt,
                                                scale=1.0 / D, bias=EPS),
                   reads=[B_st], writes=[B_st])
            dve.op(lambda: nc.vector.reciprocal(out=st[:, NT + t:NT + t + 1], in_=st[:, NT + t:NT + t + 1]),
                   reads=[B_st], writes=[B_st])
            dve.op(lambda: nc.vector.scalar_tensor_tensor(out=zb[:], in0=xst[:], scalar=st[:, NT + t:NT + t + 1],
                                                          in1=wbc[:], op0=ALU.mult, op1=ALU.mult),
                   reads=[B_xst, B_st, B_wbc], writes=[B_zb])
            for q4 in range(4):
                sl = q4 % 2
                pe.pre(reads=[B_zb, K.B_ident], writes=[B_ptr[sl]])
                for i in range(4):
                    k = q4 * 4 + i
                    ins = nc.tensor.transpose(ptr[sl][:, i * 128:(i + 1) * 128], zb[:, k * 128:(k + 1) * 128], ident[:])
                ev = pe.done(ins)
                pe.post(ev, reads=[B_zb], writes=[B_ptr[sl]])
                src_ap = ptr[sl][:, 0:512].rearrange("p (a b) -> p a b", a=4)
                dst_ap = zT[:, q4 * 4:q4 * 4 + 4, t * 128:(t + 1) * 128]
                if q4 % 2 == 0:
                    act.op(lambda: nc.scalar.copy(out=dst_ap, in_=src_ap), reads=[B_ptr[sl]], writes=[B_zT[t]])
                else:
                    dve.op(lambda: nc.vector.tensor_copy(dst_ap, src_ap), reads=[B_ptr[sl]], writes=[B_zT[t]])
        K.end_phase()


def phase_a(K, C):
    nc = K.nc
    pe, act, dve, pool, sp = K.pe, K.act, K.dve, K.pool, K.sp
    P = C["P"]
    ident = C["ident"]
    w_in = C["w_in"]
    win_v = w_in.rearrange("(k p) f -> p k f", p=128)
    ag_kT = C["agk_in"]
    ag_kiT = C["agki_in"]
    ag_v = C["agv_in"]
    with ExitStack() as es:
        def sb(n, s, d):
            return es.enter_context(nc.sbuf_tensor("a_" + n, s, d))

        def ps(n, s, d=F32):
            return es.enter_context(nc.psum_tensor("a_" + n, s, d))

        uT = sb("uT", [128, 16, TOK], BF16)
        B_uT = [Buf() for _ in range(NT)]
        ptr = [ps(f"ptr{i}", [128, 1024], BF16) for i in range(2)]
        B_ptr = [Buf(), Buf()]
        pp = [ps(f"pp{i}", [128, 512]) for i in range(2)]
        B_pp = [Buf(), Buf()]
        px = [ps(f"px{i}", [128, 512]) for i in range(2)]
        B_px = [Buf(), Buf()]
        norm_T(K, C["h1"], C["mix_pre_w"], uT, B_uT, ident, ptr, B_ptr, "a1_")

        tabs = sb("tabs", [128, NT, 384], F32)
        es_t = ExitStack()

        def sbt(n, s, d):
            return es_t.enter_context(nc.sbuf_tensor("a_" + n, s, d))
        posv = sbt("posv", [128, NT], F32)
        io = sbt("io", [128, 64], F32)
        inv = sbt("inv", [128, 96], F32)
        ang = sbt("ang", [128, NT, 96], F32)
        kf = sbt("kf", [128, NT, 96], F32)
        kint = sbt("kint", [128, NT, 96], I32)
        sn = sbt("sn", [128, NT, 96], F32)
        cs = sbt("cs", [128, NT, 96], F32)
        B_t = Buf(K, dma=True)
        sp.dma(posv[:], C["posv"], B_t.dsem, writes=[B_t])
        pool.op(lambda: nc.gpsimd.iota(io[:], pattern=[[1, 64]], base=0, channel_multiplier=0,
                                       allow_small_or_imprecise_dtypes=True), writes=[B_t])
        act.op(lambda: nc.scalar.activation(out=inv[:, 0:64], in_=io[:, 0:64], func=AF.Exp,
                                            scale=-2.0 * LN_THETA / 128.0), reads=[B_t], writes=[B_t])
        act.op(lambda: nc.scalar.activation(out=inv[:, 64:96], in_=io[:, 0:32], func=AF.Exp,
                                            scale=-2.0 * LN_THETA / 64.0), reads=[B_t], writes=[B_t])
        for s in range(NT):
            dve.op(lambda: nc.vector.tensor_scalar(out=ang[:, s, :], in0=inv[:], scalar1=posv[:, s:s + 1],
                                                   scalar2=None, op0=ALU.mult), reads=[B_t], writes=[B_t])

        def V(fn):
            return dve.op(fn, reads=[B_t], writes=[B_t])
        V(lambda: nc.vector.tensor_scalar(out=kf[:], in0=ang[:], scalar1=1.0 / (2 * PI), scalar2=None, op0=ALU.mult))
        V(lambda: nc.vector.tensor_copy(kint[:], kf[:]))
        V(lambda: nc.vector.tensor_copy(kf[:], kint[:]))
        V(lambda: nc.vector.scalar_tensor_tensor(out=ang[:], in0=kf[:], scalar=-2 * PI, in1=ang[:],
                                                 op0=ALU.mult, op1=ALU.add))
        act.op(lambda: nc.scalar.activation(out=sn[:], in_=ang[:], func=AF.Sin), reads=[B_t], writes=[B_t])
        V(lambda: nc.vector.tensor_scalar(out=ang[:], in0=ang[:], scalar1=PI / 2, scalar2=None, op0=ALU.add))
        V(lambda: nc.vector.tensor_scalar(out=kf[:], in0=ang[:], scalar1=PI, scalar2=-2 * PI,
                                          op0=ALU.is_gt, op1=ALU.mult))
        V(lambda: nc.vector.tensor_tensor(out=ang[:], in0=ang[:], in1=kf[:], op=ALU.add))
        act.op(lambda: nc.scalar.activation(out=cs[:], in_=ang[:], func=AF.Sin), reads=[B_t], writes=[B_t])
        V(lambda: nc.vector.tensor_copy(tabs[:, :, 0:64], cs[:, :, 0:64]))
        V(lambda: nc.vector.tensor_copy(tabs[:, :, 64:128], cs[:, :, 0:64]))
        V(lambda: nc.vector.tensor_scalar(out=tabs[:, :, 128:192], in0=sn[:, :, 0:64], scalar1=-1.0, scalar2=None,
                                          op0=ALU.mult))
        V(lambda: nc.vector.tensor_copy(tabs[:, :, 192:256], sn[:, :, 0:64]))
        V(lambda: nc.vector.tensor_copy(tabs[:, :, 256:288], cs[:, :, 64:96]))
        V(lambda: nc.vector.tensor_copy(tabs[:, :, 288:320], cs[:, :, 64:96]))
        V(lambda: nc.vector.tensor_scalar(out=tabs[:, :, 320:352], in0=sn[:, :, 64:96], scalar1=-1.0, scalar2=None,
                                          op0=ALU.mult))
        V(lambda: nc.vector.tensor_copy(tabs[:, :, 352:384], sn[:, :, 64:96]))
        B_tabs = B_t
        K.barrier()
        es_t.close()

        wbuf = sb("wbuf", [128, 2, 16, 512], BF16)
        B_w = [Buf(K, dma=True), Buf(K, dma=True)]
        xs = sb("xs", [128, 2, 512], F32)
        B_xs = [Buf(K, dma=True), Buf(K, dma=True)]
        t1 = sb("t1", [128, 2, 512], F32)
        B_t1 = [Buf(), Buf()]
        t2 = sb("t2", [128, 2, 512], F32)
        B_t2 = [Buf(), Buf()]
        ob = sb("ob", [128, 2, 512], BF16)
        B_ob = [Buf(K, dma=True), Buf(K, dma=True)]
        of = sb("of", [128, 2, 320], F32)
        B_of = [Buf(K, dma=True), Buf(K, dma=True)]
        tst = sb("tst", [128, 2, 256], BF16)
        B_tst = [Buf(K, dma=True), Buf(K, dma=True)]
        glrT = sb("glrT", [32, TOK], BF16)
        B_glr = Buf()
        w2b = sb("w2b", [16, 512], BF16)
        negb = sb("negb", [128, 4], F32)
        B_c = Buf(K, dma=True)
        ones = sb("ones", [128, 128], F32)
        eT = sb("eT", [128, 512], F32)
        lT = sb("lT", [128, 512], F32)
        cT = sb("cT", [128, 512], F32)
        B_e, B_l, B_cT = Buf(), Buf(), Buf()
        E1 = sb("E1", [128, TOK], F32)
        E2 = sb("E2", [128, TOK], F32)
        B_E = [Buf(), Buf(), Buf()]
        khT = sb("khT", [128, 512], BF16)
        B_khT = Buf()
        cnt = {"blk": 0, "pp": 0, "xs": 0, "tr": 0, "of": 0, "tst": 0, "px": 0}

        pool.dma(w2b[:], C["gla_gate_w2"], B_c.dsem, writes=[B_c])
        sp.dma(negb[:], C["gla_gate_b"].rearrange("(h p) -> p h", p=128), B_c.dsem, writes=[B_c],
               allow_slow_non_contiguous=True)
        dve.op(lambda: nc.vector.tensor_scalar(out=negb[:], in0=negb[:], scalar1=-1.0, scalar2=None, op0=ALU.mult),
               reads=[B_c], writes=[B_c])
        dve.op(lambda: nc.vector.memset(ones[:], 1.0), writes=[B_c])

        def load_w(pieces):
            slot = cnt["blk"] % 2
            cnt["blk"] += 1
            off = 0
            for (c0, w) in pieces:
                pool.dma(wbuf[:, slot, :, off:off + w], win_v[:, :, c0:c0 + w], B_w[slot].dsem, writes=[B_w[slot]])
                off += w
            return slot

        def mm_tok(slot, t, width):
            i = cnt["pp"] % 2
            cnt["pp"] += 1
            pe.pre(reads=[B_uT[t], B_w[slot]], writes=[B_pp[i]])
            for k in range(16):
                ins = nc.tensor.matmul(pp[i][:, 0:width], lhsT=uT[:, k, t * 128:(t + 1) * 128],
                                       rhs=wbuf[:, slot, k, 0:width], start=(k == 0), stop=(k == 15))
            ev = pe.done(ins)
            pe.post(ev, reads=[B_uT[t], B_w[slot]], writes=[B_pp[i]])
            return i

        def mm_feat(slot, off, m, tbi):
            t0, tn = TB[tbi]
            i = cnt["pp"] % 2
            cnt["pp"] += 1
            deps = [B_uT[t] for t in ([0, 1, 2, 3], [4, 5, 6, 7], [8])[tbi]]
            pe.pre(reads=deps + [B_w[slot]], writes=[B_pp[i]])
            for k in range(16):
                ins = nc.tensor.matmul(pp[i][0:m, 0:tn], lhsT=wbuf[:, slot, k, off:off + m],
                                       rhs=uT[:, k, t0:t0 + tn], start=(k == 0), stop=(k == 15))
            ev = pe.done(ins)
            pe.post(ev, reads=deps + [B_w[slot]], writes=[B_pp[i]])
            return i

        def evac(i, width):
            j = cnt["xs"] % 2
            cnt["xs"] += 1
            act.op(lambda: nc.scalar.copy(out=xs[:, j, 0:width], in_=pp[i][:, 0:width]),
                   reads=[B_pp[i]], writes=[B_xs[j]])
            return j

        def rope(j, c0, hs, nh, t, out_ap, B_out):
            w = nh * hs
            hh = hs // 2
            tb0 = 0 if hs == 128 else 256
            x3 = xs[:, j, c0:c0 + w].rearrange("p (h d) -> p h d", h=nh)
            cosb = tabs[:, t, tb0:tb0 + hs].unsqueeze(1).to_broadcast([128, nh, hs])
            sa = tabs[:, t, tb0 + hs:tb0 + hs + hh].unsqueeze(1).to_broadcast([128, nh, hh])
            sbb = tabs[:, t, tb0 + hs + hh:tb0 + 2 * hs].unsqueeze(1).to_broadcast([128, nh, hh])
            a3 = t1[:, j, 0:w].rearrange("p (h d) -> p h d", h=nh)
            b3 = t2[:, j, 0:w].rearrange("p (h d) -> p h d", h=nh)
            pool.op(lambda: nc.gpsimd.tensor_tensor(out=a3, in0=x3, in1=cosb, op=ALU.mult),
                    reads=[B_xs[j], B_tabs], writes=[B_t1[j]])
            dve.op(lambda: nc.vector.tensor_tensor(out=b3[:, :, 0:hh], in0=x3[:, :, hh:hs], in1=sa, op=ALU.mult),
                   reads=[B_xs[j], B_tabs], writes=[B_t2[j]])
            dve.op(lambda: nc.vector.tensor_tensor(out=b3[:, :, hh:hs], in0=x3[:, :, 0:hh], in1=sbb, op=ALU.mult),
                   reads=[B_xs[j], B_tabs], writes=[B_t2[j]])
            dve.op(lambda: nc.vector.tensor_tensor(out=out_ap, in0=t1[:, j, 0:w], in1=t2[:, j, 0:w], op=ALU.add),
                   reads=[B_t1[j], B_t2[j]], writes=[B_out])

        def transposes(j, nblk, rows, dst_fn, B_dst_fn):
            sl = cnt["tr"] % 2
            cnt["tr"] += 1
            pe.pre(reads=[B_ob[j], K.B_ident], writes=[B_ptr[sl]])
            for b in range(nblk):
                ins = nc.tensor.transpose(ptr[sl][0:rows, b * 128:(b + 1) * 128],
                                          ob[:, j, b * rows:(b + 1) * rows], ident[:])
            ev = pe.done(ins)
            pe.post(ev, reads=[B_ob[j]], writes=[B_ptr[sl]])
            return sl

        def pipelined(front, back, n=NT):
            st_ = {}
            st_[0] = front(0)
            for t in range(n):
                if t + 1 < n:
                    st_[t + 1] = front(t + 1)
                back(t, st_[t])

        for blk in range(2):
            slot = load_w([(CQ + blk * 512, 512)])

            def q_front(t, slot=slot):
                i = mm_tok(slot, t, 512)
                j = evac(i, 512)
                rope(j, 0, 128, 4, t, ob[:, j, :], B_ob[j])
                return j

            def q_back(t, j, blk=blk):
                sl = transposes(j, 4, 128, None, None)
                act.op(lambda: nc.scalar.copy(out=P["qT"][:, blk * 4:blk * 4 + 4, t * 128:(t + 1) * 128],
                                              in_=ptr[sl][:, 0:512].rearrange("p (a b) -> p a b", a=4)),
                       reads=[B_ptr[sl]], writes=[C["B_qT"]])
            pipelined(q_front, q_back)

        slot = load_w([(CK, 512)])

        def kv_front(t, slot=slot):
            i = mm_tok(slot, t, 512)
            j = evac(i, 512)
            o = cnt["of"] % 2
            cnt["of"] += 1
            rope(j, 0, 128, 2, t, of[:, o, 0:256], B_of[o])
            sp.dma(C["ko"][t * 128:(t + 1) * 128, :], of[:, o, 0:256], B_of[o].dsem, reads=[B_of[o]])
            sp.dma(C["vo"][t * 128:(t + 1) * 128, :], xs[:, j, 256:512], B_xs[j].dsem, reads=[B_xs[j]])
            pool.op(lambda: nc.gpsimd.tensor_copy(ob[:, j, 0:256], of[:, o, 0:256]), reads=[B_of[o]], writes=[B_ob[j]])
            pool.op(lambda: nc.gpsimd.tensor_copy(ob[:, j, 256:512], xs[:, j, 256:512]), reads=[B_xs[j]],
                    writes=[B_ob[j]])
            return j

        def kv_back(t, j):
            sl = transposes(j, 2, 128, None, None)
            if t < 8:
                q = cnt["tst"] % 2
                cnt["tst"] += 1
                act.op(lambda: nc.scalar.copy(out=tst[:, q, :], in_=ptr[sl][:, 0:256]), reads=[B_ptr[sl]],
                       writes=[B_tst[q]])
                for g in range(2):
                    sp.dma(ag_kT[g * 128:(g + 1) * 128, t * 64:(t + 1) * 64],
                           tst[:, q, g * 128:(g + 1) * 128].bitcast(F32),
                           B_tst[q].dsem, reads=[B_tst[q]], writes=[C["B_ag1"]])
                sp.dma(ag_v[t * 128:(t + 1) * 128, :], ob[:, j, 256:512].bitcast(F32), B_ob[j].dsem,
                       reads=[B_ob[j]], writes=[C["B_ag1"]])
            else:
                act.op(lambda: nc.scalar.copy(out=P["kT8"][:, :], in_=ptr[sl][:, 0:256]), reads=[B_ptr[sl]],
                       writes=[C["B_s8"]])
                pool.op(lambda: nc.gpsimd.tensor_copy(P["v8"][:, :], ob[:, j, 256:512]), reads=[B_ob[j]],
                        writes=[C["B_s8"]])
        pipelined(kv_front, kv_back)

        for blk in range(2):
            slot = load_w([(CQI + blk * 512, 512)])

            def qi_front(t, slot=slot):
                i = mm_tok(slot, t, 512)
                j = evac(i, 512)
                rope(j, 0, 64, 8, t, ob[:, j, :], B_ob[j])
                return j

            def qi_back(t, j, blk=blk):
                sl = transposes(j, 4, 128, None, None)
                act.op(lambda: nc.scalar.copy(out=P["qiT"][:, blk * 4:blk * 4 + 4, t * 128:(t + 1) * 128],
                                              in_=ptr[sl][:, 0:512].rearrange("p (a b) -> p a b", a=4)),
                       reads=[B_ptr[sl]], writes=[C["B_qiT"]])
            pipelined(qi_front, qi_back)

        slot = load_w([(CKI, 80)])

        def ki_front(t, slot=slot):
            i = mm_tok(slot, t, 80)
            j = evac(i, 80)
            o = cnt["of"] % 2
            cnt["of"] += 1
            rope(j, 0, 64, 1, t, of[:, o, 256:320], B_of[o])
            sp.dma(C["kio"][t * 128:(t + 1) * 128, :], of[:, o, 256:320], B_of[o].dsem, reads=[B_of[o]])
            dve.op(lambda: nc.vector.tensor_scalar(out=P["wi"][:, t, :], in0=xs[:, j, 64:80], scalar1=1.0 / 32.0,
                                                   scalar2=None, op0=ALU.mult), reads=[B_xs[j]], writes=[C["B_wi"]])
            pool.op(lambda: nc.gpsimd.tensor_copy(ob[:, j, 0:64], of[:, o, 256:320]), reads=[B_of[o]], writes=[B_ob[j]])
            pool.op(lambda: nc.gpsimd.tensor_copy(ob[:, j, 64:128], of[:, o, 256:320]), reads=[B_of[o]],
                    writes=[B_ob[j]])
            return j

        def ki_back(t, j):
            sl = transposes(j, 1, 128, None, None)
            if t < 8:
                q = cnt["tst"] % 2
                cnt["tst"] += 1
                act.op(lambda: nc.scalar.copy(out=tst[0:64, q, 0:128], in_=ptr[sl][0:64, 0:128]), reads=[B_ptr[sl]],
                       writes=[B_tst[q]])
                sp.dma(ag_kiT[:, t * 64:(t + 1) * 64], tst[0:64, q, 0:128].bitcast(F32), B_tst[q].dsem,
                       reads=[B_tst[q]], writes=[C["B_ag1"]])
            else:
                act.op(lambda: nc.scalar.copy(out=P["kiT8"][:, :], in_=ptr[sl][:, 0:128]), reads=[B_ptr[sl]],
                       writes=[C["B_s8"]])
        pipelined(ki_front, ki_back)

        for nm in ("agk", "agki", "agv"):
            all_gather(K, C[nm + "_in"], C[nm + "_out"], [C["B_ag1"]], [C["B_ag1o"]])

        for blk in range(2):
            slot = load_w([(CGV + blk * 512, 512)])
            for t in range(NT):
                i = mm_tok(slot, t, 512)
                act.op(lambda: nc.scalar.copy(out=P["gv"][:, t, blk * 512:(blk + 1) * 512], in_=pp[i][:, :]),
                       reads=[B_pp[i]], writes=[C["B_gv"]])

        slot = load_w([(CGLR, 16)])
        for tbi in range(3):
            t0, tn = TB[tbi]
            i = mm_feat(slot, 0, 16, tbi)
            act.op(lambda: nc.scalar.copy(out=glrT[0:16, t0:t0 + tn], in_=pp[i][0:16, 0:tn]), reads=[B_pp[i]],
                   writes=[B_glr])

        for h in range(4):
            slot = load_w([(CGQ + h * 128, 128), (CGK + h * 128, 128)])
            for tbi in range(3):
                t0, tn = TB[tbi]
                x = cnt["px"] % 2
                cnt["px"] += 1
                pe.op(lambda: nc.tensor.matmul(px[x][:, 0:tn], lhsT=w2b[:, h * 128:(h + 1) * 128],
                                               rhs=glrT[0:16, t0:t0 + tn], start=True, stop=True),
                      reads=[B_glr, B_c], writes=[B_px[x]])
                act.op(lambda: nc.scalar.activation(out=eT[:, 0:tn], in_=px[x][:, 0:tn], func=AF.Exp, scale=-1.0,
                                                    bias=negb[:, h:h + 1]), reads=[B_px[x], B_c], writes=[B_e])
                act.op(lambda: nc.scalar.activation(out=lT[:, 0:tn], in_=eT[:, 0:tn], func=AF.Ln, bias=1.0),
                       reads=[B_e], writes=[B_l])
                if tbi < 2:
                    for q4 in range(4):
                        dve.op(lambda: nc.vector.tensor_tensor_scan(out=cT[:, q4 * 128:(q4 + 1) * 128], data0=ones[:],
                                                                    data1=lT[:, q4 * 128:(q4 + 1) * 128], initial=0.0,
                                                                    op0=ALU.mult, op1=ALU.add),
                               reads=[B_l, B_c], writes=[B_cT])
                    act.op(lambda: nc.scalar.activation(out=E1[:, t0:t0 + tn], in_=cT[:, 0:tn], func=AF.Exp,
                                                        scale=-1.0 / 16.0), reads=[B_cT], writes=[B_E[tbi]])
                    act.op(lambda: nc.scalar.activation(out=E2[:, t0:t0 + tn], in_=cT[:, 0:tn], func=AF.Exp,
                                                        scale=1.0 / 16.0), reads=[B_cT], writes=[B_E[tbi]])
                    dve.op(lambda: nc.vector.tensor_copy(
                        P["dec"][:, h, tbi * 4:tbi * 4 + 4],
                        E1[:, t0:t0 + tn].rearrange("p (a b) -> p a b", a=4)[:, :, 127]),
                        reads=[B_E[tbi]], writes=[C["B_dec"]])
                else:
                    act.op(lambda: nc.scalar.activation(out=E1[:, t0:t0 + tn], in_=lT[:, 0:tn], func=AF.Exp,
                                                        scale=-1.0 / 16.0), reads=[B_l], writes=[B_E[tbi]])
                    dve.op(lambda: nc.vector.tensor_copy(P["dec8"][:, h, :], E1[:, t0:t0 + tn]),
                           reads=[B_E[tbi]], writes=[C["B_dec"]])
            for tbi in range(3):
                t0, tn = TB[tbi]
                i = mm_feat(slot, 0, 128, tbi)
                if tbi < 2:
                    dve.op(lambda: nc.vector.scalar_tensor_tensor(out=P["qgT"][:, h, t0:t0 + tn], in0=pp[i][:, 0:tn],
                                                                  scalar=SQ, in1=E1[:, t0:t0 + tn],
                                                                  op0=ALU.mult, op1=ALU.mult),
                           reads=[B_pp[i], B_E[tbi]], writes=[C["B_qgT"]])
                else:
                    dve.op(lambda: nc.vector.tensor_scalar(out=P["qgT"][:, h, t0:t0 + tn], in0=pp[i][:, 0:tn],
                                                           scalar1=SQ, scalar2=None, op0=ALU.mult),
                           reads=[B_pp[i]], writes=[C["B_qgT"]])
                i = mm_feat(slot, 128, 128, tbi)
                if tbi < 2:
                    dve.op(lambda: nc.vector.tensor_tensor(out=P["kgT"][:, h, t0:t0 + tn], in0=pp[i][:, 0:tn],
                                                           in1=E2[:, t0:t0 + tn], op=ALU.mult),
                           reads=[B_pp[i], B_E[tbi]], writes=[C["B_kgT"]])
                    dve.op(lambda: nc.vector.tensor_tensor(
                        out=khT[:, :].rearrange("p (a b) -> p a b", a=4),
                        in0=P["kgT"][:, h, t0:t0 + tn].rearrange("p (a b) -> p a b", a=4),
                        in1=P["dec"][:, h, tbi * 4:tbi * 4 + 4].unsqueeze(2).to_broadcast([128, 4, 128]),
                        op=ALU.mult), reads=[C["B_kgT"], C["B_dec"]], writes=[B_khT])
                    sl = cnt["tr"] % 2
                    cnt["tr"] += 1
                    pe.pre(reads=[B_khT, K.B_ident], writes=[B_ptr[sl]])
                    for b in range(4):
                        ins = nc.tensor.transpose(ptr[sl][:, b * 128:(b + 1) * 128], khT[:, b * 128:(b + 1) * 128],
                                                  ident[:])
                    ev = pe.done(ins)
                    pe.post(ev, reads=[B_khT], writes=[B_ptr[sl]])
                    act.op(lambda: nc.scalar.copy(out=P["khat"][:, tbi * 4:tbi * 4 + 4, h * 128:(h + 1) * 128],
                                                  in_=ptr[sl][:, 0:512].rearrange("p (a b) -> p a b", a=4)),
                           reads=[B_ptr[sl]], writes=[C["B_khat"]])
                else:
                    act.op(lambda: nc.scalar.copy(out=P["kgT"][:, h, t0:t0 + tn], in_=pp[i][:, 0:tn]),
                           reads=[B_pp[i]], writes=[C["B_kgT"]])
                    dve.op(lambda: nc.vector.tensor_copy(P["kg8f"][:, h, :], pp[i][:, 0:tn]),
                           reads=[B_pp[i]], writes=[C["B_kgT"]])
        K.end_phase()
NIT = 14
TOPK = 256
NEG = -1.0e4


def topk_threshold(K, S3, np_, junk3, st, B_S, B_junk, B_st, pw2, B_c):
    nc = K.nc
    dve = K.dve

    def V(fn, r=(), w=()):
        return dve.op(fn, reads=list(r), writes=list(w))
    V(lambda: nc.vector.tensor_scalar(out=st[0:np_, 8:8 + NIT], in0=pw2[0:np_, 0:NIT], scalar1=st[0:np_, 0:1],
                                      scalar2=None, op0=ALU.mult), r=[B_st, B_c], w=[B_st])
    V(lambda: nc.vector.tensor_scalar(out=st[0:np_, 1:2], in0=st[0:np_, 0:1], scalar1=-1.0, scalar2=None,
                                      op0=ALU.mult), r=[B_st], w=[B_st])
    V(lambda: nc.vector.memset(st[0:np_, 40:40 + NIT], 0.0), r=[B_st], w=[B_st])
    for k in range(NIT):
        V(lambda: nc.vector.tensor_tensor(out=st[0:np_, 2:3], in0=st[0:np_, 1:2], in1=st[0:np_, 8 + k:9 + k],
                                          op=ALU.add), r=[B_st], w=[B_st])
        V(lambda: nc.vector.tensor_scalar(out=junk3, in0=S3, scalar1=st[0:np_, 2:3], scalar2=0.0, op0=ALU.is_ge,
                                          op1=ALU.add, accum_out=st[0:np_, 40 + k:41 + k]),
          r=[B_S, B_st], w=[B_junk, B_st])
        V(lambda: nc.vector.tensor_scalar(out=st[0:np_, 4:5], in0=st[0:np_, 40 + k:41 + k], scalar1=TOPK - 0.5,
                                          scalar2=None, op0=ALU.is_ge), r=[B_st], w=[B_st])
        V(lambda: nc.vector.scalar_tensor_tensor(out=st[0:np_, 1:2], in0=st[0:np_, 4:5], scalar=st[0:np_, 8 + k:9 + k],
                                                 in1=st[0:np_, 1:2], op0=ALU.mult, op1=ALU.add), r=[B_st], w=[B_st])


def phase_b(K, C):
    nc = K.nc
    pe, act, dve, pool, sp = K.pe, K.act, K.dve, K.pool, K.sp
    P = C["P"]
    ident = C["ident"]
    with ExitStack() as es:
        def sb(n, s, d):
            return es.enter_context(nc.sbuf_tensor("b_" + n, s, d))

        def ps(n, s, d=F32):
            return es.enter_context(nc.psum_tensor("b_" + n, s, d))

        kT_all = sb("kT", [128, 2, 4, 1024], BF16)
        kiT2 = sb("kiT2", [128, 4, 1024], BF16)
        V1 = sb("V1", [128, 4, 8, 2, 130], BF16)
        S = sb("S", [128, 4096], F32)
        selm = sb("selm", [128, 4096], BF16)
        selT = sb("selT", [128, 4, 8, 128], BF16)
        diagw = sb("diagw", [128, 16, 128], BF16)
        rh = sb("rh", [128, 3, 512], BF16)
        pex = sb("pex", [128, 3, 512], BF16)
        pm = sb("pm", [128, 3, 512], BF16)
        cmask = sb("cmask", [128, 512], F32)
        pw2 = sb("pw2", [128, 32], F32)
        st = sb("st", [128, 64], F32)
        rec = sb("rec", [128, 8], F32)
        psh = [ps(f"psh{i}", [128, 512]) for i in range(3)]
        psc = [ps(f"psc{i}", [128, 512]) for i in range(2)]
        ptr = [ps(f"ptr{i}", [128, 1024], BF16) for i in range(2)]
        B_kv = Buf(K, dma=True)
        B_S, B_selm, B_selT, B_diagw, B_st, B_c, B_rec = Buf(), Buf(), Buf(), Buf(), Buf(), Buf(K, dma=True), Buf()
        B_rh = [Buf(), Buf(), Buf()]
        B_pex = [Buf(), Buf(), Buf()]
        B_pm = [Buf(), Buf(), Buf()]
        B_psh = [Buf(), Buf(), Buf()]
        B_psc = [Buf(), Buf()]
        B_ptr = [Buf(), Buf()]
        cnt = {"h": 0, "c": 0, "r": 0, "e": 0, "t": 0}

        agk, agki, agv = C["agk_out"], C["agki_out"], C["agv_out"]
        for r in range(4):
            for g in range(2):
                sp.dma(kT_all[:, g, r, :].bitcast(F32), agk[r * 256 + g * 128:r * 256 + (g + 1) * 128, :], B_kv.dsem,
                       reads=[C["B_ag1o"]], writes=[B_kv])
            for hf in range(2):
                sp.dma(kiT2[hf * 64:(hf + 1) * 64, r, :].bitcast(F32), agki[r * 64:(r + 1) * 64, :], B_kv.dsem,
                       reads=[C["B_ag1o"]], writes=[B_kv])
            for g in range(2):
                sp.dma(V1[:, r, :, g, 0:128],
                       agv.bitcast(BF16)[r * 1024:(r + 1) * 1024, g * 128:(g + 1) * 128].rearrange(
                           "(s p) c -> p s c", p=128),
                       B_kv.dsem, reads=[C["B_ag1o"]], writes=[B_kv])
        pool.op(lambda: nc.gpsimd.memset(V1[:, :, :, :, 128:130], 1.0), writes=[B_kv])
        sp.dma(cmask[:], C["cmask"], B_c.dsem, writes=[B_c])
        for k in range(NIT):
            dve.op(lambda: nc.vector.memset(pw2[:, k:k + 1], 2.0 ** (-k)), writes=[B_c])

        for s in range(8):
            Lr = (s + 1) * 128
            qs = slice(s * 128, (s + 1) * 128)
            for h in range(16):
                dve.op(lambda: nc.vector.tensor_scalar(out=diagw[:, h, :], in0=ident[:], scalar1=P["wi"][:, s, h:h + 1],
                                                       scalar2=None, op0=ALU.mult),
                       reads=[C["B_wi"], K.B_ident], writes=[B_diagw])
            LA = 2
            units = [(r, c0, min(512, Lr - c0), h) for r in range(4) for c0 in range(0, Lr, 512) for h in range(16)]
            hi_of = {}
            ci_of = {}

            def sc_front(u):
                r, c0, cw, h = units[u]
                hi = cnt["h"] % 3
                cnt["h"] += 1
                hi_of[u] = hi
                p0 = (h % 2) * 64
                pe.op(lambda: nc.tensor.matmul(psh[hi][:, 0:cw], lhsT=P["qiT"][p0:p0 + 64, h // 2, qs],
                                               rhs=kiT2[p0:p0 + 64, r, c0:c0 + cw], start=True, stop=True),
                      reads=[C["B_qiT"], B_kv], writes=[B_psh[hi]])
                if u % 2 == 0:
                    act.op(lambda: nc.scalar.activation(out=rh[:, hi, 0:cw], in_=psh[hi][:, 0:cw], func=AF.Relu),
                           reads=[B_psh[hi]], writes=[B_rh[hi]])
                else:
                    dve.op(lambda: nc.vector.tensor_scalar(out=rh[:, hi, 0:cw], in0=psh[hi][:, 0:cw], scalar1=0.0,
                                                           scalar2=None, op0=ALU.max),
                           reads=[B_psh[hi]], writes=[B_rh[hi]])

            def sc_back(u):
                r, c0, cw, h = units[u]
                hi = hi_of[u]
                if h == 0:
                    ci_of[(r, c0)] = cnt["c"] % 2
                    cnt["c"] += 1
                ci = ci_of[(r, c0)]
                pe.op(lambda: nc.tensor.matmul(psc[ci][:, 0:cw], lhsT=diagw[:, h, :], rhs=rh[:, hi, 0:cw],
                                               start=(h == 0), stop=(h == 15)),
                      reads=[B_diagw, B_rh[hi]], writes=[B_psc[ci]])
                if h == 15:
                    dve.op(lambda: nc.vector.tensor_copy(S[:, r * Lr + c0:r * Lr + c0 + cw], psc[ci][:, 0:cw]),
                           reads=[B_psc[ci]], writes=[B_S])
            for u in range(min(LA, len(units))):
                sc_front(u)
            for u in range(len(units)):
                if u + LA < len(units):
                    sc_front(u + LA)
                sc_back(u)
            S3 = S[:, 0:4 * Lr]
            dve.op(lambda: nc.vector.tensor_reduce(out=st[:, 32:33], in_=S3, axis=AX.X, op=ALU.max),
                   reads=[B_S], writes=[B_st])
            dve.op(lambda: nc.vector.tensor_reduce(out=st[:, 33:34], in_=S3, axis=AX.X, op=ALU.min),
                   reads=[B_S], writes=[B_st])
            dve.op(lambda: nc.vector.tensor_scalar(out=st[:, 33:34], in0=st[:, 33:34], scalar1=-1.0, scalar2=None,
                                                   op0=ALU.mult), reads=[B_st], writes=[B_st])
            dve.op(lambda: nc.vector.tensor_tensor(out=st[:, 0:1], in0=st[:, 32:33], in1=st[:, 33:34], op=ALU.max),
                   reads=[B_st], writes=[B_st])
            Sl = S3.rearrange("p (r l) -> p r l", r=4)[:, :, s * 128:(s + 1) * 128]
            dve.op(lambda: nc.vector.tensor_tensor(out=Sl, in0=Sl,
                                                   in1=cmask[:, :].rearrange("p (a b) -> p a b", a=4), op=ALU.add),
                   reads=[B_S, B_c], writes=[B_S])
            topk_threshold(K, S3, 128, selm[:, 0:4 * Lr], st, B_S, B_selm, B_st, pw2, B_c)
            dve.op(lambda: nc.vector.tensor_scalar(out=selm[:, 0:4 * Lr], in0=S3, scalar1=st[:, 1:2], scalar2=None,
                                                   op0=ALU.is_ge), reads=[B_S, B_st], writes=[B_selm])
            if s == 7 and "dbg2" in C:
                B_S.dsem = K.new_sem("d")
                B_st.dsem = B_S.dsem
                sp.dma(C["dbg2"][:, 0:4096], S[:], B_S.dsem, reads=[B_S])
                sp.dma(C["dbg2"][:, 4096:4136], st[:, 0:40], B_S.dsem, reads=[B_st])
            for r in range(4):
                for s0 in range(0, s + 1, 4):
                    nb = min(4, s + 1 - s0)
                    ti = cnt["t"] % 2
                    cnt["t"] += 1
                    pe.pre(reads=[B_selm, K.B_ident], writes=[B_ptr[ti]])
                    for b in range(nb):
                        ins = nc.tensor.transpose(ptr[ti][:, b * 128:(b + 1) * 128],
                                                  selm[:, r * Lr + (s0 + b) * 128:r * Lr + (s0 + b + 1) * 128], ident[:])
                    ev = pe.done(ins)
                    pe.post(ev, reads=[B_selm], writes=[B_ptr[ti]])
                    act.op(lambda: nc.scalar.copy(out=selT[:, r, s0:s0 + nb, :],
                                                  in_=ptr[ti][:, 0:nb * 128].rearrange("p (a b) -> p a b", a=nb)),
                           reads=[B_ptr[ti]], writes=[B_selT])
            tiles = [(r, s1) for r in range(4) for s1 in range(s + 1)]
            nt_ = len(tiles)
            aunits = [(g, n, r, s1) for g in range(2) for n, (r, s1) in enumerate(tiles)]
            bi_of = {}

            def at_front(u):
                g, n, r, s1 = aunits[u]
                hi = cnt["h"] % 3
                cnt["h"] += 1
                ei = cnt["e"] % 3
                cnt["e"] += 1
                bi_of[u] = ei
                pe.op(lambda: nc.tensor.matmul(psh[hi][:, :], lhsT=kT_all[:, g, r, s1 * 128:(s1 + 1) * 128],
                                               rhs=P["qT"][:, g * 4:(g + 1) * 4, qs], start=True, stop=True),
                      reads=[B_kv, C["B_qT"]], writes=[B_psh[hi]])
                act.op(lambda: nc.scalar.activation(out=pex[:, ei, :], in_=psh[hi][:, :], func=AF.Exp, scale=SQ),
                       reads=[B_psh[hi]], writes=[B_pex[ei]])
                eng = dve if u % 2 == 0 else pool
                veng = nc.vector if u % 2 == 0 else nc.gpsimd
                eng.op(lambda: veng.tensor_tensor(
                    out=pm[:, ei, :].rearrange("p (a b) -> p a b", a=4),
                    in0=pex[:, ei, :].rearrange("p (a b) -> p a b", a=4),
                    in1=selT[:, r, s1, :].unsqueeze(1).to_broadcast([128, 4, 128]), op=ALU.mult),
                    reads=[B_pex[ei], B_selT], writes=[B_pm[ei]])

            def at_back(u):
                g, n, r, s1 = aunits[u]
                ei = bi_of[u]
                pe.pre(reads=[B_pm[ei], B_kv], writes=[B_psc[0], B_psc[1]])
                for hh in range(4):
                    po = psc[hh // 3][:, (hh % 3) * 129:(hh % 3) * 129 + 129]
                    ins = nc.tensor.matmul(po, lhsT=pm[:, ei, hh * 128:(hh + 1) * 128], rhs=V1[:, r, s1, g, 0:129],
                                           start=(n == 0 and hh % 3 == 0), stop=(n == nt_ - 1),
                                           skip_group_check=True)
                ev = pe.done(ins)
                pe.post(ev, reads=[B_pm[ei], B_kv], writes=[B_psc[0], B_psc[1]])
                if n == nt_ - 1:
                    for hh in range(4):
                        po = psc[hh // 3][:, (hh % 3) * 129:(hh % 3) * 129 + 129]
                        hcol = g * 4 + hh
                        dve.op(lambda: nc.vector.reciprocal(out=rec[:, hcol:hcol + 1], in_=po[:, 128:129]),
                               reads=[B_psc[hh // 3]], writes=[B_rec])
                        dve.op(lambda: nc.vector.tensor_scalar(out=P["oat"][:, s, hcol * 128:(hcol + 1) * 128],
                                                               in0=po[:, 0:128], scalar1=rec[:, hcol:hcol + 1],
                                                               scalar2=None, op0=ALU.mult),
                               reads=[B_psc[hh // 3], B_rec], writes=[C["B_oat"]])
            for u in range(min(LA, len(aunits))):
                at_front(u)
            for u in range(len(aunits)):
                if u + LA < len(aunits):
                    at_front(u + LA)
                at_back(u)
        K.end_phase()
def phase_bs(K, C):
    nc = K.nc
    pe, act, dve, pool, sp = K.pe, K.act, K.dve, K.pool, K.sp
    P = C["P"]
    ident = C["ident"]
    NS = 16
    LK = 2049
    with ExitStack() as es:
        def sb(n, s, d):
            return es.enter_context(nc.sbuf_tensor("s_" + n, s, d))
        ptb = sb("ptb", [128, 256], I32)
        iop = sb("iop", [128, 1], I32)
        idx = sb("idx", [128, 256], I32)
        B_idx = Buf(K, dma=True)
        sp.dma(ptb[:], C["pt"].rearrange("i p -> (i p)").rearrange("(o n) -> o n", o=1).to_broadcast([128, 256]),
               B_idx.dsem, writes=[B_idx])
        pool.op(lambda: nc.gpsimd.iota(iop[:], pattern=[[0, 1]], base=0, channel_multiplier=1), writes=[B_idx])
        pool.op(lambda: nc.gpsimd.tensor_scalar(out=idx[:], in0=ptb[:], scalar1=128, scalar2=None, op0=ALU.mult),
                reads=[B_idx], writes=[B_idx])
        pool.op(lambda: nc.gpsimd.tensor_tensor(out=idx[:], in0=idx[:], in1=iop[:].to_broadcast([128, 256]), op=ALU.add),
                reads=[B_idx], writes=[B_idx])

        wperm = sb("wperm", [128, 64], BF16)
        wTp = sb("wTp", [32, 2, 128], BF16)
        B_w = Buf()
        Ssmp = sb("Ssmp", [NS, 2176], F32)
        B_Ss = Buf(K, dma=True)
        selms = sb("selms", [NS, 2176], BF16)
        B_selms = Buf()
        selTs = sb("selTs", [128, 16, 16], BF16)
        selfs = sb("selfs", [1, 16], F32)
        B_selT = Buf()
        st = sb("st", [NS, 64], F32)
        B_st = Buf()
        pw2 = sb("pw2", [NS, 32], F32)
        B_c = Buf()
        for k in range(NIT):
            dve.op(lambda: nc.vector.memset(pw2[:, k:k + 1], 2.0 ** (-k)), writes=[B_c])

        with ExitStack() as es1:
            def sb1(n, s, d):
                return es1.enter_context(nc.sbuf_tensor("s1_" + n, s, d))

            def ps1(n, s, d=F32):
                return es1.enter_context(nc.psum_tensor("s1_" + n, s, d))
            kig = sb1("kig", [128, 2, 16, 128], BF16)
            B_kig = [Buf(K, dma=True), Buf(K, dma=True)]
            kiTs = sb1("kiTs", [128, 2, 2048], BF16)
            B_kiTs = [Buf(), Buf()]
            rh = sb1("rh", [32, 2, 2, 512], BF16)
            B_rh = [Buf(), Buf()]
            srow = sb1("srow", [1, 1, 2176], F32)
            B_srow = [Buf(K, dma=True)] * 2
            ptr = [ps1(f"ptr{i}", [128, 1024], BF16) for i in range(2)]
            B_ptr = [Buf(), Buf()]
            psh = [ps1(f"psh{i}", [128, 512]) for i in range(2)]
            pso = [ps1(f"pso{i}", [128, 512]) for i in range(2)]
            B_psh = [Buf(), Buf()]
            pss = [ps1(f"pss{i}", [128, 512]) for i in range(2)]
            B_pss = [Buf(), Buf()]
            dve.op(lambda: nc.vector.memset(wperm[:], 0.0), writes=[B_w])
            wi8 = P["wi"][:, 8, :].rearrange("p (a b) -> p a b", b=2)
            dve.op(lambda: nc.vector.tensor_copy(wperm[:, 0:8], wi8[:, :, 0]), reads=[C["B_wi"]], writes=[B_w])
            dve.op(lambda: nc.vector.tensor_copy(wperm[:, 32:40], wi8[:, :, 1]), reads=[C["B_wi"]], writes=[B_w])
            for e in range(2):
                pe.op(lambda: nc.tensor.transpose(ptr[e][0:32, 0:128], wperm[:, e * 32:(e + 1) * 32], ident[:]),
                      reads=[B_w, K.B_ident], writes=[B_ptr[e]])
                act.op(lambda: nc.scalar.copy(out=wTp[:, e, :], in_=ptr[e][0:32, 0:128]), reads=[B_ptr[e]], writes=[B_w])
            nt = 1
            nh = 0
            for i in range(NS):
                o = i % 2
                tok = 1024 + i
                pool.pre(reads=[B_idx], writes=[B_kig[o]])
                for pg in range(16):
                    ins = nc.gpsimd.indirect_dma_start(
                        out=kig[:, o, pg, 0:64], out_offset=None, in_=C["cache_kidx"],
                        in_offset=bass.IndirectOffsetOnAxis(ap=idx[:, i * 16 + pg:i * 16 + pg + 1], axis=0))
                    B_kig[o].dsem.cnt += 16
                    ins.then_inc(B_kig[o].dsem.h, 16)
                    pool.nins += 1
                K.dma_sems[B_kig[o].dsem.uid] = B_kig[o].dsem
                ev = Ev(B_kig[o].dsem, B_kig[o].dsem.cnt)
                pool.post(ev, writes=[B_kig[o]])
                dve.op(lambda: nc.vector.tensor_copy(kig[:, o, :, 64:128], kig[:, o, :, 0:64]), reads=[B_kig[o]],
                       writes=[B_kig[o]])
                for q4 in range(4):
                    x = nt % 2
                    nt += 1
                    pe.pre(reads=[B_kig[o], K.B_ident], writes=[B_ptr[x]])
                    for b in range(4):
                        ins = nc.tensor.transpose(ptr[x][:, b * 128:(b + 1) * 128], kig[:, o, q4 * 4 + b, :], ident[:])
                    ev = pe.done(ins)
                    pe.post(ev, reads=[B_kig[o]], writes=[B_ptr[x]])
                    act.op(lambda: nc.scalar.copy(out=kiTs[:, o, q4 * 512:(q4 + 1) * 512], in_=ptr[x][:, 0:512]),
                           reads=[B_ptr[x]], writes=[B_kiTs[o]])
                for c in range(5):
                    c0 = c * 512
                    cw = 512 if c < 4 else 1
                    x = nh % 2
                    nh += 1
                    if c < 4:
                        rhs_e, rhs_o = kiTs[0:64, o, c0:c0 + cw], kiTs[64:128, o, c0:c0 + cw]
                        deps = [B_kiTs[o]]
                    else:
                        rhs_e, rhs_o = P["kiT8"][0:64, i:i + 1], P["kiT8"][64:128, i:i + 1]
                        deps = [C["B_s8"]]
                    pe.pre(reads=deps + [C["B_qiT"]], writes=[B_psh[x]])
                    nc.tensor.matmul(psh[x][0:8, 0:cw], lhsT=P["qiT"][0:64, :, tok], rhs=rhs_e, start=True, stop=True)
                    ins = nc.tensor.matmul(pso[x][0:8, 0:cw], lhsT=P["qiT"][64:128, :, tok], rhs=rhs_o, start=True,
                                           stop=True)
                    ev = pe.done(ins)
                    pe.post(ev, reads=deps + [C["B_qiT"]], writes=[B_psh[x]])
                    act.op(lambda: nc.scalar.activation(out=rh[0:8, x, 0, 0:cw], in_=psh[x][0:8, 0:cw], func=AF.Relu),
                           reads=[B_psh[x]], writes=[B_rh[x]])
                    act.op(lambda: nc.scalar.activation(out=rh[0:8, x, 1, 0:cw], in_=pso[x][0:8, 0:cw], func=AF.Relu),
                           reads=[B_psh[x]], writes=[B_rh[x]])
                    pe.pre(reads=[B_rh[x], B_w], writes=[B_pss[x]])
                    nc.tensor.matmul(pss[x][0:1, 0:cw], lhsT=wTp[0:8, 0, i:i + 1], rhs=rh[0:8, x, 0, 0:cw], start=True,
                                     stop=False)
                    ins = nc.tensor.matmul(pss[x][0:1, 0:cw], lhsT=wTp[0:8, 1, i:i + 1], rhs=rh[0:8, x, 1, 0:cw],
                                           start=False, stop=True)
                    ev = pe.done(ins)
                    pe.post(ev, reads=[B_rh[x], B_w], writes=[B_pss[x]])
                    dve.op(lambda: nc.vector.tensor_copy(srow[0:1, 0, c0:c0 + cw], pss[x][0:1, 0:cw]), reads=[B_pss[x]],
                           writes=[B_srow[o]])
                sp.dma(C["sscr"][i:i + 1, 0:LK], srow[0:1, 0, 0:LK], B_srow[o].dsem, reads=[B_srow[o]],
                       writes=[C["B_sscr"]])
            K.barrier()
        sp.dma(Ssmp[:, 0:LK], C["sscr"][:, 0:LK], B_Ss.dsem, reads=[C["B_sscr"]], writes=[B_Ss])
        S3 = Ssmp[:, 0:LK]
        dve.op(lambda: nc.vector.tensor_reduce(out=st[:, 32:33], in_=S3, axis=AX.X, op=ALU.max), reads=[B_Ss],
               writes=[B_st])
        dve.op(lambda: nc.vector.tensor_reduce(out=st[:, 33:34], in_=S3, axis=AX.X, op=ALU.min), reads=[B_Ss],
               writes=[B_st])
        dve.op(lambda: nc.vector.tensor_scalar(out=st[:, 33:34], in0=st[:, 33:34], scalar1=-1.0, scalar2=None,
                                               op0=ALU.mult), reads=[B_st], writes=[B_st])
        dve.op(lambda: nc.vector.tensor_tensor(out=st[:, 0:1], in0=st[:, 32:33], in1=st[:, 33:34], op=ALU.max),
               reads=[B_st], writes=[B_st])
        topk_threshold(K, S3, NS, selms[:, 0:LK], st, B_Ss, B_selms, B_st, pw2, B_c)
        dve.op(lambda: nc.vector.tensor_scalar(out=selms[:, 0:LK], in0=S3, scalar1=st[:, 1:2], scalar2=None,
                                               op0=ALU.is_ge), reads=[B_Ss, B_st], writes=[B_selms])

        with ExitStack() as es2:
            def sb2(n, s, d):
                return es2.enter_context(nc.sbuf_tensor("s2_" + n, s, d))

            def ps2(n, s, d=F32):
                return es2.enter_context(nc.psum_tensor("s2_" + n, s, d))
            Kg = sb2("Kg", [128, 2, 16, 256], BF16)
            B_Kg = [Buf(K, dma=True), Buf(K, dma=True)]
            Vc = sb2("Vc", [128, 1, 16, 256], BF16)
            B_Vc = [Buf(K, dma=True)] * 2
            Vg = sb2("Vg", [128, 2, 16, 2, 130], BF16)
            B_Vg = [Buf(), Buf()]
            kTs = sb2("kTs", [128, 2, 2, 2048], BF16)
            B_kTs = [Buf(), Buf()]
            vself = sb2("vself", [1, 16, 2, 130], BF16)
            B_vs = Buf(K, dma=True)
            pTs = sb2("pTs", [128, 2, 128], BF16)
            B_pTs = [Buf(), Buf()]
            pms = sb2("pms", [128, 2, 128], BF16)
            B_pms = [Buf(), Buf()]
            pself = sb2("pself", [1, 2, 8], BF16)
            pselfm = sb2("pselfm", [1, 2, 8], BF16)
            B_pself = [Buf(), Buf()]
            osm = sb2("osm", [4, 2, 2, 128], F32)
            B_osm = [Buf(K, dma=True), Buf(K, dma=True)]
            rec = sb2("rec", [4, 4], F32)
            B_rec = Buf()
            ptr = [ps2(f"ptr{i}", [128, 1024], BF16) for i in range(2)]
            B_ptr = [Buf(), Buf()]
            pl = [ps2(f"pl{i}", [128, 512]) for i in range(2)]
            B_pl = [Buf(), Buf()]
            psf = ps2("psf", [128, 512])
            B_psf = Buf()
            pos = [ps2(f"pos{i}", [128, 512]) for i in range(2)]
            B_pos = [Buf(), Buf()]
            pe.pre(reads=[B_selms, K.B_ident], writes=[B_ptr[0]])
            for pg in range(16):
                ins = nc.tensor.transpose(ptr[0][:, pg * 16:(pg + 1) * 16], selms[0:NS, pg * 128:(pg + 1) * 128],
                                          ident[0:NS, 0:NS])
            ev = pe.done(ins)
            pe.post(ev, reads=[B_selms], writes=[B_ptr[0]])
            act.op(lambda: nc.scalar.copy(out=selTs[:].rearrange("p a b -> p (a b)"), in_=ptr[0][:, 0:256]),
                   reads=[B_ptr[0]], writes=[B_selT])
            pe.op(lambda: nc.tensor.transpose(ptr[1][0:1, 0:NS], selms[0:NS, 2048:2049], ident[0:NS, 0:NS]),
                  reads=[B_selms, K.B_ident], writes=[B_ptr[1]])
            act.op(lambda: nc.scalar.copy(out=selfs[0:1, :], in_=ptr[1][0:1, 0:NS]), reads=[B_ptr[1]], writes=[B_selT])
            pool.op(lambda: nc.gpsimd.memset(vself[:], 1.0), writes=[B_vs])
            pool.dma(vself[0:1, :, :, 0:128], C["vo"][1024:1040, :].rearrange("(o i) (g d) -> o i g d", o=1, g=2),
                     B_vs.dsem, writes=[B_vs])
            for o in range(2):
                pool.op(lambda: nc.gpsimd.memset(Vg[:, o, :, :, 128:130], 1.0), writes=[B_Vg[o]])
            nt = 0
            for i in range(NS):
                o = i % 2
                tok = 1024 + i
                for (dst, Bd, srcc, oo) in ((Kg, B_Kg, C["cache_k"], o), (Vc, B_Vc, C["cache_v"], 0)):
                    pool.pre(reads=[B_idx], writes=[Bd[o]])
                    for pg in range(16):
                        ins = nc.gpsimd.indirect_dma_start(
                            out=dst[:, oo, pg, :], out_offset=None, in_=srcc,
                            in_offset=bass.IndirectOffsetOnAxis(ap=idx[:, i * 16 + pg:i * 16 + pg + 1], axis=0))
                        Bd[o].dsem.cnt += 16
                        ins.then_inc(Bd[o].dsem.h, 16)
                        pool.nins += 1
                    K.dma_sems[Bd[o].dsem.uid] = Bd[o].dsem
                    ev = Ev(Bd[o].dsem, Bd[o].dsem.cnt)
                    pool.post(ev, writes=[Bd[o]])
                act.op(lambda: nc.scalar.copy(out=Vg[:, o, :, :, 0:128],
                                              in_=Vc[:, 0, :, :].rearrange("p a (g d) -> p a g d", g=2)),
                       reads=[B_Vc[o]], writes=[B_Vg[o]])
                for pg4 in range(8):
                    x = nt % 2
                    nt += 1
                    pe.pre(reads=[B_Kg[o], K.B_ident], writes=[B_ptr[x]])
                    for b in range(4):
                        pg, g = (pg4 * 4 + b) // 2, (pg4 * 4 + b) % 2
                        ins = nc.tensor.transpose(ptr[x][:, b * 128:(b + 1) * 128], Kg[:, o, pg, g * 128:(g + 1) * 128],
                                                  ident[:])
                    ev = pe.done(ins)
                    pe.post(ev, reads=[B_Kg[o]], writes=[B_ptr[x]])
                    dstv = kTs[:, o, :, pg4 * 256:(pg4 + 1) * 256].rearrange("p g (a l) -> p a g l", a=2)
                    srcv = ptr[x][:, 0:512].rearrange("p (a g l) -> p a g l", a=2, g=2)
                    eng, veng = (act, None) if pg4 % 2 == 0 else (dve, None)
                    if pg4 % 2 == 0:
                        act.op(lambda: nc.scalar.copy(out=dstv, in_=srcv), reads=[B_ptr[x]], writes=[B_kTs[o]])
                    else:
                        dve.op(lambda: nc.vector.tensor_copy(dstv, srcv), reads=[B_ptr[x]], writes=[B_kTs[o]])
                pe.pre(reads=[B_kTs[o], C["B_qT"]], writes=[B_pl[o]])
                for pg in range(16):
                    for g in range(2):
                        ins = nc.tensor.matmul(pl[o][:, (pg * 2 + g) * 4:(pg * 2 + g) * 4 + 4],
                                               lhsT=kTs[:, o, g, pg * 128:(pg + 1) * 128],
                                               rhs=P["qT"][:, g * 4:(g + 1) * 4, tok], start=True, stop=True,
                                               skip_group_check=True)
                ev = pe.done(ins)
                pe.post(ev, reads=[B_kTs[o], C["B_qT"]], writes=[B_pl[o]])
                pe.pre(reads=[C["B_s8"], C["B_qT"]], writes=[B_psf])
                for g in range(2):
                    ins = nc.tensor.matmul(psf[0:1, g * 4:(g + 1) * 4], lhsT=P["kT8"][:, g * 128 + i:g * 128 + i + 1],
                                           rhs=P["qT"][:, g * 4:(g + 1) * 4, tok], start=True, stop=True,
                                           skip_group_check=True)
                ev = pe.done(ins)
                pe.post(ev, reads=[C["B_s8"], C["B_qT"]], writes=[B_psf])
                act.op(lambda: nc.scalar.activation(out=pTs[:, o, :], in_=pl[o][:, 0:128], func=AF.Exp, scale=SQ),
                       reads=[B_pl[o]], writes=[B_pTs[o]])
                act.op(lambda: nc.scalar.activation(out=pself[0:1, o, :], in_=psf[0:1, 0:8], func=AF.Exp, scale=SQ),
                       reads=[B_psf], writes=[B_pself[o]])
                dve.op(lambda: nc.vector.tensor_tensor(
                    out=pms[:, o, :].rearrange("p (a b) -> p a b", a=16),
                    in0=pTs[:, o, :].rearrange("p (a b) -> p a b", a=16),
                    in1=selTs[:, :, i].unsqueeze(2).to_broadcast([128, 16, 8]), op=ALU.mult),
                    reads=[B_pTs[o], B_selT], writes=[B_pms[o]])
                dve.op(lambda: nc.vector.tensor_scalar(out=pselfm[0:1, o, :], in0=pself[0:1, o, :],
                                                       scalar1=selfs[0:1, i:i + 1], scalar2=None, op0=ALU.mult),
                       reads=[B_pself[o], B_selT], writes=[B_pself[o]])
                for g in range(2):
                    pe.pre(reads=[B_pms[o], B_Vg[o], B_pself[o], B_vs], writes=[B_pos[g]])
                    for pg in range(16):
                        nc.tensor.matmul(pos[g][0:4, 0:129], lhsT=pms[:, o, (pg * 2 + g) * 4:(pg * 2 + g) * 4 + 4],
                                         rhs=Vg[:, o, pg, g, 0:129], start=(pg == 0), stop=False)
                    ins = nc.tensor.matmul(pos[g][0:4, 0:129], lhsT=pselfm[0:1, o, g * 4:(g + 1) * 4],
                                           rhs=vself[0:1, i, g, 0:129], start=False, stop=True)
                    ev = pe.done(ins)
                    pe.post(ev, reads=[B_pms[o], B_Vg[o], B_pself[o], B_vs], writes=[B_pos[g]])
                    dve.op(lambda: nc.vector.reciprocal(out=rec[0:4, g:g + 1], in_=pos[g][0:4, 128:129]),
                           reads=[B_pos[g]], writes=[B_rec])
                    dve.op(lambda: nc.vector.tensor_scalar(out=osm[0:4, o, g, :], in0=pos[g][0:4, 0:128],
                                                           scalar1=rec[0:4, g:g + 1], scalar2=None, op0=ALU.mult),
                           reads=[B_pos[g], B_rec], writes=[B_osm[o]])
                    sp.dma(C["oscr"][i, g * 512:(g + 1) * 512].rearrange("(h d) -> h d", h=4), osm[0:4, o, g, :],
                           B_osm[o].dsem, reads=[B_osm[o]], writes=[C["B_oscr"]])
            K.barrier()
        pool.dma(P["oat"][0:NS, 8, :], C["oscr"][:, :], C["B_oat"].dsem, reads=[C["B_oscr"]], writes=[C["B_oat"]])
        K.end_phase()
def phase_g1(K, C):
    nc = K.nc
    pe, act, dve, pool, sp = K.pe, K.act, K.dve, K.pool, K.sp
    P = C["P"]
    with ExitStack() as es:
        def sb(n, s, d):
            return es.enter_context(nc.sbuf_tensor("g1_" + n, s, d))

        def ps(n, s, d=F32):
            return es.enter_context(nc.psum_tensor("g1_" + n, s, d))
        triu = sb("triu", [128, 128], F32)
        B_tri = Buf()
        sst = sb("sst", [128, 2, 4, 256], F32)
        B_sst = [Buf(K, dma=True), Buf(K, dma=True)]
        pa = [ps(f"pa{i}", [128, 512]) for i in range(2)]
        B_pa = [Buf(), Buf()]
        pl = [ps(f"pl{i}", [128, 512]) for i in range(2)]
        B_pl = [Buf(), Buf()]
        pool.op(lambda: nc.gpsimd.memset(triu[:], 1.0), writes=[B_tri])
        pool.op(lambda: nc.gpsimd.affine_select(out=triu[:], in_=triu[:], pattern=[[1, 128]], compare_op=ALU.is_ge,
                                                fill=0.0, base=0, channel_multiplier=-1),
                reads=[B_tri], writes=[B_tri])
        sp.dma(C["dec_in"], P["dec"][:].rearrange("p h t -> p (h t)"), C["B_dec"].dsem, reads=[C["B_dec"]],
               writes=[C["B_decin"]])
        all_gather(K, C["dec_in"], C["dec_out"], [C["B_decin"]], [C["B_deco"]])
        n = 0
        for t in range(8):
            ts = slice(t * 128, (t + 1) * 128)
            o = t % 2
            for h in range(4):
                i = n % 2
                n += 1
                pe.op(lambda: nc.tensor.matmul(pa[i][:, 0:128], lhsT=P["kgT"][:, h, ts], rhs=P["qgT"][:, h, ts],
                                               start=True, stop=True),
                      reads=[C["B_kgT"], C["B_qgT"]], writes=[B_pa[i]])
                dve.op(lambda: nc.vector.tensor_tensor(out=P["AT"][:, t, h, :], in0=pa[i][:, 0:128], in1=triu[:],
                                                       op=ALU.mult), reads=[B_pa[i], B_tri], writes=[C["B_AT"]])
                pe.op(lambda: nc.tensor.matmul(pl[i][:, 0:256], lhsT=P["khat"][:, t, h * 128:(h + 1) * 128],
                                               rhs=P["gv"][:, t, h * 256:(h + 1) * 256], start=True, stop=True),
                      reads=[C["B_khat"], C["B_gv"]], writes=[B_pl[i]])
                act.op(lambda: nc.scalar.copy(out=sst[:, o, h, :], in_=pl[i][:, 0:256]), reads=[B_pl[i]],
                       writes=[B_sst[o]])
            sp.dma(C["gst_in"][t].rearrange("(h p) v -> p h v", p=128), sst[:, o], B_sst[o].dsem, reads=[B_sst[o]],
                   writes=[C["B_gin"][t]])
            all_gather(K, C["gst_in"][t], C["gst_out"][t], [C["B_gin"][t]], [C["B_gout"][t]])
        K.end_phase()


def phase_g2(K, C):
    nc = K.nc
    pe, act, dve, pool, sp = K.pe, K.act, K.dve, K.pool, K.sp
    P = C["P"]
    ident = C["ident"]
    with ExitStack() as es:
        def sb(n, s, d):
            return es.enter_context(nc.sbuf_tensor("g2_" + n, s, d))

        def ps(n, s, d=F32):
            return es.enter_context(nc.psum_tensor("g2_" + n, s, d))
        Sg = sb("Sg", [128, 4, 4, 256], F32)
        B_Sg = Buf(K, dma=True)
        dg = sb("dg", [128, 4, 32], F32)
        B_dg = Buf(K, dma=True)
        oh = sb("oh", [128, 4], F32)
        gnw = sb("gnw", [128, 256], F32)
        B_c = Buf(K, dma=True)
        Srun = sb("Srun", [128, 4, 256], F32)
        B_run = Buf(K, dma=True)
        Sin = sb("Sin", [128, 4, 256], F32)
        B_in = Buf()
        Sinb = sb("Sinb", [128, 4, 256], BF16)
        B_inb = Buf()
        st = sb("st", [128, 16], F32)
        B_st = Buf()
        junk = sb("junk", [128, 256], BF16)
        B_junk = Buf()
        po = [ps(f"po{i}", [128, 512]) for i in range(2)]
        B_po = [Buf(), Buf()]

        sp.dma(oh[:], C["onehot"], B_c.dsem, writes=[B_c])
        sp.dma(gnw[:], C["gla_norm_w"].rearrange("(o d) -> o d", o=1).to_broadcast([128, 256]), B_c.dsem, writes=[B_c])
        sp.dma(dg[:], C["dec_out"].rearrange("(r p) c -> p r c", p=128), B_dg.dsem, reads=[C["B_deco"]], writes=[B_dg])
        dve.op(lambda: nc.vector.memset(Srun[:], 0.0), writes=[B_run])

        def head_norm(pz, B_pz, np_, out_ap, B_out):
            dve.op(lambda: nc.vector.memset(st[0:np_, 0:1], 0.0), writes=[B_st])
            act.op(lambda: nc.scalar.activation(out=junk[0:np_, :], in_=pz, func=AF.Square,
                                                accum_out=st[0:np_, 0:1]), reads=[B_pz, B_st], writes=[B_junk, B_st])
            act.op(lambda: nc.scalar.activation(out=st[0:np_, 1:2], in_=st[0:np_, 0:1], func=AF.Sqrt,
                                                scale=1.0 / 256.0, bias=EPS), reads=[B_st], writes=[B_st])
            dve.op(lambda: nc.vector.reciprocal(out=st[0:np_, 1:2], in_=st[0:np_, 1:2]), reads=[B_st], writes=[B_st])
            dve.op(lambda: nc.vector.scalar_tensor_tensor(out=out_ap, in0=pz, scalar=st[0:np_, 1:2],
                                                          in1=gnw[0:np_, :], op0=ALU.mult, op1=ALU.mult),
                   reads=[B_pz, B_st, B_c], writes=[B_out])

        n = 0
        for s in range(8):
            ts = slice(s * 128, (s + 1) * 128)
            sp.dma(Sg[:], C["gst_out"][s].rearrange("(r h p) v -> p r h v", p=128, h=4), B_Sg.dsem,
                   reads=[C["B_gout"][s]], writes=[B_Sg])
            dve.op(lambda: nc.vector.memset(Sin[:], 0.0), reads=[], writes=[B_in])
            for r in range(4):
                dve.op(lambda: nc.vector.scalar_tensor_tensor(out=Sin[:], in0=Srun[:], scalar=oh[:, r:r + 1],
                                                              in1=Sin[:], op0=ALU.mult, op1=ALU.add),
                       reads=[B_run, B_c, B_in], writes=[B_in])
                for h in range(4):
                    dve.op(lambda: nc.vector.scalar_tensor_tensor(out=Srun[:, h, :], in0=Srun[:, h, :],
                                                                  scalar=dg[:, r, h * 8 + s:h * 8 + s + 1],
                                                                  in1=Sg[:, r, h, :], op0=ALU.mult, op1=ALU.add),
                           reads=[B_run, B_dg, B_Sg], writes=[B_run])
            act.op(lambda: nc.scalar.copy(out=Sinb[:], in_=Sin[:]), reads=[B_in], writes=[B_inb])
            for h in range(4):
                i = n % 2
                n += 1
                pe.pre(reads=[C["B_AT"], C["B_gv"], C["B_qgT"], B_inb], writes=[B_po[i]])
                nc.tensor.matmul(po[i][:, 0:256], lhsT=P["AT"][:, s, h, :], rhs=P["gv"][:, s, h * 256:(h + 1) * 256],
                                 start=True, stop=False)
                ins = nc.tensor.matmul(po[i][:, 0:256], lhsT=P["qgT"][:, h, ts], rhs=Sinb[:, h, :],
                                       start=False, stop=True)
                ev = pe.done(ins)
                pe.post(ev, reads=[C["B_AT"], C["B_gv"], C["B_qgT"], B_inb], writes=[B_po[i]])
                head_norm(po[i][:, 0:256], B_po[i], 128, P["og"][:, s, h * 256:(h + 1) * 256], C["B_og"])
        sp.dma(C["gla_p"].rearrange("(h p) v -> p h v", p=128), Srun[:], B_run.dsem, reads=[B_run])

        S0 = sb("S0", [128, 2, 4, 256], F32)
        B_S0 = [Buf(K, dma=True), Buf(K, dma=True)]
        Sn = sb("Sn", [128, 2, 4, 256], F32)
        B_Sn = [Buf(K, dma=True), Buf(K, dma=True)]
        Snb = sb("Snb", [128, 2, 4, 256], BF16)
        B_Snb = [Buf(), Buf()]
        tmp = sb("tmp", [128, 2, 256], F32)
        B_tmp = [Buf(), Buf()]
        Bsel = sb("Bsel", [128, 16, 128], BF16)
        I16 = sb("I16", [128, 16, 16], BF16)
        Qpad = sb("Qpad", [128, 4, 16, 16], BF16)
        B_q = Buf()
        pb = [ps(f"pb{i}", [128, 512]) for i in range(2)]
        B_pb = [Buf(), Buf()]
        pso = [ps(f"pso{i}", [128, 512]) for i in range(2)]
        B_pso = Buf()
        dve.op(lambda: nc.vector.tensor_copy(Bsel[:], ident[:, 0:16].unsqueeze(2).to_broadcast([128, 16, 128])),
               reads=[K.B_ident], writes=[B_q])
        dve.op(lambda: nc.vector.memset(I16[:], 0.0), writes=[B_q])
        for i in range(16):
            dve.op(lambda: nc.vector.memset(I16[:, i, i:i + 1], 1.0), writes=[B_q])
        for h in range(4):
            dve.op(lambda: nc.vector.tensor_tensor(out=Qpad[:, h], in0=P["qgT"][:, h, 1024:1040].unsqueeze(2).to_broadcast(
                [128, 16, 16]), in1=I16[:], op=ALU.mult), reads=[C["B_qgT"], B_q], writes=[B_q])
        stin = C["state_in"].rearrange("(i h p) v -> i p h v", p=128, h=4)
        stout = C["gla_s"].rearrange("(i h p) v -> i p h v", p=128, h=4)
        pe.pre(writes=[B_pso])
        for i in range(16):
            o = i % 2
            sp.dma(S0[:, o], stin[i], B_S0[o].dsem, writes=[B_S0[o]])
            for h in range(4):
                x = n % 2
                n += 1
                pe.op(lambda: nc.tensor.matmul(pb[x][:, 0:256], lhsT=Bsel[:, i, :], rhs=P["gv"][:, 8, h * 256:(h + 1) * 256],
                                               start=True, stop=True), reads=[B_q, C["B_gv"]], writes=[B_pb[x]])
                dve.op(lambda: nc.vector.tensor_scalar(out=tmp[:, x, :], in0=pb[x][:, 0:256],
                                                       scalar1=P["kg8f"][:, h, i:i + 1], scalar2=None, op0=ALU.mult),
                       reads=[B_pb[x], C["B_kgT"]], writes=[B_tmp[x]])
                dve.op(lambda: nc.vector.scalar_tensor_tensor(out=Sn[:, o, h, :], in0=S0[:, o, h, :],
                                                              scalar=P["dec8"][:, h, i:i + 1], in1=tmp[:, x, :],
                                                              op0=ALU.mult, op1=ALU.add),
                       reads=[B_S0[o], B_tmp[x], C["B_dec"]], writes=[B_Sn[o]])
            sp.dma(stout[i], Sn[:, o], B_Sn[o].dsem, reads=[B_Sn[o]])
            act.op(lambda: nc.scalar.copy(out=Snb[:, o], in_=Sn[:, o]), reads=[B_Sn[o]], writes=[B_Snb[o]])
            pe.pre(reads=[B_Snb[o], B_q])
            for h in range(4):
                ins = nc.tensor.matmul(pso[h // 2][0:16, (h % 2) * 256:(h % 2) * 256 + 256], lhsT=Qpad[:, h, i, :],
                                       rhs=Snb[:, o, h, :], start=(i == 0 and h % 2 == 0), stop=(i == 15),
                                       skip_group_check=True)
            ev = pe.done(ins)
            pe.post(ev, reads=[B_Snb[o], B_q], writes=[B_pso])
        for h in range(4):
            head_norm(pso[h // 2][0:16, (h % 2) * 256:(h % 2) * 256 + 256], B_pso, 16,
                      P["og"][0:16, 8, h * 256:(h + 1) * 256], C["B_og"])
        K.end_phase()
def phase_c(K, C):
    nc = K.nc
    pe, act, dve, pool, sp = K.pe, K.act, K.dve, K.pool, K.sp
    ident = C["ident"]
    win_v = C["w_in"].rearrange("(k p) f -> p k f", p=128)
    wpa_v = C["w_proj_attn"].rearrange("(k p) f -> p k f", p=128)
    wpg_v = C["w_proj_gla"].rearrange("(k p) f -> p k f", p=128)
    wo_v = C["w_out"].rearrange("(k p) f -> p k f", p=128)
    with ExitStack() as esm:
        def sbm(n, s, d):
            return esm.enter_context(nc.sbuf_tensor("c_" + n, s, d))
        mT = sbm("mT", [128, 16, TOK], BF16)
        B_mT = [Buf() for _ in range(NT)]
        with ExitStack() as esg:
            merged = esg.enter_context(nc.sbuf_tensor("c_merged", [128, NT, D], BF16))
            B_mg = [Buf() for _ in range(NT)]
            with ExitStack() as es:
                def sb(n, s, d):
                    return es.enter_context(nc.sbuf_tensor("c_" + n, s, d))

                def ps(n, s, d=F32):
                    return es.enter_context(nc.psum_tensor("c_" + n, s, d))
                uT = sb("uT", [128, 16, TOK], BF16)
                B_uT = [Buf() for _ in range(NT)]
                oatT = sb("oatT", [128, 8, TOK], BF16)
                ogT = sb("ogT", [128, 8, TOK], BF16)
                B_oatT = [Buf() for _ in range(NT)]
                B_ogT = [Buf() for _ in range(NT)]
                ptr = [ps(f"ptr{i}", [128, 1024], BF16) for i in range(2)]
                B_ptr = [Buf(), Buf()]
                norm_T(K, C["h1"], C["mix_pre_w"], uT, B_uT, ident, ptr, B_ptr, "c1_")
                pp = [ps(f"pp{i}", [128, 512]) for i in range(4)]
                B_pp = [Buf() for _ in range(4)]
                wg = sb("wg", [128, 2, 16, 512], BF16)
                B_wg = [Buf(K, dma=True), Buf(K, dma=True)]
                wp = sb("wp", [128, 2, 8, 512], BF16)
                B_wp = [Buf(K, dma=True), Buf(K, dma=True)]
                ost = sb("ost", [128, 1, 1024], BF16)
                B_ost = [Buf(K, dma=True)] * 2
                gst = sb("gst", [128, 1, 1024], BF16)
                B_gst = [Buf(K, dma=True)] * 2
                sg = sb("sg", [128, 4, 512], BF16)
                B_sg = [Buf() for _ in range(4)]
                tm = sb("tm", [128, 2, 512], F32)
                B_tm = [Buf(), Buf()]
                npp = [0]
                ntr = [0]

                def tr8(src_slot_ap, B_src, dstT, B_dst, t):
                    for half in range(2):
                        x = ntr[0] % 2
                        ntr[0] += 1
                        pe.pre(reads=[B_src, K.B_ident], writes=[B_ptr[x]])
                        for b in range(4):
                            k = half * 4 + b
                            ins = nc.tensor.transpose(ptr[x][:, b * 128:(b + 1) * 128],
                                                      src_slot_ap[:, k * 128:(k + 1) * 128], ident[:])
                        ev = pe.done(ins)
                        pe.post(ev, reads=[B_src], writes=[B_ptr[x]])
                        act.op(lambda: nc.scalar.copy(out=dstT[:, half * 4:half * 4 + 4, t * 128:(t + 1) * 128],
                                                      in_=ptr[x][:, 0:512].rearrange("p (a b) -> p a b", a=4)),
                               reads=[B_ptr[x]], writes=[B_dst[t]])

                oat_d = C["oat_d"].bitcast(BF16)
                og_d = C["og_d"].bitcast(BF16)
                for t in range(NT):
                    o = t % 2
                    sp.dma(ost[:, 0, :], oat_d[t * 128:(t + 1) * 128, :], B_ost[o].dsem, writes=[B_ost[o]])
                    tr8(ost[:, 0, :], B_ost[o], oatT, B_oatT, t)
                for blk in range(2):
                    pool.dma(wg[:, blk, :, :], win_v[:, :, CGR + blk * 512:CGR + (blk + 1) * 512], B_wg[blk].dsem,
                             writes=[B_wg[blk]])
                for t in range(NT):
                    o = t % 2
                    sp.dma(gst[:, 0, :], og_d[t * 128:(t + 1) * 128, :], B_gst[o].dsem, writes=[B_gst[o]])
                    for blk in range(2):
                        i = npp[0] % 4
                        npp[0] += 1
                        pe.pre(reads=[B_uT[t], B_wg[blk]], writes=[B_pp[i]])
                        for k in range(16):
                            ins = nc.tensor.matmul(pp[i][:, :], lhsT=uT[:, k, t * 128:(t + 1) * 128], rhs=wg[:, blk, k, :],
                                                   start=(k == 0), stop=(k == 15))
                        ev = pe.done(ins)
                        pe.post(ev, reads=[B_uT[t], B_wg[blk]], writes=[B_pp[i]])
                        act.op(lambda: nc.scalar.activation(out=sg[:, i, :], in_=pp[i][:, :], func=AF.Silu),
                               reads=[B_pp[i]], writes=[B_sg[i]])
                        dve.op(lambda: nc.vector.tensor_tensor(out=gst[:, 0, blk * 512:(blk + 1) * 512],
                                                               in0=gst[:, 0, blk * 512:(blk + 1) * 512], in1=sg[:, i, :],
                                                               op=ALU.mult), reads=[B_sg[i], B_gst[o]], writes=[B_gst[o]])
                    tr8(gst[:, 0, :], B_gst[o], ogT, B_ogT, t)
                for nb in range(4):
                    cs = slice(nb * 512, (nb + 1) * 512)
                    pool.dma(wg[:, 0, :, :], win_v[:, :, CGA + nb * 512:CGA + (nb + 1) * 512], B_wg[0].dsem,
                             writes=[B_wg[0]])
                    pool.dma(wg[:, 1, :, :], win_v[:, :, CGG + nb * 512:CGG + (nb + 1) * 512], B_wg[1].dsem,
                             writes=[B_wg[1]])
                    pool.dma(wp[:, 0, :, :], wpa_v[:, :, cs], B_wp[0].dsem, writes=[B_wp[0]])
                    pool.dma(wp[:, 1, :, :], wpg_v[:, :, cs], B_wp[1].dsem, writes=[B_wp[1]])
                    for t in range(NT):
                        ts = slice(t * 128, (t + 1) * 128)
                        ids = []
                        for which in range(4):
                            i = npp[0] % 4
                            npp[0] += 1
                            ids.append(i)
                            if which < 2:
                                srcT, Bs, w, Bw, nk = uT, B_uT, wg[:, which], B_wg[which], 16
                            elif which == 2:
                                srcT, Bs, w, Bw, nk = oatT, B_oatT, wp[:, 0], B_wp[0], 8
                            else:
                                srcT, Bs, w, Bw, nk = ogT, B_ogT, wp[:, 1], B_wp[1], 8
                            pe.pre(reads=[Bs[t], Bw], writes=[B_pp[i]])
                            for k in range(nk):
                                ins = nc.tensor.matmul(pp[i][:, :], lhsT=srcT[:, k, ts], rhs=w[:, k, :],
                                                       start=(k == 0), stop=(k == nk - 1))
                            ev = pe.done(ins)
                            pe.post(ev, reads=[Bs[t], Bw], writes=[B_pp[i]])
                            if which < 2:
                                act.op(lambda: nc.scalar.activation(out=sg[:, i, :], in_=pp[i][:, :], func=AF.Sigmoid),
                                       reads=[B_pp[i]], writes=[B_sg[i]])
                        ia, ig, ipa, ipg = ids
                        x = t % 2
                        dve.op(lambda: nc.vector.tensor_tensor(out=tm[:, x, :], in0=sg[:, ia, :], in1=pp[ipa][:, :],
                                                               op=ALU.mult), reads=[B_sg[ia], B_pp[ipa]], writes=[B_tm[x]])
                        dve.op(lambda: nc.vector.tensor_tensor(out=sg[:, ig, :], in0=sg[:, ig, :], in1=pp[ipg][:, :],
                                                               op=ALU.mult), reads=[B_sg[ig], B_pp[ipg]], writes=[B_sg[ig]])
                        pool.op(lambda: nc.gpsimd.tensor_tensor(out=merged[:, t, cs], in0=tm[:, x, :], in1=sg[:, ig, :],
                                                                op=ALU.add), reads=[B_tm[x], B_sg[ig]], writes=[B_mg[t]])
                K.barrier()
            with ExitStack() as es:
                ptr = [es.enter_context(nc.psum_tensor(f"c2_ptr{i}", [128, 1024], BF16)) for i in range(2)]
                B_ptr = [Buf(), Buf()]
                n = 0
                for t in range(NT):
                    for q4 in range(4):
                        x = n % 2
                        n += 1
                        pe.pre(reads=[B_mg[t], K.B_ident], writes=[B_ptr[x]])
                        for b in range(4):
                            k = q4 * 4 + b
                            ins = nc.tensor.transpose(ptr[x][:, b * 128:(b + 1) * 128], merged[:, t, k * 128:(k + 1) * 128],
                                                      ident[:])
                        ev = pe.done(ins)
                        pe.post(ev, reads=[B_mg[t]], writes=[B_ptr[x]])
                        if q4 % 2 == 0:
                            act.op(lambda: nc.scalar.copy(out=mT[:, q4 * 4:q4 * 4 + 4, t * 128:(t + 1) * 128],
                                                          in_=ptr[x][:, 0:512].rearrange("p (a b) -> p a b", a=4)),
                                   reads=[B_ptr[x]], writes=[B_mT[t]])
                        else:
                            dve.op(lambda: nc.vector.tensor_copy(mT[:, q4 * 4:q4 * 4 + 4, t * 128:(t + 1) * 128],
                                                                 ptr[x][:, 0:512].rearrange("p (a b) -> p a b", a=4)),
                                   reads=[B_ptr[x]], writes=[B_mT[t]])
                K.barrier()
        with ExitStack() as es:
            def sb(n, s, d):
                return es.enter_context(nc.sbuf_tensor("c3_" + n, s, d))
            wo = sb("wo", [128, 16, D], BF16)
            B_wo = Buf(K, dma=True)
            wbc = sb("wbc", [128, D], F32)
            hst = sb("hst", [128, 2, D], F32)
            B_hst = [Buf(K, dma=True), Buf(K, dma=True)]
            ot = sb("ot", [128, 2, D], F32)
            B_ot = [Buf(K, dma=True), Buf(K, dma=True)]
            junk = sb("junk", [128, 512], BF16)
            B_junk = Buf()
            st = sb("st", [128, 8 * NT], F32)
            B_st = Buf()
            po = [es.enter_context(nc.psum_tensor(f"c3_po{i}", [128, 512])) for i in range(8)]
            B_po = [Buf() for _ in range(8)]
            for nb in range(4):
                pool.dma(wo[:, :, nb * 512:(nb + 1) * 512], wo_v[:, :, nb * 512:(nb + 1) * 512], B_wo.dsem, writes=[B_wo])
            sp.dma(wbc[:], C["mix_post_w"].rearrange("(o d) -> o d", o=1).to_broadcast([128, D]), B_wo.dsem,
                   writes=[B_wo])
            dve.op(lambda: nc.vector.memset(st[:], 0.0), writes=[B_st])
            for t in range(NT):
                o = t % 2
                ts = slice(t * 128, (t + 1) * 128)
                sp.dma(hst[:, o, :], C["h1"][ts, :], B_hst[o].dsem, writes=[B_hst[o]])
                for nb in range(4):
                    i = o * 4 + nb
                    pe.pre(reads=[B_mT[t], B_wo], writes=[B_po[i]])
                    for k in range(16):
                        ins = nc.tensor.matmul(po[i][:, :], lhsT=mT[:, k, ts], rhs=wo[:, k, nb * 512:(nb + 1) * 512],
                                               start=(k == 0), stop=(k == 15))
                    ev = pe.done(ins)
                    pe.post(ev, reads=[B_mT[t], B_wo], writes=[B_po[i]])
                    act.op(lambda: nc.scalar.activation(out=junk[:], in_=po[i][:, :], func=AF.Square,
                                                        accum_out=st[:, t * 8 + nb:t * 8 + nb + 1]),
                           reads=[B_po[i], B_st], writes=[B_junk, B_st])
                c = t * 8
                dve.op(lambda: nc.vector.tensor_reduce(out=st[:, c + 4:c + 5], in_=st[:, c:c + 4], axis=AX.X, op=ALU.add),
                       reads=[B_st], writes=[B_st])
                act.op(lambda: nc.scalar.activation(out=st[:, c + 5:c + 6], in_=st[:, c + 4:c + 5], func=AF.Sqrt,
                                                    scale=1.0 / D, bias=EPS), reads=[B_st], writes=[B_st])
                dve.op(lambda: nc.vector.reciprocal(out=st[:, c + 5:c + 6], in_=st[:, c + 5:c + 6]), reads=[B_st],
                       writes=[B_st])
                for nb in range(4):
                    i = o * 4 + nb
                    cs = slice(nb * 512, (nb + 1) * 512)
                    dve.op(lambda: nc.vector.scalar_tensor_tensor(out=ot[:, o, cs], in0=po[i][:, :],
                                                                  scalar=st[:, c + 5:c + 6], in1=wbc[:, cs],
                                                                  op0=ALU.mult, op1=ALU.mult),
                           reads=[B_po[i], B_st, B_wo], writes=[B_ot[o]])
                pool.op(lambda: nc.gpsimd.tensor_tensor(out=ot[:, o, :], in0=ot[:, o, :], in1=hst[:, o, :], op=ALU.add),
                        reads=[B_hst[o], B_ot[o]], writes=[B_ot[o]])
                sp.dma(C["h2"][ts, :], ot[:, o, :], B_ot[o].dsem, reads=[B_ot[o]])
            K.barrier()


WNAMES = [("ffn1_pre_w", [D]), ("ffn1_w_gate", [D, DFF]), ("ffn1_w_up", [D, DFF]), ("ffn1_w_down", [DFF, D]),
          ("ffn1_post_w", [D]), ("mix_pre_w", [D]), ("w_in", [D, DIN]), ("gla_gate_w2", [16, 512]),
          ("gla_gate_b", [512]), ("gla_norm_w", [256]), ("w_proj_attn", [1024, D]), ("w_proj_gla", [1024, D]),
          ("w_out", [D, D]), ("mix_post_w", [D]), ("ffn2_pre_w", [D]), ("ffn2_w_gate", [D, DFF]),
          ("ffn2_w_up", [D, DFF]), ("ffn2_w_down", [DFF, D]), ("ffn2_post_w", [D])]
NPOOL_ROWS = 2560 * 128


def build(stage=99, debug=False):
    K = Kern()
    nc = K.nc
    C = {}
    full = stage >= 4
    x = K.dram("x", [TOK, D], F32, "ExternalInput").ap()
    C["posv"] = K.dram("posv", [128, NT], F32, "ExternalInput").ap()
    K.used = WNAMES[:5] if stage == 1 else (WNAMES[:9] if stage in (2, 3) else WNAMES)
    for name, shape in K.used:
        C[name] = K.dram(name, shape, F32, "ExternalInput").ap()
    y = K.dram("y", [TOK, D], F32, "ExternalOutput").ap()
    C["ko"] = K.dram("ko", [TOK, 256], F32, "ExternalOutput").ap()
    C["vo"] = K.dram("vo", [TOK, 256], F32, "ExternalOutput").ap()
    C["kio"] = K.dram("kio", [TOK, 64], F32, "ExternalOutput").ap()
    dk = "ExternalOutput" if debug else "Internal"
    C["h1"] = K.dram("h1s", [TOK, D], F32, dk).ap()
    C["h2"] = K.dram("h2s", [TOK, D], F32, dk).ap()
    C["cmask"] = K.dram("cmask", [128, 512], F32, "ExternalInput").ap()
    if stage == 3:
        C["dbg"] = K.dram("dbg", [TOK, 1024], F32, "ExternalOutput").ap()
        C["dbg2"] = K.dram("dbg2", [128, 4136], F32, "ExternalOutput").ap()
    for nm, r, c in (("agk", 256, 512), ("agki", 64, 512), ("agv", 1024, 128), ("dec", 128, 32)):
        C[nm + "_in"] = K.dram(nm + "_in", [r, c], F32).ap()
        C[nm + "_out"] = K.dram(nm + "_out", [4 * r, c], F32).ap()
    if full:
        C["onehot"] = K.dram("onehot", [128, 4], F32, "ExternalInput").ap()
        C["pt"] = K.dram("pt", [16, 16], I32, "ExternalInput").ap()
        C["state_in"] = K.dram("state_in", [16 * 512, 256], F32, "ExternalInput").ap()
        C["cache_k"] = K.dram("cache_k", [NPOOL_ROWS, 256], F32, "ExternalInput").ap()
        C["cache_v"] = K.dram("cache_v", [NPOOL_ROWS, 256], F32, "ExternalInput").ap()
        C["cache_kidx"] = K.dram("cache_kidx", [NPOOL_ROWS, 64], F32, "ExternalInput").ap()
        C["gla_p"] = K.dram("gla_p", [512, 256], F32, "ExternalOutput").ap()
        C["gla_s"] = K.dram("gla_s", [16 * 512, 256], F32, "ExternalOutput").ap()
        C["gst_in"] = [K.dram(f"gst_in{t}", [512, 256], F32).ap() for t in range(8)]
        C["gst_out"] = [K.dram(f"gst_out{t}", [2048, 256], F32).ap() for t in range(8)]
        C["sscr"] = K.dram("sscr", [16, 2176], F32).ap()
        C["oscr"] = K.dram("oscr", [16, 1024], F32).ap()
        C["oat_d"] = K.dram("oat_d", [TOK, 512], F32, dk).ap()
        C["og_d"] = K.dram("og_d", [TOK, 512], F32, dk).ap()

    with ExitStack() as es0:
        ident = es0.enter_context(nc.sbuf_tensor("ident", [128, 128], BF16))
        C["ident"] = ident
        K.B_ident = Buf()
        K.pool.op(lambda: nc.gpsimd.memset(ident[:], 1.0), writes=[K.B_ident])
        K.pool.op(lambda: nc.gpsimd.affine_select(out=ident[:], in_=ident[:], pattern=[[-1, 128]],
                                                  compare_op=ALU.is_equal, fill=0.0, base=0, channel_multiplier=1),
                  reads=[K.B_ident], writes=[K.B_ident])
        K.barrier()

        ffn_phase(K, x, (y if stage == 1 else C["h1"]), C["ffn1_pre_w"], C["ffn1_w_gate"], C["ffn1_w_up"],
                  C["ffn1_w_down"], C["ffn1_post_w"], ident)
        if stage >= 2:
            with ExitStack() as es:
                def sb(n, s, d):
                    return es.enter_context(nc.sbuf_tensor(n, s, d))
                P = {}
                P["qT"] = sb("p_qT", [128, 8, TOK], BF16)
                P["qiT"] = sb("p_qiT", [128, 8, TOK], BF16)
                P["wi"] = sb("p_wi", [128, NT, 16], F32)
                P["qgT"] = sb("p_qgT", [128, 4, TOK], BF16)
                P["kgT"] = sb("p_kgT", [128, 4, TOK], BF16)
                P["khat"] = sb("p_khat", [128, NT, 512], BF16)
                P["gv"] = sb("p_gv", [128, NT, 1024], BF16)
                P["dec"] = sb("p_dec", [128, 4, 8], F32)
                P["dec8"] = sb("p_dec8", [128, 4, 128], F32)
                P["kg8f"] = sb("p_kg8f", [128, 4, 128], F32)
                P["kT8"] = sb("p_kT8", [128, 256], BF16)
                P["v8"] = sb("p_v8", [128, 256], BF16)
                P["kiT8"] = sb("p_kiT8", [128, 128], BF16)
                C["P"] = P
                for n in ("B_qT", "B_qiT", "B_wi", "B_qgT", "B_kgT", "B_khat", "B_gv", "B_s8", "B_AT", "B_og",
                          "B_ag1", "B_ag1o", "B_decin", "B_deco", "B_sscr", "B_oscr"):
                    C[n] = Buf()
                C["B_dec"] = Buf(K, dma=True, persist=True)
                C["B_gin"] = [Buf() for _ in range(8)]
                C["B_gout"] = [Buf() for _ in range(8)]
                phase_a(K, C)
                if full:
                    P["AT"] = sb("p_AT", [128, 8, 4, 128], BF16)
                    phase_g1(K, C)
                if stage >= 3:
                    P["oat"] = sb("p_oat", [128, NT, 1024], BF16)
                    C["B_oat"] = Buf(K, dma=True, persist=True)
                    K.dve.op(lambda: nc.vector.memset(P["oat"][:, 8, :], 0.0), writes=[C["B_oat"]])
                    phase_b(K, C)
                if stage == 3:
                    K.pool.dma(C["dbg"].rearrange("(t p) c -> p t c", p=128), P["oat"][:], C["B_oat"].dsem,
                               reads=[C["B_oat"]])
                if full:
                    phase_bs(K, C)
                    K.sp.dma(C["oat_d"].bitcast(BF16).rearrange("(t p) c -> p t c", p=128), P["oat"][:],
                             C["B_oat"].dsem, reads=[C["B_oat"]])
                    P["og"] = sb("p_og", [128, NT, 1024], BF16)
                    C["B_og"] = Buf(K, dma=True, persist=True)
                    K.dve.op(lambda: nc.vector.memset(P["og"][:, 8, :], 0.0), writes=[C["B_og"]])
                    phase_g2(K, C)
                    K.sp.dma(C["og_d"].bitcast(BF16).rearrange("(t p) c -> p t c", p=128), P["og"][:],
                             C["B_og"].dsem, reads=[C["B_og"]])
                K.barrier()
            if full:
                phase_c(K, C)
                ffn_phase(K, C["h2"], y, C["ffn2_pre_w"], C["ffn2_w_gate"], C["ffn2_w_up"], C["ffn2_w_down"],
                          C["ffn2_post_w"], ident, tag="f2")
    K.barrier()
    K.es.close()
    return K


def core_rows(x_prompt, x_sample, c):
    b, j = c // 4, c % 4
    rows = [x_prompt[b, (4 * s + j) * 128:(4 * s + j + 1) * 128] for s in range(8)]
    pad = np.zeros((128, x_sample.shape[-1]), np.float32)
    pad[:16] = x_sample[16 * c:16 * c + 16, 0]
    rows.append(pad)
    return np.ascontiguousarray(np.concatenate(rows, 0))


def make_in_maps(inputs, used=WNAMES, full=True):
    in_maps = []
    shared = {k: np.ascontiguousarray(inputs[k], dtype=np.float32) for k, _ in used}
    if full:
        shared["cache_k"] = np.ascontiguousarray(inputs["cache_k"]).reshape(NPOOL_ROWS, 256)
        shared["cache_v"] = np.ascontiguousarray(inputs["cache_v"]).reshape(NPOOL_ROWS, 256)
        shared["cache_kidx"] = np.ascontiguousarray(inputs["cache_kidx"]).reshape(NPOOL_ROWS, 64)
    for c in range(NCORES):
        j = c % 4
        m = {"x": core_rows(inputs["x_prompt"], inputs["x_sample"], c)}
        posv = np.zeros((128, NT), np.float32)
        for s in range(8):
            posv[:, s] = (4 * s + j) * 128 + np.arange(128)
        posv[:, 8] = 2048.0
        m["posv"] = posv
        cm = np.zeros((128, 4, 128), np.float32)
        for r in range(4):
            if r > j:
                cm[:, r, :] = -1.0e4
            elif r == j:
                cm[:, r, :] = np.where(np.arange(128)[None, :] > np.arange(128)[:, None], -1.0e4, 0.0)
        m["cmask"] = cm.reshape(128, 512)
        if full:
            oh = np.zeros((128, 4), np.float32)
            oh[:, j] = 1.0
            m["onehot"] = oh
            m["pt"] = np.ascontiguousarray(inputs["page_table"][16 * c:16 * c + 16], dtype=np.int32)
            m["state_in"] = np.ascontiguousarray(inputs["state_gla"][16 * c:16 * c + 16], dtype=np.float32).reshape(
                16 * 512, 256)
        m.update(shared)
        in_maps.append(m)
    return in_maps


def assemble(results):
    y_p = np.zeros((2, 4096, D), np.float32)
    y_s = np.zeros((128, 1, D), np.float32)
    k_p = np.zeros((2, 4096, 2, 128), np.float32)
    v_p = np.zeros((2, 4096, 2, 128), np.float32)
    ki_p = np.zeros((2, 4096, 64), np.float32)
    k_s = np.zeros((128, 1, 2, 128), np.float32)
    v_s = np.zeros((128, 1, 2, 128), np.float32)
    ki_s = np.zeros((128, 1, 64), np.float32)
    gla_p = np.zeros((2, 4, 128, 256), np.float32)
    gla_s = np.zeros((128, 4, 128, 256), np.float32)
    for c, r in enumerate(results):
        b, j = c // 4, c % 4
        for s in range(8):
            sl = slice((4 * s + j) * 128, (4 * s + j + 1) * 128)
            rs = slice(s * 128, (s + 1) * 128)
            y_p[b, sl] = r["y"][rs]
            k_p[b, sl] = r["ko"][rs].reshape(128, 2, 128)
            v_p[b, sl] = r["vo"][rs].reshape(128, 2, 128)
            ki_p[b, sl] = r["kio"][rs]
        ss = slice(16 * c, 16 * c + 16)
        y_s[ss, 0] = r["y"][1024:1040]
        k_s[ss, 0] = r["ko"][1024:1040].reshape(16, 2, 128)
        v_s[ss, 0] = r["vo"][1024:1040].reshape(16, 2, 128)
        ki_s[ss, 0] = r["kio"][1024:1040]
        gla_s[ss] = r["gla_s"].reshape(16, 4, 128, 256)
        if j == 0:
            gla_p[b] = r["gla_p"].reshape(4, 128, 256)
    return (y_p, y_s, k_p, v_p, ki_p, gla_p, k_s, v_s, ki_s, gla_s)


def kernel(**inputs):
    K = build()
    in_maps = make_in_maps(inputs, K.used, True)
    res = run_bass_kernel_spmd(K.nc, in_maps, core_ids=list(range(NCORES)))
    return assemble(res.results)
```

```python
import numpy as np
from contextlib import ExitStack
import concourse.bass as bass
import concourse.mybir as mybir
from concourse.bass_utils import run_bass_kernel_spmd

F32 = mybir.dt.float32
BF16 = mybir.dt.bfloat16
I32 = mybir.dt.int32
ALU = mybir.AluOpType
AF = mybir.ActivationFunctionType
AX = mybir.AxisListType

NCORES = 8
NT = 9
TOK = NT * 128
D = 2048
DFF = 5632
DIN = 9824
EPS = 1e-6
TB = [(0, 512), (512, 512), (1024, 128)]


class Sem:
    def __init__(self, h, uid):
        self.h = h
        self.uid = uid
        self.cnt = 0


class Ev:
    __slots__ = ("sem", "val", "q", "idx")

    def __init__(self, sem, val, q=None, idx=0):
        self.sem = sem
        self.val = val
        self.q = q
        self.idx = idx


class Buf:
    def __init__(self, K=None, dma=False, persist=False):
        self.w = None
        self.r = {}
        self.dsem = K.new_sem("d", persist) if dma else None


class Q:
    def __init__(self, K, eng, name):
        self.K = K
        self.eng = eng
        self.name = name
        self.sem = K.new_sem(name, True)
        self.seen = {}
        self.nins = 0

    def wait(self, *evs):
        for ev in evs:
            if ev is None:
                continue
            if ev.q is self and self.nins - ev.idx >= 4:
                continue
            if self.seen.get(ev.sem.uid, -1) >= ev.val:
                continue
            self.seen[ev.sem.uid] = ev.val
            self.eng.wait_ge(ev.sem.h, ev.val)

    def done(self, ins):
        self.sem.cnt += 1
        self.nins += 1
        ins.then_inc(self.sem.h, 1)
        return Ev(self.sem, self.sem.cnt, self, self.nins)

    def tick(self, n=1):
        self.nins += n

    def pre(self, reads=(), writes=()):
        for b in reads:
            self.wait(b.w)
        for b in writes:
            self.wait(b.w)
            self.wait(*b.r.values())

    def post(self, ev, reads=(), writes=()):
        for b in reads:
            b.r[ev.sem.uid] = ev
        for b in writes:
            b.w = ev
            b.r = {}

    def op(self, fn, reads=(), writes=()):
        self.pre(reads, writes)
        ev = self.done(fn())
        self.post(ev, reads, writes)
        return ev

    def dma(self, out, in_, sem, reads=(), writes=(), **kw):
        for b in reads:
            self.wait(b.w)
        for b in writes:
            if not (b.w is not None and b.w.sem is sem):
                self.wait(b.w)
            self.wait(*b.r.values())
        ins = self.eng.dma_start(out=out, in_=in_, **kw)
        sem.cnt += 16
        ins.then_inc(sem.h, 16)
        self.nins += 1
        ev = Ev(sem, sem.cnt)
        self.post(ev, reads, writes)
        self.K.dma_sems[sem.uid] = sem
        return ev


class Kern:
    def __init__(self):
        self.nc = bass.Bass("TRN2", target_bir_lowering=False)
        self.es = ExitStack()
        self.nsem = 0
        self.dma_sems = {}
        self.free_sems = []
        self.phase_sems = []
        nc = self.nc
        self.pe = Q(self, nc.tensor, "pe")
        self.act = Q(self, nc.scalar, "act")
        self.dve = Q(self, nc.vector, "dve")
        self.pool = Q(self, nc.gpsimd, "pool")
        self.sp = Q(self, nc.sync, "sp")
        self.queues = [self.pe, self.act, self.dve, self.pool, self.sp]

    def new_sem(self, name, persist=False):
        if not persist and self.free_sems:
            s = self.free_sems.pop()
        else:
            self.nsem += 1
            h = self.es.enter_context(self.nc.semaphore(f"{name}{self.nsem}"))
            s = Sem(h, self.nsem)
        if not persist:
            self.phase_sems.append(s)
        return s

    def end_phase(self):
        self.barrier()
        self.free_sems.extend(self.phase_sems)
        self.phase_sems = []

    def barrier(self):
        evs = []
        for q in self.queues:
            if q.sem.cnt > 0:
                evs.append(Ev(q.sem, q.sem.cnt))
        for s in self.dma_sems.values():
            if s.cnt > 0:
                evs.append(Ev(s, s.cnt))
        for q in self.queues:
            q.wait(*evs)

    def dram(self, name, shape, dt, kind="Internal"):
        return self.nc.dram_tensor(name, list(shape), dt, kind=kind)


def ffn_phase(K, src, dst, pre_w, wg, wu, wd, post_w, ident, tag="f1"):
    nc = K.nc
    pe, act, dve, pool, sp = K.pe, K.act, K.dve, K.pool, K.sp
    NG = DFF // 256
    with ExitStack() as es:
        def sb(n, s, d):
            return es.enter_context(nc.sbuf_tensor(tag + n, s, d))

        def ps(n, s, d=F32):
            return es.enter_context(nc.psum_tensor(tag + n, s, d))

        acc = sb("f_acc", [128, NT, D], F32)
        zT = sb("f_zT", [128, 16, TOK], BF16)
        xst = sb("f_xst", [128, D], F32)
        zb = sb("f_zb", [128, D], BF16)
        wbc = sb("f_wbc", [128, D], F32)
        wgb = sb("f_wgb", [128, 2, 16, 256], BF16)
        wub = sb("f_wub", [128, 2, 16, 256], BF16)
        wdb = sb("f_wdb", [128, 3, 2, D], BF16)
        aT = sb("f_aT", [128, 2, 2, TOK], BF16)
        sg = sb("f_sg", [128, 2, 512], BF16)
        dtmp = sb("f_dtmp", [128, 2, 512], F32)
        B_dtmp = [Buf(), Buf()]
        st = sb("f_st", [128, 4 * NT], F32)
        ptr = [ps(f"f_ptr{i}", [128, 1024], BF16) for i in range(2)]
        pg = [ps(f"f_pg{i}", [128, 512]) for i in range(2)]
        pu = [ps(f"f_pu{i}", [128, 512]) for i in range(2)]
        pd = [ps(f"f_pd{i}", [128, 512]) for i in range(2)]

        B_xst = Buf(K, dma=True)
        B_zb = Buf()
        B_wbc = Buf(K, dma=True)
        B_st = Buf()
        B_ptr = [Buf(), Buf()]
        B_zT = [Buf() for _ in range(NT)]
        B_wgu = [Buf(K, dma=True) for _ in range(2)]
        B_wd = [Buf(K, dma=True) for _ in range(3)]
        B_aT = [[[Buf() for _ in range(3)] for _ in range(2)] for _ in range(2)]
        B_pg = [Buf(), Buf()]
        B_pu = [Buf(), Buf()]
        B_sg = [Buf(), Buf()]
        B_pd = [Buf(), Buf()]
        B_acc = [[Buf() for _ in range(4)] for _ in range(NT)]
        B_accst = [Buf(K, dma=True) for _ in range(NT)]

        wg_v = wg.rearrange("(k p) f -> p k f", p=128)
        wu_v = wu.rearrange("(k p) f -> p k f", p=128)
        wd_v = wd.rearrange("(c p) n -> p c n", p=128)

        def load_group(gi):
            s2, s3 = gi % 2, gi % 3
            c0 = gi * 256
            pool.dma(wgb[:, s2], wg_v[:, :, c0:c0 + 256], B_wgu[s2].dsem, writes=[B_wgu[s2]])
            pool.dma(wub[:, s2], wu_v[:, :, c0:c0 + 256], B_wgu[s2].dsem, writes=[B_wgu[s2]])
            pool.dma(wdb[:, s3], wd_v[:, 2 * gi:2 * gi + 2, :], B_wd[s3].dsem, writes=[B_wd[s3]])

        sp.dma(wbc[:], pre_w.rearrange("(o d) -> o d", o=1).to_broadcast([128, D]), B_wbc.dsem, writes=[B_wbc])
        load_group(0)
        dve.op(lambda: nc.vector.memset(st[:], 0.0), writes=[B_st])

        for t in range(NT):
            sp.dma(xst[:], src[t * 128:(t + 1) * 128, :], B_xst.dsem, writes=[B_xst])
            act.op(lambda: nc.scalar.activation(out=zb[:], in_=xst[:], func=AF.Square,
                                                accum_out=st[:, t:t + 1]),
                   reads=[B_xst], writes=[B_zb, B_st])
            act.op(lambda: nc.scalar.activation(out=st[:, NT + t:NT + t + 1], in_=st[:, t:t + 1], func=AF.Sqrt,
                                                scale=1.0 / D, bias=EPS),
                   reads=[B_st], writes=[B_st])
            dve.op(lambda: nc.vector.reciprocal(out=st[:, NT + t:NT + t + 1], in_=st[:, NT + t:NT + t + 1]),
                   reads=[B_st], writes=[B_st])
            dve.op(lambda: nc.vector.scalar_tensor_tensor(out=zb[:], in0=xst[:], scalar=st[:, NT + t:NT + t + 1],
                                                          in1=wbc[:], op0=ALU.mult, op1=ALU.mult),
                   reads=[B_xst, B_st, B_wbc], writes=[B_zb])
            for q4 in range(4):
                sl = q4 % 2
                pe.pre(reads=[B_zb, K.B_ident], writes=[B_ptr[sl]])
                for i in range(4):
                    k = q4 * 4 + i
                    ins = nc.tensor.transpose(ptr[sl][:, i * 128:(i + 1) * 128], zb[:, k * 128:(k + 1) * 128], ident[:])
                    pe.tick()
                ev = pe.done(ins)
                pe.nins -= 1
                pe.post(ev, reads=[B_zb], writes=[B_ptr[sl]])
                src_ap = ptr[sl][:, 0:512].rearrange("p (a b) -> p a b", a=4)
                dst_ap = zT[:, q4 * 4:q4 * 4 + 4, t * 128:(t + 1) * 128]
                if q4 % 2 == 0:
                    act.op(lambda: nc.scalar.copy(out=dst_ap, in_=src_ap), reads=[B_ptr[sl]], writes=[B_zT[t]])
                else:
                    dve.op(lambda: nc.vector.tensor_copy(dst_ap, src_ap), reads=[B_ptr[sl]], writes=[B_zT[t]])

        sp.dma(wbc[:], post_w.rearrange("(o d) -> o d", o=1).to_broadcast([128, D]), B_wbc.dsem, writes=[B_wbc])

        tiles_of_tb = [[0, 1, 2, 3], [4, 5, 6, 7], [8]]
        down_units = []

        def emit_down(gi, t, nb, idx):
            s2, s3 = gi % 2, gi % 3
            tb = t // 4
            sl = idx % 2
            pe.pre(reads=[B_aT[s2][0][tb], B_aT[s2][1][tb], B_wd[s3]], writes=[B_pd[sl]])
            for ci in range(2):
                ins = nc.tensor.matmul(pd[sl][:], lhsT=aT[:, s2, ci, t * 128:(t + 1) * 128],
                                       rhs=wdb[:, s3, ci, nb * 512:(nb + 1) * 512],
                                       start=(ci == 0), stop=(ci == 1))
                pe.tick()
            ev = pe.done(ins)
            pe.nins -= 1
            pe.post(ev, reads=[B_aT[s2][0][tb], B_aT[s2][1][tb], B_wd[s3]], writes=[B_pd[sl]])
            a_ap = acc[:, t, nb * 512:(nb + 1) * 512]
            if gi == 0:
                dve.op(lambda: nc.vector.tensor_copy(a_ap, pd[sl][:]), reads=[B_pd[sl]], writes=[B_acc[t][nb]])
            elif idx % 4 == 3:
                x = (idx // 4) % 2
                act.op(lambda: nc.scalar.copy(out=dtmp[:, x, :], in_=pd[sl][:]), reads=[B_pd[sl]], writes=[B_dtmp[x]])
                pool.op(lambda: nc.gpsimd.tensor_tensor(out=a_ap, in0=a_ap, in1=dtmp[:, x, :], op=ALU.add),
                        reads=[B_dtmp[x], B_acc[t][nb]], writes=[B_acc[t][nb]])
            else:
                dve.op(lambda: nc.vector.tensor_tensor(out=a_ap, in0=a_ap, in1=pd[sl][:], op=ALU.add),
                       reads=[B_pd[sl], B_acc[t][nb]], writes=[B_acc[t][nb]])

        didx = 0
        for gi in range(NG):
            s2 = gi % 2
            if gi + 1 < NG:
                load_group(gi + 1)
            step = 0
            for ci in range(2):
                for tbi, (t0, tn) in enumerate(TB):
                    sl = step % 2
                    zdeps = [B_zT[t] for t in tiles_of_tb[tbi]]
                    for (pp, Bp, wb) in ((pg, B_pg, wgb), (pu, B_pu, wub)):
                        pe.pre(reads=zdeps + [B_wgu[s2]], writes=[Bp[sl]])
                        for k in range(16):
                            ins = nc.tensor.matmul(pp[sl][:, 0:tn], lhsT=wb[:, s2, k, ci * 128:(ci + 1) * 128],
                                                   rhs=zT[:, k, t0:t0 + tn], start=(k == 0), stop=(k == 15))
                            pe.tick()
                        ev = pe.done(ins)
                        pe.nins -= 1
                        pe.post(ev, reads=zdeps + [B_wgu[s2]], writes=[Bp[sl]])
                    act.op(lambda: nc.scalar.activation(out=sg[:, sl, 0:tn], in_=pg[sl][:, 0:tn], func=AF.Silu),
                           reads=[B_pg[sl]], writes=[B_sg[sl]])
                    dve.op(lambda: nc.vector.tensor_tensor(out=aT[:, s2, ci, t0:t0 + tn], in0=sg[:, sl, 0:tn],
                                                           in1=pu[sl][:, 0:tn], op=ALU.mult),
                           reads=[B_sg[sl], B_pu[sl]], writes=[B_aT[s2][ci][tbi]])
                    step += 1
                    for _ in range(6):
                        if down_units:
                            g0, t, nb = down_units.pop(0)
                            emit_down(g0, t, nb, didx)
                            didx += 1
            down_units = [(gi, t, nb) for t in range(NT) for nb in range(4)]
        while down_units:
            g0, t, nb = down_units.pop(0)
            emit_down(g0, t, nb, didx)
            didx += 1

        for t in range(NT):
            sp.dma(xst[:], src[t * 128:(t + 1) * 128, :], B_xst.dsem, writes=[B_xst])
            act.op(lambda: nc.scalar.activation(out=zb[:], in_=acc[:, t, :], func=AF.Square,
                                                accum_out=st[:, 2 * NT + t:2 * NT + t + 1]),
                   reads=B_acc[t], writes=[B_zb, B_st])
            c = 3 * NT + t
            act.op(lambda: nc.scalar.activation(out=st[:, c:c + 1], in_=st[:, 2 * NT + t:2 * NT + t + 1], func=AF.Sqrt,
                                                scale=4.0 / D, bias=4.0 * EPS),
                   reads=[B_st], writes=[B_st])
            dve.op(lambda: nc.vector.reciprocal(out=st[:, c:c + 1], in_=st[:, c:c + 1]),
                   reads=[B_st], writes=[B_st])
            dve.op(lambda: nc.vector.scalar_tensor_tensor(out=acc[:, t, :], in0=acc[:, t, :], scalar=st[:, c:c + 1],
                                                          in1=wbc[:], op0=ALU.mult, op1=ALU.mult),
                   reads=[B_st, B_wbc] + B_acc[t], writes=B_acc[t])
            dve.op(lambda: nc.vector.tensor_tensor(out=acc[:, t, :], in0=acc[:, t, :], in1=xst[:], op=ALU.add),
                   reads=[B_xst] + B_acc[t], writes=B_acc[t])
            sp.dma(dst[t * 128:(t + 1) * 128, :], acc[:, t, :], B_accst[t].dsem, reads=B_acc[t])
        K.end_phase()


import math
LN_THETA = math.log(10000.0)
PI = math.pi
CQ, CK, CV, CQI, CKI, CWI, CGQ, CGK, CGV, CGLR, CGR, CGA, CGG = (
    0, 1024, 1280, 1536, 2560, 2624, 2640, 3152, 3664, 4688, 4704, 5728, 7776)
AG1_ROWS = 576
SQ = 1.0 / math.sqrt(128.0)


def all_gather(K, src, dst, rbufs, wbufs):
    pool = K.pool
    pool.pre(reads=rbufs, writes=wbufs)
    ins = K.nc.gpsimd.collective_compute("AllGather", ALU.bypass, replica_groups=[[0, 1, 2, 3], [4, 5, 6, 7]],
                                         ins=[src.opt()], outs=[dst.opt()])
    csem = K.new_sem("cc", True)
    ins.then_inc(csem.h)
    csem.cnt += 1
    pool.nins += 1
    ev = Ev(csem, 1)
    for b in rbufs:
        b.r[csem.uid] = ev
    for b in wbufs:
        b.r[csem.uid] = ev
    return ev


def norm_T(K, src, wvec, zT, B_zT, ident, ptr, B_ptr, tag):
    nc = K.nc
    pe, act, dve, pool, sp = K.pe, K.act, K.dve, K.pool, K.sp
    with ExitStack() as es:
        def sb(n, s, d):
            return es.enter_context(nc.sbuf_tensor(tag + n, s, d))
        xst = sb("xst", [128, D], F32)
        zb = sb("zb", [128, D], BF16)
        wbc = sb("wbc", [128, D], F32)
        st = sb("st", [128, 2 * NT], F32)
        B_xst = Buf(K, dma=True)
        B_zb = Buf()
        B_wbc = Buf(K, dma=True)
        B_st = Buf()
        sp.dma(wbc[:], wvec.rearrange("(o d) -> o d", o=1).to_broadcast([128, D]), B_wbc.dsem, writes=[B_wbc])
        dve.op(lambda: nc.vector.memset(st[:], 0.0), writes=[B_st])
        for t in range(NT):
            sp.dma(xst[:], src[t * 128:(t + 1) * 128, :], B_xst.dsem, writes=[B_xst])
            act.op(lambda: nc.scalar.activation(out=zb[:], in_=xst[:], func=AF.Square,
                                                accum_out=st[:, t:t + 1]),
                   reads=[B_xst], writes=[B_zb, B_st])
            act.op(lambda: nc.scalar.activation(out=st[:, NT + t:NT + t + 1], in_=st[:, t:t + 1], func=AF.Sqrt,
                                                scale=1.0 / D, bias=EPS),
                   reads=[B_st], writes=[B_st])
            dve.op(lambda: nc.vector.reciprocal(out=st[:, NT + t:NT + t + 1], in_=st[:, NT + t:NT + t + 1]),
                   reads=[B_st], writes=[B_st])
            dve.op(lambda: nc.vector.scalar_tensor_tensor(out=zb[:], in0=xst[:], scalar=st[:, NT + t:NT + t + 1],
                                                          in1=wbc[:], op0=ALU.mult, op1=ALU.mult),
                   reads=[B_xst, B_st, B_wbc], writes=[B_zb])
            for q4 in range(4):
                sl = q4 % 2
                pe.pre(reads=[B_zb, K.B_ident], writes=[B_ptr[sl]])
                for i in range(4):
                    k = q4 * 4 + i
                    ins = nc.tensor.transpose(ptr[sl][:, i * 128:(i + 1) * 128], zb[:, k * 128:(k + 1) * 128], ident[:])
                ev = pe.done(ins)
                pe.post(ev, reads=[B_zb], writes=[B_ptr[sl]])
                src_ap = ptr[sl][:, 0:512].rearrange("p (a b) -> p a b", a=4)
                dst_ap = zT[:, q4 * 4:q4 * 4 + 4, t * 128:(t + 1) * 128]
                if q4 % 2 == 0:
                    act.op(lambda: nc.scalar.copy(out=dst_ap, in_=src_ap), reads=[B_ptr[sl]], writes=[B_zT[t]])
                else:
                    dve.op(lambda: nc.vector.tensor_copy(dst_ap, src_ap), reads=[B_ptr[sl]], writes=[B_zT[t]])
        K.end_phase()


def phase_a(K, C):
    nc = K.nc
    pe, act, dve, pool, sp = K.pe, K.act, K.dve, K.pool, K.sp
    P = C["P"]
    ident = C["ident"]
    w_in = C["w_in"]
    win_v = w_in.rearrange("(k p) f -> p k f", p=128)
    ag_kT = C["agk_in"]
    ag_kiT = C["agki_in"]
    ag_v = C["agv_in"]
    with ExitStack() as es:
        def sb(n, s, d):
            return es.enter_context(nc.sbuf_tensor("a_" + n, s, d))

        def ps(n, s, d=F32):
            return es.enter_context(nc.psum_tensor("a_" + n, s, d))

        uT = sb("uT", [128, 16, TOK], BF16)
        B_uT = [Buf() for _ in range(NT)]
        ptr = [ps(f"ptr{i}", [128, 1024], BF16) for i in range(2)]
        B_ptr = [Buf(), Buf()]
        pp = [ps(f"pp{i}", [128, 512]) for i in range(2)]
        B_pp = [Buf(), Buf()]
        px = [ps(f"px{i}", [128, 512]) for i in range(2)]
        B_px = [Buf(), Buf()]
        norm_T(K, C["h1"], C["mix_pre_w"], uT, B_uT, ident, ptr, B_ptr, "a1_")

        tabs = sb("tabs", [128, NT, 384], F32)
        es_t = ExitStack()

        def sbt(n, s, d):
            return es_t.enter_context(nc.sbuf_tensor("a_" + n, s, d))
        posv = sbt("posv", [128, NT], F32)
        io = sbt("io", [128, 64], F32)
        inv = sbt("inv", [128, 96], F32)
        ang = sbt("ang", [128, NT, 96], F32)
        kf = sbt("kf", [128, NT, 96], F32)
        kint = sbt("kint", [128, NT, 96], I32)
        sn = sbt("sn", [128, NT, 96], F32)
        cs = sbt("cs", [128, NT, 96], F32)
        B_t = Buf(K, dma=True)
        sp.dma(posv[:], C["posv"], B_t.dsem, writes=[B_t])
        pool.op(lambda: nc.gpsimd.iota(io[:], pattern=[[1, 64]], base=0, channel_multiplier=0,
                                       allow_small_or_imprecise_dtypes=True), writes=[B_t])
        act.op(lambda: nc.scalar.activation(out=inv[:, 0:64], in_=io[:, 0:64], func=AF.Exp,
                                            scale=-2.0 * LN_THETA / 128.0), reads=[B_t], writes=[B_t])
        act.op(lambda: nc.scalar.activation(out=inv[:, 64:96], in_=io[:, 0:32], func=AF.Exp,
                                            scale=-2.0 * LN_THETA / 64.0), reads=[B_t], writes=[B_t])
        for s in range(NT):
            dve.op(lambda: nc.vector.tensor_scalar(out=ang[:, s, :], in0=inv[:], scalar1=posv[:, s:s + 1],
                                                   scalar2=None, op0=ALU.mult), reads=[B_t], writes=[B_t])

        def V(fn):
            return dve.op(fn, reads=[B_t], writes=[B_t])
        V(lambda: nc.vector.tensor_scalar(out=kf[:], in0=ang[:], scalar1=1.0 / (2 * PI), scalar2=None, op0=ALU.mult))
        V(lambda: nc.vector.tensor_copy(kint[:], kf[:]))
        V(lambda: nc.vector.tensor_copy(kf[:], kint[:]))
        V(lambda: nc.vector.scalar_tensor_tensor(out=ang[:], in0=kf[:], scalar=-2 * PI, in1=ang[:],
                                                 op0=ALU.mult, op1=ALU.add))
        act.op(lambda: nc.scalar.activation(out=sn[:], in_=ang[:], func=AF.Sin), reads=[B_t], writes=[B_t])
        V(lambda: nc.vector.tensor_scalar(out=ang[:], in0=ang[:], scalar1=PI / 2, scalar2=None, op0=ALU.add))
        V(lambda: nc.vector.tensor_scalar(out=kf[:], in0=ang[:], scalar1=PI, scalar2=-2 * PI,
                                          op0=ALU.is_gt, op1=ALU.mult))
        V(lambda: nc.vector.tensor_tensor(out=ang[:], in0=ang[:], in1=kf[:], op=ALU.add))
        act.op(lambda: nc.scalar.activation(out=cs[:], in_=ang[:], func=AF.Sin), reads=[B_t], writes=[B_t])
        V(lambda: nc.vector.tensor_copy(tabs[:, :, 0:64], cs[:, :, 0:64]))
        V(lambda: nc.vector.tensor_copy(tabs[:, :, 64:128], cs[:, :, 0:64]))
        V(lambda: nc.vector.tensor_scalar(out=tabs[:, :, 128:192], in0=sn[:, :, 0:64], scalar1=-1.0, scalar2=None,
                                          op0=ALU.mult))
        V(lambda: nc.vector.tensor_copy(tabs[:, :, 192:256], sn[:, :, 0:64]))
        V(lambda: nc.vector.tensor_copy(tabs[:, :, 256:288], cs[:, :, 64:96]))
        V(lambda: nc.vector.tensor_copy(tabs[:, :, 288:320], cs[:, :, 64:96]))
        V(lambda: nc.vector.tensor_scalar(out=tabs[:, :, 320:352], in0=sn[:, :, 64:96], scalar1=-1.0, scalar2=None,
                                          op0=ALU.mult))
        V(lambda: nc.vector.tensor_copy(tabs[:, :, 352:384], sn[:, :, 64:96]))
        B_tabs = B_t
        K.barrier()
        es_t.close()

        wbuf = sb("wbuf", [128, 2, 16, 512], BF16)
        B_w = [Buf(K, dma=True), Buf(K, dma=True)]
        xs = sb("xs", [128, 2, 512], F32)
        B_xs = [Buf(K, dma=True), Buf(K, dma=True)]
        t1 = sb("t1", [128, 2, 512], F32)
        B_t1 = [Buf(), Buf()]
        t2 = sb("t2", [128, 2, 512], F32)
        B_t2 = [Buf(), Buf()]
        ob = sb("ob", [128, 2, 512], BF16)
        B_ob = [Buf(K, dma=True), Buf(K, dma=True)]
        of = sb("of", [128, 2, 320], F32)
        B_of = [Buf(K, dma=True), Buf(K, dma=True)]
        tst = sb("tst", [128, 2, 256], BF16)
        B_tst = [Buf(K, dma=True), Buf(K, dma=True)]
        glrT = sb("glrT", [32, TOK], BF16)
        B_glr = Buf()
        w2b = sb("w2b", [16, 512], BF16)
        negb = sb("negb", [128, 4], F32)
        B_c = Buf(K, dma=True)
        ones = sb("ones", [128, 128], F32)
        eT = sb("eT", [128, 512], F32)
        lT = sb("lT", [128, 512], F32)
        cT = eT
        B_e = Buf()
        B_l = Buf()
        B_cT = B_e
        E1 = sb("E1", [128, 512], F32)
        E2 = sb("E2", [128, 512], F32)
        B_E = [Buf()] * 3
        khT = sb("khT", [128, 512], BF16)
        B_khT = Buf()
        cnt = {"blk": 0, "pp": 0, "xs": 0, "tr": 0, "of": 0, "tst": 0, "px": 0}

        pool.dma(w2b[:], C["gla_gate_w2"], B_c.dsem, writes=[B_c])
        sp.dma(negb[:], C["gla_gate_b"].rearrange("(h p) -> p h", p=128), B_c.dsem, writes=[B_c],
               allow_slow_non_contiguous=True)
        dve.op(lambda: nc.vector.tensor_scalar(out=negb[:], in0=negb[:], scalar1=-1.0, scalar2=None, op0=ALU.mult),
               reads=[B_c], writes=[B_c])
        dve.op(lambda: nc.vector.memset(ones[:], 1.0), writes=[B_c])

        def load_w(pieces):
            slot = cnt["blk"] % 2
            cnt["blk"] += 1
            off = 0
            for (c0, w) in pieces:
                pool.dma(wbuf[:, slot, :, off:off + w], win_v[:, :, c0:c0 + w], B_w[slot].dsem, writes=[B_w[slot]])
                off += w
            return slot

        def mm_tok(slot, t, width):
            i = cnt["pp"] % 2
            cnt["pp"] += 1
            pe.pre(reads=[B_uT[t], B_w[slot]], writes=[B_pp[i]])
            for k in range(16):
                ins = nc.tensor.matmul(pp[i][:, 0:width], lhsT=uT[:, k, t * 128:(t + 1) * 128],
                                       rhs=wbuf[:, slot, k, 0:width], start=(k == 0), stop=(k == 15))
            ev = pe.done(ins)
            pe.post(ev, reads=[B_uT[t], B_w[slot]], writes=[B_pp[i]])
            return i

        def mm_feat(slot, off, m, tbi):
            t0, tn = TB[tbi]
            i = cnt["pp"] % 2
            cnt["pp"] += 1
            deps = [B_uT[t] for t in ([0, 1, 2, 3], [4, 5, 6, 7], [8])[tbi]]
            pe.pre(reads=deps + [B_w[slot]], writes=[B_pp[i]])
            for k in range(16):
                ins = nc.tensor.matmul(pp[i][0:m, 0:tn], lhsT=wbuf[:, slot, k, off:off + m],
                                       rhs=uT[:, k, t0:t0 + tn], start=(k == 0), stop=(k == 15))
            ev = pe.done(ins)
            pe.post(ev, reads=deps + [B_w[slot]], writes=[B_pp[i]])
            return i

        def evac(i, width):
            j = cnt["xs"] % 2
            cnt["xs"] += 1
            act.op(lambda: nc.scalar.copy(out=xs[:, j, 0:width], in_=pp[i][:, 0:width]),
                   reads=[B_pp[i]], writes=[B_xs[j]])
            return j

        def rope(j, c0, hs, nh, t, out_ap, B_out):
            w = nh * hs
            hh = hs // 2
            tb0 = 0 if hs == 128 else 256
            x3 = xs[:, j, c0:c0 + w].rearrange("p (h d) -> p h d", h=nh)
            cosb = tabs[:, t, tb0:tb0 + hs].unsqueeze(1).to_broadcast([128, nh, hs])
            sa = tabs[:, t, tb0 + hs:tb0 + hs + hh].unsqueeze(1).to_broadcast([128, nh, hh])
            sbb = tabs[:, t, tb0 + hs + hh:tb0 + 2 * hs].unsqueeze(1).to_broadcast([128, nh, hh])
            a3 = t1[:, j, 0:w].rearrange("p (h d) -> p h d", h=nh)
            b3 = t2[:, j, 0:w].rearrange("p (h d) -> p h d", h=nh)
            pool.op(lambda: nc.gpsimd.tensor_tensor(out=a3, in0=x3, in1=cosb, op=ALU.mult),
                    reads=[B_xs[j], B_tabs], writes=[B_t1[j]])
            dve.op(lambda: nc.vector.tensor_tensor(out=b3[:, :, 0:hh], in0=x3[:, :, hh:hs], in1=sa, op=ALU.mult),
                   reads=[B_xs[j], B_tabs], writes=[B_t2[j]])
            dve.op(lambda: nc.vector.tensor_tensor(out=b3[:, :, hh:hs], in0=x3[:, :, 0:hh], in1=sbb, op=ALU.mult),
                   reads=[B_xs[j], B_tabs], writes=[B_t2[j]])
            dve.op(lambda: nc.vector.tensor_tensor(out=out_ap, in0=t1[:, j, 0:w], in1=t2[:, j, 0:w], op=ALU.add),
                   reads=[B_t1[j], B_t2[j]], writes=[B_out])

        def transposes(j, nblk, rows, dst_fn, B_dst_fn):
            sl = cnt["tr"] % 2
            cnt["tr"] += 1
            pe.pre(reads=[B_ob[j], K.B_ident], writes=[B_ptr[sl]])
            for b in range(nblk):
                ins = nc.tensor.transpose(ptr[sl][0:rows, b * 128:(b + 1) * 128],
                                          ob[:, j, b * rows:(b + 1) * rows], ident[:])
            ev = pe.done(ins)
            pe.post(ev, reads=[B_ob[j]], writes=[B_ptr[sl]])
            return sl

        def pipelined(front, back, n=NT):
            st_ = {}
            st_[0] = front(0)
            for t in range(n):
                if t + 1 < n:
                    st_[t + 1] = front(t + 1)
                back(t, st_[t])

        for blk in range(2):
            slot = load_w([(CQ + blk * 512, 512)])

            def q_front(t, slot=slot):
                i = mm_tok(slot, t, 512)
                j = evac(i, 512)
                rope(j, 0, 128, 4, t, ob[:, j, :], B_ob[j])
                return j

            def q_back(t, j, blk=blk):
                sl = transposes(j, 4, 128, None, None)
                act.op(lambda: nc.scalar.copy(out=P["qT"][:, blk * 4:blk * 4 + 4, t * 128:(t + 1) * 128],
                                              in_=ptr[sl][:, 0:512].rearrange("p (a b) -> p a b", a=4)),
                       reads=[B_ptr[sl]], writes=[C["B_qT"]])
            pipelined(q_front, q_back)

        slot = load_w([(CK, 512)])

        def kv_front(t, slot=slot):
            i = mm_tok(slot, t, 512)
            j = evac(i, 512)
            o = cnt["of"] % 2
            cnt["of"] += 1
            rope(j, 0, 128, 2, t, of[:, o, 0:256], B_of[o])
            sp.dma(C["ko"][t * 128:(t + 1) * 128, :], of[:, o, 0:256], B_of[o].dsem, reads=[B_of[o]])
            sp.dma(C["vo"][t * 128:(t + 1) * 128, :], xs[:, j, 256:512], B_xs[j].dsem, reads=[B_xs[j]])
            pool.op(lambda: nc.gpsimd.tensor_copy(ob[:, j, 0:256], of[:, o, 0:256]), reads=[B_of[o]], writes=[B_ob[j]])
            pool.op(lambda: nc.gpsimd.tensor_copy(ob[:, j, 256:512], xs[:, j, 256:512]), reads=[B_xs[j]],
                    writes=[B_ob[j]])
            return j

        def kv_back(t, j):
            sl = transposes(j, 2, 128, None, None)
            if t < 8:
                q = cnt["tst"] % 2
                cnt["tst"] += 1
                act.op(lambda: nc.scalar.copy(out=tst[:, q, :], in_=ptr[sl][:, 0:256]), reads=[B_ptr[sl]],
                       writes=[B_tst[q]])
                for g in range(2):
                    sp.dma(ag_kT[g * 128:(g + 1) * 128, t * 64:(t + 1) * 64],
                           tst[:, q, g * 128:(g + 1) * 128].bitcast(F32),
                           B_tst[q].dsem, reads=[B_tst[q]], writes=[C["B_ag1"]])
                sp.dma(ag_v[t * 128:(t + 1) * 128, :], ob[:, j, 256:512].bitcast(F32), B_ob[j].dsem,
                       reads=[B_ob[j]], writes=[C["B_ag1"]])
            else:
                act.op(lambda: nc.scalar.copy(out=P["kT8"][:, :], in_=ptr[sl][:, 0:256]), reads=[B_ptr[sl]],
                       writes=[C["B_s8"]])
                pool.op(lambda: nc.gpsimd.tensor_copy(P["v8"][:, :], ob[:, j, 256:512]), reads=[B_ob[j]],
                        writes=[C["B_s8"]])
        pipelined(kv_front, kv_back)

        for blk in range(2):
            slot = load_w([(CQI + blk * 512, 512)])

            def qi_front(t, slot=slot):
                i = mm_tok(slot, t, 512)
                j = evac(i, 512)
                rope(j, 0, 64, 8, t, ob[:, j, :], B_ob[j])
                return j

            def qi_back(t, j, blk=blk):
                sl = transposes(j, 4, 128, None, None)
                act.op(lambda: nc.scalar.copy(out=P["qiT"][:, blk * 4:blk * 4 + 4, t * 128:(t + 1) * 128],
                                              in_=ptr[sl][:, 0:512].rearrange("p (a b) -> p a b", a=4)),
                       reads=[B_ptr[sl]], writes=[C["B_qiT"]])
            pipelined(qi_front, qi_back)

        slot = load_w([(CKI, 80)])

        def ki_front(t, slot=slot):
            i = mm_tok(slot, t, 80)
            j = evac(i, 80)
            o = cnt["of"] % 2
            cnt["of"] += 1
            rope(j, 0, 64, 1, t, of[:, o, 256:320], B_of[o])
            sp.dma(C["kio"][t * 128:(t + 1) * 128, :], of[:, o, 256:320], B_of[o].dsem, reads=[B_of[o]])
            dve.op(lambda: nc.vector.tensor_scalar(out=P["wi"][:, t, :], in0=xs[:, j, 64:80], scalar1=1.0 / 32.0,
                                                   scalar2=None, op0=ALU.mult), reads=[B_xs[j]], writes=[C["B_wi"]])
            pool.op(lambda: nc.gpsimd.tensor_copy(ob[:, j, 0:64], of[:, o, 256:320]), reads=[B_of[o]], writes=[B_ob[j]])
            pool.op(lambda: nc.gpsimd.tensor_copy(ob[:, j, 64:128], of[:, o, 256:320]), reads=[B_of[o]],
                    writes=[B_ob[j]])
            return j

        def ki_back(t, j):
            sl = transposes(j, 1, 128, None, None)
            if t < 8:
                q = cnt["tst"] % 2
                cnt["tst"] += 1
                act.op(lambda: nc.scalar.copy(out=tst[0:64, q, 0:128], in_=ptr[sl][0:64, 0:128]), reads=[B_ptr[sl]],
                       writes=[B_tst[q]])
                sp.dma(ag_kiT[:, t * 64:(t + 1) * 64], tst[0:64, q, 0:128].bitcast(F32), B_tst[q].dsem,
                       reads=[B_tst[q]], writes=[C["B_ag1"]])
            else:
                act.op(lambda: nc.scalar.copy(out=P["kiT8"][:, :], in_=ptr[sl][:, 0:128]), reads=[B_ptr[sl]],
                       writes=[C["B_s8"]])
        pipelined(ki_front, ki_back)

        for nm in ("agk", "agki", "agv"):
            all_gather(K, C[nm + "_in"], C[nm + "_out"], [C["B_ag1"]], [C["B_ag1o"]])

        for blk in range(2):
            slot = load_w([(CGV + blk * 512, 512)])
            for t in range(NT):
                i = mm_tok(slot, t, 512)
                act.op(lambda: nc.scalar.copy(out=P["gv"][:, t, blk * 512:(blk + 1) * 512], in_=pp[i][:, :]),
                       reads=[B_pp[i]], writes=[C["B_gv"]])

        slot = load_w([(CGLR, 16)])
        for tbi in range(3):
            t0, tn = TB[tbi]
            i = mm_feat(slot, 0, 16, tbi)
            act.op(lambda: nc.scalar.copy(out=glrT[0:16, t0:t0 + tn], in_=pp[i][0:16, 0:tn]), reads=[B_pp[i]],
                   writes=[B_glr])

        for h in range(4):
            slot = load_w([(CGQ + h * 128, 128), (CGK + h * 128, 128)])
            for tbi in range(3):
                t0, tn = TB[tbi]
                x = cnt["px"] % 2
                cnt["px"] += 1
                pe.op(lambda: nc.tensor.matmul(px[x][:, 0:tn], lhsT=w2b[:, h * 128:(h + 1) * 128],
                                               rhs=glrT[0:16, t0:t0 + tn], start=True, stop=True),
                      reads=[B_glr, B_c], writes=[B_px[x]])
                act.op(lambda: nc.scalar.activation(out=eT[:, 0:tn], in_=px[x][:, 0:tn], func=AF.Exp, scale=-1.0,
                                                    bias=negb[:, h:h + 1]), reads=[B_px[x], B_c], writes=[B_e])
                act.op(lambda: nc.scalar.activation(out=lT[:, 0:tn], in_=eT[:, 0:tn], func=AF.Ln, bias=1.0),
                       reads=[B_e], writes=[B_l])
                if tbi < 2:
                    for q4 in range(4):
                        dve.op(lambda: nc.vector.tensor_tensor_scan(out=cT[:, q4 * 128:(q4 + 1) * 128], data0=ones[:],
                                                                    data1=lT[:, q4 * 128:(q4 + 1) * 128], initial=0.0,
                                                                    op0=ALU.mult, op1=ALU.add),
                               reads=[B_l, B_c], writes=[B_cT])
                    act.op(lambda: nc.scalar.activation(out=E1[:, 0:tn], in_=cT[:, 0:tn], func=AF.Exp,
                                                        scale=-1.0 / 16.0), reads=[B_cT], writes=[B_E[tbi]])
                    act.op(lambda: nc.scalar.activation(out=E2[:, 0:tn], in_=cT[:, 0:tn], func=AF.Exp,
                                                        scale=1.0 / 16.0), reads=[B_cT], writes=[B_E[tbi]])
                    dve.op(lambda: nc.vector.tensor_copy(
                        P["dec"][:, h, tbi * 4:tbi * 4 + 4],
                        E1[:, 0:tn].rearrange("p (a b) -> p a b", a=4)[:, :, 127]),
                        reads=[B_E[tbi]], writes=[C["B_dec"]])
                else:
                    act.op(lambda: nc.scalar.activation(out=E1[:, 0:tn], in_=lT[:, 0:tn], func=AF.Exp,
                                                        scale=-1.0 / 16.0), reads=[B_l], writes=[B_E[tbi]])
                    dve.op(lambda: nc.vector.tensor_copy(P["dec8"][:, h, :], E1[:, 0:tn]),
                           reads=[B_E[tbi]], writes=[C["B_dec"]])
                i = mm_feat(slot, 0, 128, tbi)
                if tbi < 2:
                    dve.op(lambda: nc.vector.scalar_tensor_tensor(out=P["qgT"][:, h, t0:t0 + tn], in0=pp[i][:, 0:tn],
                                                                  scalar=SQ, in1=E1[:, 0:tn],
                                                                  op0=ALU.mult, op1=ALU.mult),
                           reads=[B_pp[i], B_E[tbi]], writes=[C["B_qgT"]])
                else:
                    dve.op(lambda: nc.vector.tensor_scalar(out=P["qgT"][:, h, t0:t0 + tn], in0=pp[i][:, 0:tn],
                                                           scalar1=SQ, scalar2=None, op0=ALU.mult),
                           reads=[B_pp[i]], writes=[C["B_qgT"]])
                i = mm_feat(slot, 128, 128, tbi)
                if tbi < 2:
                    dve.op(lambda: nc.vector.tensor_tensor(out=P["kgT"][:, h, t0:t0 + tn], in0=pp[i][:, 0:tn],
                                                           in1=E2[:, 0:tn], op=ALU.mult),
                           reads=[B_pp[i], B_E[tbi]], writes=[C["B_kgT"]])
                    dve.op(lambda: nc.vector.tensor_tensor(
                        out=khT[:, :].rearrange("p (a b) -> p a b", a=4),
                        in0=P["kgT"][:, h, t0:t0 + tn].rearrange("p (a b) -> p a b", a=4),
                        in1=P["dec"][:, h, tbi * 4:tbi * 4 + 4].unsqueeze(2).to_broadcast([128, 4, 128]),
                        op=ALU.mult), reads=[C["B_kgT"], C["B_dec"]], writes=[B_khT])
                    sl = cnt["tr"] % 2
                    cnt["tr"] += 1
                    pe.pre(reads=[B_khT, K.B_ident], writes=[B_ptr[sl]])
                    for b in range(4):
                        ins = nc.tensor.transpose(ptr[sl][:, b * 128:(b + 1) * 128], khT[:, b * 128:(b + 1) * 128],
                                                  ident[:])
                    ev = pe.done(ins)
                    pe.post(ev, reads=[B_khT], writes=[B_ptr[sl]])
                    act.op(lambda: nc.scalar.copy(out=P["khat"][:, tbi * 4:tbi * 4 + 4, h * 128:(h + 1) * 128],
                                                  in_=ptr[sl][:, 0:512].rearrange("p (a b) -> p a b", a=4)),
                           reads=[B_ptr[sl]], writes=[C["B_khat"]])
                else:
                    act.op(lambda: nc.scalar.copy(out=P["kgT"][:, h, t0:t0 + tn], in_=pp[i][:, 0:tn]),
                           reads=[B_pp[i]], writes=[C["B_kgT"]])
                    dve.op(lambda: nc.vector.tensor_copy(P["kg8f"][:, h, :], pp[i][:, 0:tn]),
                           reads=[B_pp[i]], writes=[C["B_kgT"]])
        K.end_phase()
NIT = 14
TOPK = 256
NEG = -1.0e4


def topk_threshold(K, S3, np_, junk3, st, B_S, B_junk, B_st, pw2, B_c):
    nc = K.nc
    dve = K.dve

    def V(fn, r=(), w=()):
        return dve.op(fn, reads=list(r), writes=list(w))
    V(lambda: nc.vector.tensor_scalar(out=st[0:np_, 8:8 + NIT], in0=pw2[0:np_, 0:NIT], scalar1=st[0:np_, 0:1],
                                      scalar2=None, op0=ALU.mult), r=[B_st, B_c], w=[B_st])
    V(lambda: nc.vector.tensor_scalar(out=st[0:np_, 1:2], in0=st[0:np_, 0:1], scalar1=-1.0, scalar2=None,
                                      op0=ALU.mult), r=[B_st], w=[B_st])
    V(lambda: nc.vector.memset(st[0:np_, 40:40 + NIT], 0.0), r=[B_st], w=[B_st])
    for k in range(NIT):
        V(lambda: nc.vector.tensor_tensor(out=st[0:np_, 2:3], in0=st[0:np_, 1:2], in1=st[0:np_, 8 + k:9 + k],
                                          op=ALU.add), r=[B_st], w=[B_st])
        V(lambda: nc.vector.tensor_scalar(out=junk3, in0=S3, scalar1=st[0:np_, 2:3], scalar2=0.0, op0=ALU.is_ge,
                                          op1=ALU.add, accum_out=st[0:np_, 40 + k:41 + k]),
          r=[B_S, B_st], w=[B_junk, B_st])
        V(lambda: nc.vector.tensor_scalar(out=st[0:np_, 4:5], in0=st[0:np_, 40 + k:41 + k], scalar1=TOPK - 0.5,
                                          scalar2=None, op0=ALU.is_ge), r=[B_st], w=[B_st])
        V(lambda: nc.vector.scalar_tensor_tensor(out=st[0:np_, 1:2], in0=st[0:np_, 4:5], scalar=st[0:np_, 8 + k:9 + k],
                                                 in1=st[0:np_, 1:2], op0=ALU.mult, op1=ALU.add), r=[B_st], w=[B_st])


def phase_b(K, C):
    nc = K.nc
    pe, act, dve, pool, sp = K.pe, K.act, K.dve, K.pool, K.sp
    P = C["P"]
    ident = C["ident"]
    with ExitStack() as es:
        def sb(n, s, d):
            return es.enter_context(nc.sbuf_tensor("b_" + n, s, d))

        def ps(n, s, d=F32):
            return es.enter_context(nc.psum_tensor("b_" + n, s, d))

        kT_all = sb("kT", [128, 2, 4, 1024], BF16)
        kiT2 = sb("kiT2", [128, 4, 1024], BF16)
        V1 = sb("V1", [128, 4, 8, 2, 130], BF16)
        S2 = sb("S", [128, 2, 4096], F32)
        selm = sb("selm", [128, 4096], BF16)
        selT = sb("selT", [128, 4, 8, 128], BF16)
        diagw = sb("diagw", [128, 16, 128], BF16)
        rh = sb("rh", [128, 3, 512], BF16)
        pex = sb("pex", [128, 3, 512], BF16)
        pm = sb("pm", [128, 3, 512], BF16)
        cmask = sb("cmask", [128, 512], F32)
        pw2 = sb("pw2", [128, 32], F32)
        st = sb("st", [128, 64], F32)
        rec = sb("rec", [128, 8], F32)
        psh = [ps(f"psh{i}", [128, 512]) for i in range(3)]
        psc = [ps(f"psc{i}", [128, 512]) for i in range(2)]
        ptr = [ps(f"ptr{i}", [128, 1024], BF16) for i in range(2)]
        B_kv = Buf(K, dma=True)
        B_S2, B_selm, B_selT, B_diagw, B_st, B_c, B_rec = [Buf(), Buf()], Buf(), Buf(), Buf(), Buf(), Buf(K, dma=True), Buf()
        B_rh = [Buf(), Buf(), Buf()]
        B_pex = [Buf(), Buf(), Buf()]
        B_pm = [Buf(), Buf(), Buf()]
        B_psh = [Buf(), Buf(), Buf()]
        B_psc = [Buf(), Buf()]
        B_ptr = [Buf(), Buf()]
        cnt = {"h": 0, "c": 0, "r": 0, "e": 0, "t": 0}

        agk, agki, agv = C["agk_out"], C["agki_out"], C["agv_out"]
        for r in range(4):
            for g in range(2):
                sp.dma(kT_all[:, g, r, :].bitcast(F32), agk[r * 256 + g * 128:r * 256 + (g + 1) * 128, :], B_kv.dsem,
                       reads=[C["B_ag1o"]], writes=[B_kv])
            for hf in range(2):
                sp.dma(kiT2[hf * 64:(hf + 1) * 64, r, :].bitcast(F32), agki[r * 64:(r + 1) * 64, :], B_kv.dsem,
                       reads=[C["B_ag1o"]], writes=[B_kv])
            for g in range(2):
                sp.dma(V1[:, r, :, g, 0:128],
                       agv.bitcast(BF16)[r * 1024:(r + 1) * 1024, g * 128:(g + 1) * 128].rearrange(
                           "(s p) c -> p s c", p=128),
                       B_kv.dsem, reads=[C["B_ag1o"]], writes=[B_kv])
        pool.op(lambda: nc.gpsimd.memset(V1[:, :, :, :, 128:130], 1.0), writes=[B_kv])
        sp.dma(cmask[:], C["cmask"], B_c.dsem, writes=[B_c])
        for k in range(NIT):
            dve.op(lambda: nc.vector.memset(pw2[:, k:k + 1], 2.0 ** (-k)), writes=[B_c])

        def scores(s):
            S = S2[:, s % 2, :]
            B_S = B_S2[s % 2]
            Lr = (s + 1) * 128
            qs = slice(s * 128, (s + 1) * 128)
            for h in range(16):
                dve.op(lambda: nc.vector.tensor_scalar(out=diagw[:, h, :], in0=ident[:], scalar1=P["wi"][:, s, h:h + 1],
                                                       scalar2=None, op0=ALU.mult),
                       reads=[C["B_wi"], K.B_ident], writes=[B_diagw])
            LA = 2
            units = [(r, c0, min(512, Lr - c0), h) for r in range(4) for c0 in range(0, Lr, 512) for h in range(16)]
            hi_of = {}
            ci_of = {}

            def sc_front(u):
                r, c0, cw, h = units[u]
                hi = cnt["h"] % 3
                cnt["h"] += 1
                hi_of[u] = hi
                p0 = (h % 2) * 64
                pe.op(lambda: nc.tensor.matmul(psh[hi][:, 0:cw], lhsT=P["qiT"][p0:p0 + 64, h // 2, qs],
                                               rhs=kiT2[p0:p0 + 64, r, c0:c0 + cw], start=True, stop=True),
                      reads=[C["B_qiT"], B_kv], writes=[B_psh[hi]])
                act.op(lambda: nc.scalar.activation(out=rh[:, hi, 0:cw], in_=psh[hi][:, 0:cw], func=AF.Relu),
                       reads=[B_psh[hi]], writes=[B_rh[hi]])

            def sc_back(u):
                r, c0, cw, h = units[u]
                hi = hi_of[u]
                if h == 0:
                    ci_of[(r, c0)] = cnt["c"] % 2
                    cnt["c"] += 1
                ci = ci_of[(r, c0)]
                pe.op(lambda: nc.tensor.matmul(psc[ci][:, 0:cw], lhsT=diagw[:, h, :], rhs=rh[:, hi, 0:cw],
                                               start=(h == 0), stop=(h == 15)),
                      reads=[B_diagw, B_rh[hi]], writes=[B_psc[ci]])
                if h == 15:
                    act.op(lambda: nc.scalar.copy(out=S[:, r * Lr + c0:r * Lr + c0 + cw], in_=psc[ci][:, 0:cw]),
                           reads=[B_psc[ci]], writes=[B_S])
            for u in range(min(LA, len(units))):
                sc_front(u)
            for u in range(len(units)):
                if u + LA < len(units):
                    sc_front(u + LA)
                sc_back(u)

        def rest(s):
            S = S2[:, s % 2, :]
            B_S = B_S2[s % 2]
            Lr = (s + 1) * 128
            qs = slice(s * 128, (s + 1) * 128)
            LA = 2
            S3 = S[:, 0:4 * Lr]
            dve.op(lambda: nc.vector.tensor_reduce(out=st[:, 32:33], in_=S3, axis=AX.X, op=ALU.max),
                   reads=[B_S], writes=[B_st])
            dve.op(lambda: nc.vector.tensor_reduce(out=st[:, 33:34], in_=S3, axis=AX.X, op=ALU.min),
                   reads=[B_S], writes=[B_st])
            dve.op(lambda: nc.vector.tensor_scalar(out=st[:, 33:34], in0=st[:, 33:34], scalar1=-1.0, scalar2=None,
                                                   op0=ALU.mult), reads=[B_st], writes=[B_st])
            dve.op(lambda: nc.vector.tensor_tensor(out=st[:, 0:1], in0=st[:, 32:33], in1=st[:, 33:34], op=ALU.max),
                   reads=[B_st], writes=[B_st])
            Sl = S3.rearrange("p (r l) -> p r l", r=4)[:, :, s * 128:(s + 1) * 128]
            dve.op(lambda: nc.vector.tensor_tensor(out=Sl, in0=Sl,
                                                   in1=cmask[:, :].rearrange("p (a b) -> p a b", a=4), op=ALU.add),
                   reads=[B_S, B_c], writes=[B_S])
            topk_threshold(K, S3, 128, selm[:, 0:4 * Lr], st, B_S, B_selm, B_st, pw2, B_c)
            dve.op(lambda: nc.vector.tensor_scalar(out=selm[:, 0:4 * Lr], in0=S3, scalar1=st[:, 1:2], scalar2=None,
                                                   op0=ALU.is_ge), reads=[B_S, B_st], writes=[B_selm])
            if s == 7 and "dbg2" in C:
                B_S.dsem = K.new_sem("d")
                B_st.dsem = B_S.dsem
                sp.dma(C["dbg2"][:, 0:4096], S[:], B_S.dsem, reads=[B_S])
                sp.dma(C["dbg2"][:, 4096:4136], st[:, 0:40], B_S.dsem, reads=[B_st])
            for r in range(4):
                for s0 in range(0, s + 1, 4):
                    nb = min(4, s + 1 - s0)
                    ti = cnt["t"] % 2
                    cnt["t"] += 1
                    pe.pre(reads=[B_selm, K.B_ident], writes=[B_ptr[ti]])
                    for b in range(nb):
                        ins = nc.tensor.transpose(ptr[ti][:, b * 128:(b + 1) * 128],
                                                  selm[:, r * Lr + (s0 + b) * 128:r * Lr + (s0 + b + 1) * 128], ident[:])
                    ev = pe.done(ins)
                    pe.post(ev, reads=[B_selm], writes=[B_ptr[ti]])
                    act.op(lambda: nc.scalar.copy(out=selT[:, r, s0:s0 + nb, :],
                                                  in_=ptr[ti][:, 0:nb * 128].rearrange("p (a b) -> p a b", a=nb)),
                           reads=[B_ptr[ti]], writes=[B_selT])
            tiles = [(r, s1) for r in range(4) for s1 in range(s + 1)]
            nt_ = len(tiles)
            aunits = [(g, n, r, s1) for g in range(2) for n, (r, s1) in enumerate(tiles)]
            bi_of = {}

            def at_front(u):
                g, n, r, s1 = aunits[u]
                hi = cnt["h"] % 3
                cnt["h"] += 1
                ei = cnt["e"] % 3
                cnt["e"] += 1
                bi_of[u] = ei
                pe.op(lambda: nc.tensor.matmul(psh[hi][:, :], lhsT=kT_all[:, g, r, s1 * 128:(s1 + 1) * 128],
                                               rhs=P["qT"][:, g * 4:(g + 1) * 4, qs], start=True, stop=True),
                      reads=[B_kv, C["B_qT"]], writes=[B_psh[hi]])
                act.op(lambda: nc.scalar.activation(out=pex[:, ei, :], in_=psh[hi][:, :], func=AF.Exp, scale=SQ),
                       reads=[B_psh[hi]], writes=[B_pex[ei]])
                eng = dve if u % 2 == 0 else pool
                veng = nc.vector if u % 2 == 0 else nc.gpsimd
                eng.op(lambda: veng.tensor_tensor(
                    out=pm[:, ei, :].rearrange("p (a b) -> p a b", a=4),
                    in0=pex[:, ei, :].rearrange("p (a b) -> p a b", a=4),
                    in1=selT[:, r, s1, :].unsqueeze(1).to_broadcast([128, 4, 128]), op=ALU.mult),
                    reads=[B_pex[ei], B_selT], writes=[B_pm[ei]])

            def at_back(u):
                g, n, r, s1 = aunits[u]
                ei = bi_of[u]
                pe.pre(reads=[B_pm[ei], B_kv], writes=[B_psc[0], B_psc[1]])
                for hh in range(4):
                    po = psc[hh // 3][:, (hh % 3) * 129:(hh % 3) * 129 + 129]
                    ins = nc.tensor.matmul(po, lhsT=pm[:, ei, hh * 128:(hh + 1) * 128], rhs=V1[:, r, s1, g, 0:129],
                                           start=(n == 0 and hh % 3 == 0), stop=(n == nt_ - 1),
                                           skip_group_check=True)
                ev = pe.done(ins)
                pe.post(ev, reads=[B_pm[ei], B_kv], writes=[B_psc[0], B_psc[1]])
                if n == nt_ - 1:
                    for hh in range(4):
                        po = psc[hh // 3][:, (hh % 3) * 129:(hh % 3) * 129 + 129]
                        hcol = g * 4 + hh
                        dve.op(lambda: nc.vector.reciprocal(out=rec[:, hcol:hcol + 1], in_=po[:, 128:129]),
                               reads=[B_psc[hh // 3]], writes=[B_rec])
                        dve.op(lambda: nc.vector.tensor_scalar(out=P["oat"][:, s, hcol * 128:(hcol + 1) * 128],
                                                               in0=po[:, 0:128], scalar1=rec[:, hcol:hcol + 1],
                                                               scalar2=None, op0=ALU.mult),
                               reads=[B_psc[hh // 3], B_rec], writes=[C["B_oat"]])
            for u in range(min(LA, len(aunits))):
                at_front(u)
            for u in range(len(aunits)):
                if u + LA < len(aunits):
                    at_front(u + LA)
                at_back(u)

        scores(0)
        for s in range(8):
            if s + 1 < 8:
                scores(s + 1)
            rest(s)
        K.end_phase()
def phase_bs(K, C):
    nc = K.nc
    pe, act, dve, pool, sp = K.pe, K.act, K.dve, K.pool, K.sp
    P = C["P"]
    ident = C["ident"]
    NS = 16
    LK = 2049
    with ExitStack() as es:
        def sb(n, s, d):
            return es.enter_context(nc.sbuf_tensor("s_" + n, s, d))
        ptb = sb("ptb", [128, 256], I32)
        iop = sb("iop", [128, 1], I32)
        idx = sb("idx", [128, 256], I32)
        B_idx = Buf(K, dma=True)
        sp.dma(ptb[:], C["pt"].rearrange("i p -> (i p)").rearrange("(o n) -> o n", o=1).to_broadcast([128, 256]),
               B_idx.dsem, writes=[B_idx])
        pool.op(lambda: nc.gpsimd.iota(iop[:], pattern=[[0, 1]], base=0, channel_multiplier=1), writes=[B_idx])
        pool.op(lambda: nc.gpsimd.tensor_scalar(out=idx[:], in0=ptb[:], scalar1=128, scalar2=None, op0=ALU.mult),
                reads=[B_idx], writes=[B_idx])
        pool.op(lambda: nc.gpsimd.tensor_tensor(out=idx[:], in0=idx[:], in1=iop[:].to_broadcast([128, 256]), op=ALU.add),
                reads=[B_idx], writes=[B_idx])

        wperm = sb("wperm", [128, 64], BF16)
        wTp = sb("wTp", [32, 2, 128], BF16)
        B_w = Buf()
        Ssmp = sb("Ssmp", [NS, 2176], F32)
        B_Ss = Buf(K, dma=True)
        selms = sb("selms", [NS, 2176], BF16)
        B_selms = Buf()
        selTs = sb("selTs", [128, 16, 16], BF16)
        selfs = sb("selfs", [1, 16], F32)
        B_selT = Buf()
        st = sb("st", [NS, 64], F32)
        B_st = Buf()
        pw2 = sb("pw2", [NS, 32], F32)
        B_c = Buf()
        for k in range(NIT):
            dve.op(lambda: nc.vector.memset(pw2[:, k:k + 1], 2.0 ** (-k)), writes=[B_c])

        with ExitStack() as es1:
            def sb1(n, s, d):
                return es1.enter_context(nc.sbuf_tensor("s1_" + n, s, d))

            def ps1(n, s, d=F32):
                return es1.enter_context(nc.psum_tensor("s1_" + n, s, d))
            kig = sb1("kig", [128, 2, 16, 128], BF16)
            B_kig = [Buf(K, dma=True), Buf(K, dma=True)]
            kiTs = sb1("kiTs", [128, 2, 2048], BF16)
            B_kiTs = [Buf(), Buf()]
            rh = sb1("rh", [32, 2, 2, 512], BF16)
            B_rh = [Buf(), Buf()]
            srow = sb1("srow", [1, 1, 2176], F32)
            B_srow = [Buf(K, dma=True)] * 2
            ptr = [ps1(f"ptr{i}", [128, 1024], BF16) for i in range(2)]
            B_ptr = [Buf(), Buf()]
            psh = [ps1(f"psh{i}", [128, 512]) for i in range(2)]
            pso = [ps1(f"pso{i}", [128, 512]) for i in range(2)]
            B_psh = [Buf(), Buf()]
            pss = [ps1(f"pss{i}", [128, 512]) for i in range(2)]
            B_pss = [Buf(), Buf()]
            dve.op(lambda: nc.vector.memset(wperm[:], 0.0), writes=[B_w])
            wi8 = P["wi"][:, 8, :].rearrange("p (a b) -> p a b", b=2)
            dve.op(lambda: nc.vector.tensor_copy(wperm[:, 0:8], wi8[:, :, 0]), reads=[C["B_wi"]], writes=[B_w])
            dve.op(lambda: nc.vector.tensor_copy(wperm[:, 32:40], wi8[:, :, 1]), reads=[C["B_wi"]], writes=[B_w])
            for e in range(2):
                pe.op(lambda: nc.tensor.transpose(ptr[e][0:32, 0:128], wperm[:, e * 32:(e + 1) * 32], ident[:]),
                      reads=[B_w, K.B_ident], writes=[B_ptr[e]])
                act.op(lambda: nc.scalar.copy(out=wTp[:, e, :], in_=ptr[e][0:32, 0:128]), reads=[B_ptr[e]], writes=[B_w])
            nt = 1
            nh = 0
            for i in range(NS):
                o = i % 2
                tok = 1024 + i
                pool.pre(reads=[B_idx], writes=[B_kig[o]])
                for pg in range(16):
                    ins = nc.gpsimd.indirect_dma_start(
                        out=kig[:, o, pg, 0:64], out_offset=None, in_=C["cache_kidx"],
                        in_offset=bass.IndirectOffsetOnAxis(ap=idx[:, i * 16 + pg:i * 16 + pg + 1], axis=0))
                    B_kig[o].dsem.cnt += 16
                    ins.then_inc(B_kig[o].dsem.h, 16)
                    pool.nins += 1
                K.dma_sems[B_kig[o].dsem.uid] = B_kig[o].dsem
                ev = Ev(B_kig[o].dsem, B_kig[o].dsem.cnt)
                pool.post(ev, writes=[B_kig[o]])
                dve.op(lambda: nc.vector.tensor_copy(kig[:, o, :, 64:128], kig[:, o, :, 0:64]), reads=[B_kig[o]],
                       writes=[B_kig[o]])
                for q4 in range(4):
                    x = nt % 2
                    nt += 1
                    pe.pre(reads=[B_kig[o], K.B_ident], writes=[B_ptr[x]])
                    for b in range(4):
                        ins = nc.tensor.transpose(ptr[x][:, b * 128:(b + 1) * 128], kig[:, o, q4 * 4 + b, :], ident[:])
                    ev = pe.done(ins)
                    pe.post(ev, reads=[B_kig[o]], writes=[B_ptr[x]])
                    act.op(lambda: nc.scalar.copy(out=kiTs[:, o, q4 * 512:(q4 + 1) * 512], in_=ptr[x][:, 0:512]),
                           reads=[B_ptr[x]], writes=[B_kiTs[o]])
                for c in range(5):
                    c0 = c * 512
                    cw = 512 if c < 4 else 1
                    x = nh % 2
                    nh += 1
                    if c < 4:
                        rhs_e, rhs_o = kiTs[0:64, o, c0:c0 + cw], kiTs[64:128, o, c0:c0 + cw]
                        deps = [B_kiTs[o]]
                    else:
                        rhs_e, rhs_o = P["kiT8"][0:64, i:i + 1], P["kiT8"][64:128, i:i + 1]
                        deps = [C["B_s8"]]
                    pe.pre(reads=deps + [C["B_qiT"]], writes=[B_psh[x]])
                    nc.tensor.matmul(psh[x][0:8, 0:cw], lhsT=P["qiT"][0:64, :, tok], rhs=rhs_e, start=True, stop=True)
                    ins = nc.tensor.matmul(pso[x][0:8, 0:cw], lhsT=P["qiT"][64:128, :, tok], rhs=rhs_o, start=True,
                                           stop=True)
                    ev = pe.done(ins)
                    pe.post(ev, reads=deps + [C["B_qiT"]], writes=[B_psh[x]])
                    act.op(lambda: nc.scalar.activation(out=rh[0:8, x, 0, 0:cw], in_=psh[x][0:8, 0:cw], func=AF.Relu),
                           reads=[B_psh[x]], writes=[B_rh[x]])
                    act.op(lambda: nc.scalar.activation(out=rh[0:8, x, 1, 0:cw], in_=pso[x][0:8, 0:cw], func=AF.Relu),
                           reads=[B_psh[x]], writes=[B_rh[x]])
                    pe.pre(reads=[B_rh[x], B_w], writes=[B_pss[x]])
                    nc.tensor.matmul(pss[x][0:1, 0:cw], lhsT=wTp[0:8, 0, i:i + 1], rhs=rh[0:8, x, 0, 0:cw], start=True,
                                     stop=False)
                    ins = nc.tensor.matmul(pss[x][0:1, 0:cw], lhsT=wTp[0:8, 1, i:i + 1], rhs=rh[0:8, x, 1, 0:cw],
                                           start=False, stop=True)
                    ev = pe.done(ins)
                    pe.post(ev, reads=[B_rh[x], B_w], writes=[B_pss[x]])
                    dve.op(lambda: nc.vector.tensor_copy(srow[0:1, 0, c0:c0 + cw], pss[x][0:1, 0:cw]), reads=[B_pss[x]],
                           writes=[B_srow[o]])
                sp.dma(C["sscr"][i:i + 1, 0:LK], srow[0:1, 0, 0:LK], B_srow[o].dsem, reads=[B_srow[o]],
                       writes=[C["B_sscr"]])
            K.barrier()
        sp.dma(Ssmp[:, 0:LK], C["sscr"][:, 0:LK], B_Ss.dsem, reads=[C["B_sscr"]], writes=[B_Ss])
        S3 = Ssmp[:, 0:LK]
        dve.op(lambda: nc.vector.tensor_reduce(out=st[:, 32:33], in_=S3, axis=AX.X, op=ALU.max), reads=[B_Ss],
               writes=[B_st])
        dve.op(lambda: nc.vector.tensor_reduce(out=st[:, 33:34], in_=S3, axis=AX.X, op=ALU.min), reads=[B_Ss],
               writes=[B_st])
        dve.op(lambda: nc.vector.tensor_scalar(out=st[:, 33:34], in0=st[:, 33:34], scalar1=-1.0, scalar2=None,
                                               op0=ALU.mult), reads=[B_st], writes=[B_st])
        dve.op(lambda: nc.vector.tensor_tensor(out=st[:, 0:1], in0=st[:, 32:33], in1=st[:, 33:34], op=ALU.max),
               reads=[B_st], writes=[B_st])
        topk_threshold(K, S3, NS, selms[:, 0:LK], st, B_Ss, B_selms, B_st, pw2, B_c)
        dve.op(lambda: nc.vector.tensor_scalar(out=selms[:, 0:LK], in0=S3, scalar1=st[:, 1:2], scalar2=None,
                                               op0=ALU.is_ge), reads=[B_Ss, B_st], writes=[B_selms])

        with ExitStack() as es2:
            def sb2(n, s, d):
                return es2.enter_context(nc.sbuf_tensor("s2_" + n, s, d))

            def ps2(n, s, d=F32):
                return es2.enter_context(nc.psum_tensor("s2_" + n, s, d))
            Kg = sb2("Kg", [128, 2, 16, 256], BF16)
            B_Kg = [Buf(K, dma=True), Buf(K, dma=True)]
            Vc = sb2("Vc", [128, 1, 16, 256], BF16)
            B_Vc = [Buf(K, dma=True)] * 2
            Vg = sb2("Vg", [128, 2, 16, 2, 130], BF16)
            B_Vg = [Buf(), Buf()]
            kTs = sb2("kTs", [128, 2, 2, 2048], BF16)
            B_kTs = [Buf(), Buf()]
            vself = sb2("vself", [1, 16, 2, 130], BF16)
            B_vs = Buf(K, dma=True)
            pTs = sb2("pTs", [128, 2, 128], BF16)
            B_pTs = [Buf(), Buf()]
            pms = sb2("pms", [128, 2, 128], BF16)
            B_pms = [Buf(), Buf()]
            pself = sb2("pself", [1, 2, 8], BF16)
            pselfm = sb2("pselfm", [1, 2, 8], BF16)
            B_pself = [Buf(), Buf()]
            osm = sb2("osm", [4, 2, 2, 128], F32)
            B_osm = [Buf(K, dma=True), Buf(K, dma=True)]
            rec = sb2("rec", [4, 4], F32)
            B_rec = Buf()
            ptr = [ps2(f"ptr{i}", [128, 1024], BF16) for i in range(2)]
            B_ptr = [Buf(), Buf()]
            pl = [ps2(f"pl{i}", [128, 512]) for i in range(2)]
            B_pl = [Buf(), Buf()]
            psf = ps2("psf", [128, 512])
            B_psf = Buf()
            pos = [ps2(f"pos{i}", [128, 512]) for i in range(2)]
            B_pos = [Buf(), Buf()]
            pe.pre(reads=[B_selms, K.B_ident], writes=[B_ptr[0]])
            for pg in range(16):
                ins = nc.tensor.transpose(ptr[0][:, pg * 16:(pg + 1) * 16], selms[0:NS, pg * 128:(pg + 1) * 128],
                                          ident[0:NS, 0:NS])
            ev = pe.done(ins)
            pe.post(ev, reads=[B_selms], writes=[B_ptr[0]])
            act.op(lambda: nc.scalar.copy(out=selTs[:].rearrange("p a b -> p (a b)"), in_=ptr[0][:, 0:256]),
                   reads=[B_ptr[0]], writes=[B_selT])
            pe.op(lambda: nc.tensor.transpose(ptr[1][0:1, 0:NS], selms[0:NS, 2048:2049], ident[0:NS, 0:NS]),
                  reads=[B_selms, K.B_ident], writes=[B_ptr[1]])
            act.op(lambda: nc.scalar.copy(out=selfs[0:1, :], in_=ptr[1][0:1, 0:NS]), reads=[B_ptr[1]], writes=[B_selT])
            pool.op(lambda: nc.gpsimd.memset(vself[:], 1.0), writes=[B_vs])
            pool.dma(vself[0:1, :, :, 0:128], C["vo"][1024:1040, :].rearrange("(o i) (g d) -> o i g d", o=1, g=2),
                     B_vs.dsem, writes=[B_vs])
            for o in range(2):
                pool.op(lambda: nc.gpsimd.memset(Vg[:, o, :, :, 128:130], 1.0), writes=[B_Vg[o]])
            nt = 0
            for i in range(NS):
                o = i % 2
                tok = 1024 + i
                for (dst, Bd, srcc, oo) in ((Kg, B_Kg, C["cache_k"], o), (Vc, B_Vc, C["cache_v"], 0)):
                    pool.pre(reads=[B_idx], writes=[Bd[o]])
                    for pg in range(16):
                        ins = nc.gpsimd.indirect_dma_start(
                            out=dst[:, oo, pg, :], out_offset=None, in_=srcc,
                            in_offset=bass.IndirectOffsetOnAxis(ap=idx[:, i * 16 + pg:i * 16 + pg + 1], axis=0))
                        Bd[o].dsem.cnt += 16
                        ins.then_inc(Bd[o].dsem.h, 16)
                        pool.nins += 1
                    K.dma_sems[Bd[o].dsem.uid] = Bd[o].dsem
                    ev = Ev(Bd[o].dsem, Bd[o].dsem.cnt)
                    pool.post(ev, writes=[Bd[o]])
                act.op(lambda: nc.scalar.copy(out=Vg[:, o, :, :, 0:128],
                                              in_=Vc[:, 0, :, :].rearrange("p a (g d) -> p a g d", g=2)),
                       reads=[B_Vc[o]], writes=[B_Vg[o]])
                for pg4 in range(8):
                    x = nt % 2
                    nt += 1
                    pe.pre(reads=[B_Kg[o], K.B_ident], writes=[B_ptr[x]])
                    for b in range(4):
                        pg, g = (pg4 * 4 + b) // 2, (pg4 * 4 + b) % 2
                        ins = nc.tensor.transpose(ptr[x][:, b * 128:(b + 1) * 128], Kg[:, o, pg, g * 128:(g + 1) * 128],
                                                  ident[:])
                    ev = pe.done(ins)
                    pe.post(ev, reads=[B_Kg[o]], writes=[B_ptr[x]])
                    dstv = kTs[:, o, :, pg4 * 256:(pg4 + 1) * 256].rearrange("p g (a l) -> p a g l", a=2)
                    srcv = ptr[x][:, 0:512].rearrange("p (a g l) -> p a g l", a=2, g=2)
                    eng, veng = (act, None) if pg4 % 2 == 0 else (dve, None)
                    if pg4 % 2 == 0:
                        act.op(lambda: nc.scalar.copy(out=dstv, in_=srcv), reads=[B_ptr[x]], writes=[B_kTs[o]])
                    else:
                        dve.op(lambda: nc.vector.tensor_copy(dstv, srcv), reads=[B_ptr[x]], writes=[B_kTs[o]])
                pe.pre(reads=[B_kTs[o], C["B_qT"]], writes=[B_pl[o]])
                for pg in range(16):
                    for g in range(2):
                        ins = nc.tensor.matmul(pl[o][:, (pg * 2 + g) * 4:(pg * 2 + g) * 4 + 4],
                                               lhsT=kTs[:, o, g, pg * 128:(pg + 1) * 128],
                                               rhs=P["qT"][:, g * 4:(g + 1) * 4, tok], start=True, stop=True,
                                               skip_group_check=True)
                ev = pe.done(ins)
                pe.post(ev, reads=[B_kTs[o], C["B_qT"]], writes=[B_pl[o]])
                pe.pre(reads=[C["B_s8"], C["B_qT"]], writes=[B_psf])
                for g in range(2):
                    ins = nc.tensor.matmul(psf[0:1, g * 4:(g + 1) * 4], lhsT=P["kT8"][:, g * 128 + i:g * 128 + i + 1],
                                           rhs=P["qT"][:, g * 4:(g + 1) * 4, tok], start=True, stop=True,
                                           skip_group_check=True)
                ev = pe.done(ins)
                pe.post(ev, reads=[C["B_s8"], C["B_qT"]], writes=[B_psf])
                act.op(lambda: nc.scalar.activation(out=pTs[:, o, :], in_=pl[o][:, 0:128], func=AF.Exp, scale=SQ),
                       reads=[B_pl[o]], writes=[B_pTs[o]])
                act.op(lambda: nc.scalar.activation(out=pself[0:1, o, :], in_=psf[0:1, 0:8], func=AF.Exp, scale=SQ),
                       reads=[B_psf], writes=[B_pself[o]])
                dve.op(lambda: nc.vector.tensor_tensor(
                    out=pms[:, o, :].rearrange("p (a b) -> p a b", a=16),
                    in0=pTs[:, o, :].rearrange("p (a b) -> p a b", a=16),
                    in1=selTs[:, :, i].unsqueeze(2).to_broadcast([128, 16, 8]), op=ALU.mult),
                    reads=[B_pTs[o], B_selT], writes=[B_pms[o]])
                dve.op(lambda: nc.vector.tensor_scalar(out=pselfm[0:1, o, :], in0=pself[0:1, o, :],
                                                       scalar1=selfs[0:1, i:i + 1], scalar2=None, op0=ALU.mult),
                       reads=[B_pself[o], B_selT], writes=[B_pself[o]])
                for g in range(2):
                    pe.pre(reads=[B_pms[o], B_Vg[o], B_pself[o], B_vs], writes=[B_pos[g]])
                    for pg in range(16):
                        nc.tensor.matmul(pos[g][0:4, 0:129], lhsT=pms[:, o, (pg * 2 + g) * 4:(pg * 2 + g) * 4 + 4],
                                         rhs=Vg[:, o, pg, g, 0:129], start=(pg == 0), stop=False)
                    ins = nc.tensor.matmul(pos[g][0:4, 0:129], lhsT=pselfm[0:1, o, g * 4:(g + 1) * 4],
                                           rhs=vself[0:1, i, g, 0:129], start=False, stop=True)
                    ev = pe.done(ins)
                    pe.post(ev, reads=[B_pms[o], B_Vg[o], B_pself[o], B_vs], writes=[B_pos[g]])
                    dve.op(lambda: nc.vector.reciprocal(out=rec[0:4, g:g + 1], in_=pos[g][0:4, 128:129]),
                           reads=[B_pos[g]], writes=[B_rec])
                    dve.op(lambda: nc.vector.tensor_scalar(out=osm[0:4, o, g, :], in0=pos[g][0:4, 0:128],
                                                           scalar1=rec[0:4, g:g + 1], scalar2=None, op0=ALU.mult),
                           reads=[B_pos[g], B_rec], writes=[B_osm[o]])
                    sp.dma(C["oscr"][i, g * 512:(g + 1) * 512].rearrange("(h d) -> h d", h=4), osm[0:4, o, g, :],
                           B_osm[o].dsem, reads=[B_osm[o]], writes=[C["B_oscr"]])
            K.barrier()
        pool.dma(P["oat"][0:NS, 8, :], C["oscr"][:, :], C["B_oat"].dsem, reads=[C["B_oscr"]], writes=[C["B_oat"]])
        K.end_phase()
def phase_g1(K, C):
    nc = K.nc
    pe, act, dve, pool, sp = K.pe, K.act, K.dve, K.pool, K.sp
    P = C["P"]
    with ExitStack() as es:
        def sb(n, s, d):
            return es.enter_context(nc.sbuf_tensor("g1_" + n, s, d))

        def ps(n, s, d=F32):
            return es.enter_context(nc.psum_tensor("g1_" + n, s, d))
        triu = sb("triu", [128, 128], F32)
        B_tri = Buf()
        sst = sb("sst", [128, 2, 4, 256], F32)
        B_sst = [Buf(K, dma=True), Buf(K, dma=True)]
        pa = [ps(f"pa{i}", [128, 512]) for i in range(2)]
        B_pa = [Buf(), Buf()]
        pl = [ps(f"pl{i}", [128, 512]) for i in range(2)]
        B_pl = [Buf(), Buf()]
        pool.op(lambda: nc.gpsimd.memset(triu[:], 1.0), writes=[B_tri])
        pool.op(lambda: nc.gpsimd.affine_select(out=triu[:], in_=triu[:], pattern=[[1, 128]], compare_op=ALU.is_ge,
                                                fill=0.0, base=0, channel_multiplier=-1),
                reads=[B_tri], writes=[B_tri])
        sp.dma(C["dec_in"], P["dec"][:].rearrange("p h t -> p (h t)"), C["B_dec"].dsem, reads=[C["B_dec"]],
               writes=[C["B_decin"]])
        all_gather(K, C["dec_in"], C["dec_out"], [C["B_decin"]], [C["B_deco"]])
        n = 0
        for t in range(8):
            ts = slice(t * 128, (t + 1) * 128)
            o = t % 2
            for h in range(4):
                i = n % 2
                n += 1
                pe.op(lambda: nc.tensor.matmul(pa[i][:, 0:128], lhsT=P["kgT"][:, h, ts], rhs=P["qgT"][:, h, ts],
                                               start=True, stop=True),
                      reads=[C["B_kgT"], C["B_qgT"]], writes=[B_pa[i]])
                dve.op(lambda: nc.vector.tensor_tensor(out=P["AT"][:, t, h, :], in0=pa[i][:, 0:128], in1=triu[:],
                                                       op=ALU.mult), reads=[B_pa[i], B_tri], writes=[C["B_AT"]])
                pe.op(lambda: nc.tensor.matmul(pl[i][:, 0:256], lhsT=P["khat"][:, t, h * 128:(h + 1) * 128],
                                               rhs=P["gv"][:, t, h * 256:(h + 1) * 256], start=True, stop=True),
                      reads=[C["B_khat"], C["B_gv"]], writes=[B_pl[i]])
                act.op(lambda: nc.scalar.copy(out=sst[:, o, h, :], in_=pl[i][:, 0:256]), reads=[B_pl[i]],
                       writes=[B_sst[o]])
            sp.dma(C["gst_in"][t].rearrange("(h p) v -> p h v", p=128), sst[:, o], B_sst[o].dsem, reads=[B_sst[o]],
                   writes=[C["B_gin"][t]])
            all_gather(K, C["gst_in"][t], C["gst_out"][t], [C["B_gin"][t]], [C["B_gout"][t]])
        K.end_phase()


def phase_g2(K, C):
    nc = K.nc
    pe, act, dve, pool, sp = K.pe, K.act, K.dve, K.pool, K.sp
    P = C["P"]
    ident = C["ident"]
    with ExitStack() as es:
        def sb(n, s, d):
            return es.enter_context(nc.sbuf_tensor("g2_" + n, s, d))

        def ps(n, s, d=F32):
            return es.enter_context(nc.psum_tensor("g2_" + n, s, d))
        Sg = sb("Sg", [128, 4, 4, 256], F32)
        B_Sg = Buf(K, dma=True)
        dg = sb("dg", [128, 4, 32], F32)
        B_dg = Buf(K, dma=True)
        oh = sb("oh", [128, 4], F32)
        gnw = sb("gnw", [128, 256], F32)
        B_c = Buf(K, dma=True)
        Srun = sb("Srun", [128, 4, 256], F32)
        B_run = Buf(K, dma=True)
        Sin = sb("Sin", [128, 4, 256], F32)
        B_in = Buf()
        Sinb = sb("Sinb", [128, 4, 256], BF16)
        B_inb = Buf()
        st = sb("st", [128, 16], F32)
        B_st = Buf()
        junk = sb("junk", [128, 256], BF16)
        B_junk = Buf()
        po = [ps(f"po{i}", [128, 512]) for i in range(2)]
        B_po = [Buf(), Buf()]

        sp.dma(oh[:], C["onehot"], B_c.dsem, writes=[B_c])
        sp.dma(gnw[:], C["gla_norm_w"].rearrange("(o d) -> o d", o=1).to_broadcast([128, 256]), B_c.dsem, writes=[B_c])
        sp.dma(dg[:], C["dec_out"].rearrange("(r p) c -> p r c", p=128), B_dg.dsem, reads=[C["B_deco"]], writes=[B_dg])
        dve.op(lambda: nc.vector.memset(Srun[:], 0.0), writes=[B_run])

        def head_norm(pz, B_pz, np_, out_ap, B_out):
            dve.op(lambda: nc.vector.memset(st[0:np_, 0:1], 0.0), writes=[B_st])
            act.op(lambda: nc.scalar.activation(out=junk[0:np_, :], in_=pz, func=AF.Square,
                                                accum_out=st[0:np_, 0:1]), reads=[B_pz, B_st], writes=[B_junk, B_st])
            act.op(lambda: nc.scalar.activation(out=st[0:np_, 1:2], in_=st[0:np_, 0:1], func=AF.Sqrt,
                                                scale=1.0 / 256.0, bias=EPS), reads=[B_st], writes=[B_st])
            dve.op(lambda: nc.vector.reciprocal(out=st[0:np_, 1:2], in_=st[0:np_, 1:2]), reads=[B_st], writes=[B_st])
            dve.op(lambda: nc.vector.scalar_tensor_tensor(out=out_ap, in0=pz, scalar=st[0:np_, 1:2],
                                                          in1=gnw[0:np_, :], op0=ALU.mult, op1=ALU.mult),
                   reads=[B_pz, B_st, B_c], writes=[B_out])

        n = 0
        for s in range(8):
            ts = slice(s * 128, (s + 1) * 128)
            sp.dma(Sg[:], C["gst_out"][s].rearrange("(r h p) v -> p r h v", p=128, h=4), B_Sg.dsem,
                   reads=[C["B_gout"][s]], writes=[B_Sg])
            dve.op(lambda: nc.vector.memset(Sin[:], 0.0), reads=[], writes=[B_in])
            for r in range(4):
                dve.op(lambda: nc.vector.scalar_tensor_tensor(out=Sin[:], in0=Srun[:], scalar=oh[:, r:r + 1],
                                                              in1=Sin[:], op0=ALU.mult, op1=ALU.add),
                       reads=[B_run, B_c, B_in], writes=[B_in])
                for h in range(4):
                    dve.op(lambda: nc.vector.scalar_tensor_tensor(out=Srun[:, h, :], in0=Srun[:, h, :],
                                                                  scalar=dg[:, r, h * 8 + s:h * 8 + s + 1],
                                                                  in1=Sg[:, r, h, :], op0=ALU.mult, op1=ALU.add),
                           reads=[B_run, B_dg, B_Sg], writes=[B_run])
            act.op(lambda: nc.scalar.copy(out=Sinb[:], in_=Sin[:]), reads=[B_in], writes=[B_inb])
            for h in range(4):
                i = n % 2
                n += 1
                pe.pre(reads=[C["B_AT"], C["B_gv"], C["B_qgT"], B_inb], writes=[B_po[i]])
                nc.tensor.matmul(po[i][:, 0:256], lhsT=P["AT"][:, s, h, :], rhs=P["gv"][:, s, h * 256:(h + 1) * 256],
                                 start=True, stop=False)
                ins = nc.tensor.matmul(po[i][:, 0:256], lhsT=P["qgT"][:, h, ts], rhs=Sinb[:, h, :],
                                       start=False, stop=True)
                ev = pe.done(ins)
                pe.post(ev, reads=[C["B_AT"], C["B_gv"], C["B_qgT"], B_inb], writes=[B_po[i]])
                head_norm(po[i][:, 0:256], B_po[i], 128, P["og"][:, s, h * 256:(h + 1) * 256], C["B_og"])
        sp.dma(C["gla_p"].rearrange("(h p) v -> p h v", p=128), Srun[:], B_run.dsem, reads=[B_run])

        S0 = sb("S0", [128, 2, 4, 256], F32)
        B_S0 = [Buf(K, dma=True), Buf(K, dma=True)]
        Sn = sb("Sn", [128, 2, 4, 256], F32)
        B_Sn = [Buf(K, dma=True), Buf(K, dma=True)]
        Snb = sb("Snb", [128, 2, 4, 256], BF16)
        B_Snb = [Buf(), Buf()]
        tmp = sb("tmp", [128, 2, 256], F32)
        B_tmp = [Buf(), Buf()]
        Bsel = sb("Bsel", [128, 16, 128], BF16)
        I16 = sb("I16", [128, 16, 16], BF16)
        Qpad = sb("Qpad", [128, 4, 16, 16], BF16)
        B_q = Buf()
        pb = [ps(f"pb{i}", [128, 512]) for i in range(2)]
        B_pb = [Buf(), Buf()]
        pso = [ps(f"pso{i}", [128, 512]) for i in range(2)]
        B_pso = Buf()
        dve.op(lambda: nc.vector.tensor_copy(Bsel[:], ident[:, 0:16].unsqueeze(2).to_broadcast([128, 16, 128])),
               reads=[K.B_ident], writes=[B_q])
        dve.op(lambda: nc.vector.memset(I16[:], 0.0), writes=[B_q])
        for i in range(16):
            dve.op(lambda: nc.vector.memset(I16[:, i, i:i + 1], 1.0), writes=[B_q])
        for h in range(4):
            dve.op(lambda: nc.vector.tensor_tensor(out=Qpad[:, h], in0=P["qgT"][:, h, 1024:1040].unsqueeze(2).to_broadcast(
                [128, 16, 16]), in1=I16[:], op=ALU.mult), reads=[C["B_qgT"], B_q], writes=[B_q])
        stin = C["state_in"].rearrange("(i h p) v -> i p h v", p=128, h=4)
        stout = C["gla_s"].rearrange("(i h p) v -> i p h v", p=128, h=4)
        pe.pre(writes=[B_pso])
        for i in range(16):
            o = i % 2
            sp.dma(S0[:, o], stin[i], B_S0[o].dsem, writes=[B_S0[o]])
            for h in range(4):
                x = n % 2
                n += 1
                pe.op(lambda: nc.tensor.matmul(pb[x][:, 0:256], lhsT=Bsel[:, i, :], rhs=P["gv"][:, 8, h * 256:(h + 1) * 256],
                                               start=True, stop=True), reads=[B_q, C["B_gv"]], writes=[B_pb[x]])
                dve.op(lambda: nc.vector.tensor_scalar(out=tmp[:, x, :], in0=pb[x][:, 0:256],
                                                       scalar1=P["kg8f"][:, h, i:i + 1], scalar2=None, op0=ALU.mult),
                       reads=[B_pb[x], C["B_kgT"]], writes=[B_tmp[x]])
                dve.op(lambda: nc.vector.scalar_tensor_tensor(out=Sn[:, o, h, :], in0=S0[:, o, h, :],
                                                              scalar=P["dec8"][:, h, i:i + 1], in1=tmp[:, x, :],
                                                              op0=ALU.mult, op1=ALU.add),
                       reads=[B_S0[o], B_tmp[x], C["B_dec"]], writes=[B_Sn[o]])
            sp.dma(stout[i], Sn[:, o], B_Sn[o].dsem, reads=[B_Sn[o]])
            act.op(lambda: nc.scalar.copy(out=Snb[:, o], in_=Sn[:, o]), reads=[B_Sn[o]], writes=[B_Snb[o]])
            pe.pre(reads=[B_Snb[o], B_q])
            for h in range(4):
                ins = nc.tensor.matmul(pso[h // 2][0:16, (h % 2) * 256:(h % 2) * 256 + 256], lhsT=Qpad[:, h, i, :],
                                       rhs=Snb[:, o, h, :], start=(i == 0 and h % 2 == 0), stop=(i == 15),
                                       skip_group_check=True)
            ev = pe.done(ins)
            pe.post(ev, reads=[B_Snb[o], B_q], writes=[B_pso])
        for h in range(4):
            head_norm(pso[h // 2][0:16, (h % 2) * 256:(h % 2) * 256 + 256], B_pso, 16,
                      P["og"][0:16, 8, h * 256:(h + 1) * 256], C["B_og"])
        K.end_phase()
def phase_c(K, C):
    nc = K.nc
    pe, act, dve, pool, sp = K.pe, K.act, K.dve, K.pool, K.sp
    ident = C["ident"]
    win_v = C["w_in"].rearrange("(k p) f -> p k f", p=128)
    wpa_v = C["w_proj_attn"].rearrange("(k p) f -> p k f", p=128)
    wpg_v = C["w_proj_gla"].rearrange("(k p) f -> p k f", p=128)
    wo_v = C["w_out"].rearrange("(k p) f -> p k f", p=128)
    with ExitStack() as esm:
        def sbm(n, s, d):
            return esm.enter_context(nc.sbuf_tensor("c_" + n, s, d))
        mT = sbm("mT", [128, 16, TOK], BF16)
        B_mT = [Buf() for _ in range(NT)]
        with ExitStack() as esg:
            merged = esg.enter_context(nc.sbuf_tensor("c_merged", [128, NT, D], BF16))
            B_mg = [Buf() for _ in range(NT)]
            with ExitStack() as es:
                def sb(n, s, d):
                    return es.enter_context(nc.sbuf_tensor("c_" + n, s, d))

                def ps(n, s, d=F32):
                    return es.enter_context(nc.psum_tensor("c_" + n, s, d))
                uT = sb("uT", [128, 16, TOK], BF16)
                B_uT = [Buf() for _ in range(NT)]
                oatT = sb("oatT", [128, 8, TOK], BF16)
                ogT = sb("ogT", [128, 8, TOK], BF16)
                B_oatT = [Buf() for _ in range(NT)]
                B_ogT = [Buf() for _ in range(NT)]
                ptr = [ps(f"ptr{i}", [128, 1024], BF16) for i in range(2)]
                B_ptr = [Buf(), Buf()]
                norm_T(K, C["h1"], C["mix_pre_w"], uT, B_uT, ident, ptr, B_ptr, "c1_")
                pp = [ps(f"pp{i}", [128, 512]) for i in range(4)]
                B_pp = [Buf() for _ in range(4)]
                wg = sb("wg", [128, 2, 16, 512], BF16)
                B_wg = [Buf(K, dma=True), Buf(K, dma=True)]
                wp = sb("wp", [128, 2, 8, 512], BF16)
                B_wp = [Buf(K, dma=True), Buf(K, dma=True)]
                ost = sb("ost", [128, 1, 1024], BF16)
                B_ost = [Buf(K, dma=True)] * 2
                gst = sb("gst", [128, 1, 1024], BF16)
                B_gst = [Buf(K, dma=True)] * 2
                sg = sb("sg", [128, 4, 512], BF16)
                B_sg = [Buf() for _ in range(4)]
                tm = sb("tm", [128, 2, 512], F32)
                B_tm = [Buf(), Buf()]
                npp = [0]
                ntr = [0]

                def tr8(src_slot_ap, B_src, dstT, B_dst, t):
                    for half in range(2):
                        x = ntr[0] % 2
                        ntr[0] += 1
                        pe.pre(reads=[B_src, K.B_ident], writes=[B_ptr[x]])
                        for b in range(4):
                            k = half * 4 + b
                            ins = nc.tensor.transpose(ptr[x][:, b * 128:(b + 1) * 128],
                                                      src_slot_ap[:, k * 128:(k + 1) * 128], ident[:])
                        ev = pe.done(ins)
                        pe.post(ev, reads=[B_src], writes=[B_ptr[x]])
                        act.op(lambda: nc.scalar.copy(out=dstT[:, half * 4:half * 4 + 4, t * 128:(t + 1) * 128],
                                                      in_=ptr[x][:, 0:512].rearrange("p (a b) -> p a b", a=4)),
                               reads=[B_ptr[x]], writes=[B_dst[t]])

                oat_d = C["oat_d"].bitcast(BF16)
                og_d = C["og_d"].bitcast(BF16)
                for t in range(NT):
                    o = t % 2
                    sp.dma(ost[:, 0, :], oat_d[t * 128:(t + 1) * 128, :], B_ost[o].dsem, writes=[B_ost[o]])
                    tr8(ost[:, 0, :], B_ost[o], oatT, B_oatT, t)
                for blk in range(2):
                    pool.dma(wg[:, blk, :, :], win_v[:, :, CGR + blk * 512:CGR + (blk + 1) * 512], B_wg[blk].dsem,
                             writes=[B_wg[blk]])
                for t in range(NT):
                    o = t % 2
                    sp.dma(gst[:, 0, :], og_d[t * 128:(t + 1) * 128, :], B_gst[o].dsem, writes=[B_gst[o]])
                    for blk in range(2):
                        i = npp[0] % 4
                        npp[0] += 1
                        pe.pre(reads=[B_uT[t], B_wg[blk]], writes=[B_pp[i]])
                        for k in range(16):
                            ins = nc.tensor.matmul(pp[i][:, :], lhsT=uT[:, k, t * 128:(t + 1) * 128], rhs=wg[:, blk, k, :],
                                                   start=(k == 0), stop=(k == 15))
                        ev = pe.done(ins)
                        pe.post(ev, reads=[B_uT[t], B_wg[blk]], writes=[B_pp[i]])
                        act.op(lambda: nc.scalar.activation(out=sg[:, i, :], in_=pp[i][:, :], func=AF.Silu),
                               reads=[B_pp[i]], writes=[B_sg[i]])
                        dve.op(lambda: nc.vector.tensor_tensor(out=gst[:, 0, blk * 512:(blk + 1) * 512],
                                                               in0=gst[:, 0, blk * 512:(blk + 1) * 512], in1=sg[:, i, :],
                                                               op=ALU.mult), reads=[B_sg[i], B_gst[o]], writes=[B_gst[o]])
                    tr8(gst[:, 0, :], B_gst[o], ogT, B_ogT, t)
                for nb in range(4):
                    cs = slice(nb * 512, (nb + 1) * 512)
                    pool.dma(wg[:, 0, :, :], win_v[:, :, CGA + nb * 512:CGA + (nb + 1) * 512], B_wg[0].dsem,
                             writes=[B_wg[0]])
                    pool.dma(wg[:, 1, :, :], win_v[:, :, CGG + nb * 512:CGG + (nb + 1) * 512], B_wg[1].dsem,
                             writes=[B_wg[1]])
                    pool.dma(wp[:, 0, :, :], wpa_v[:, :, cs], B_wp[0].dsem, writes=[B_wp[0]])
                    pool.dma(wp[:, 1, :, :], wpg_v[:, :, cs], B_wp[1].dsem, writes=[B_wp[1]])
                    for t in range(NT):
                        ts = slice(t * 128, (t + 1) * 128)
                        ids = []
                        for which in range(4):
                            i = npp[0] % 4
                            npp[0] += 1
                            ids.append(i)
                            if which < 2:
                                srcT, Bs, w, Bw, nk = uT, B_uT, wg[:, which], B_wg[which], 16
                            elif which == 2:
                                srcT, Bs, w, Bw, nk = oatT, B_oatT, wp[:, 0], B_wp[0], 8
                            else:
                                srcT, Bs, w, Bw, nk = ogT, B_ogT, wp[:, 1], B_wp[1], 8
                            pe.pre(reads=[Bs[t], Bw], writes=[B_pp[i]])
                            for k in range(nk):
                                ins = nc.tensor.matmul(pp[i][:, :], lhsT=srcT[:, k, ts], rhs=w[:, k, :],
                                                       start=(k == 0), stop=(k == nk - 1))
                            ev = pe.done(ins)
                            pe.post(ev, reads=[Bs[t], Bw], writes=[B_pp[i]])
                            if which < 2:
                                act.op(lambda: nc.scalar.activation(out=sg[:, i, :], in_=pp[i][:, :], func=AF.Sigmoid),
                                       reads=[B_pp[i]], writes=[B_sg[i]])
                        ia, ig, ipa, ipg = ids
                        x = t % 2
                        dve.op(lambda: nc.vector.tensor_tensor(out=tm[:, x, :], in0=sg[:, ia, :], in1=pp[ipa][:, :],
                                                               op=ALU.mult), reads=[B_sg[ia], B_pp[ipa]], writes=[B_tm[x]])
                        dve.op(lambda: nc.vector.tensor_tensor(out=sg[:, ig, :], in0=sg[:, ig, :], in1=pp[ipg][:, :],
                                                               op=ALU.mult), reads=[B_sg[ig], B_pp[ipg]], writes=[B_sg[ig]])
                        pool.op(lambda: nc.gpsimd.tensor_tensor(out=merged[:, t, cs], in0=tm[:, x, :], in1=sg[:, ig, :],
                                                                op=ALU.add), reads=[B_tm[x], B_sg[ig]], writes=[B_mg[t]])
                K.barrier()
            with ExitStack() as es:
                ptr = [es.enter_context(nc.psum_tensor(f"c2_ptr{i}", [128, 1024], BF16)) for i in range(2)]
                B_ptr = [Buf(), Buf()]
                n = 0
                for t in range(NT):
                    for q4 in range(4):
                        x = n % 2
                        n += 1
                        pe.pre(reads=[B_mg[t], K.B_ident], writes=[B_ptr[x]])
                        for b in range(4):
                            k = q4 * 4 + b
                            ins = nc.tensor.transpose(ptr[x][:, b * 128:(b + 1) * 128], merged[:, t, k * 128:(k + 1) * 128],
                                                      ident[:])
                        ev = pe.done(ins)
                        pe.post(ev, reads=[B_mg[t]], writes=[B_ptr[x]])
                        if q4 % 2 == 0:
                            act.op(lambda: nc.scalar.copy(out=mT[:, q4 * 4:q4 * 4 + 4, t * 128:(t + 1) * 128],
                                                          in_=ptr[x][:, 0:512].rearrange("p (a b) -> p a b", a=4)),
                                   reads=[B_ptr[x]], writes=[B_mT[t]])
                        else:
                            dve.op(lambda: nc.vector.tensor_copy(mT[:, q4 * 4:q4 * 4 + 4, t * 128:(t + 1) * 128],
                                                                 ptr[x][:, 0:512].rearrange("p (a b) -> p a b", a=4)),
                                   reads=[B_ptr[x]], writes=[B_mT[t]])
                K.barrier()
        with ExitStack() as es:
            def sb(n, s, d):
                return es.enter_context(nc.sbuf_tensor("c3_" + n, s, d))
            wo = sb("wo", [128, 16, D], BF16)
            B_wo = Buf(K, dma=True)
            wbc = sb("wbc", [128, D], F32)
            hst = sb("hst", [128, 2, D], F32)
            B_hst = [Buf(K, dma=True), Buf(K, dma=True)]
            ot = sb("ot", [128, 2, D], F32)
            B_ot = [Buf(K, dma=True), Buf(K, dma=True)]
            junk = sb("junk", [128, 512], BF16)
            B_junk = Buf()
            st = sb("st", [128, 8 * NT], F32)
            B_st = Buf()
            po = [es.enter_context(nc.psum_tensor(f"c3_po{i}", [128, 512])) for i in range(8)]
            B_po = [Buf() for _ in range(8)]
            for nb in range(4):
                pool.dma(wo[:, :, nb * 512:(nb + 1) * 512], wo_v[:, :, nb * 512:(nb + 1) * 512], B_wo.dsem, writes=[B_wo])
            sp.dma(wbc[:], C["mix_post_w"].rearrange("(o d) -> o d", o=1).to_broadcast([128, D]), B_wo.dsem,
                   writes=[B_wo])
            dve.op(lambda: nc.vector.memset(st[:], 0.0), writes=[B_st])
            for t in range(NT):
                o = t % 2
                ts = slice(t * 128, (t + 1) * 128)
                sp.dma(hst[:, o, :], C["h1"][ts, :], B_hst[o].dsem, writes=[B_hst[o]])
                for nb in range(4):
                    i = o * 4 + nb
                    pe.pre(reads=[B_mT[t], B_wo], writes=[B_po[i]])
                    for k in range(16):
                        ins = nc.tensor.matmul(po[i][:, :], lhsT=mT[:, k, ts], rhs=wo[:, k, nb * 512:(nb + 1) * 512],
                                               start=(k == 0), stop=(k == 15))
                    ev = pe.done(ins)
                    pe.post(ev, reads=[B_mT[t], B_wo], writes=[B_po[i]])
                    act.op(lambda: nc.scalar.activation(out=junk[:], in_=po[i][:, :], func=AF.Square,
                                                        accum_out=st[:, t * 8 + nb:t * 8 + nb + 1]),
                           reads=[B_po[i], B_st], writes=[B_junk, B_st])
                c = t * 8
                dve.op(lambda: nc.vector.tensor_reduce(out=st[:, c + 4:c + 5], in_=st[:, c:c + 4], axis=AX.X, op=ALU.add),
                       reads=[B_st], writes=[B_st])
                act.op(lambda: nc.scalar.activation(out=st[:, c + 5:c + 6], in_=st[:, c + 4:c + 5], func=AF.Sqrt,
                                                    scale=1.0 / D, bias=EPS), reads=[B_st], writes=[B_st])
                dve.op(lambda: nc.vector.reciprocal(out=st[:, c + 5:c + 6], in_=st[:, c + 5:c + 6]), reads=[B_st],
                       writes=[B_st])
                for nb in range(4):
                    i = o * 4 + nb
                    cs = slice(nb * 512, (nb + 1) * 512)
                    dve.op(lambda: nc.vector.scalar_tensor_tensor(out=ot[:, o, cs], in0=po[i][:, :],
                                                                  scalar=st[:, c + 5:c + 6], in1=wbc[:, cs],
                                                                  op0=ALU.mult, op1=ALU.mult),
                           reads=[B_po[i], B_st, B_wo], writes=[B_ot[o]])
                pool.op(lambda: nc.gpsimd.tensor_tensor(out=ot[:, o, :], in0=ot[:, o, :], in1=hst[:, o, :], op=ALU.add),
                        reads=[B_hst[o], B_ot[o]], writes=[B_ot[o]])
                sp.dma(C["h2"][ts, :], ot[:, o, :], B_ot[o].dsem, reads=[B_ot[o]])
            K.barrier()


WNAMES = [("ffn1_pre_w", [D]), ("ffn1_w_gate", [D, DFF]), ("ffn1_w_up", [D, DFF]), ("ffn1_w_down", [DFF, D]),
          ("ffn1_post_w", [D]), ("mix_pre_w", [D]), ("w_in", [D, DIN]), ("gla_gate_w2", [16, 512]),
          ("gla_gate_b", [512]), ("gla_norm_w", [256]), ("w_proj_attn", [1024, D]), ("w_proj_gla", [1024, D]),
          ("w_out", [D, D]), ("mix_post_w", [D]), ("ffn2_pre_w", [D]), ("ffn2_w_gate", [D, DFF]),
          ("ffn2_w_up", [D, DFF]), ("ffn2_w_down", [DFF, D]), ("ffn2_post_w", [D])]
NPOOL_ROWS = 2560 * 128


def build(stage=99, debug=False):
    K = Kern()
    nc = K.nc
    C = {}
    full = stage >= 4
    x = K.dram("x", [TOK, D], F32, "ExternalInput").ap()
    C["posv"] = K.dram("posv", [128, NT], F32, "ExternalInput").ap()
    K.used = WNAMES[:5] if stage == 1 else (WNAMES[:9] if stage in (2, 3) else WNAMES)
    for name, shape in K.used:
        C[name] = K.dram(name, shape, F32, "ExternalInput").ap()
    y = K.dram("y", [TOK, D], F32, "ExternalOutput").ap()
    C["ko"] = K.dram("ko", [TOK, 256], F32, "ExternalOutput").ap()
    C["vo"] = K.dram("vo", [TOK, 256], F32, "ExternalOutput").ap()
    C["kio"] = K.dram("kio", [TOK, 64], F32, "ExternalOutput").ap()
    dk = "ExternalOutput" if debug else "Internal"
    C["h1"] = K.dram("h1s", [TOK, D], F32, dk).ap()
    C["h2"] = K.dram("h2s", [TOK, D], F32, dk).ap()
    C["cmask"] = K.dram("cmask", [128, 512], F32, "ExternalInput").ap()
    if stage == 3:
        C["dbg"] = K.dram("dbg", [TOK, 1024], F32, "ExternalOutput").ap()
        C["dbg2"] = K.dram("dbg2", [128, 4136], F32, "ExternalOutput").ap()
    for nm, r, c in (("agk", 256, 512), ("agki", 64, 512), ("agv", 1024, 128), ("dec", 128, 32)):
        C[nm + "_in"] = K.dram(nm + "_in", [r, c], F32).ap()
        C[nm + "_out"] = K.dram(nm + "_out", [4 * r, c], F32).ap()
    if full:
        C["onehot"] = K.dram("onehot", [128, 4], F32, "ExternalInput").ap()
        C["pt"] = K.dram("pt", [16, 16], I32, "ExternalInput").ap()
        C["state_in"] = K.dram("state_in", [16 * 512, 256], F32, "ExternalInput").ap()
        C["cache_k"] = K.dram("cache_k", [NPOOL_ROWS, 256], F32, "ExternalInput").ap()
        C["cache_v"] = K.dram("cache_v", [NPOOL_ROWS, 256], F32, "ExternalInput").ap()
        C["cache_kidx"] = K.dram("cache_kidx", [NPOOL_ROWS, 64], F32, "ExternalInput").ap()
        C["gla_p"] = K.dram("gla_p", [512, 256], F32, "ExternalOutput").ap()
        C["gla_s"] = K.dram("gla_s", [16 * 512, 256], F32, "ExternalOutput").ap()
        C["gst_in"] = [K.dram(f"gst_in{t}", [512, 256], F32).ap() for t in range(8)]
        C["gst_out"] = [K.dram(f"gst_out{t}", [2048, 256], F32).ap() for t in range(8)]
        C["sscr"] = K.dram("sscr", [16, 2176], F32).ap()
        C["oscr"] = K.dram("oscr", [16, 1024], F32).ap()
        C["oat_d"] = K.dram("oat_d", [TOK, 512], F32, dk).ap()
        C["og_d"] = K.dram("og_d", [TOK, 512], F32, dk).ap()

    with ExitStack() as es0:
        ident = es0.enter_context(nc.sbuf_tensor("ident", [128, 128], BF16))
        C["ident"] = ident
        K.B_ident = Buf()
        K.pool.op(lambda: nc.gpsimd.memset(ident[:], 1.0), writes=[K.B_ident])
        K.pool.op(lambda: nc.gpsimd.affine_select(out=ident[:], in_=ident[:], pattern=[[-1, 128]],
                                                  compare_op=ALU.is_equal, fill=0.0, base=0, channel_multiplier=1),
                  reads=[K.B_ident], writes=[K.B_ident])
        K.barrier()

        ffn_phase(K, x, (y if stage == 1 else C["h1"]), C["ffn1_pre_w"], C["ffn1_w_gate"], C["ffn1_w_up"],
                  C["ffn1_w_down"], C["ffn1_post_w"], ident)
        if stage >= 2:
            with ExitStack() as es:
                def sb(n, s, d):
                    return es.enter_context(nc.sbuf_tensor(n, s, d))
                P = {}
                P["qT"] = sb("p_qT", [128, 8, TOK], BF16)
                P["qiT"] = sb("p_qiT", [128, 8, TOK], BF16)
                P["wi"] = sb("p_wi", [128, NT, 16], F32)
                P["qgT"] = sb("p_qgT", [128, 4, TOK], BF16)
                P["gv"] = sb("p_gv", [128, NT, 1024], BF16)
                P["dec"] = sb("p_dec", [128, 4, 8], F32)
                P["dec8"] = sb("p_dec8", [128, 4, 128], F32)
                P["kg8f"] = sb("p_kg8f", [128, 4, 128], F32)
                P["kT8"] = sb("p_kT8", [128, 256], BF16)
                P["v8"] = sb("p_v8", [128, 256], BF16)
                P["kiT8"] = sb("p_kiT8", [128, 128], BF16)
                if full:
                    P["AT"] = sb("p_AT", [128, 8, 4, 128], BF16)
                C["P"] = P
                for n in ("B_qT", "B_qiT", "B_wi", "B_qgT", "B_kgT", "B_khat", "B_gv", "B_s8", "B_AT", "B_og",
                          "B_ag1", "B_ag1o", "B_decin", "B_deco", "B_sscr", "B_oscr"):
                    C[n] = Buf()
                C["B_dec"] = Buf(K, dma=True, persist=True)
                C["B_gin"] = [Buf() for _ in range(8)]
                C["B_gout"] = [Buf() for _ in range(8)]
                with ExitStack() as esk:
                    P["kgT"] = esk.enter_context(nc.sbuf_tensor("p_kgT", [128, 4, TOK], BF16))
                    P["khat"] = esk.enter_context(nc.sbuf_tensor("p_khat", [128, NT, 512], BF16))
                    phase_a(K, C)
                    if full:
                        phase_g1(K, C)
                if stage >= 3:
                    P["oat"] = sb("p_oat", [128, NT, 1024], BF16)
                    C["B_oat"] = Buf(K, dma=True, persist=True)
                    K.dve.op(lambda: nc.vector.memset(P["oat"][:, 8, :], 0.0), writes=[C["B_oat"]])
                    phase_b(K, C)
                if stage == 3:
                    K.pool.dma(C["dbg"].rearrange("(t p) c -> p t c", p=128), P["oat"][:], C["B_oat"].dsem,
                               reads=[C["B_oat"]])
                if full:
                    phase_bs(K, C)
                    K.sp.dma(C["oat_d"].bitcast(BF16).rearrange("(t p) c -> p t c", p=128), P["oat"][:],
                             C["B_oat"].dsem, reads=[C["B_oat"]])
                    P["og"] = sb("p_og", [128, NT, 1024], BF16)
                    C["B_og"] = Buf(K, dma=True, persist=True)
                    K.dve.op(lambda: nc.vector.memset(P["og"][:, 8, :], 0.0), writes=[C["B_og"]])
                    phase_g2(K, C)
                    K.sp.dma(C["og_d"].bitcast(BF16).rearrange("(t p) c -> p t c", p=128), P["og"][:],
                             C["B_og"].dsem, reads=[C["B_og"]])
                K.barrier()
            if full:
                phase_c(K, C)
                ffn_phase(K, C["h2"], y, C["ffn2_pre_w"], C["ffn2_w_gate"], C["ffn2_w_up"], C["ffn2_w_down"],
                          C["ffn2_post_w"], ident, tag="f2")
    K.barrier()
    K.es.close()
    return K


def core_rows(x_prompt, x_sample, c):
    b, j = c // 4, c % 4
    rows = [x_prompt[b, (4 * s + j) * 128:(4 * s + j + 1) * 128] for s in range(8)]
    pad = np.zeros((128, x_sample.shape[-1]), np.float32)
    pad[:16] = x_sample[16 * c:16 * c + 16, 0]
    rows.append(pad)
    return np.ascontiguousarray(np.concatenate(rows, 0))


def make_in_maps(inputs, used=WNAMES, full=True):
    in_maps = []
    shared = {k: np.ascontiguousarray(inputs[k], dtype=np.float32) for k, _ in used}
    if full:
        shared["cache_k"] = np.ascontiguousarray(inputs["cache_k"]).reshape(NPOOL_ROWS, 256)
        shared["cache_v"] = np.ascontiguousarray(inputs["cache_v"]).reshape(NPOOL_ROWS, 256)
        shared["cache_kidx"] = np.ascontiguousarray(inputs["cache_kidx"]).reshape(NPOOL_ROWS, 64)
    for c in range(NCORES):
        j = c % 4
        m = {"x": core_rows(inputs["x_prompt"], inputs["x_sample"], c)}
        posv = np.zeros((128, NT), np.float32)
        for s in range(8):
            posv[:, s] = (4 * s + j) * 128 + np.arange(128)
        posv[:, 8] = 2048.0
        m["posv"] = posv
        cm = np.zeros((128, 4, 128), np.float32)
        for r in range(4):
            if r > j:
                cm[:, r, :] = -1.0e4
            elif r == j:
                cm[:, r, :] = np.where(np.arange(128)[None, :] > np.arange(128)[:, None], -1.0e4, 0.0)
        m["cmask"] = cm.reshape(128, 512)
        if full:
            oh = np.zeros((128, 4), np.float32)
            oh[:, j] = 1.0
            m["onehot"] = oh
            m["pt"] = np.ascontiguousarray(inputs["page_table"][16 * c:16 * c + 16], dtype=np.int32)
            m["state_in"] = np.ascontiguousarray(inputs["state_gla"][16 * c:16 * c + 16], dtype=np.float32).reshape(
                16 * 512, 256)
        m.update(shared)
        in_maps.append(m)
    return in_maps


def assemble(results):
    y_p = np.zeros((2, 4096, D), np.float32)
    y_s = np.zeros((128, 1, D), np.float32)
    k_p = np.zeros((2, 4096, 2, 128), np.float32)
    v_p = np.zeros((2, 4096, 2, 128), np.float32)
    ki_p = np.zeros((2, 4096, 64), np.float32)
    k_s = np.zeros((128, 1, 2, 128), np.float32)
    v_s = np.zeros((128, 1, 2, 128), np.float32)
    ki_s = np.zeros((128, 1, 64), np.float32)
    gla_p = np.zeros((2, 4, 128, 256), np.float32)
    gla_s = np.zeros((128, 4, 128, 256), np.float32)
    for c, r in enumerate(results):
        b, j = c // 4, c % 4
        for s in range(8):
            sl = slice((4 * s + j) * 128, (4 * s + j + 1) * 128)
            rs = slice(s * 128, (s + 1) * 128)
            y_p[b, sl] = r["y"][rs]
            k_p[b, sl] = r["ko"][rs].reshape(128, 2, 128)
            v_p[b, sl] = r["vo"][rs].reshape(128, 2, 128)
            ki_p[b, sl] = r["kio"][rs]
        ss = slice(16 * c, 16 * c + 16)
        y_s[ss, 0] = r["y"][1024:1040]
        k_s[ss, 0] = r["ko"][1024:1040].reshape(16, 2, 128)
        v_s[ss, 0] = r["vo"][1024:1040].reshape(16, 2, 128)
        ki_s[ss, 0] = r["kio"][1024:1040]
        gla_s[ss] = r["gla_s"].reshape(16, 4, 128, 256)
        if j == 0:
            gla_p[b] = r["gla_p"].reshape(4, 128, 256)
    return (y_p, y_s, k_p, v_p, ki_p, gla_p, k_s, v_s, ki_s, gla_s)


def kernel(**inputs):
    K = build()
    in_maps = make_in_maps(inputs, K.used, True)
    res = run_bass_kernel_spmd(K.nc, in_maps, core_ids=list(range(NCORES)))
    return assemble(res.results)
```

```python
import numpy as np
from contextlib import ExitStack
import concourse.bass as bass
import concourse.mybir as mybir
from concourse.bass_utils import run_bass_kernel_spmd

F32 = mybir.dt.float32
BF16 = mybir.dt.bfloat16
I32 = mybir.dt.int32
ALU = mybir.AluOpType
AF = mybir.ActivationFunctionType
AX = mybir.AxisListType

NCORES = 8
NT = 9
TOK = NT * 128
D = 2048
DFF = 5632
DIN = 9824
EPS = 1e-6
TB = [(0, 512), (512, 512), (1024, 128)]


class Sem:
    def __init__(self, h, uid):
        self.h = h
        self.uid = uid
        self.cnt = 0


class Ev:
    __slots__ = ("sem", "val", "q", "idx")

    def __init__(self, sem, val, q=None, idx=0):
        self.sem = sem
        self.val = val
        self.q = q
        self.idx = idx


class Buf:
    def __init__(self, K=None, dma=False, persist=False):
        self.w = None
        self.r = {}
        self.dsem = K.new_sem("d", persist) if dma else None


class Q:
    def __init__(self, K, eng, name):
        self.K = K
        self.eng = eng
        self.name = name
        self.sem = K.new_sem(name, True)
        self.seen = {}
        self.nins = 0

    def wait(self, *evs):
        for ev in evs:
            if ev is None:
                continue
            if ev.q is self and self.nins - ev.idx >= 4:
                continue
            if self.seen.get(ev.sem.uid, -1) >= ev.val:
                continue
            self.seen[ev.sem.uid] = ev.val
            self.eng.wait_ge(ev.sem.h, ev.val)

    def done(self, ins):
        self.sem.cnt += 1
        self.nins += 1
        ins.then_inc(self.sem.h, 1)
        return Ev(self.sem, self.sem.cnt, self, self.nins)

    def tick(self, n=1):
        self.nins += n

    def pre(self, reads=(), writes=()):
        for b in reads:
            self.wait(b.w)
        for b in writes:
            self.wait(b.w)
            self.wait(*b.r.values())

    def post(self, ev, reads=(), writes=()):
        for b in reads:
            b.r[ev.sem.uid] = ev
        for b in writes:
            b.w = ev
            b.r = {}

    def op(self, fn, reads=(), writes=()):
        self.pre(reads, writes)
        ev = self.done(fn())
        self.post(ev, reads, writes)
        return ev

    def dma(self, out, in_, sem, reads=(), writes=(), **kw):
        for b in reads:
            self.wait(b.w)
        for b in writes:
            if not (b.w is not None and b.w.sem is sem):
                self.wait(b.w)
            self.wait(*b.r.values())
        ins = self.eng.dma_start(out=out, in_=in_, **kw)
        sem.cnt += 16
        ins.then_inc(sem.h, 16)
        self.nins += 1
        ev = Ev(sem, sem.cnt)
        self.post(ev, reads, writes)
        self.K.dma_sems[sem.uid] = sem
        return ev


class Kern:
    def __init__(self):
        self.nc = bass.Bass("TRN2", target_bir_lowering=False)
        self.es = ExitStack()
        self.nsem = 0
        self.dma_sems = {}
        self.free_sems = []
        self.phase_sems = []
        nc = self.nc
        self.pe = Q(self, nc.tensor, "pe")
        self.act = Q(self, nc.scalar, "act")
        self.dve = Q(self, nc.vector, "dve")
        self.pool = Q(self, nc.gpsimd, "pool")
        self.sp = Q(self, nc.sync, "sp")
        self.queues = [self.pe, self.act, self.dve, self.pool, self.sp]

    def new_sem(self, name, persist=False):
        if not persist and self.free_sems:
            s = self.free_sems.pop()
        else:
            self.nsem += 1
            h = self.es.enter_context(self.nc.semaphore(f"{name}{self.nsem}"))
            s = Sem(h, self.nsem)
        if not persist:
            self.phase_sems.append(s)
        return s

    def end_phase(self):
        self.barrier()
        self.free_sems.extend(self.phase_sems)
        self.phase_sems = []

    def barrier(self):
        evs = []
        for q in self.queues:
            if q.sem.cnt > 0:
                evs.append(Ev(q.sem, q.sem.cnt))
        for s in self.dma_sems.values():
            if s.cnt > 0:
                evs.append(Ev(s, s.cnt))
        for q in self.queues:
            q.wait(*evs)

    def dram(self, name, shape, dt, kind="Internal"):
        return self.nc.dram_tensor(name, list(shape), dt, kind=kind)


def ffn_phase(K, src, dst, pre_w, wg, wu, wd, post_w, ident, tag="f1"):
    nc = K.nc
    pe, act, dve, pool, sp = K.pe, K.act, K.dve, K.pool, K.sp
    NG = DFF // 256
    with ExitStack() as es:
        def sb(n, s, d):
            return es.enter_context(nc.sbuf_tensor(tag + n, s, d))

        def ps(n, s, d=F32):
            return es.enter_context(nc.psum_tensor(tag + n, s, d))

        acc = sb("f_acc", [128, NT, D], F32)
        zT = sb("f_zT", [128, 16, TOK], BF16)
        xst = sb("f_xst", [128, D], F32)
        zb = sb("f_zb", [128, D], BF16)
        wbc = sb("f_wbc", [128, D], F32)
        wgb = sb("f_wgb", [128, 2, 16, 256], BF16)
        wub = sb("f_wub", [128, 2, 16, 256], BF16)
        wdb = sb("f_wdb", [128, 3, 2, D], BF16)
        aT = sb("f_aT", [128, 2, 2, TOK], BF16)
        sg = sb("f_sg", [128, 2, 512], BF16)
        dtmp = sb("f_dtmp", [128, 2, 512], F32)
        B_dtmp = [Buf(), Buf()]
        st = sb("f_st", [128, 4 * NT], F32)
        ptr = [ps(f"f_ptr{i}", [128, 1024], BF16) for i in range(2)]
        pg = [ps(f"f_pg{i}", [128, 512]) for i in range(2)]
        pu = [ps(f"f_pu{i}", [128, 512]) for i in range(2)]
        pd = [ps(f"f_pd{i}", [128, 512]) for i in range(2)]

        B_xst = Buf(K, dma=True)
        B_zb = Buf()
        B_wbc = Buf(K, dma=True)
        B_st = Buf()
        B_ptr = [Buf(), Buf()]
        B_zT = [Buf() for _ in range(NT)]
        B_wgu = [Buf(K, dma=True) for _ in range(2)]
        B_wd = [Buf(K, dma=True) for _ in range(3)]
        B_aT = [[[Buf() for _ in range(3)] for _ in range(2)] for _ in range(2)]
        B_pg = [Buf(), Buf()]
        B_pu = [Buf(), Buf()]
        B_sg = [Buf(), Buf()]
        B_pd = [Buf(), Buf()]
        B_acc = [[Buf() for _ in range(4)] for _ in range(NT)]
        B_accst = [Buf(K, dma=True) for _ in range(NT)]

        wg_v = wg.rearrange("(k p) f -> p k f", p=128)
        wu_v = wu.rearrange("(k p) f -> p k f", p=128)
        wd_v = wd.rearrange("(c p) n -> p c n", p=128)

        def load_group(gi):
            s2, s3 = gi % 2, gi % 3
            c0 = gi * 256
            pool.dma(wgb[:, s2], wg_v[:, :, c0:c0 + 256], B_wgu[s2].dsem, writes=[B_wgu[s2]])
            pool.dma(wub[:, s2], wu_v[:, :, c0:c0 + 256], B_wgu[s2].dsem, writes=[B_wgu[s2]])
            pool.dma(wdb[:, s3], wd_v[:, 2 * gi:2 * gi + 2, :], B_wd[s3].dsem, writes=[B_wd[s3]])

        sp.dma(wbc[:], pre_w.rearrange("(o d) -> o d", o=1).to_broadcast([128, D]), B_wbc.dsem, writes=[B_wbc])
        load_group(0)
        dve.op(lambda: nc.vector.memset(st[:], 0.0), writes=[B_st])

        for t in range(NT):
            sp.dma(xst[:], src[t * 128:(t + 1) * 128, :], B_xst.dsem, writes=[B_xst])
            act.op(lambda: nc.scalar.activation(out=zb[:], in_=xst[:], func=AF.Square,
                                                accum_out=st[:, t:t + 1]),
                   reads=[B_xst], writes=[B_zb, B_st])
            act.op(lambda: nc.scalar.activation(out=st[:, NT + t:NT + t + 1], in_=st[:, t:t + 1], func=AF.Sqrt,
                                                scale=1.0 / D, bias=EPS),
                   reads=[B_st], writes=[B_st])
            dve.op(lambda: nc.vector.reciprocal(out=st[:, NT + t:NT + t + 1], in_=st[:, NT + t:NT + t + 1]),
                   reads=[B_st], writes=[B_st])
            dve.op(lambda: nc.vector.scalar_tensor_tensor(out=zb[:], in0=xst[:], scalar=st[:, NT + t:NT + t + 1],
                                                          in1=wbc[:], op0=ALU.mult, op1=ALU.mult),
                   reads=[B_xst, B_st, B_wbc], writes=[B_zb])
            for q4 in range(4):
                sl = q4 % 2
                pe.pre(reads=[B_zb, K.B_ident], writes=[B_ptr[sl]])
                for i in range(4):
                    k = q4 * 4 + i
                    ins = nc.tensor.transpose(ptr[sl][:, i * 128:(i + 1) * 128], zb[:, k * 128:(k + 1) * 128], ident[:])
                    pe.tick()
                ev = pe.done(ins)
                pe.nins -= 1
                pe.post(ev, reads=[B_zb], writes=[B_ptr[sl]])
                src_ap = ptr[sl][:, 0:512].rearrange("p (a b) -> p a b", a=4)
                dst_ap = zT[:, q4 * 4:q4 * 4 + 4, t * 128:(t + 1) * 128]
                if q4 % 2 == 0:
                    act.op(lambda: nc.scalar.copy(out=dst_ap, in_=src_ap), reads=[B_ptr[sl]], writes=[B_zT[t]])
                else:
                    dve.op(lambda: nc.vector.tensor_copy(dst_ap, src_ap), reads=[B_ptr[sl]], writes=[B_zT[t]])

        sp.dma(wbc[:], post_w.rearrange("(o d) -> o d", o=1).to_broadcast([128, D]), B_wbc.dsem, writes=[B_wbc])

        tiles_of_tb = [[0, 1, 2, 3], [4, 5, 6, 7], [8]]
        down_units = []

        def emit_down(gi, t, nb, idx):
            s2, s3 = gi % 2, gi % 3
            tb = t // 4
            sl = idx % 2
            pe.pre(reads=[B_aT[s2][0][tb], B_aT[s2][1][tb], B_wd[s3]], writes=[B_pd[sl]])
            for ci in range(2):
                ins = nc.tensor.matmul(pd[sl][:], lhsT=aT[:, s2, ci, t * 128:(t + 1) * 128],
                                       rhs=wdb[:, s3, ci, nb * 512:(nb + 1) * 512],
                                       start=(ci == 0), stop=(ci == 1))
                pe.tick()
            ev = pe.done(ins)
            pe.nins -= 1
            pe.post(ev, reads=[B_aT[s2][0][tb], B_aT[s2][1][tb], B_wd[s3]], writes=[B_pd[sl]])
            a_ap = acc[:, t, nb * 512:(nb + 1) * 512]
            if gi == 0:
                dve.op(lambda: nc.vector.tensor_copy(a_ap, pd[sl][:]), reads=[B_pd[sl]], writes=[B_acc[t][nb]])
            elif idx % 4 == 3:
                x = (idx // 4) % 2
                act.op(lambda: nc.scalar.copy(out=dtmp[:, x, :], in_=pd[sl][:]), reads=[B_pd[sl]], writes=[B_dtmp[x]])
                pool.op(lambda: nc.gpsimd.tensor_tensor(out=a_ap, in0=a_ap, in1=dtmp[:, x, :], op=ALU.add),
                        reads=[B_dtmp[x], B_acc[t][nb]], writes=[B_acc[t][nb]])
            else:
                dve.op(lambda: nc.vector.tensor_tensor(out=a_ap, in0=a_ap, in1=pd[sl][:], op=ALU.add),
                       reads=[B_pd[sl], B_acc[t][nb]], writes=[B_acc[t][nb]])

        didx = 0
        for gi in range(NG):
            s2 = gi % 2
            if gi + 1 < NG:
                load_group(gi + 1)
            step = 0
            for ci in range(2):
                for tbi, (t0, tn) in enumerate(TB):
                    sl = step % 2
                    zdeps = [B_zT[t] for t in tiles_of_tb[tbi]]
                    for (pp, Bp, wb) in ((pg, B_pg, wgb), (pu, B_pu, wub)):
                        pe.pre(reads=zdeps + [B_wgu[s2]], writes=[Bp[sl]])
                        for k in range(16):
                            ins = nc.tensor.matmul(pp[sl][:, 0:tn], lhsT=wb[:, s2, k, ci * 128:(ci + 1) * 128],
                                                   rhs=zT[:, k, t0:t0 + tn], start=(k == 0), stop=(k == 15))
                            pe.tick()
                        ev = pe.done(ins)
                        pe.nins -= 1
                        pe.post(ev, reads=zdeps + [B_wgu[s2]], writes=[Bp[sl]])
                    act.op(lambda: nc.scalar.activation(out=sg[:, sl, 0:tn], in_=pg[sl][:, 0:tn], func=AF.Silu),
                           reads=[B_pg[sl]], writes=[B_sg[sl]])
                    dve.op(lambda: nc.vector.tensor_tensor(out=aT[:, s2, ci, t0:t0 + tn], in0=sg[:, sl, 0:tn],
                                                           in1=pu[sl][:, 0:tn], op=ALU.mult),
                           reads=[B_sg[sl], B_pu[sl]], writes=[B_aT[s2][ci][tbi]])
                    step += 1
                    for _ in range(6):
                        if down_units:
                            g0, t, nb = down_units.pop(0)
                            emit_down(g0, t, nb, didx)
                            didx += 1
            down_units = [(gi, t, nb) for t in range(NT) for nb in range(4)]
        while down_units:
            g0, t, nb = down_units.pop(0)
            emit_down(g0, t, nb, didx)
            didx += 1

        for t in range(NT):
            sp.dma(xst[:], src[t * 128:(t + 1) * 128, :], B_xst.dsem, writes=[B_xst])
            act.op(lambda: nc.scalar.activation(out=zb[:], in_=acc[:, t, :], func=AF.Square,
                                                accum_out=st[:, 2 * NT + t:2 * NT + t + 1]),
                   reads=B_acc[t], writes=[B_zb, B_st])
            c = 3 * NT + t
            act.op(lambda: nc.scalar.activation(out=st[:, c:c + 1], in_=st[:, 2 * NT + t:2 * NT + t + 1], func=AF.Sqrt,
                                                scale=4.0 / D, bias=4.0 * EPS),
                   reads=[B_st], writes=[B_st])
            dve.op(lambda: nc.vector.reciprocal(out=st[:, c:c + 1], in_=st[:, c:c + 1]),
                   reads=[B_st], writes=[B_st])
            dve.op(lambda: nc.vector.scalar_tensor_tensor(out=acc[:, t, :], in0=acc[:, t, :], scalar=st[:, c:c + 1],
                                                          in1=wbc[:], op0=ALU.mult, op1=ALU.mult),
                   reads=[B_st, B_wbc] + B_acc[t], writes=B_acc[t])
            dve.op(lambda: nc.vector.tensor_tensor(out=acc[:, t, :], in0=acc[:, t, :], in1=xst[:], op=ALU.add),
                   reads=[B_xst] + B_acc[t], writes=B_acc[t])
            sp.dma(dst[t * 128:(t + 1) * 128, :], acc[:, t, :], B_accst[t].dsem, reads=B_acc[t])
        K.end_phase()


import math
LN_THETA = math.log(10000.0)
PI = math.pi
CQ, CK, CV, CQI, CKI, CWI, CGQ, CGK, CGV, CGLR, CGR, CGA, CGG = (
    0, 1024, 1280, 1536, 2560, 2624, 2640, 3152, 3664, 4688, 4704, 5728, 7776)
AG1_ROWS = 576
SQ = 1.0 / math.sqrt(128.0)


def all_gather(K, src, dst, rbufs, wbufs):
    pool = K.pool
    pool.pre(reads=rbufs, writes=wbufs)
    ins = K.nc.gpsimd.collective_compute("AllGather", ALU.bypass, replica_groups=[[0, 1, 2, 3], [4, 5, 6, 7]],
                                         ins=[src.opt()], outs=[dst.opt()])
    csem = K.new_sem("cc", True)
    ins.then_inc(csem.h)
    csem.cnt += 1
    pool.nins += 1
    ev = Ev(csem, 1)
    for b in rbufs:
        b.r[csem.uid] = ev
    for b in wbufs:
        b.r[csem.uid] = ev
    return ev


def norm_T(K, src, wvec, zT, B_zT, ident, ptr, B_ptr, tag):
    nc = K.nc
    pe, act, dve, pool, sp = K.pe, K.act, K.dve, K.pool, K.sp
    with ExitStack() as es:
        def sb(n, s, d):
            return es.enter_context(nc.sbuf_tensor(tag + n, s, d))
        xst = sb("xst", [128, D], F32)
        zb = sb("zb", [128, D], BF16)
        wbc = sb("wbc", [128, D], F32)
        st = sb("st", [128, 2 * NT], F32)
        B_xst = Buf(K, dma=True)
        B_zb = Buf()
        B_wbc = Buf(K, dma=True)
        B_st = Buf()
        sp.dma(wbc[:], wvec.rearrange("(o d) -> o d", o=1).to_broadcast([128, D]), B_wbc.dsem, writes=[B_wbc])
        dve.op(lambda: nc.vector.memset(st[:], 0.0), writes=[B_st])
        for t in range(NT):
            sp.dma(xst[:], src[t * 128:(t + 1) * 128, :], B_xst.dsem, writes=[B_xst])
            act.op(lambda: nc.scalar.activation(out=zb[:], in_=xst[:], func=AF.Square,
                                                accum_out=st[:, t:t + 1]),
                   reads=[B_xst], writes=[B_zb, B_st])
            act.op(lambda: nc.scalar.activation(out=st[:, NT + t:NT + t + 1], in_=st[:, t:t + 1], func=AF.Sqrt,
                                                scale=1.0 / D, bias=EPS),
                   reads=[B_st], writes=[B_st])
            dve.op(lambda: nc.vector.reciprocal(out=st[:, NT + t:NT + t + 1], in_=st[:, NT + t:NT + t + 1]),
                   reads=[B_st], writes=[B_st])
            dve.op(lambda: nc.vector.scalar_tensor_tensor(out=zb[:], in0=xst[:], scalar=st[:, NT + t:NT + t + 1],
                                                          in1=wbc[:], op0=ALU.mult, op1=ALU.mult),
                   reads=[B_xst, B_st, B_wbc], writes=[B_zb])
            for q4 in range(4):
                sl = q4 % 2
                pe.pre(reads=[B_zb, K.B_ident], writes=[B_ptr[sl]])
                for i in range(4):
                    k = q4 * 4 + i
                    ins = nc.tensor.transpose(ptr[sl][:, i * 128:(i + 1) * 128], zb[:, k * 128:(k + 1) * 128], ident[:])
                ev = pe.done(ins)
                pe.post(ev, reads=[B_zb], writes=[B_ptr[sl]])
                src_ap = ptr[sl][:, 0:512].rearrange("p (a b) -> p a b", a=4)
                dst_ap = zT[:, q4 * 4:q4 * 4 + 4, t * 128:(t + 1) * 128]
                if q4 % 2 == 0:
                    act.op(lambda: nc.scalar.copy(out=dst_ap, in_=src_ap), reads=[B_ptr[sl]], writes=[B_zT[t]])
                else:
                    dve.op(lambda: nc.vector.tensor_copy(dst_ap, src_ap), reads=[B_ptr[sl]], writes=[B_zT[t]])
        K.end_phase()


def phase_a(K, C):
    nc = K.nc
    pe, act, dve, pool, sp = K.pe, K.act, K.dve, K.pool, K.sp
    P = C["P"]
    ident = C["ident"]
    w_in = C["w_in"]
    win_v = w_in.rearrange("(k p) f -> p k f", p=128)
    ag_kT = C["agk_in"]
    ag_kiT = C["agki_in"]
    ag_v = C["agv_in"]
    with ExitStack() as es:
        def sb(n, s, d):
            return es.enter_context(nc.sbuf_tensor("a_" + n, s, d))

        def ps(n, s, d=F32):
            return es.enter_context(nc.psum_tensor("a_" + n, s, d))

        uT = sb("uT", [128, 16, TOK], BF16)
        B_uT = [Buf() for _ in range(NT)]
        ptr = [ps(f"ptr{i}", [128, 1024], BF16) for i in range(2)]
        B_ptr = [Buf(), Buf()]
        pp = [ps(f"pp{i}", [128, 512]) for i in range(2)]
        B_pp = [Buf(), Buf()]
        px = [ps(f"px{i}", [128, 512]) for i in range(2)]
        B_px = [Buf(), Buf()]
        norm_T(K, C["h1"], C["mix_pre_w"], uT, B_uT, ident, ptr, B_ptr, "a1_")

        tabs = sb("tabs", [128, NT, 384], F32)
        es_t = ExitStack()

        def sbt(n, s, d):
            return es_t.enter_context(nc.sbuf_tensor("a_" + n, s, d))
        posv = sbt("posv", [128, NT], F32)
        io = sbt("io", [128, 64], F32)
        inv = sbt("inv", [128, 96], F32)
        ang = sbt("ang", [128, NT, 96], F32)
        kf = sbt("kf", [128, NT, 96], F32)
        kint = sbt("kint", [128, NT, 96], I32)
        sn = sbt("sn", [128, NT, 96], F32)
        cs = sbt("cs", [128, NT, 96], F32)
        B_t = Buf(K, dma=True)
        sp.dma(posv[:], C["posv"], B_t.dsem, writes=[B_t])
        pool.op(lambda: nc.gpsimd.iota(io[:], pattern=[[1, 64]], base=0, channel_multiplier=0,
                                       allow_small_or_imprecise_dtypes=True), writes=[B_t])
        act.op(lambda: nc.scalar.activation(out=inv[:, 0:64], in_=io[:, 0:64], func=AF.Exp,
                                            scale=-2.0 * LN_THETA / 128.0), reads=[B_t], writes=[B_t])
        act.op(lambda: nc.scalar.activation(out=inv[:, 64:96], in_=io[:, 0:32], func=AF.Exp,
                                            scale=-2.0 * LN_THETA / 64.0), reads=[B_t], writes=[B_t])
        for s in range(NT):
            dve.op(lambda: nc.vector.tensor_scalar(out=ang[:, s, :], in0=inv[:], scalar1=posv[:, s:s + 1],
                                                   scalar2=None, op0=ALU.mult), reads=[B_t], writes=[B_t])

        def V(fn):
            return dve.op(fn, reads=[B_t], writes=[B_t])
        V(lambda: nc.vector.tensor_scalar(out=kf[:], in0=ang[:], scalar1=1.0 / (2 * PI), scalar2=None, op0=ALU.mult))
        V(lambda: nc.vector.tensor_copy(kint[:], kf[:]))
        V(lambda: nc.vector.tensor_copy(kf[:], kint[:]))
        V(lambda: nc.vector.scalar_tensor_tensor(out=ang[:], in0=kf[:], scalar=-2 * PI, in1=ang[:],
                                                 op0=ALU.mult, op1=ALU.add))
        act.op(lambda: nc.scalar.activation(out=sn[:], in_=ang[:], func=AF.Sin), reads=[B_t], writes=[B_t])
        V(lambda: nc.vector.tensor_scalar(out=ang[:], in0=ang[:], scalar1=PI / 2, scalar2=None, op0=ALU.add))
        V(lambda: nc.vector.tensor_scalar(out=kf[:], in0=ang[:], scalar1=PI, scalar2=-2 * PI,
                                          op0=ALU.is_gt, op1=ALU.mult))
        V(lambda: nc.vector.tensor_tensor(out=ang[:], in0=ang[:], in1=kf[:], op=ALU.add))
        act.op(lambda: nc.scalar.activation(out=cs[:], in_=ang[:], func=AF.Sin), reads=[B_t], writes=[B_t])
        V(lambda: nc.vector.tensor_copy(tabs[:, :, 0:64], cs[:, :, 0:64]))
        V(lambda: nc.vector.tensor_copy(tabs[:, :, 64:128], cs[:, :, 0:64]))
        V(lambda: nc.vector.tensor_scalar(out=tabs[:, :, 128:192], in0=sn[:, :, 0:64], scalar1=-1.0, scalar2=None,
                                          op0=ALU.mult))
        V(lambda: nc.vector.tensor_copy(tabs[:, :, 192:256], sn[:, :, 0:64]))
        V(lambda: nc.vector.tensor_copy(tabs[:, :, 256:288], cs[:, :, 64:96]))
        V(lambda: nc.vector.tensor_copy(tabs[:, :, 288:320], cs[:, :, 64:96]))
        V(lambda: nc.vector.tensor_scalar(out=tabs[:, :, 320:352], in0=sn[:, :, 64:96], scalar1=-1.0, scalar2=None,
                                          op0=ALU.mult))
        V(lambda: nc.vector.tensor_copy(tabs[:, :, 352:384], sn[:, :, 64:96]))
        B_tabs = B_t
        K.barrier()
        es_t.close()

        wbuf = sb("wbuf", [128, 2, 16, 512], BF16)
        B_w = [Buf(K, dma=True), Buf(K, dma=True)]
        xs = sb("xs", [128, 2, 512], F32)
        B_xs = [Buf(K, dma=True), Buf(K, dma=True)]
        t1 = sb("t1", [128, 2, 512], F32)
        B_t1 = [Buf(), Buf()]
        t2 = sb("t2", [128, 2, 512], F32)
        B_t2 = [Buf(), Buf()]
        ob = sb("ob", [128, 2, 512], BF16)
        B_ob = [Buf(K, dma=True), Buf(K, dma=True)]
        of = sb("of", [128, 2, 320], F32)
        B_of = [Buf(K, dma=True), Buf(K, dma=True)]
        tst = sb("tst", [128, 2, 256], BF16)
        B_tst = [Buf(K, dma=True), Buf(K, dma=True)]
        glrT = sb("glrT", [32, TOK], BF16)
        B_glr = Buf()
        w2b = sb("w2b", [16, 512], BF16)
        negb = sb("negb", [128, 4], F32)
        B_c = Buf(K, dma=True)
        ones = sb("ones", [128, 128], F32)
        eT = sb("eT", [128, 512], F32)
        lT = sb("lT", [128, 512], F32)
        cT = eT
        B_e = Buf()
        B_l = Buf()
        B_cT = B_e
        E1 = sb("E1", [128, 512], F32)
        E2 = sb("E2", [128, 512], F32)
        B_E = [Buf()] * 3
        khT = sb("khT", [128, 512], BF16)
        B_khT = Buf()
        cnt = {"blk": 0, "pp": 0, "xs": 0, "tr": 0, "of": 0, "tst": 0, "px": 0}

        pool.dma(w2b[:], C["gla_gate_w2"], B_c.dsem, writes=[B_c])
        sp.dma(negb[:], C["gla_gate_b"].rearrange("(h p) -> p h", p=128), B_c.dsem, writes=[B_c],
               allow_slow_non_contiguous=True)
        dve.op(lambda: nc.vector.tensor_scalar(out=negb[:], in0=negb[:], scalar1=-1.0, scalar2=None, op0=ALU.mult),
               reads=[B_c], writes=[B_c])
        dve.op(lambda: nc.vector.memset(ones[:], 1.0), writes=[B_c])

        def load_w(pieces):
            slot = cnt["blk"] % 2
            cnt["blk"] += 1
            off = 0
            for (c0, w) in pieces:
                pool.dma(wbuf[:, slot, :, off:off + w], win_v[:, :, c0:c0 + w], B_w[slot].dsem, writes=[B_w[slot]])
                off += w
            return slot

        def mm_tok(slot, t, width):
            i = cnt["pp"] % 2
            cnt["pp"] += 1
            pe.pre(reads=[B_uT[t], B_w[slot]], writes=[B_pp[i]])
            for k in range(16):
                ins = nc.tensor.matmul(pp[i][:, 0:width], lhsT=uT[:, k, t * 128:(t + 1) * 128],
                                       rhs=wbuf[:, slot, k, 0:width], start=(k == 0), stop=(k == 15))
            ev = pe.done(ins)
            pe.post(ev, reads=[B_uT[t], B_w[slot]], writes=[B_pp[i]])
            return i

        def mm_feat(slot, off, m, tbi):
            t0, tn = TB[tbi]
            i = cnt["pp"] % 2
            cnt["pp"] += 1
            deps = [B_uT[t] for t in ([0, 1, 2, 3], [4, 5, 6, 7], [8])[tbi]]
            pe.pre(reads=deps + [B_w[slot]], writes=[B_pp[i]])
            for k in range(16):
                ins = nc.tensor.matmul(pp[i][0:m, 0:tn], lhsT=wbuf[:, slot, k, off:off + m],
                                       rhs=uT[:, k, t0:t0 + tn], start=(k == 0), stop=(k == 15))
            ev = pe.done(ins)
            pe.post(ev, reads=deps + [B_w[slot]], writes=[B_pp[i]])
            return i

        def evac(i, width):
            j = cnt["xs"] % 2
            cnt["xs"] += 1
            act.op(lambda: nc.scalar.copy(out=xs[:, j, 0:width], in_=pp[i][:, 0:width]),
                   reads=[B_pp[i]], writes=[B_xs[j]])
            return j

        def rope(j, c0, hs, nh, t, out_ap, B_out):
            w = nh * hs
            hh = hs // 2
            tb0 = 0 if hs == 128 else 256
            x3 = xs[:, j, c0:c0 + w].rearrange("p (h d) -> p h d", h=nh)
            cosb = tabs[:, t, tb0:tb0 + hs].unsqueeze(1).to_broadcast([128, nh, hs])
            sa = tabs[:, t, tb0 + hs:tb0 + hs + hh].unsqueeze(1).to_broadcast([128, nh, hh])
            sbb = tabs[:, t, tb0 + hs + hh:tb0 + 2 * hs].unsqueeze(1).to_broadcast([128, nh, hh])
            a3 = t1[:, j, 0:w].rearrange("p (h d) -> p h d", h=nh)
            b3 = t2[:, j, 0:w].rearrange("p (h d) -> p h d", h=nh)
            pool.op(lambda: nc.gpsimd.tensor_tensor(out=a3, in0=x3, in1=cosb, op=ALU.mult),
                    reads=[B_xs[j], B_tabs], writes=[B_t1[j]])
            dve.op(lambda: nc.vector.tensor_tensor(out=b3[:, :, 0:hh], in0=x3[:, :, hh:hs], in1=sa, op=ALU.mult),
                   reads=[B_xs[j], B_tabs], writes=[B_t2[j]])
            dve.op(lambda: nc.vector.tensor_tensor(out=b3[:, :, hh:hs], in0=x3[:, :, 0:hh], in1=sbb, op=ALU.mult),
                   reads=[B_xs[j], B_tabs], writes=[B_t2[j]])
            dve.op(lambda: nc.vector.tensor_tensor(out=out_ap, in0=t1[:, j, 0:w], in1=t2[:, j, 0:w], op=ALU.add),
                   reads=[B_t1[j], B_t2[j]], writes=[B_out])

        def transposes(j, nblk, rows, dst_fn, B_dst_fn):
            sl = cnt["tr"] % 2
            cnt["tr"] += 1
            pe.pre(reads=[B_ob[j], K.B_ident], writes=[B_ptr[sl]])
            for b in range(nblk):
                ins = nc.tensor.transpose(ptr[sl][0:rows, b * 128:(b + 1) * 128],
                                          ob[:, j, b * rows:(b + 1) * rows], ident[:])
            ev = pe.done(ins)
            pe.post(ev, reads=[B_ob[j]], writes=[B_ptr[sl]])
            return sl

        def pipelined(front, back, n=NT):
            st_ = {}
            st_[0] = front(0)
            for t in range(n):
                if t + 1 < n:
                    st_[t + 1] = front(t + 1)
                back(t, st_[t])

        for blk in range(2):
            slot = load_w([(CQ + blk * 512, 512)])

            def q_front(t, slot=slot):
                i = mm_tok(slot, t, 512)
                j = evac(i, 512)
                rope(j, 0, 128, 4, t, ob[:, j, :], B_ob[j])
                return j

            def q_back(t, j, blk=blk):
                sl = transposes(j, 4, 128, None, None)
                act.op(lambda: nc.scalar.copy(out=P["qT"][:, blk * 4:blk * 4 + 4, t * 128:(t + 1) * 128],
                                              in_=ptr[sl][:, 0:512].rearrange("p (a b) -> p a b", a=4)),
                       reads=[B_ptr[sl]], writes=[C["B_qT"]])
            pipelined(q_front, q_back)

        slot = load_w([(CK, 512)])

        def kv_front(t, slot=slot):
            i = mm_tok(slot, t, 512)
            j = evac(i, 512)
            o = cnt["of"] % 2
            cnt["of"] += 1
            rope(j, 0, 128, 2, t, of[:, o, 0:256], B_of[o])
            sp.dma(C["ko"][t * 128:(t + 1) * 128, :], of[:, o, 0:256], B_of[o].dsem, reads=[B_of[o]])
            sp.dma(C["vo"][t * 128:(t + 1) * 128, :], xs[:, j, 256:512], B_xs[j].dsem, reads=[B_xs[j]])
            pool.op(lambda: nc.gpsimd.tensor_copy(ob[:, j, 0:256], of[:, o, 0:256]), reads=[B_of[o]], writes=[B_ob[j]])
            pool.op(lambda: nc.gpsimd.tensor_copy(ob[:, j, 256:512], xs[:, j, 256:512]), reads=[B_xs[j]],
                    writes=[B_ob[j]])
            return j

        def kv_back(t, j):
            sl = transposes(j, 2, 128, None, None)
            if t < 8:
                q = cnt["tst"] % 2
                cnt["tst"] += 1
                act.op(lambda: nc.scalar.copy(out=tst[:, q, :], in_=ptr[sl][:, 0:256]), reads=[B_ptr[sl]],
                       writes=[B_tst[q]])
                for g in range(2):
                    sp.dma(ag_kT[g * 128:(g + 1) * 128, t * 64:(t + 1) * 64],
                           tst[:, q, g * 128:(g + 1) * 128].bitcast(F32),
                           B_tst[q].dsem, reads=[B_tst[q]], writes=[C["B_ag1"]])
                sp.dma(ag_v[t * 128:(t + 1) * 128, :], ob[:, j, 256:512].bitcast(F32), B_ob[j].dsem,
                       reads=[B_ob[j]], writes=[C["B_ag1"]])
            else:
                act.op(lambda: nc.scalar.copy(out=P["kT8"][:, :], in_=ptr[sl][:, 0:256]), reads=[B_ptr[sl]],
                       writes=[C["B_s8"]])
                pool.op(lambda: nc.gpsimd.tensor_copy(P["v8"][:, :], ob[:, j, 256:512]), reads=[B_ob[j]],
                        writes=[C["B_s8"]])
        pipelined(kv_front, kv_back)

        for blk in range(2):
            slot = load_w([(CQI + blk * 512, 512)])

            def qi_front(t, slot=slot):
                i = mm_tok(slot, t, 512)
                j = evac(i, 512)
                rope(j, 0, 64, 8, t, ob[:, j, :], B_ob[j])
                return j

            def qi_back(t, j, blk=blk):
                sl = transposes(j, 4, 128, None, None)
                act.op(lambda: nc.scalar.copy(out=P["qiT"][:, blk * 4:blk * 4 + 4, t * 128:(t + 1) * 128],
                                              in_=ptr[sl][:, 0:512].rearrange("p (a b) -> p a b", a=4)),
                       reads=[B_ptr[sl]], writes=[C["B_qiT"]])
            pipelined(qi_front, qi_back)

        slot = load_w([(CKI, 80)])

        def ki_front(t, slot=slot):
            i = mm_tok(slot, t, 80)
            j = evac(i, 80)
            o = cnt["of"] % 2
            cnt["of"] += 1
            rope(j, 0, 64, 1, t, of[:, o, 256:320], B_of[o])
            sp.dma(C["kio"][t * 128:(t + 1) * 128, :], of[:, o, 256:320], B_of[o].dsem, reads=[B_of[o]])
            dve.op(lambda: nc.vector.tensor_scalar(out=P["wi"][:, t, :], in0=xs[:, j, 64:80], scalar1=1.0 / 32.0,
                                                   scalar2=None, op0=ALU.mult), reads=[B_xs[j]], writes=[C["B_wi"]])
            pool.op(lambda: nc.gpsimd.tensor_copy(ob[:, j, 0:64], of[:, o, 256:320]), reads=[B_of[o]], writes=[B_ob[j]])
            pool.op(lambda: nc.gpsimd.tensor_copy(ob[:, j, 64:128], of[:, o, 256:320]), reads=[B_of[o]],
                    writes=[B_ob[j]])
            return j

        def ki_back(t, j):
            sl = transposes(j, 1, 128, None, None)
            if t < 8:
                q = cnt["tst"] % 2
                cnt["tst"] += 1
                act.op(lambda: nc.scalar.copy(out=tst[0:64, q, 0:128], in_=ptr[sl][0:64, 0:128]), reads=[B_ptr[sl]],
                       writes=[B_tst[q]])
                sp.dma(ag_kiT[:, t * 64:(t + 1) * 64], tst[0:64, q, 0:128].bitcast(F32), B_tst[q].dsem,
                       reads=[B_tst[q]], writes=[C["B_ag1"]])
            else:
                act.op(lambda: nc.scalar.copy(out=P["kiT8"][:, :], in_=ptr[sl][:, 0:128]), reads=[B_ptr[sl]],
                       writes=[C["B_s8"]])
        pipelined(ki_front, ki_back)

        for nm in ("agk", "agki", "agv"):
            all_gather(K, C[nm + "_in"], C[nm + "_out"], [C["B_ag1"]], [C["B_ag1o"]])

        for blk in range(2):
            slot = load_w([(CGV + blk * 512, 512)])
            for t in range(NT):
                i = mm_tok(slot, t, 512)
                act.op(lambda: nc.scalar.copy(out=P["gv"][:, t, blk * 512:(blk + 1) * 512], in_=pp[i][:, :]),
                       reads=[B_pp[i]], writes=[C["B_gv"]])

        slot = load_w([(CGLR, 16)])
        for tbi in range(3):
            t0, tn = TB[tbi]
            i = mm_feat(slot, 0, 16, tbi)
            act.op(lambda: nc.scalar.copy(out=glrT[0:16, t0:t0 + tn], in_=pp[i][0:16, 0:tn]), reads=[B_pp[i]],
                   writes=[B_glr])

        for h in range(4):
            slot = load_w([(CGQ + h * 128, 128), (CGK + h * 128, 128)])
            for tbi in range(3):
                t0, tn = TB[tbi]
                x = cnt["px"] % 2
                cnt["px"] += 1
                pe.op(lambda: nc.tensor.matmul(px[x][:, 0:tn], lhsT=w2b[:, h * 128:(h + 1) * 128],
                                               rhs=glrT[0:16, t0:t0 + tn], start=True, stop=True),
                      reads=[B_glr, B_c], writes=[B_px[x]])
                act.op(lambda: nc.scalar.activation(out=eT[:, 0:tn], in_=px[x][:, 0:tn], func=AF.Exp, scale=-1.0,
                                                    bias=negb[:, h:h + 1]), reads=[B_px[x], B_c], writes=[B_e])
                act.op(lambda: nc.scalar.activation(out=lT[:, 0:tn], in_=eT[:, 0:tn], func=AF.Ln, bias=1.0),
                       reads=[B_e], writes=[B_l])
                if tbi < 2:
                    for q4 in range(4):
                        dve.op(lambda: nc.vector.tensor_tensor_scan(out=cT[:, q4 * 128:(q4 + 1) * 128], data0=ones[:],
                                                                    data1=lT[:, q4 * 128:(q4 + 1) * 128], initial=0.0,
                                                                    op0=ALU.mult, op1=ALU.add),
                               reads=[B_l, B_c], writes=[B_cT])
                    act.op(lambda: nc.scalar.activation(out=E1[:, 0:tn], in_=cT[:, 0:tn], func=AF.Exp,
                                                        scale=-1.0 / 16.0), reads=[B_cT], writes=[B_E[tbi]])
                    act.op(lambda: nc.scalar.activation(out=E2[:, 0:tn], in_=cT[:, 0:tn], func=AF.Exp,
                                                        scale=1.0 / 16.0), reads=[B_cT], writes=[B_E[tbi]])
                    dve.op(lambda: nc.vector.tensor_copy(
                        P["dec"][:, h, tbi * 4:tbi * 4 + 4],
                        E1[:, 0:tn].rearrange("p (a b) -> p a b", a=4)[:, :, 127]),
                        reads=[B_E[tbi]], writes=[C["B_dec"]])
                else:
                    act.op(lambda: nc.scalar.activation(out=E1[:, 0:tn], in_=lT[:, 0:tn], func=AF.Exp,
                                                        scale=-1.0 / 16.0), reads=[B_l], writes=[B_E[tbi]])
                    dve.op(lambda: nc.vector.tensor_copy(P["dec8"][:, h, :], E1[:, 0:tn]),
                           reads=[B_E[tbi]], writes=[C["B_dec"]])
                i = mm_feat(slot, 0, 128, tbi)
                if tbi < 2:
                    dve.op(lambda: nc.vector.scalar_tensor_tensor(out=P["qgT"][:, h, t0:t0 + tn], in0=pp[i][:, 0:tn],
                                                                  scalar=SQ, in1=E1[:, 0:tn],
                                                                  op0=ALU.mult, op1=ALU.mult),
                           reads=[B_pp[i], B_E[tbi]], writes=[C["B_qgT"]])
                else:
                    dve.op(lambda: nc.vector.tensor_scalar(out=P["qgT"][:, h, t0:t0 + tn], in0=pp[i][:, 0:tn],
                                                           scalar1=SQ, scalar2=None, op0=ALU.mult),
                           reads=[B_pp[i]], writes=[C["B_qgT"]])
                i = mm_feat(slot, 128, 128, tbi)
                if tbi < 2:
                    dve.op(lambda: nc.vector.tensor_tensor(out=P["kgT"][:, h, t0:t0 + tn], in0=pp[i][:, 0:tn],
                                                           in1=E2[:, 0:tn], op=ALU.mult),
                           reads=[B_pp[i], B_E[tbi]], writes=[C["B_kgT"]])
                    dve.op(lambda: nc.vector.tensor_tensor(
                        out=khT[:, :].rearrange("p (a b) -> p a b", a=4),
                        in0=P["kgT"][:, h, t0:t0 + tn].rearrange("p (a b) -> p a b", a=4),
                        in1=P["dec"][:, h, tbi * 4:tbi * 4 + 4].unsqueeze(2).to_broadcast([128, 4, 128]),
                        op=ALU.mult), reads=[C["B_kgT"], C["B_dec"]], writes=[B_khT])
                    sl = cnt["tr"] % 2
                    cnt["tr"] += 1
                    pe.pre(reads=[B_khT, K.B_ident], writes=[B_ptr[sl]])
                    for b in range(4):
                        ins = nc.tensor.transpose(ptr[sl][:, b * 128:(b + 1) * 128], khT[:, b * 128:(b + 1) * 128],
                                                  ident[:])
                    ev = pe.done(ins)
                    pe.post(ev, reads=[B_khT], writes=[B_ptr[sl]])
                    act.op(lambda: nc.scalar.copy(out=P["khat"][:, tbi * 4:tbi * 4 + 4, h * 128:(h + 1) * 128],
                                                  in_=ptr[sl][:, 0:512].rearrange("p (a b) -> p a b", a=4)),
                           reads=[B_ptr[sl]], writes=[C["B_khat"]])
                else:
                    act.op(lambda: nc.scalar.copy(out=P["kgT"][:, h, t0:t0 + tn], in_=pp[i][:, 0:tn]),
                           reads=[B_pp[i]], writes=[C["B_kgT"]])
                    dve.op(lambda: nc.vector.tensor_copy(P["kg8f"][:, h, :], pp[i][:, 0:tn]),
                           reads=[B_pp[i]], writes=[C["B_kgT"]])
        K.end_phase()
NIT = 14
TOPK = 256
NEG = -1.0e4


def topk_threshold(K, S3, np_, junk3, st, B_S, B_junk, B_st, pw2, B_c):
    nc = K.nc
    dve = K.dve

    def V(fn, r=(), w=()):
        return dve.op(fn, reads=list(r), writes=list(w))
    V(lambda: nc.vector.tensor_scalar(out=st[0:np_, 8:8 + NIT], in0=pw2[0:np_, 0:NIT], scalar1=st[0:np_, 0:1],
                                      scalar2=None, op0=ALU.mult), r=[B_st, B_c], w=[B_st])
    V(lambda: nc.vector.tensor_scalar(out=st[0:np_, 1:2], in0=st[0:np_, 0:1], scalar1=-1.0, scalar2=None,
                                      op0=ALU.mult), r=[B_st], w=[B_st])
    V(lambda: nc.vector.memset(st[0:np_, 40:40 + NIT], 0.0), r=[B_st], w=[B_st])
    for k in range(NIT):
        V(lambda: nc.vector.tensor_tensor(out=st[0:np_, 2:3], in0=st[0:np_, 1:2], in1=st[0:np_, 8 + k:9 + k],
                                          op=ALU.add), r=[B_st], w=[B_st])
        V(lambda: nc.vector.tensor_scalar(out=junk3, in0=S3, scalar1=st[0:np_, 2:3], scalar2=0.0, op0=ALU.is_ge,
                                          op1=ALU.add, accum_out=st[0:np_, 40 + k:41 + k]),
          r=[B_S, B_st], w=[B_junk, B_st])
        V(lambda: nc.vector.tensor_scalar(out=st[0:np_, 4:5], in0=st[0:np_, 40 + k:41 + k], scalar1=TOPK - 0.5,
                                          scalar2=None, op0=ALU.is_ge), r=[B_st], w=[B_st])
        V(lambda: nc.vector.scalar_tensor_tensor(out=st[0:np_, 1:2], in0=st[0:np_, 4:5], scalar=st[0:np_, 8 + k:9 + k],
                                                 in1=st[0:np_, 1:2], op0=ALU.mult, op1=ALU.add), r=[B_st], w=[B_st])


def phase_b(K, C):
    nc = K.nc
    pe, act, dve, pool, sp = K.pe, K.act, K.dve, K.pool, K.sp
    P = C["P"]
    ident = C["ident"]
    with ExitStack() as es:
        def sb(n, s, d):
            return es.enter_context(nc.sbuf_tensor("b_" + n, s, d))

        def ps(n, s, d=F32):
            return es.enter_context(nc.psum_tensor("b_" + n, s, d))

        kT_all = sb("kT", [128, 2, 4, 1024], BF16)
        kiT2 = sb("kiT2", [128, 4, 1024], BF16)
        V1 = sb("V1", [128, 4, 8, 2, 130], BF16)
        S2 = sb("S", [128, 2, 4096], F32)
        selm = sb("selm", [128, 4096], BF16)
        selT = sb("selT", [128, 4, 8, 128], BF16)
        diagw = sb("diagw", [128, 16, 128], BF16)
        rh = sb("rh", [128, 3, 512], BF16)
        pex = sb("pex", [128, 3, 512], BF16)
        pm = sb("pm", [128, 3, 512], BF16)
        cmask = sb("cmask", [128, 512], F32)
        pw2 = sb("pw2", [128, 32], F32)
        st = sb("st", [128, 64], F32)
        rec = sb("rec", [128, 8], F32)
        psh = [ps(f"psh{i}", [128, 512]) for i in range(3)]
        psc = [ps(f"psc{i}", [128, 512]) for i in range(2)]
        ptr = [ps(f"ptr{i}", [128, 1024], BF16) for i in range(2)]
        B_kv = Buf(K, dma=True)
        B_S2, B_selm, B_selT, B_diagw, B_st, B_c, B_rec = [Buf(), Buf()], Buf(), Buf(), Buf(), Buf(), Buf(K, dma=True), Buf()
        B_rh = [Buf(), Buf(), Buf()]
        B_pex = [Buf(), Buf(), Buf()]
        B_pm = [Buf(), Buf(), Buf()]
        B_psh = [Buf(), Buf(), Buf()]
        B_psc = [Buf(), Buf()]
        B_ptr = [Buf(), Buf()]
        cnt = {"h": 0, "c": 0, "r": 0, "e": 0, "t": 0}

        agk, agki, agv = C["agk_out"], C["agki_out"], C["agv_out"]
        for r in range(4):
            for g in range(2):
                sp.dma(kT_all[:, g, r, :].bitcast(F32), agk[r * 256 + g * 128:r * 256 + (g + 1) * 128, :], B_kv.dsem,
                       reads=[C["B_ag1o"]], writes=[B_kv])
            for hf in range(2):
                sp.dma(kiT2[hf * 64:(hf + 1) * 64, r, :].bitcast(F32), agki[r * 64:(r + 1) * 64, :], B_kv.dsem,
                       reads=[C["B_ag1o"]], writes=[B_kv])
            for g in range(2):
                sp.dma(V1[:, r, :, g, 0:128],
                       agv.bitcast(BF16)[r * 1024:(r + 1) * 1024, g * 128:(g + 1) * 128].rearrange(
                           "(s p) c -> p s c", p=128),
                       B_kv.dsem, reads=[C["B_ag1o"]], writes=[B_kv])
        pool.op(lambda: nc.gpsimd.memset(V1[:, :, :, :, 128:130], 1.0), writes=[B_kv])
        sp.dma(cmask[:], C["cmask"], B_c.dsem, writes=[B_c])
        for k in range(NIT):
            dve.op(lambda: nc.vector.memset(pw2[:, k:k + 1], 2.0 ** (-k)), writes=[B_c])

        def scores(s):
            S = S2[:, s % 2, :]
            B_S = B_S2[s % 2]
            Lr = (s + 1) * 128
            qs = slice(s * 128, (s + 1) * 128)
            for h in range(16):
                dve.op(lambda: nc.vector.tensor_scalar(out=diagw[:, h, :], in0=ident[:], scalar1=P["wi"][:, s, h:h + 1],
                                                       scalar2=None, op0=ALU.mult),
                       reads=[C["B_wi"], K.B_ident], writes=[B_diagw])
            LA = 2
            units = [(r, c0, min(512, Lr - c0), h) for r in range(4) for c0 in range(0, Lr, 512) for h in range(16)]
            hi_of = {}
            ci_of = {}

            def sc_front(u):
                r, c0, cw, h = units[u]
                hi = cnt["h"] % 3
                cnt["h"] += 1
                hi_of[u] = hi
                p0 = (h % 2) * 64
                pe.op(lambda: nc.tensor.matmul(psh[hi][:, 0:cw], lhsT=P["qiT"][p0:p0 + 64, h // 2, qs],
                                               rhs=kiT2[p0:p0 + 64, r, c0:c0 + cw], start=True, stop=True),
                      reads=[C["B_qiT"], B_kv], writes=[B_psh[hi]])
                act.op(lambda: nc.scalar.activation(out=rh[:, hi, 0:cw], in_=psh[hi][:, 0:cw], func=AF.Relu),
                       reads=[B_psh[hi]], writes=[B_rh[hi]])

            def sc_back(u):
                r, c0, cw, h = units[u]
                hi = hi_of[u]
                if h == 0:
                    ci_of[(r, c0)] = cnt["c"] % 2
                    cnt["c"] += 1
                ci = ci_of[(r, c0)]
                pe.op(lambda: nc.tensor.matmul(psc[ci][:, 0:cw], lhsT=diagw[:, h, :], rhs=rh[:, hi, 0:cw],
                                               start=(h == 0), stop=(h == 15)),
                      reads=[B_diagw, B_rh[hi]], writes=[B_psc[ci]])
                if h == 15:
                    act.op(lambda: nc.scalar.copy(out=S[:, r * Lr + c0:r * Lr + c0 + cw], in_=psc[ci][:, 0:cw]),
                           reads=[B_psc[ci]], writes=[B_S])
            for u in range(min(LA, len(units))):
                sc_front(u)
            for u in range(len(units)):
                if u + LA < len(units):
                    sc_front(u + LA)
                sc_back(u)

        def rest(s):
            S = S2[:, s % 2, :]
            B_S = B_S2[s % 2]
            Lr = (s + 1) * 128
            qs = slice(s * 128, (s + 1) * 128)
            LA = 2
            S3 = S[:, 0:4 * Lr]
            dve.op(lambda: nc.vector.tensor_reduce(out=st[:, 32:33], in_=S3, axis=AX.X, op=ALU.max),
                   reads=[B_S], writes=[B_st])
            dve.op(lambda: nc.vector.tensor_reduce(out=st[:, 33:34], in_=S3, axis=AX.X, op=ALU.min),
                   reads=[B_S], writes=[B_st])
            dve.op(lambda: nc.vector.tensor_scalar(out=st[:, 33:34], in0=st[:, 33:34], scalar1=-1.0, scalar2=None,
                                                   op0=ALU.mult), reads=[B_st], writes=[B_st])
            dve.op(lambda: nc.vector.tensor_tensor(out=st[:, 0:1], in0=st[:, 32:33], in1=st[:, 33:34], op=ALU.max),
                   reads=[B_st], writes=[B_st])
            Sl = S3.rearrange("p (r l) -> p r l", r=4)[:, :, s * 128:(s + 1) * 128]
            dve.op(lambda: nc.vector.tensor_tensor(out=Sl, in0=Sl,
                                                   in1=cmask[:, :].rearrange("p (a b) -> p a b", a=4), op=ALU.add),
                   reads=[B_S, B_c], writes=[B_S])
            topk_threshold(K, S3, 128, selm[:, 0:4 * Lr], st, B_S, B_selm, B_st, pw2, B_c)
            dve.op(lambda: nc.vector.tensor_scalar(out=selm[:, 0:4 * Lr], in0=S3, scalar1=st[:, 1:2], scalar2=None,
                                                   op0=ALU.is_ge), reads=[B_S, B_st], writes=[B_selm])
            if s == 7 and "dbg2" in C:
                B_S.dsem = K.new_sem("d")
                B_st.dsem = B_S.dsem
                sp.dma(C["dbg2"][:, 0:4096], S[:], B_S.dsem, reads=[B_S])
                sp.dma(C["dbg2"][:, 4096:4136], st[:, 0:40], B_S.dsem, reads=[B_st])
            for r in range(4):
                for s0 in range(0, s + 1, 4):
                    nb = min(4, s + 1 - s0)
                    ti = cnt["t"] % 2
                    cnt["t"] += 1
                    pe.pre(reads=[B_selm, K.B_ident], writes=[B_ptr[ti]])
                    for b in range(nb):
                        ins = nc.tensor.transpose(ptr[ti][:, b * 128:(b + 1) * 128],
                                                  selm[:, r * Lr + (s0 + b) * 128:r * Lr + (s0 + b + 1) * 128], ident[:])
                    ev = pe.done(ins)
                    pe.post(ev, reads=[B_selm], writes=[B_ptr[ti]])
                    act.op(lambda: nc.scalar.copy(out=selT[:, r, s0:s0 + nb, :],
                                                  in_=ptr[ti][:, 0:nb * 128].rearrange("p (a b) -> p a b", a=nb)),
                           reads=[B_ptr[ti]], writes=[B_selT])
            tiles = [(r, s1) for r in range(4) for s1 in range(s + 1)]
            nt_ = len(tiles)
            aunits = [(g, n, r, s1) for g in range(2) for n, (r, s1) in enumerate(tiles)]
            bi_of = {}

            def at_front(u):
                g, n, r, s1 = aunits[u]
                hi = cnt["h"] % 3
                cnt["h"] += 1
                ei = cnt["e"] % 3
                cnt["e"] += 1
                bi_of[u] = ei
                pe.op(lambda: nc.tensor.matmul(psh[hi][:, :], lhsT=kT_all[:, g, r, s1 * 128:(s1 + 1) * 128],
                                               rhs=P["qT"][:, g * 4:(g + 1) * 4, qs], start=True, stop=True),
                      reads=[B_kv, C["B_qT"]], writes=[B_psh[hi]])
                act.op(lambda: nc.scalar.activation(out=pex[:, ei, :], in_=psh[hi][:, :], func=AF.Exp, scale=SQ),
                       reads=[B_psh[hi]], writes=[B_pex[ei]])
                eng = dve if u % 2 == 0 else pool
                veng = nc.vector if u % 2 == 0 else nc.gpsimd
                eng.op(lambda: veng.tensor_tensor(
                    out=pm[:, ei, :].rearrange("p (a b) -> p a b", a=4),
                    in0=pex[:, ei, :].rearrange("p (a b) -> p a b", a=4),
                    in1=selT[:, r, s1, :].unsqueeze(1).to_broadcast([128, 4, 128]), op=ALU.mult),
                    reads=[B_pex[ei], B_selT], writes=[B_pm[ei]])

            def at_back(u):
                g, n, r, s1 = aunits[u]
                ei = bi_of[u]
                pe.pre(reads=[B_pm[ei], B_kv], writes=[B_psc[0], B_psc[1]])
                for hh in range(4):
                    po = psc[hh // 3][:, (hh % 3) * 129:(hh % 3) * 129 + 129]
                    ins = nc.tensor.matmul(po, lhsT=pm[:, ei, hh * 128:(hh + 1) * 128], rhs=V1[:, r, s1, g, 0:129],
                                           start=(n == 0 and hh % 3 == 0), stop=(n == nt_ - 1),
                                           skip_group_check=True)
                ev = pe.done(ins)
                pe.post(ev, reads=[B_pm[ei], B_kv], writes=[B_psc[0], B_psc[1]])
                if n == nt_ - 1:
                    for hh in range(4):
                        po = psc[hh // 3][:, (hh % 3) * 129:(hh % 3) * 129 + 129]
                        hcol = g * 4 + hh
                        dve.op(lambda: nc.vector.reciprocal(out=rec[:, hcol:hcol + 1], in_=po[:, 128:129]),
                               reads=[B_psc[hh // 3]], writes=[B_rec])
                        dve.op(lambda: nc.vector.tensor_scalar(out=P["oat"][:, s, hcol * 128:(hcol + 1) * 128],
                                                               in0=po[:, 0:128], scalar1=rec[:, hcol:hcol + 1],
                                                               scalar2=None, op0=ALU.mult),
                               reads=[B_psc[hh // 3], B_rec], writes=[C["B_oat"]])
            for u in range(min(LA, len(aunits))):
                at_front(u)
            for u in range(len(aunits)):
                if u + LA < len(aunits):
                    at_front(u + LA)
                at_back(u)

        scores(0)
        for s in range(8):
            if s + 1 < 8:
                scores(s + 1)
            rest(s)
        K.end_phase()
def phase_bs(K, C):
    nc = K.nc
    pe, act, dve, pool, sp = K.pe, K.act, K.dve, K.pool, K.sp
    P = C["P"]
    ident = C["ident"]
    NS = 16
    LK = 2049
    with ExitStack() as es:
        def sb(n, s, d):
            return es.enter_context(nc.sbuf_tensor("s_" + n, s, d))
        ptb = sb("ptb", [128, 256], I32)
        iop = sb("iop", [128, 1], I32)
        idx = sb("idx", [128, 256], I32)
        B_idx = Buf(K, dma=True)
        sp.dma(ptb[:], C["pt"].rearrange("i p -> (i p)").rearrange("(o n) -> o n", o=1).to_broadcast([128, 256]),
               B_idx.dsem, writes=[B_idx])
        pool.op(lambda: nc.gpsimd.iota(iop[:], pattern=[[0, 1]], base=0, channel_multiplier=1), writes=[B_idx])
        pool.op(lambda: nc.gpsimd.tensor_scalar(out=idx[:], in0=ptb[:], scalar1=128, scalar2=None, op0=ALU.mult),
                reads=[B_idx], writes=[B_idx])
        pool.op(lambda: nc.gpsimd.tensor_tensor(out=idx[:], in0=idx[:], in1=iop[:].to_broadcast([128, 256]), op=ALU.add),
                reads=[B_idx], writes=[B_idx])

        wperm = sb("wperm", [128, 64], BF16)
        wTp = sb("wTp", [32, 2, 128], BF16)
        B_w = Buf()
        Ssmp = sb("Ssmp", [NS, 2176], F32)
        B_Ss = Buf(K, dma=True)
        selms = sb("selms", [NS, 2176], BF16)
        B_selms = Buf()
        selTs = sb("selTs", [128, 16, 16], BF16)
        selfs = sb("selfs", [1, 16], F32)
        B_selT = Buf()
        st = sb("st", [NS, 64], F32)
        B_st = Buf()
        pw2 = sb("pw2", [NS, 32], F32)
        B_c = Buf()
        for k in range(NIT):
            dve.op(lambda: nc.vector.memset(pw2[:, k:k + 1], 2.0 ** (-k)), writes=[B_c])

        with ExitStack() as es1:
            def sb1(n, s, d):
                return es1.enter_context(nc.sbuf_tensor("s1_" + n, s, d))

            def ps1(n, s, d=F32):
                return es1.enter_context(nc.psum_tensor("s1_" + n, s, d))
            kig = sb1("kig", [128, 2, 16, 128], BF16)
            B_kig = [Buf(K, dma=True), Buf(K, dma=True)]
            kiTs = sb1("kiTs", [128, 2, 2048], BF16)
            B_kiTs = [Buf(), Buf()]
            rh = sb1("rh", [32, 2, 2, 512], BF16)
            B_rh = [Buf(), Buf()]
            srow = sb1("srow", [1, 1, 2176], F32)
            B_srow = [Buf(K, dma=True)] * 2
            ptr = [ps1(f"ptr{i}", [128, 1024], BF16) for i in range(2)]
            B_ptr = [Buf(), Buf()]
            psh = [ps1(f"psh{i}", [128, 512]) for i in range(2)]
            pso = [ps1(f"pso{i}", [128, 512]) for i in range(2)]
            B_psh = [Buf(), Buf()]
            pss = [ps1(f"pss{i}", [128, 512]) for i in range(2)]
            B_pss = [Buf(), Buf()]
            dve.op(lambda: nc.vector.memset(wperm[:], 0.0), writes=[B_w])
            wi8 = P["wi"][:, 8, :].rearrange("p (a b) -> p a b", b=2)
            dve.op(lambda: nc.vector.tensor_copy(wperm[:, 0:8], wi8[:, :, 0]), reads=[C["B_wi"]], writes=[B_w])
            dve.op(lambda: nc.vector.tensor_copy(wperm[:, 32:40], wi8[:, :, 1]), reads=[C["B_wi"]], writes=[B_w])
            for e in range(2):
                pe.op(lambda: nc.tensor.transpose(ptr[e][0:32, 0:128], wperm[:, e * 32:(e + 1) * 32], ident[:]),
                      reads=[B_w, K.B_ident], writes=[B_ptr[e]])
                act.op(lambda: nc.scalar.copy(out=wTp[:, e, :], in_=ptr[e][0:32, 0:128]), reads=[B_ptr[e]], writes=[B_w])
            nt = 1
            nh = 0
            for i in range(NS):
                o = i % 2
                tok = 1024 + i
                pool.pre(reads=[B_idx], writes=[B_kig[o]])
                for pg in range(16):
                    ins = nc.gpsimd.indirect_dma_start(
                        out=kig[:, o, pg, 0:64], out_offset=None, in_=C["cache_kidx"],
                        in_offset=bass.IndirectOffsetOnAxis(ap=idx[:, i * 16 + pg:i * 16 + pg + 1], axis=0))
                    B_kig[o].dsem.cnt += 16
                    ins.then_inc(B_kig[o].dsem.h, 16)
                    pool.nins += 1
                K.dma_sems[B_kig[o].dsem.uid] = B_kig[o].dsem
                ev = Ev(B_kig[o].dsem, B_kig[o].dsem.cnt)
                pool.post(ev, writes=[B_kig[o]])
                dve.op(lambda: nc.vector.tensor_copy(kig[:, o, :, 64:128], kig[:, o, :, 0:64]), reads=[B_kig[o]],
                       writes=[B_kig[o]])
                for q4 in range(4):
                    x = nt % 2
                    nt += 1
                    pe.pre(reads=[B_kig[o], K.B_ident], writes=[B_ptr[x]])
                    for b in range(4):
                        ins = nc.tensor.transpose(ptr[x][:, b * 128:(b + 1) * 128], kig[:, o, q4 * 4 + b, :], ident[:])
                    ev = pe.done(ins)
                    pe.post(ev, reads=[B_kig[o]], writes=[B_ptr[x]])
                    act.op(lambda: nc.scalar.copy(out=kiTs[:, o, q4 * 512:(q4 + 1) * 512], in_=ptr[x][:, 0:512]),
                           reads=[B_ptr[x]], writes=[B_kiTs[o]])
                for c in range(5):
                    c0 = c * 512
                    cw = 512 if c < 4 else 1
                    x = nh % 2
                    nh += 1
                    if c < 4:
                        rhs_e, rhs_o = kiTs[0:64, o, c0:c0 + cw], kiTs[64:128, o, c0:c0 + cw]
                        deps = [B_kiTs[o]]
                    else:
                        rhs_e, rhs_o = P["kiT8"][0:64, i:i + 1], P["kiT8"][64:128, i:i + 1]
                        deps = [C["B_s8"]]
                    pe.pre(reads=deps + [C["B_qiT"]], writes=[B_psh[x]])
                    nc.tensor.matmul(psh[x][0:8, 0:cw], lhsT=P["qiT"][0:64, :, tok], rhs=rhs_e, start=True, stop=True)
                    ins = nc.tensor.matmul(pso[x][0:8, 0:cw], lhsT=P["qiT"][64:128, :, tok], rhs=rhs_o, start=True,
                                           stop=True)
                    ev = pe.done(ins)
                    pe.post(ev, reads=deps + [C["B_qiT"]], writes=[B_psh[x]])
                    act.op(lambda: nc.scalar.activation(out=rh[0:8, x, 0, 0:cw], in_=psh[x][0:8, 0:cw], func=AF.Relu),
                           reads=[B_psh[x]], writes=[B_rh[x]])
                    act.op(lambda: nc.scalar.activation(out=rh[0:8, x, 1, 0:cw], in_=pso[x][0:8, 0:cw], func=AF.Relu),
                           reads=[B_psh[x]], writes=[B_rh[x]])
                    pe.pre(reads=[B_rh[x], B_w], writes=[B_pss[x]])
                    nc.tensor.matmul(pss[x][0:1, 0:cw], lhsT=wTp[0:8, 0, i:i + 1], rhs=rh[0:8, x, 0, 0:cw], start=True,
                                     stop=False)
                    ins = nc.tensor.matmul(pss[x][0:1, 0:cw], lhsT=wTp[0:8, 1, i:i + 1], rhs=rh[0:8, x, 1, 0:cw],
                                           start=False, stop=True)
                    ev = pe.done(ins)
                    pe.post(ev, reads=[B_rh[x], B_w], writes=[B_pss[x]])
                    dve.op(lambda: nc.vector.tensor_copy(srow[0:1, 0, c0:c0 + cw], pss[x][0:1, 0:cw]), reads=[B_pss[x]],
                           writes=[B_srow[o]])
                sp.dma(C["sscr"][i:i + 1, 0:LK], srow[0:1, 0, 0:LK], B_srow[o].dsem, reads=[B_srow[o]],
                       writes=[C["B_sscr"]])
            K.barrier()
        sp.dma(Ssmp[:, 0:LK], C["sscr"][:, 0:LK], B_Ss.dsem, reads=[C["B_sscr"]], writes=[B_Ss])
        S3 = Ssmp[:, 0:LK]
        dve.op(lambda: nc.vector.tensor_reduce(out=st[:, 32:33], in_=S3, axis=AX.X, op=ALU.max), reads=[B_Ss],
               writes=[B_st])
        dve.op(lambda: nc.vector.tensor_reduce(out=st[:, 33:34], in_=S3, axis=AX.X, op=ALU.min), reads=[B_Ss],
               writes=[B_st])
        dve.op(lambda: nc.vector.tensor_scalar(out=st[:, 33:34], in0=st[:, 33:34], scalar1=-1.0, scalar2=None,
                                               op0=ALU.mult), reads=[B_st], writes=[B_st])
        dve.op(lambda: nc.vector.tensor_tensor(out=st[:, 0:1], in0=st[:, 32:33], in1=st[:, 33:34], op=ALU.max),
               reads=[B_st], writes=[B_st])
        topk_threshold(K, S3, NS, selms[:, 0:LK], st, B_Ss, B_selms, B_st, pw2, B_c)
        dve.op(lambda: nc.vector.tensor_scalar(out=selms[:, 0:LK], in0=S3, scalar1=st[:, 1:2], scalar2=None,
                                               op0=ALU.is_ge), reads=[B_Ss, B_st], writes=[B_selms])

        with ExitStack() as es2:
            def sb2(n, s, d):
                return es2.enter_context(nc.sbuf_tensor("s2_" + n, s, d))

            def ps2(n, s, d=F32):
                return es2.enter_context(nc.psum_tensor("s2_" + n, s, d))
            Kg = sb2("Kg", [128, 2, 16, 256], BF16)
            B_Kg = [Buf(K, dma=True), Buf(K, dma=True)]
            Vc = sb2("Vc", [128, 1, 16, 256], BF16)
            B_Vc = [Buf(K, dma=True)] * 2
            Vg = sb2("Vg", [128, 2, 16, 2, 130], BF16)
            B_Vg = [Buf(), Buf()]
            kTs = sb2("kTs", [128, 2, 2, 2048], BF16)
            B_kTs = [Buf(), Buf()]
            vself = sb2("vself", [1, 16, 2, 130], BF16)
            B_vs = Buf(K, dma=True)
            pTs = sb2("pTs", [128, 2, 128], BF16)
            B_pTs = [Buf(), Buf()]
            pms = sb2("pms", [128, 2, 128], BF16)
            B_pms = [Buf(), Buf()]
            pself = sb2("pself", [1, 2, 8], BF16)
            pselfm = sb2("pselfm", [1, 2, 8], BF16)
            B_pself = [Buf(), Buf()]
            osm = sb2("osm", [4, 2, 2, 128], F32)
            B_osm = [Buf(K, dma=True), Buf(K, dma=True)]
            rec = sb2("rec", [4, 4], F32)
            B_rec = Buf()
            ptr = [ps2(f"ptr{i}", [128, 1024], BF16) for i in range(2)]
            B_ptr = [Buf(), Buf()]
            pl = [ps2(f"pl{i}", [128, 512]) for i in range(2)]
            B_pl = [Buf(), Buf()]
            psf = ps2("psf", [128, 512])
            B_psf = Buf()
            pos = [ps2(f"pos{i}", [128, 512]) for i in range(2)]
            B_pos = [Buf(), Buf()]
            pe.pre(reads=[B_selms, K.B_ident], writes=[B_ptr[0]])
            for pg in range(16):
                ins = nc.tensor.transpose(ptr[0][:, pg * 16:(pg + 1) * 16], selms[0:NS, pg * 128:(pg + 1) * 128],
                                          ident[0:NS, 0:NS])
            ev = pe.done(ins)
            pe.post(ev, reads=[B_selms], writes=[B_ptr[0]])
            act.op(lambda: nc.scalar.copy(out=selTs[:].rearrange("p a b -> p (a b)"), in_=ptr[0][:, 0:256]),
                   reads=[B_ptr[0]], writes=[B_selT])
            pe.op(lambda: nc.tensor.transpose(ptr[1][0:1, 0:NS], selms[0:NS, 2048:2049], ident[0:NS, 0:NS]),
                  reads=[B_selms, K.B_ident], writes=[B_ptr[1]])
            act.op(lambda: nc.scalar.copy(out=selfs[0:1, :], in_=ptr[1][0:1, 0:NS]), reads=[B_ptr[1]], writes=[B_selT])
            pool.op(lambda: nc.gpsimd.memset(vself[:], 1.0), writes=[B_vs])
            pool.dma(vself[0:1, :, :, 0:128], C["vo"][1024:1040, :].rearrange("(o i) (g d) -> o i g d", o=1, g=2),
                     B_vs.dsem, writes=[B_vs])
            for o in range(2):
                pool.op(lambda: nc.gpsimd.memset(Vg[:, o, :, :, 128:130], 1.0), writes=[B_Vg[o]])
            nt = 0
            for i in range(NS):
                o = i % 2
                tok = 1024 + i
                for (dst, Bd, srcc, oo) in ((Kg, B_Kg, C["cache_k"], o), (Vc, B_Vc, C["cache_v"], 0)):
                    pool.pre(reads=[B_idx], writes=[Bd[o]])
                    for pg in range(16):
                        ins = nc.gpsimd.indirect_dma_start(
                            out=dst[:, oo, pg, :], out_offset=None, in_=srcc,
                            in_offset=bass.IndirectOffsetOnAxis(ap=idx[:, i * 16 + pg:i * 16 + pg + 1], axis=0))
                        Bd[o].dsem.cnt += 16
                        ins.then_inc(Bd[o].dsem.h, 16)
                        pool.nins += 1
                    K.dma_sems[Bd[o].dsem.uid] = Bd[o].dsem
                    ev = Ev(Bd[o].dsem, Bd[o].dsem.cnt)
                    pool.post(ev, writes=[Bd[o]])
                act.op(lambda: nc.scalar.copy(out=Vg[:, o, :, :, 0:128],
                                              in_=Vc[:, 0, :, :].rearrange("p a (g d) -> p a g d", g=2)),
                       reads=[B_Vc[o]], writes=[B_Vg[o]])
                for pg4 in range(8):
                    x = nt % 2
                    nt += 1
                    pe.pre(reads=[B_Kg[o], K.B_ident], writes=[B_ptr[x]])
                    for b in range(4):
                        pg, g = (pg4 * 4 + b) // 2, (pg4 * 4 + b) % 2
                        ins = nc.tensor.transpose(ptr[x][:, b * 128:(b + 1) * 128], Kg[:, o, pg, g * 128:(g + 1) * 128],
                                                  ident[:])
                    ev = pe.done(ins)
                    pe.post(ev, reads=[B_Kg[o]], writes=[B_ptr[x]])
                    dstv = kTs[:, o, :, pg4 * 256:(pg4 + 1) * 256].rearrange("p g (a l) -> p a g l", a=2)
                    srcv = ptr[x][:, 0:512].rearrange("p (a g l) -> p a g l", a=2, g=2)
                    eng, veng = (act, None) if pg4 % 2 == 0 else (dve, None)
                    if pg4 % 2 == 0:
                        act.op(lambda: nc.scalar.copy(out=dstv, in_=srcv), reads=[B_ptr[x]], writes=[B_kTs[o]])
                    else:
                        dve.op(lambda: nc.vector.tensor_copy(dstv, srcv), reads=[B_ptr[x]], writes=[B_kTs[o]])
                pe.pre(reads=[B_kTs[o], C["B_qT"]], writes=[B_pl[o]])
                for pg in range(16):
                    for g in range(2):
                        ins = nc.tensor.matmul(pl[o][:, (pg * 2 + g) * 4:(pg * 2 + g) * 4 + 4],
                                               lhsT=kTs[:, o, g, pg * 128:(pg + 1) * 128],
                                               rhs=P["qT"][:, g * 4:(g + 1) * 4, tok], start=True, stop=True,
                                               skip_group_check=True)
                ev = pe.done(ins)
                pe.post(ev, reads=[B_kTs[o], C["B_qT"]], writes=[B_pl[o]])
                pe.pre(reads=[C["B_s8"], C["B_qT"]], writes=[B_psf])
                for g in range(2):
                    ins = nc.tensor.matmul(psf[0:1, g * 4:(g + 1) * 4], lhsT=P["kT8"][:, g * 128 + i:g * 128 + i + 1],
                                           rhs=P["qT"][:, g * 4:(g + 1) * 4, tok], start=True, stop=True,
                                           skip_group_check=True)
                ev = pe.done(ins)
                pe.post(ev, reads=[C["B_s8"], C["B_qT"]], writes=[B_psf])
                act.op(lambda: nc.scalar.activation(out=pTs[:, o, :], in_=pl[o][:, 0:128], func=AF.Exp, scale=SQ),
                       reads=[B_pl[o]], writes=[B_pTs[o]])
                act.op(lambda: nc.scalar.activation(out=pself[0:1, o, :], in_=psf[0:1, 0:8], func=AF.Exp, scale=SQ),
                       reads=[B_psf], writes=[B_pself[o]])
                dve.op(lambda: nc.vector.tensor_tensor(
                    out=pms[:, o, :].rearrange("p (a b) -> p a b", a=16),
                    in0=pTs[:, o, :].rearrange("p (a b) -> p a b", a=16),
                    in1=selTs[:, :, i].unsqueeze(2).to_broadcast([128, 16, 8]), op=ALU.mult),
                    reads=[B_pTs[o], B_selT], writes=[B_pms[o]])
                dve.op(lambda: nc.vector.tensor_scalar(out=pselfm[0:1, o, :], in0=pself[0:1, o, :],
                                                       scalar1=selfs[0:1, i:i + 1], scalar2=None, op0=ALU.mult),
                       reads=[B_pself[o], B_selT], writes=[B_pself[o]])
                for g in range(2):
                    pe.pre(reads=[B_pms[o], B_Vg[o], B_pself[o], B_vs], writes=[B_pos[g]])
                    for pg in range(16):
                        nc.tensor.matmul(pos[g][0:4, 0:129], lhsT=pms[:, o, (pg * 2 + g) * 4:(pg * 2 + g) * 4 + 4],
                                         rhs=Vg[:, o, pg, g, 0:129], start=(pg == 0), stop=False)
                    ins = nc.tensor.matmul(pos[g][0:4, 0:129], lhsT=pselfm[0:1, o, g * 4:(g + 1) * 4],
                                           rhs=vself[0:1, i, g, 0:129], start=False, stop=True)
                    ev = pe.done(ins)
                    pe.post(ev, reads=[B_pms[o], B_Vg[o], B_pself[o], B_vs], writes=[B_pos[g]])
                    dve.op(lambda: nc.vector.reciprocal(out=rec[0:4, g:g + 1], in_=pos[g][0:4, 128:129]),
                           reads=[B_pos[g]], writes=[B_rec])
                    dve.op(lambda: nc.vector.tensor_scalar(out=osm[0:4, o, g, :], in0=pos[g][0:4, 0:128],
                                                           scalar1=rec[0:4, g:g + 1], scalar2=None, op0=ALU.mult),
                           reads=[B_pos[g], B_rec], writes=[B_osm[o]])
                    sp.dma(C["oscr"][i, g * 512:(g + 1) * 512].rearrange("(h d) -> h d", h=4), osm[0:4, o, g, :],
                           B_osm[o].dsem, reads=[B_osm[o]], writes=[C["B_oscr"]])
            K.barrier()
        pool.dma(P["oat"][0:NS, 8, :], C["oscr"][:, :], C["B_oat"].dsem, reads=[C["B_oscr"]], writes=[C["B_oat"]])
        K.end_phase()
def phase_g1(K, C):
    nc = K.nc
    pe, act, dve, pool, sp = K.pe, K.act, K.dve, K.pool, K.sp
    P = C["P"]
    with ExitStack() as es:
        def sb(n, s, d):
            return es.enter_context(nc.sbuf_tensor("g1_" + n, s, d))

        def ps(n, s, d=F32):
            return es.enter_context(nc.psum_tensor("g1_" + n, s, d))
        triu = sb("triu", [128, 128], F32)
        B_tri = Buf()
        sst = sb("sst", [128, 2, 4, 256], F32)
        B_sst = [Buf(K, dma=True), Buf(K, dma=True)]
        pa = [ps(f"pa{i}", [128, 512]) for i in range(2)]
        B_pa = [Buf(), Buf()]
        pl = [ps(f"pl{i}", [128, 512]) for i in range(2)]
        B_pl = [Buf(), Buf()]
        pool.op(lambda: nc.gpsimd.memset(triu[:], 1.0), writes=[B_tri])
        pool.op(lambda: nc.gpsimd.affine_select(out=triu[:], in_=triu[:], pattern=[[1, 128]], compare_op=ALU.is_ge,
                                                fill=0.0, base=0, channel_multiplier=-1),
                reads=[B_tri], writes=[B_tri])
        sp.dma(C["dec_in"], P["dec"][:].rearrange("p h t -> p (h t)"), C["B_dec"].dsem, reads=[C["B_dec"]],
               writes=[C["B_decin"]])
        all_gather(K, C["dec_in"], C["dec_out"], [C["B_decin"]], [C["B_deco"]])
        n = 0
        for t in range(8):
            ts = slice(t * 128, (t + 1) * 128)
            o = t % 2
            for h in range(4):
                i = n % 2
                n += 1
                pe.op(lambda: nc.tensor.matmul(pa[i][:, 0:128], lhsT=P["kgT"][:, h, ts], rhs=P["qgT"][:, h, ts],
                                               start=True, stop=True),
                      reads=[C["B_kgT"], C["B_qgT"]], writes=[B_pa[i]])
                dve.op(lambda: nc.vector.tensor_tensor(out=P["AT"][:, t, h, :], in0=pa[i][:, 0:128], in1=triu[:],
                                                       op=ALU.mult), reads=[B_pa[i], B_tri], writes=[C["B_AT"]])
                pe.op(lambda: nc.tensor.matmul(pl[i][:, 0:256], lhsT=P["khat"][:, t, h * 128:(h + 1) * 128],
                                               rhs=P["gv"][:, t, h * 256:(h + 1) * 256], start=True, stop=True),
                      reads=[C["B_khat"], C["B_gv"]], writes=[B_pl[i]])
                act.op(lambda: nc.scalar.copy(out=sst[:, o, h, :], in_=pl[i][:, 0:256]), reads=[B_pl[i]],
                       writes=[B_sst[o]])
            sp.dma(C["gst_in"][t].rearrange("(h p) v -> p h v", p=128), sst[:, o], B_sst[o].dsem, reads=[B_sst[o]],
                   writes=[C["B_gin"][t]])
            all_gather(K, C["gst_in"][t], C["gst_out"][t], [C["B_gin"][t]], [C["B_gout"][t]])
        K.end_phase()


def phase_g2(K, C):
    nc = K.nc
    pe, act, dve, pool, sp = K.pe, K.act, K.dve, K.pool, K.sp
    P = C["P"]
    ident = C["ident"]
    with ExitStack() as es:
        def sb(n, s, d):
            return es.enter_context(nc.sbuf_tensor("g2_" + n, s, d))

        def ps(n, s, d=F32):
            return es.enter_context(nc.psum_tensor("g2_" + n, s, d))
        Sg = sb("Sg", [128, 4, 4, 256], F32)
        B_Sg = Buf(K, dma=True)
        dg = sb("dg", [128, 4, 32], F32)
        B_dg = Buf(K, dma=True)
        oh = sb("oh", [128, 4], F32)
        gnw = sb("gnw", [128, 256], F32)
        B_c = Buf(K, dma=True)
        Srun = sb("Srun", [128, 4, 256], F32)
        B_run = Buf(K, dma=True)
        Sin = sb("Sin", [128, 4, 256], F32)
        B_in = Buf()
        Sinb = sb("Sinb", [128, 4, 256], BF16)
        B_inb = Buf()
        st = sb("st", [128, 16], F32)
        B_st = Buf()
        junk = sb("junk", [128, 256], BF16)
        B_junk = Buf()
        po = [ps(f"po{i}", [128, 512]) for i in range(2)]
        B_po = [Buf(), Buf()]

        sp.dma(oh[:], C["onehot"], B_c.dsem, writes=[B_c])
        sp.dma(gnw[:], C["gla_norm_w"].rearrange("(o d) -> o d", o=1).to_broadcast([128, 256]), B_c.dsem, writes=[B_c])
        sp.dma(dg[:], C["dec_out"].rearrange("(r p) c -> p r c", p=128), B_dg.dsem, reads=[C["B_deco"]], writes=[B_dg])
        dve.op(lambda: nc.vector.memset(Srun[:], 0.0), writes=[B_run])

        def head_norm(pz, B_pz, np_, out_ap, B_out):
            dve.op(lambda: nc.vector.memset(st[0:np_, 0:1], 0.0), writes=[B_st])
            act.op(lambda: nc.scalar.activation(out=junk[0:np_, :], in_=pz, func=AF.Square,
                                                accum_out=st[0:np_, 0:1]), reads=[B_pz, B_st], writes=[B_junk, B_st])
            act.op(lambda: nc.scalar.activation(out=st[0:np_, 1:2], in_=st[0:np_, 0:1], func=AF.Sqrt,
                                                scale=1.0 / 256.0, bias=EPS), reads=[B_st], writes=[B_st])
            dve.op(lambda: nc.vector.reciprocal(out=st[0:np_, 1:2], in_=st[0:np_, 1:2]), reads=[B_st], writes=[B_st])
            dve.op(lambda: nc.vector.scalar_tensor_tensor(out=out_ap, in0=pz, scalar=st[0:np_, 1:2],
                                                          in1=gnw[0:np_, :], op0=ALU.mult, op1=ALU.mult),
                   reads=[B_pz, B_st, B_c], writes=[B_out])

        n = 0
        for s in range(8):
            ts = slice(s * 128, (s + 1) * 128)
            sp.dma(Sg[:], C["gst_out"][s].rearrange("(r h p) v -> p r h v", p=128, h=4), B_Sg.dsem,
                   reads=[C["B_gout"][s]], writes=[B_Sg])
            dve.op(lambda: nc.vector.memset(Sin[:], 0.0), reads=[], writes=[B_in])
            for r in range(4):
                dve.op(lambda: nc.vector.scalar_tensor_tensor(out=Sin[:], in0=Srun[:], scalar=oh[:, r:r + 1],
                                                              in1=Sin[:], op0=ALU.mult, op1=ALU.add),
                       reads=[B_run, B_c, B_in], writes=[B_in])
                for h in range(4):
                    dve.op(lambda: nc.vector.scalar_tensor_tensor(out=Srun[:, h, :], in0=Srun[:, h, :],
                                                                  scalar=dg[:, r, h * 8 + s:h * 8 + s + 1],
                                                                  in1=Sg[:, r, h, :], op0=ALU.mult, op1=ALU.add),
                           reads=[B_run, B_dg, B_Sg], writes=[B_run])
            act.op(lambda: nc.scalar.copy(out=Sinb[:], in_=Sin[:]), reads=[B_in], writes=[B_inb])
            for h in range(4):
                i = n % 2
                n += 1
                pe.pre(reads=[C["B_AT"], C["B_gv"], C["B_qgT"], B_inb], writes=[B_po[i]])
                nc.tensor.matmul(po[i][:, 0:256], lhsT=P["AT"][:, s, h, :], rhs=P["gv"][:, s, h * 256:(h + 1) * 256],
                                 start=True, stop=False)
                ins = nc.tensor.matmul(po[i][:, 0:256], lhsT=P["qgT"][:, h, ts], rhs=Sinb[:, h, :],
                                       start=False, stop=True)
                ev = pe.done(ins)
                pe.post(ev, reads=[C["B_AT"], C["B_gv"], C["B_qgT"], B_inb], writes=[B_po[i]])
                head_norm(po[i][:, 0:256], B_po[i], 128, P["og"][:, s, h * 256:(h + 1) * 256], C["B_og"])
        sp.dma(C["gla_p"].rearrange("(h p) v -> p h v", p=128), Srun[:], B_run.dsem, reads=[B_run])

        S0 = sb("S0", [128, 2, 4, 256], F32)
        B_S0 = [Buf(K, dma=True), Buf(K, dma=True)]
        Sn = sb("Sn", [128, 2, 4, 256], F32)
        B_Sn = [Buf(K, dma=True), Buf(K, dma=True)]
        Snb = sb("Snb", [128, 2, 4, 256], BF16)
        B_Snb = [Buf(), Buf()]
        tmp = sb("tmp", [128, 2, 256], F32)
        B_tmp = [Buf(), Buf()]
        Bsel = sb("Bsel", [128, 16, 128], BF16)
        I16 = sb("I16", [128, 16, 16], BF16)
        Qpad = sb("Qpad", [128, 4, 16, 16], BF16)
        B_q = Buf()
        pb = [ps(f"pb{i}", [128, 512]) for i in range(2)]
        B_pb = [Buf(), Buf()]
        pso = [ps(f"pso{i}", [128, 512]) for i in range(2)]
        B_pso = Buf()
        dve.op(lambda: nc.vector.tensor_copy(Bsel[:], ident[:, 0:16].unsqueeze(2).to_broadcast([128, 16, 128])),
               reads=[K.B_ident], writes=[B_q])
        dve.op(lambda: nc.vector.memset(I16[:], 0.0), writes=[B_q])
        for i in range(16):
            dve.op(lambda: nc.vector.memset(I16[:, i, i:i + 1], 1.0), writes=[B_q])
        for h in range(4):
            dve.op(lambda: nc.vector.tensor_tensor(out=Qpad[:, h], in0=P["qgT"][:, h, 1024:1040].unsqueeze(2).to_broadcast(
                [128, 16, 16]), in1=I16[:], op=ALU.mult), reads=[C["B_qgT"], B_q], writes=[B_q])
        stin = C["state_in"].rearrange("(i h p) v -> i p h v", p=128, h=4)
        stout = C["gla_s"].rearrange("(i h p) v -> i p h v", p=128, h=4)
        pe.pre(writes=[B_pso])
        for i in range(16):
            o = i % 2
            sp.dma(S0[:, o], stin[i], B_S0[o].dsem, writes=[B_S0[o]])
            for h in range(4):
                x = n % 2
                n += 1
                pe.op(lambda: nc.tensor.matmul(pb[x][:, 0:256], lhsT=Bsel[:, i, :], rhs=P["gv"][:, 8, h * 256:(h + 1) * 256],
                                               start=True, stop=True), reads=[B_q, C["B_gv"]], writes=[B_pb[x]])
                dve.op(lambda: nc.vector.tensor_scalar(out=tmp[:, x, :], in0=pb[x][:, 0:256],
                                                       scalar1=P["kg8f"][:, h, i:i + 1], scalar2=None, op0=ALU.mult),
                       reads=[B_pb[x], C["B_kgT"]], writes=[B_tmp[x]])
                dve.op(lambda: nc.vector.scalar_tensor_tensor(out=Sn[:, o, h, :], in0=S0[:, o, h, :],
                                                              scalar=P["dec8"][:, h, i:i + 1], in1=tmp[:, x, :],
                                                              op0=ALU.mult, op1=ALU.add),
                       reads=[B_S0[o], B_tmp[x], C["B_dec"]], writes=[B_Sn[o]])
            sp.dma(stout[i], Sn[:, o], B_Sn[o].dsem, reads=[B_Sn[o]])
            act.op(lambda: nc.scalar.copy(out=Snb[:, o], in_=Sn[:, o]), reads=[B_Sn[o]], writes=[B_Snb[o]])
            pe.pre(reads=[B_Snb[o], B_q])
            for h in range(4):
                ins = nc.tensor.matmul(pso[h // 2][0:16, (h % 2) * 256:(h % 2) * 256 + 256], lhsT=Qpad[:, h, i, :],
                                       rhs=Snb[:, o, h, :], start=(i == 0 and h % 2 == 0), stop=(i == 15),
                                       skip_group_check=True)
            ev = pe.done(ins)
            pe.post(ev, reads=[B_Snb[o], B_q], writes=[B_pso])
        for h in range(4):
            head_norm(pso[h // 2][0:16, (h % 2) * 256:(h % 2) * 256 + 256], B_pso, 16,
                      P["og"][0:16, 8, h * 256:(h + 1) * 256], C["B_og"])
        K.end_phase()
def phase_c(K, C):
    nc = K.nc
    pe, act, dve, pool, sp = K.pe, K.act, K.dve, K.pool, K.sp
    ident = C["ident"]
    win_v = C["w_in"].rearrange("(k p) f -> p k f", p=128)
    wpa_v = C["w_proj_attn"].rearrange("(k p) f -> p k f", p=128)
    wpg_v = C["w_proj_gla"].rearrange("(k p) f -> p k f", p=128)
    wo_v = C["w_out"].rearrange("(k p) f -> p k f", p=128)
    with ExitStack() as esm:
        def sbm(n, s, d):
            return esm.enter_context(nc.sbuf_tensor("c_" + n, s, d))
        mT = sbm("mT", [128, 16, TOK], BF16)
        B_mT = [Buf() for _ in range(NT)]
        with ExitStack() as esg:
            merged = esg.enter_context(nc.sbuf_tensor("c_merged", [128, NT, D], BF16))
            B_mg = [Buf() for _ in range(NT)]
            with ExitStack() as es:
                def sb(n, s, d):
                    return es.enter_context(nc.sbuf_tensor("c_" + n, s, d))

                def ps(n, s, d=F32):
                    return es.enter_context(nc.psum_tensor("c_" + n, s, d))
                uT = sb("uT", [128, 16, TOK], BF16)
                B_uT = [Buf() for _ in range(NT)]
                oatT = sb("oatT", [128, 8, TOK], BF16)
                ogT = sb("ogT", [128, 8, TOK], BF16)
                B_oatT = [Buf() for _ in range(NT)]
                B_ogT = [Buf() for _ in range(NT)]
                ptr = [ps(f"ptr{i}", [128, 1024], BF16) for i in range(2)]
                B_ptr = [Buf(), Buf()]
                norm_T(K, C["h1"], C["mix_pre_w"], uT, B_uT, ident, ptr, B_ptr, "c1_")
                pp = [ps(f"pp{i}", [128, 512]) for i in range(4)]
                B_pp = [Buf() for _ in range(4)]
                wg = sb("wg", [128, 2, 16, 512], BF16)
                B_wg = [Buf(K, dma=True), Buf(K, dma=True)]
                wp = sb("wp", [128, 2, 8, 512], BF16)
                B_wp = [Buf(K, dma=True), Buf(K, dma=True)]
                ost = sb("ost", [128, 1, 1024], BF16)
                B_ost = [Buf(K, dma=True)] * 2
                gst = sb("gst", [128, 1, 1024], BF16)
                B_gst = [Buf(K, dma=True)] * 2
                sg = sb("sg", [128, 4, 512], BF16)
                B_sg = [Buf() for _ in range(4)]
                tm = sb("tm", [128, 2, 512], F32)
                B_tm = [Buf(), Buf()]
                npp = [0]
                ntr = [0]

                def tr8(src_slot_ap, B_src, dstT, B_dst, t):
                    for half in range(2):
                        x = ntr[0] % 2
                        ntr[0] += 1
                        pe.pre(reads=[B_src, K.B_ident], writes=[B_ptr[x]])
                        for b in range(4):
                            k = half * 4 + b
                            ins = nc.tensor.transpose(ptr[x][:, b * 128:(b + 1) * 128],
                                                      src_slot_ap[:, k * 128:(k + 1) * 128], ident[:])
                        ev = pe.done(ins)
                        pe.post(ev, reads=[B_src], writes=[B_ptr[x]])
                        act.op(lambda: nc.scalar.copy(out=dstT[:, half * 4:half * 4 + 4, t * 128:(t + 1) * 128],
                                                      in_=ptr[x][:, 0:512].rearrange("p (a b) -> p a b", a=4)),
                               reads=[B_ptr[x]], writes=[B_dst[t]])

                oat_d = C["oat_d"].bitcast(BF16)
                og_d = C["og_d"].bitcast(BF16)
                for t in range(NT):
                    o = t % 2
                    sp.dma(ost[:, 0, :], oat_d[t * 128:(t + 1) * 128, :], B_ost[o].dsem, writes=[B_ost[o]])
                    tr8(ost[:, 0, :], B_ost[o], oatT, B_oatT, t)
                for blk in range(2):
                    pool.dma(wg[:, blk, :, :], win_v[:, :, CGR + blk * 512:CGR + (blk + 1) * 512], B_wg[blk].dsem,
                             writes=[B_wg[blk]])
                for t in range(NT):
                    o = t % 2
                    sp.dma(gst[:, 0, :], og_d[t * 128:(t + 1) * 128, :], B_gst[o].dsem, writes=[B_gst[o]])
                    for blk in range(2):
                        i = npp[0] % 4
                        npp[0] += 1
                        pe.pre(reads=[B_uT[t], B_wg[blk]], writes=[B_pp[i]])
                        for k in range(16):
                            ins = nc.tensor.matmul(pp[i][:, :], lhsT=uT[:, k, t * 128:(t + 1) * 128], rhs=wg[:, blk, k, :],
                                                   start=(k == 0), stop=(k == 15))
                        ev = pe.done(ins)
                        pe.post(ev, reads=[B_uT[t], B_wg[blk]], writes=[B_pp[i]])
                        act.op(lambda: nc.scalar.activation(out=sg[:, i, :], in_=pp[i][:, :], func=AF.Silu),
                               reads=[B_pp[i]], writes=[B_sg[i]])
                        dve.op(lambda: nc.vector.tensor_tensor(out=gst[:, 0, blk * 512:(blk + 1) * 512],
                                                               in0=gst[:, 0, blk * 512:(blk + 1) * 512], in1=sg[:, i, :],
                                                               op=ALU.mult), reads=[B_sg[i], B_gst[o]], writes=[B_gst[o]])
                    tr8(gst[:, 0, :], B_gst[o], ogT, B_ogT, t)
                B_wgb = [[Buf(K, dma=True), Buf(K, dma=True)] for _ in range(2)]
                B_wpb = [[Buf(K, dma=True), Buf(K, dma=True)] for _ in range(2)]
                NB = D // 256

                def load_blk(nb):
                    b = nb % 2
                    bs_ = slice(b * 256, (b + 1) * 256)
                    c0 = nb * 256
                    pool.dma(wg[:, 0, :, bs_], win_v[:, :, CGA + c0:CGA + c0 + 256], B_wgb[0][b].dsem,
                             writes=[B_wgb[0][b], B_wg[0]])
                    pool.dma(wg[:, 1, :, bs_], win_v[:, :, CGG + c0:CGG + c0 + 256], B_wgb[1][b].dsem,
                             writes=[B_wgb[1][b], B_wg[1]])
                    pool.dma(wp[:, 0, :, bs_], wpa_v[:, :, c0:c0 + 256], B_wpb[0][b].dsem, writes=[B_wpb[0][b]])
                    pool.dma(wp[:, 1, :, bs_], wpg_v[:, :, c0:c0 + 256], B_wpb[1][b].dsem, writes=[B_wpb[1][b]])
                load_blk(0)
                for nb in range(NB):
                    b = nb % 2
                    bs_ = slice(b * 256, (b + 1) * 256)
                    cs = slice(nb * 256, (nb + 1) * 256)
                    if nb + 1 < NB:
                        load_blk(nb + 1)
                    for t in range(NT):
                        ts = slice(t * 128, (t + 1) * 128)
                        ids = []
                        for which in range(4):
                            i = npp[0] % 4
                            npp[0] += 1
                            ids.append(i)
                            if which < 2:
                                srcT, Bs, w, Bw, nk = uT, B_uT, wg[:, which], B_wgb[which][b], 16
                            elif which == 2:
                                srcT, Bs, w, Bw, nk = oatT, B_oatT, wp[:, 0], B_wpb[0][b], 8
                            else:
                                srcT, Bs, w, Bw, nk = ogT, B_ogT, wp[:, 1], B_wpb[1][b], 8
                            pe.pre(reads=[Bs[t], Bw], writes=[B_pp[i]])
                            for k in range(nk):
                                ins = nc.tensor.matmul(pp[i][:, 0:256], lhsT=srcT[:, k, ts], rhs=w[:, k, bs_],
                                                       start=(k == 0), stop=(k == nk - 1))
                            ev = pe.done(ins)
                            pe.post(ev, reads=[Bs[t], Bw], writes=[B_pp[i]])
                            if which < 2:
                                act.op(lambda: nc.scalar.activation(out=sg[:, i, 0:256], in_=pp[i][:, 0:256],
                                                                    func=AF.Sigmoid),
                                       reads=[B_pp[i]], writes=[B_sg[i]])
                        ia, ig, ipa, ipg = ids
                        x = t % 2
                        dve.op(lambda: nc.vector.tensor_tensor(out=tm[:, x, 0:256], in0=sg[:, ia, 0:256],
                                                               in1=pp[ipa][:, 0:256], op=ALU.mult),
                               reads=[B_sg[ia], B_pp[ipa]], writes=[B_tm[x]])
                        dve.op(lambda: nc.vector.tensor_tensor(out=sg[:, ig, 0:256], in0=sg[:, ig, 0:256],
                                                               in1=pp[ipg][:, 0:256], op=ALU.mult),
                               reads=[B_sg[ig], B_pp[ipg]], writes=[B_sg[ig]])
                        pool.op(lambda: nc.gpsimd.tensor_tensor(out=merged[:, t, cs], in0=tm[:, x, 0:256],
                                                                in1=sg[:, ig, 0:256], op=ALU.add),
                                reads=[B_tm[x], B_sg[ig]], writes=[B_mg[t]])
                K.barrier()
            with ExitStack() as es:
                ptr = [es.enter_context(nc.psum_tensor(f"c2_ptr{i}", [128, 1024], BF16)) for i in range(2)]
                B_ptr = [Buf(), Buf()]
                n = 0
                for t in range(NT):
                    for q4 in range(4):
                        x = n % 2
                        n += 1
                        pe.pre(reads=[B_mg[t], K.B_ident], writes=[B_ptr[x]])
                        for b in range(4):
                            k = q4 * 4 + b
                            ins = nc.tensor.transpose(ptr[x][:, b * 128:(b + 1) * 128], merged[:, t, k * 128:(k + 1) * 128],
                                                      ident[:])
                        ev = pe.done(ins)
                        pe.post(ev, reads=[B_mg[t]], writes=[B_ptr[x]])
                        if q4 % 2 == 0:
                            act.op(lambda: nc.scalar.copy(out=mT[:, q4 * 4:q4 * 4 + 4, t * 128:(t + 1) * 128],
                                                          in_=ptr[x][:, 0:512].rearrange("p (a b) -> p a b", a=4)),
                                   reads=[B_ptr[x]], writes=[B_mT[t]])
                        else:
                            dve.op(lambda: nc.vector.tensor_copy(mT[:, q4 * 4:q4 * 4 + 4, t * 128:(t + 1) * 128],
                                                                 ptr[x][:, 0:512].rearrange("p (a b) -> p a b", a=4)),
                                   reads=[B_ptr[x]], writes=[B_mT[t]])
                K.barrier()
        with ExitStack() as es:
            def sb(n, s, d):
                return es.enter_context(nc.sbuf_tensor("c3_" + n, s, d))
            wo = sb("wo", [128, 16, D], BF16)
            B_wo = Buf(K, dma=True)
            wbc = sb("wbc", [128, D], F32)
            hst = sb("hst", [128, 2, D], F32)
            B_hst = [Buf(K, dma=True), Buf(K, dma=True)]
            ot = sb("ot", [128, 2, D], F32)
            B_ot = [Buf(K, dma=True), Buf(K, dma=True)]
            junk = sb("junk", [128, 512], BF16)
            B_junk = Buf()
            st = sb("st", [128, 8 * NT], F32)
            B_st = Buf()
            po = [es.enter_context(nc.psum_tensor(f"c3_po{i}", [128, 512])) for i in range(8)]
            B_po = [Buf() for _ in range(8)]
            for nb in range(4):
                pool.dma(wo[:, :, nb * 512:(nb + 1) * 512], wo_v[:, :, nb * 512:(nb + 1) * 512], B_wo.dsem, writes=[B_wo])
            sp.dma(wbc[:], C["mix_post_w"].rearrange("(o d) -> o d", o=1).to_broadcast([128, D]), B_wo.dsem,
                   writes=[B_wo])
            dve.op(lambda: nc.vector.memset(st[:], 0.0), writes=[B_st])
            for t in range(NT):
                o = t % 2
                ts = slice(t * 128, (t + 1) * 128)
                sp.dma(hst[:, o, :], C["h1"][ts, :], B_hst[o].dsem, writes=[B_hst[o]])
                for nb in range(4):
                    i = o * 4 + nb
                    pe.pre(reads=[B_mT[t], B_wo], writes=[B_po[i]])
                    for k in range(16):
                        ins = nc.tensor.matmul(po[i][:, :], lhsT=mT[:, k, ts], rhs=wo[:, k, nb * 512:(nb + 1) * 512],
                                               start=(k == 0), stop=(k == 15))
                    ev = pe.done(ins)
                    pe.post(ev, reads=[B_mT[t], B_wo], writes=[B_po[i]])
                    act.op(lambda: nc.scalar.activation(out=junk[:], in_=po[i][:, :], func=AF.Square,
                                                        accum_out=st[:, t * 8 + nb:t * 8 + nb + 1]),
                           reads=[B_po[i], B_st], writes=[B_junk, B_st])
                c = t * 8
                dve.op(lambda: nc.vector.tensor_reduce(out=st[:, c + 4:c + 5], in_=st[:, c:c + 4], axis=AX.X, op=ALU.add),
                       reads=[B_st], writes=[B_st])
                act.op(lambda: nc.scalar.activation(out=st[:, c + 5:c + 6], in_=st[:, c + 4:c + 5], func=AF.Sqrt,
                                                    scale=1.0 / D, bias=EPS), reads=[B_st], writes=[B_st])
                dve.op(lambda: nc.vector.reciprocal(out=st[:, c + 5:c + 6], in_=st[:, c + 5:c + 6]), reads=[B_st],
                       writes=[B_st])
                for nb in range(4):
                    i = o * 4 + nb
                    cs = slice(nb * 512, (nb + 1) * 512)
                    dve.op(lambda: nc.vector.scalar_tensor_tensor(out=ot[:, o, cs], in0=po[i][:, :],
                                                                  scalar=st[:, c + 5:c + 6], in1=wbc[:, cs],
                                                                  op0=ALU.mult, op1=ALU.mult),
                           reads=[B_po[i], B_st, B_wo], writes=[B_ot[o]])
                pool.op(lambda: nc.gpsimd.tensor_tensor(out=ot[:, o, :], in0=ot[:, o, :], in1=hst[:, o, :], op=ALU.add),
                        reads=[B_hst[o], B_ot[o]], writes=[B_ot[o]])
                sp.dma(C["h2"][ts, :], ot[:, o, :], B_ot[o].dsem, reads=[B_ot[o]])
            K.barrier()


WNAMES = [("ffn1_pre_w", [D]), ("ffn1_w_gate", [D, DFF]), ("ffn1_w_up", [D, DFF]), ("ffn1_w_down", [DFF, D]),
          ("ffn1_post_w", [D]), ("mix_pre_w", [D]), ("w_in", [D, DIN]), ("gla_gate_w2", [16, 512]),
          ("gla_gate_b", [512]), ("gla_norm_w", [256]), ("w_proj_attn", [1024, D]), ("w_proj_gla", [1024, D]),
          ("w_out", [D, D]), ("mix_post_w", [D]), ("ffn2_pre_w", [D]), ("ffn2_w_gate", [D, DFF]),
          ("ffn2_w_up", [D, DFF]), ("ffn2_w_down", [DFF, D]), ("ffn2_post_w", [D])]
NPOOL_ROWS = 2560 * 128


def build(stage=99, debug=False):
    K = Kern()
    nc = K.nc
    C = {}
    full = stage >= 4
    x = K.dram("x", [TOK, D], F32, "ExternalInput").ap()
    C["posv"] = K.dram("posv", [128, NT], F32, "ExternalInput").ap()
    K.used = WNAMES[:5] if stage == 1 else (WNAMES[:9] if stage in (2, 3) else WNAMES)
    for name, shape in K.used:
        C[name] = K.dram(name, shape, F32, "ExternalInput").ap()
    y = K.dram("y", [TOK, D], F32, "ExternalOutput").ap()
    C["ko"] = K.dram("ko", [TOK, 256], F32, "ExternalOutput").ap()
    C["vo"] = K.dram("vo", [TOK, 256], F32, "ExternalOutput").ap()
    C["kio"] = K.dram("kio", [TOK, 64], F32, "ExternalOutput").ap()
    dk = "ExternalOutput" if debug else "Internal"
    C["h1"] = K.dram("h1s", [TOK, D], F32, dk).ap()
    C["h2"] = K.dram("h2s", [TOK, D], F32, dk).ap()
    C["cmask"] = K.dram("cmask", [128, 512], F32, "ExternalInput").ap()
    if stage == 3:
        C["dbg"] = K.dram("dbg", [TOK, 1024], F32, "ExternalOutput").ap()
        C["dbg2"] = K.dram("dbg2", [128, 4136], F32, "ExternalOutput").ap()
    for nm, r, c in (("agk", 256, 512), ("agki", 64, 512), ("agv", 1024, 128), ("dec", 128, 32)):
        C[nm + "_in"] = K.dram(nm + "_in", [r, c], F32).ap()
        C[nm + "_out"] = K.dram(nm + "_out", [4 * r, c], F32).ap()
    if full:
        C["onehot"] = K.dram("onehot", [128, 4], F32, "ExternalInput").ap()
        C["pt"] = K.dram("pt", [16, 16], I32, "ExternalInput").ap()
        C["state_in"] = K.dram("state_in", [16 * 512, 256], F32, "ExternalInput").ap()
        C["cache_k"] = K.dram("cache_k", [NPOOL_ROWS, 256], F32, "ExternalInput").ap()
        C["cache_v"] = K.dram("cache_v", [NPOOL_ROWS, 256], F32, "ExternalInput").ap()
        C["cache_kidx"] = K.dram("cache_kidx", [NPOOL_ROWS, 64], F32, "ExternalInput").ap()
        C["gla_p"] = K.dram("gla_p", [512, 256], F32, "ExternalOutput").ap()
        C["gla_s"] = K.dram("gla_s", [16 * 512, 256], F32, "ExternalOutput").ap()
        C["gst_in"] = [K.dram(f"gst_in{t}", [512, 256], F32).ap() for t in range(8)]
        C["gst_out"] = [K.dram(f"gst_out{t}", [2048, 256], F32).ap() for t in range(8)]
        C["sscr"] = K.dram("sscr", [16, 2176], F32).ap()
        C["oscr"] = K.dram("oscr", [16, 1024], F32).ap()
        C["oat_d"] = K.dram("oat_d", [TOK, 512], F32, dk).ap()
        C["og_d"] = K.dram("og_d", [TOK, 512], F32, dk).ap()

    with ExitStack() as es0:
        ident = es0.enter_context(nc.sbuf_tensor("ident", [128, 128], BF16))
        C["ident"] = ident
        K.B_ident = Buf()
        K.pool.op(lambda: nc.gpsimd.memset(ident[:], 1.0), writes=[K.B_ident])
        K.pool.op(lambda: nc.gpsimd.affine_select(out=ident[:], in_=ident[:], pattern=[[-1, 128]],
                                                  compare_op=ALU.is_equal, fill=0.0, base=0, channel_multiplier=1),
                  reads=[K.B_ident], writes=[K.B_ident])
        K.barrier()

        ffn_phase(K, x, (y if stage == 1 else C["h1"]), C["ffn1_pre_w"], C["ffn1_w_gate"], C["ffn1_w_up"],
                  C["ffn1_w_down"], C["ffn1_post_w"], ident)
        if stage >= 2:
            with ExitStack() as es:
                def sb(n, s, d):
                    return es.enter_context(nc.sbuf_tensor(n, s, d))
                P = {}
                P["qT"] = sb("p_qT", [128, 8, TOK], BF16)
                P["qiT"] = sb("p_qiT", [128, 8, TOK], BF16)
                P["wi"] = sb("p_wi", [128, NT, 16], F32)
                P["qgT"] = sb("p_qgT", [128, 4, TOK], BF16)
                P["gv"] = sb("p_gv", [128, NT, 1024], BF16)
                P["dec"] = sb("p_dec", [128, 4, 8], F32)
                P["dec8"] = sb("p_dec8", [128, 4, 128], F32)
                P["kg8f"] = sb("p_kg8f", [128, 4, 128], F32)
                P["kT8"] = sb("p_kT8", [128, 256], BF16)
                P["v8"] = sb("p_v8", [128, 256], BF16)
                P["kiT8"] = sb("p_kiT8", [128, 128], BF16)
                if full:
                    P["AT"] = sb("p_AT", [128, 8, 4, 128], BF16)
                C["P"] = P
                for n in ("B_qT", "B_qiT", "B_wi", "B_qgT", "B_kgT", "B_khat", "B_gv", "B_s8", "B_AT", "B_og",
                          "B_ag1", "B_ag1o", "B_decin", "B_deco", "B_sscr", "B_oscr"):
                    C[n] = Buf()
                C["B_dec"] = Buf(K, dma=True, persist=True)
                C["B_gin"] = [Buf() for _ in range(8)]
                C["B_gout"] = [Buf() for _ in range(8)]
                with ExitStack() as esk:
                    P["kgT"] = esk.enter_context(nc.sbuf_tensor("p_kgT", [128, 4, TOK], BF16))
                    P["khat"] = esk.enter_context(nc.sbuf_tensor("p_khat", [128, NT, 512], BF16))
                    phase_a(K, C)
                    if full:
                        phase_g1(K, C)
                if stage >= 3:
                    P["oat"] = sb("p_oat", [128, NT, 1024], BF16)
                    C["B_oat"] = Buf(K, dma=True, persist=True)
                    K.dve.op(lambda: nc.vector.memset(P["oat"][:, 8, :], 0.0), writes=[C["B_oat"]])
                    phase_b(K, C)
                if stage == 3:
                    K.pool.dma(C["dbg"].rearrange("(t p) c -> p t c", p=128), P["oat"][:], C["B_oat"].dsem,
                               reads=[C["B_oat"]])
                if full:
                    phase_bs(K, C)
                    K.sp.dma(C["oat_d"].bitcast(BF16).rearrange("(t p) c -> p t c", p=128), P["oat"][:],
                             C["B_oat"].dsem, reads=[C["B_oat"]])
                    P["og"] = sb("p_og", [128, NT, 1024], BF16)
                    C["B_og"] = Buf(K, dma=True, persist=True)
                    K.dve.op(lambda: nc.vector.memset(P["og"][:, 8, :], 0.0), writes=[C["B_og"]])
                    phase_g2(K, C)
                    K.sp.dma(C["og_d"].bitcast(BF16).rearrange("(t p) c -> p t c", p=128), P["og"][:],
                             C["B_og"].dsem, reads=[C["B_og"]])
                K.barrier()
            if full:
                phase_c(K, C)
                ffn_phase(K, C["h2"], y, C["ffn2_pre_w"], C["ffn2_w_gate"], C["ffn2_w_up"], C["ffn2_w_down"],
                          C["ffn2_post_w"], ident, tag="f2")
    K.barrier()
    K.es.close()
    return K


def core_rows(x_prompt, x_sample, c):
    b, j = c // 4, c % 4
    rows = [x_prompt[b, (4 * s + j) * 128:(4 * s + j + 1) * 128] for s in range(8)]
    pad = np.zeros((128, x_sample.shape[-1]), np.float32)
    pad[:16] = x_sample[16 * c:16 * c + 16, 0]
    rows.append(pad)
    return np.ascontiguousarray(np.concatenate(rows, 0))


def make_in_maps(inputs, used=WNAMES, full=True):
    in_maps = []
    shared = {k: np.ascontiguousarray(inputs[k], dtype=np.float32) for k, _ in used}
    if full:
        shared["cache_k"] = np.ascontiguousarray(inputs["cache_k"]).reshape(NPOOL_ROWS, 256)
        shared["cache_v"] = np.ascontiguousarray(inputs["cache_v"]).reshape(NPOOL_ROWS, 256)
        shared["cache_kidx"] = np.ascontiguousarray(inputs["cache_kidx"]).reshape(NPOOL_ROWS, 64)
    for c in range(NCORES):
        j = c % 4
        m = {"x": core_rows(inputs["x_prompt"], inputs["x_sample"], c)}
        posv = np.zeros((128, NT), np.float32)
        for s in range(8):
            posv[:, s] = (4 * s + j) * 128 + np.arange(128)
        posv[:, 8] = 2048.0
        m["posv"] = posv
        cm = np.zeros((128, 4, 128), np.float32)
        for r in range(4):
            if r > j:
                cm[:, r, :] = -1.0e4
            elif r == j:
                cm[:, r, :] = np.where(np.arange(128)[None, :] > np.arange(128)[:, None], -1.0e4, 0.0)
        m["cmask"] = cm.reshape(128, 512)
        if full:
            oh = np.zeros((128, 4), np.float32)
            oh[:, j] = 1.0
            m["onehot"] = oh
            m["pt"] = np.ascontiguousarray(inputs["page_table"][16 * c:16 * c + 16], dtype=np.int32)
            m["state_in"] = np.ascontiguousarray(inputs["state_gla"][16 * c:16 * c + 16], dtype=np.float32).reshape(
                16 * 512, 256)
        m.update(shared)
        in_maps.append(m)
    return in_maps


def assemble(results):
    y_p = np.zeros((2, 4096, D), np.float32)
    y_s = np.zeros((128, 1, D), np.float32)
    k_p = np.zeros((2, 4096, 2, 128), np.float32)
    v_p = np.zeros((2, 4096, 2, 128), np.float32)
    ki_p = np.zeros((2, 4096, 64), np.float32)
    k_s = np.zeros((128, 1, 2, 128), np.float32)
    v_s = np.zeros((128, 1, 2, 128), np.float32)
    ki_s = np.zeros((128, 1, 64), np.float32)
    gla_p = np.zeros((2, 4, 128, 256), np.float32)
    gla_s = np.zeros((128, 4, 128, 256), np.float32)
    for c, r in enumerate(results):
        b, j = c // 4, c % 4
        for s in range(8):
            sl = slice((4 * s + j) * 128, (4 * s + j + 1) * 128)
            rs = slice(s * 128, (s + 1) * 128)
            y_p[b, sl] = r["y"][rs]
            k_p[b, sl] = r["ko"][rs].reshape(128, 2, 128)
            v_p[b, sl] = r["vo"][rs].reshape(128, 2, 128)
            ki_p[b, sl] = r["kio"][rs]
        ss = slice(16 * c, 16 * c + 16)
        y_s[ss, 0] = r["y"][1024:1040]
        k_s[ss, 0] = r["ko"][1024:1040].reshape(16, 2, 128)
        v_s[ss, 0] = r["vo"][1024:1040].reshape(16, 2, 128)
        ki_s[ss, 0] = r["kio"][1024:1040]
        gla_s[ss] = r["gla_s"].reshape(16, 4, 128, 256)
        if j == 0:
            gla_p[b] = r["gla_p"].reshape(4, 128, 256)
    return (y_p, y_s, k_p, v_p, ki_p, gla_p, k_s, v_s, ki_s, gla_s)


def kernel(**inputs):
    K = build()
    in_maps = make_in_maps(inputs, K.used, True)
    res = run_bass_kernel_spmd(K.nc, in_maps, core_ids=list(range(NCORES)))
    return assemble(res.results)
```

```python
import numpy as np
from contextlib import ExitStack
import concourse.bass as bass
import concourse.mybir as mybir
from concourse.bass_utils import run_bass_kernel_spmd

F32 = mybir.dt.float32
BF16 = mybir.dt.bfloat16
I32 = mybir.dt.int32
ALU = mybir.AluOpType
AF = mybir.ActivationFunctionType
AX = mybir.AxisListType

NCORES = 8
NT = 9
TOK = NT * 128
D = 2048
DFF = 5632
DIN = 9824
EPS = 1e-6
TB = [(0, 512), (512, 512), (1024, 128)]


class Sem:
    def __init__(self, h, uid):
        self.h = h
        self.uid = uid
        self.cnt = 0


class Ev:
    __slots__ = ("sem", "val", "q", "idx")

    def __init__(self, sem, val, q=None, idx=0):
        self.sem = sem
        self.val = val
        self.q = q
        self.idx = idx


class Buf:
    def __init__(self, K=None, dma=False, persist=False):
        self.w = None
        self.r = {}
        self.dsem = K.new_sem("d", persist) if dma else None


class Q:
    def __init__(self, K, eng, name):
        self.K = K
        self.eng = eng
        self.name = name
        self.sem = K.new_sem(name, True)
        self.seen = {}
        self.nins = 0

    def wait(self, *evs):
        for ev in evs:
            if ev is None:
                continue
            if ev.q is self and self.nins - ev.idx >= 4:
                continue
            if self.seen.get(ev.sem.uid, -1) >= ev.val:
                continue
            self.seen[ev.sem.uid] = ev.val
            self.eng.wait_ge(ev.sem.h, ev.val)

    def done(self, ins):
        self.sem.cnt += 1
        self.nins += 1
        ins.then_inc(self.sem.h, 1)
        return Ev(self.sem, self.sem.cnt, self, self.nins)

    def tick(self, n=1):
        self.nins += n

    def pre(self, reads=(), writes=()):
        for b in reads:
            self.wait(b.w)
        for b in writes:
            self.wait(b.w)
            self.wait(*b.r.values())

    def post(self, ev, reads=(), writes=()):
        for b in reads:
            b.r[ev.sem.uid] = ev
        for b in writes:
            b.w = ev
            b.r = {}

    def op(self, fn, reads=(), writes=()):
        self.pre(reads, writes)
        ev = self.done(fn())
        self.post(ev, reads, writes)
        return ev

    def dma(self, out, in_, sem, reads=(), writes=(), **kw):
        for b in reads:
            self.wait(b.w)
        for b in writes:
            if not (b.w is not None and b.w.sem is sem):
                self.wait(b.w)
            self.wait(*b.r.values())
        ins = self.eng.dma_start(out=out, in_=in_, **kw)
        sem.cnt += 16
        ins.then_inc(sem.h, 16)
        self.nins += 1
        ev = Ev(sem, sem.cnt)
        self.post(ev, reads, writes)
        self.K.dma_sems[sem.uid] = sem
        return ev


class Kern:
    def __init__(self):
        self.nc = bass.Bass("TRN2", target_bir_lowering=False)
        self.es = ExitStack()
        self.nsem = 0
        self.dma_sems = {}
        self.free_sems = []
        self.phase_sems = []
        nc = self.nc
        self.pe = Q(self, nc.tensor, "pe")
        self.act = Q(self, nc.scalar, "act")
        self.dve = Q(self, nc.vector, "dve")
        self.pool = Q(self, nc.gpsimd, "pool")
        self.sp = Q(self, nc.sync, "sp")
        self.queues = [self.pe, self.act, self.dve, self.pool, self.sp]

    def new_sem(self, name, persist=False):
        if not persist and self.free_sems:
            s = self.free_sems.pop()
        else:
            self.nsem += 1
            h = self.es.enter_context(self.nc.semaphore(f"{name}{self.nsem}"))
            s = Sem(h, self.nsem)
        if not persist:
            self.phase_sems.append(s)
        return s

    def end_phase(self):
        self.barrier()
        self.free_sems.extend(self.phase_sems)
        self.phase_sems = []

    def barrier(self):
        evs = []
        for q in self.queues:
            if q.sem.cnt > 0:
                evs.append(Ev(q.sem, q.sem.cnt))
        for s in self.dma_sems.values():
            if s.cnt > 0:
                evs.append(Ev(s, s.cnt))
        for q in self.queues:
            q.wait(*evs)

    def dram(self, name, shape, dt, kind="Internal"):
        return self.nc.dram_tensor(name, list(shape), dt, kind=kind)


def ffn_phase(K, src, dst, pre_w, wg, wu, wd, post_w, ident, tag="f1"):
    nc = K.nc
    pe, act, dve, pool, sp = K.pe, K.act, K.dve, K.pool, K.sp
    NG = DFF // 256
    with ExitStack() as es:
        def sb(n, s, d):
            return es.enter_context(nc.sbuf_tensor(tag + n, s, d))

        def ps(n, s, d=F32):
            return es.enter_context(nc.psum_tensor(tag + n, s, d))

        acc = sb("f_acc", [128, NT, D], F32)
        zT = sb("f_zT", [128, 16, TOK], BF16)
        xst = sb("f_xst", [128, D], F32)
        zb = sb("f_zb", [128, D], BF16)
        wbc = sb("f_wbc", [128, D], F32)
        wgb = sb("f_wgb", [128, 2, 16, 256], BF16)
        wub = sb("f_wub", [128, 2, 16, 256], BF16)
        wdb = sb("f_wdb", [128, 3, 2, D], BF16)
        aT = sb("f_aT", [128, 2, 2, TOK], BF16)
        sg = sb("f_sg", [128, 2, 512], BF16)
        dtmp = sb("f_dtmp", [128, 2, 512], F32)
        B_dtmp = [Buf(), Buf()]
        st = sb("f_st", [128, 4 * NT], F32)
        ptr = [ps(f"f_ptr{i}", [128, 1024], BF16) for i in range(2)]
        pg = [ps(f"f_pg{i}", [128, 512]) for i in range(2)]
        pu = [ps(f"f_pu{i}", [128, 512]) for i in range(2)]
        pd = [ps(f"f_pd{i}", [128, 512]) for i in range(2)]

        B_xst = Buf(K, dma=True)
        B_zb = Buf()
        B_wbc = Buf(K, dma=True)
        B_st = Buf()
        B_ptr = [Buf(), Buf()]
        B_zT = [Buf() for _ in range(NT)]
        B_wgu = [Buf(K, dma=True) for _ in range(2)]
        B_wd = [Buf(K, dma=True) for _ in range(3)]
        B_aT = [[[Buf() for _ in range(3)] for _ in range(2)] for _ in range(2)]
        B_pg = [Buf(), Buf()]
        B_pu = [Buf(), Buf()]
        B_sg = [Buf(), Buf()]
        B_pd = [Buf(), Buf()]
        B_acc = [[Buf() for _ in range(4)] for _ in range(NT)]
        B_accst = [Buf(K, dma=True) for _ in range(NT)]

        wg_v = wg.rearrange("(k p) f -> p k f", p=128)
        wu_v = wu.rearrange("(k p) f -> p k f", p=128)
        wd_v = wd.rearrange("(c p) n -> p c n", p=128)

        def load_group(gi):
            s2, s3 = gi % 2, gi % 3
            c0 = gi * 256
            pool.dma(wgb[:, s2], wg_v[:, :, c0:c0 + 256], B_wgu[s2].dsem, writes=[B_wgu[s2]])
            pool.dma(wub[:, s2], wu_v[:, :, c0:c0 + 256], B_wgu[s2].dsem, writes=[B_wgu[s2]])
            pool.dma(wdb[:, s3], wd_v[:, 2 * gi:2 * gi + 2, :], B_wd[s3].dsem, writes=[B_wd[s3]])

        sp.dma(wbc[:], pre_w.rearrange("(o d) -> o d", o=1).to_broadcast([128, D]), B_wbc.dsem, writes=[B_wbc])
        load_group(0)
        dve.op(lambda: nc.vector.memset(st[:], 0.0), writes=[B_st])

        for t in range(NT):
            sp.dma(xst[:], src[t * 128:(t + 1) * 128, :], B_xst.dsem, writes=[B_xst])
            act.op(lambda: nc.scalar.activation(out=zb[:], in_=xst[:], func=AF.Square,
                                                accum_out=st[:, t:t + 1]),
                   reads=[B_xst], writes=[B_zb, B_st])
            act.op(lambda: nc.scalar.activation(out=st[:, NT + t:NT + t + 1], in_=st[:, t:t + 1], func=AF.Sqrt,
                                                scale=1.0 / D, bias=EPS),
                   reads=[B_st], writes=[B_st])
            dve.op(lambda: nc.vector.reciprocal(out=st[:, NT + t:NT + t + 1], in_=st[:, NT + t:NT + t + 1]),
                   reads=[B_st], writes=[B_st])
            dve.op(lambda: nc.vector.scalar_tensor_tensor(out=zb[:], in0=xst[:], scalar=st[:, NT + t:NT + t + 1],
                                                          in1=wbc[:], op0=ALU.mult, op1=ALU.mult),
                   reads=[B_xst, B_st, B_wbc], writes=[B_zb])
            for q4 in range(4):
                sl = q4 % 2
                pe.pre(reads=[B_zb, K.B_ident], writes=[B_ptr[sl]])
                for i in range(4):
                    k = q4 * 4 + i
                    ins = nc.tensor.transpose(ptr[sl][:, i * 128:(i + 1) * 128], zb[:, k * 128:(k + 1) * 128], ident[:])
                    pe.tick()
                ev = pe.done(ins)
                pe.nins -= 1
                pe.post(ev, reads=[B_zb], writes=[B_ptr[sl]])
                src_ap = ptr[sl][:, 0:512].rearrange("p (a b) -> p a b", a=4)
                dst_ap = zT[:, q4 * 4:q4 * 4 + 4, t * 128:(t + 1) * 128]
                if q4 % 2 == 0:
                    act.op(lambda: nc.scalar.copy(out=dst_ap, in_=src_ap), reads=[B_ptr[sl]], writes=[B_zT[t]])
                else:
                    dve.op(lambda: nc.vector.tensor_copy(dst_ap, src_ap), reads=[B_ptr[sl]], writes=[B_zT[t]])

        sp.dma(wbc[:], post_w.rearrange("(o d) -> o d", o=1).to_broadcast([128, D]), B_wbc.dsem, writes=[B_wbc])

        tiles_of_tb = [[0, 1, 2, 3], [4, 5, 6, 7], [8]]
        down_units = []

        def emit_down(gi, t, nb, idx):
            s2, s3 = gi % 2, gi % 3
            tb = t // 4
            sl = idx % 2
            pe.pre(reads=[B_aT[s2][0][tb], B_aT[s2][1][tb], B_wd[s3]], writes=[B_pd[sl]])
            for ci in range(2):
                ins = nc.tensor.matmul(pd[sl][:], lhsT=aT[:, s2, ci, t * 128:(t + 1) * 128],
                                       rhs=wdb[:, s3, ci, nb * 512:(nb + 1) * 512],
                                       start=(ci == 0), stop=(ci == 1))
                pe.tick()
            ev = pe.done(ins)
            pe.nins -= 1
            pe.post(ev, reads=[B_aT[s2][0][tb], B_aT[s2][1][tb], B_wd[s3]], writes=[B_pd[sl]])
            a_ap = acc[:, t, nb * 512:(nb + 1) * 512]
            if gi == 0:
                dve.op(lambda: nc.vector.tensor_copy(a_ap, pd[sl][:]), reads=[B_pd[sl]], writes=[B_acc[t][nb]])
            elif idx % 4 == 3:
                x = (idx // 4) % 2
                act.op(lambda: nc.scalar.copy(out=dtmp[:, x, :], in_=pd[sl][:]), reads=[B_pd[sl]], writes=[B_dtmp[x]])
                pool.op(lambda: nc.gpsimd.tensor_tensor(out=a_ap, in0=a_ap, in1=dtmp[:, x, :], op=ALU.add),
                        reads=[B_dtmp[x], B_acc[t][nb]], writes=[B_acc[t][nb]])
            else:
                dve.op(lambda: nc.vector.tensor_tensor(out=a_ap, in0=a_ap, in1=pd[sl][:], op=ALU.add),
                       reads=[B_pd[sl], B_acc[t][nb]], writes=[B_acc[t][nb]])

        didx = 0
        for gi in range(NG):
            s2 = gi % 2
            if gi + 1 < NG:
                load_group(gi + 1)
            step = 0
            for ci in range(2):
                for tbi, (t0, tn) in enumerate(TB):
                    sl = step % 2
                    zdeps = [B_zT[t] for t in tiles_of_tb[tbi]]
                    for (pp, Bp, wb) in ((pg, B_pg, wgb), (pu, B_pu, wub)):
                        pe.pre(reads=zdeps + [B_wgu[s2]], writes=[Bp[sl]])
                        for k in range(16):
                            ins = nc.tensor.matmul(pp[sl][:, 0:tn], lhsT=wb[:, s2, k, ci * 128:(ci + 1) * 128],
                                                   rhs=zT[:, k, t0:t0 + tn], start=(k == 0), stop=(k == 15))
                            pe.tick()
                        ev = pe.done(ins)
                        pe.nins -= 1
                        pe.post(ev, reads=zdeps + [B_wgu[s2]], writes=[Bp[sl]])
                    act.op(lambda: nc.scalar.activation(out=sg[:, sl, 0:tn], in_=pg[sl][:, 0:tn], func=AF.Silu),
                           reads=[B_pg[sl]], writes=[B_sg[sl]])
                    dve.op(lambda: nc.vector.tensor_tensor(out=aT[:, s2, ci, t0:t0 + tn], in0=sg[:, sl, 0:tn],
                                                           in1=pu[sl][:, 0:tn], op=ALU.mult),
                           reads=[B_sg[sl], B_pu[sl]], writes=[B_aT[s2][ci][tbi]])
                    step += 1
                    for _ in range(6):
                        if down_units:
                            g0, t, nb = down_units.pop(0)
                            emit_down(g0, t, nb, didx)
                            didx += 1
            down_units = [(gi, t, nb) for t in range(NT) for nb in range(4)]
        while down_units:
            g0, t, nb = down_units.pop(0)
            emit_down(g0, t, nb, didx)
            didx += 1

        for t in range(NT):
            sp.dma(xst[:], src[t * 128:(t + 1) * 128, :], B_xst.dsem, writes=[B_xst])
            act.op(lambda: nc.scalar.activation(out=zb[:], in_=acc[:, t, :], func=AF.Square,
                                                accum_out=st[:, 2 * NT + t:2 * NT + t + 1]),
                   reads=B_acc[t], writes=[B_zb, B_st])
            c = 3 * NT + t
            act.op(lambda: nc.scalar.activation(out=st[:, c:c + 1], in_=st[:, 2 * NT + t:2 * NT + t + 1], func=AF.Sqrt,
                                                scale=4.0 / D, bias=4.0 * EPS),
                   reads=[B_st], writes=[B_st])
            dve.op(lambda: nc.vector.reciprocal(out=st[:, c:c + 1], in_=st[:, c:c + 1]),
                   reads=[B_st], writes=[B_st])
            dve.op(lambda: nc.vector.scalar_tensor_tensor(out=acc[:, t, :], in0=acc[:, t, :], scalar=st[:, c:c + 1],
                                                          in1=wbc[:], op0=ALU.mult, op1=ALU.mult),
                   reads=[B_st, B_wbc] + B_acc[t], writes=B_acc[t])
            dve.op(lambda: nc.vector.tensor_tensor(out=acc[:, t, :], in0=acc[:, t, :], in1=xst[:], op=ALU.add),
                   reads=[B_xst] + B_acc[t], writes=B_acc[t])
            sp.dma(dst[t * 128:(t + 1) * 128, :], acc[:, t, :], B_accst[t].dsem, reads=B_acc[t])
        K.end_phase()


import math
LN_THETA = math.log(10000.0)
PI = math.pi
CQ, CK, CV, CQI, CKI, CWI, CGQ, CGK, CGV, CGLR, CGR, CGA, CGG = (
    0, 1024, 1280, 1536, 2560, 2624, 2640, 3152, 3664, 4688, 4704, 5728, 7776)
AG1_ROWS = 576
SQ = 1.0 / math.sqrt(128.0)


def all_gather(K, src, dst, rbufs, wbufs):
    pool = K.pool
    pool.pre(reads=rbufs, writes=wbufs)
    ins = K.nc.gpsimd.collective_compute("AllGather", ALU.bypass, replica_groups=[[0, 1, 2, 3], [4, 5, 6, 7]],
                                         ins=[src.opt()], outs=[dst.opt()])
    csem = K.new_sem("cc", True)
    ins.then_inc(csem.h)
    csem.cnt += 1
    pool.nins += 1
    ev = Ev(csem, 1)
    for b in rbufs:
        b.r[csem.uid] = ev
    for b in wbufs:
        b.r[csem.uid] = ev
    return ev


def norm_T(K, src, wvec, zT, B_zT, ident, ptr, B_ptr, tag):
    nc = K.nc
    pe, act, dve, pool, sp = K.pe, K.act, K.dve, K.pool, K.sp
    with ExitStack() as es:
        def sb(n, s, d):
            return es.enter_context(nc.sbuf_tensor(tag + n, s, d))
        xst = sb("xst", [128, D], F32)
        zb = sb("zb", [128, D], BF16)
        wbc = sb("wbc", [128, D], F32)
        st = sb("st", [128, 2 * NT], F32)
        B_xst = Buf(K, dma=True)
        B_zb = Buf()
        B_wbc = Buf(K, dma=True)
        B_st = Buf()
        sp.dma(wbc[:], wvec.rearrange("(o d) -> o d", o=1).to_broadcast([128, D]), B_wbc.dsem, writes=[B_wbc])
        dve.op(lambda: nc.vector.memset(st[:], 0.0), writes=[B_st])
        for t in range(NT):
            sp.dma(xst[:], src[t * 128:(t + 1) * 128, :], B_xst.dsem, writes=[B_xst])
            act.op(lambda: nc.scalar.activation(out=zb[:], in_=xst[:], func=AF.Square,
                                                accum_out=st[:, t:t + 1]),
                   reads=[B_xst], writes=[B_zb, B_st])
            act.op(lambda: nc.scalar.activation(out=st[:, NT + t:NT + t + 1], in_=st[:, t:t + 1], func=AF.Sqrt,
                                                scale=1.0 / D, bias=EPS),
                   reads=[B_st], writes=[B_st])
            dve.op(lambda: nc.vector.reciprocal(out=st[:, NT + t:NT + t + 1], in_=st[:, NT + t:NT + t + 1]),
                   reads=[B_st], writes=[B_st])
            dve.op(lambda: nc.vector.scalar_tensor_tensor(out=zb[:], in0=xst[:], scalar=st[:, NT + t:NT + t + 1],
                                                          in1=wbc[:], op0=ALU.mult, op1=ALU.mult),
                   reads=[B_xst, B_st, B_wbc], writes=[B_zb])
            for q4 in range(4):
                sl = q4 % 2
                pe.pre(reads=[B_zb, K.B_ident], writes=[B_ptr[sl]])
                for i in range(4):
                    k = q4 * 4 + i
                    ins = nc.tensor.transpose(ptr[sl][:, i * 128:(i + 1) * 128], zb[:, k * 128:(k + 1) * 128], ident[:])
                ev = pe.done(ins)
                pe.post(ev, reads=[B_zb], writes=[B_ptr[sl]])
                src_ap = ptr[sl][:, 0:512].rearrange("p (a b) -> p a b", a=4)
                dst_ap = zT[:, q4 * 4:q4 * 4 + 4, t * 128:(t + 1) * 128]
                if q4 % 2 == 0:
                    act.op(lambda: nc.scalar.copy(out=dst_ap, in_=src_ap), reads=[B_ptr[sl]], writes=[B_zT[t]])
                else:
                    dve.op(lambda: nc.vector.tensor_copy(dst_ap, src_ap), reads=[B_ptr[sl]], writes=[B_zT[t]])
        K.end_phase()


def phase_a(K, C):
    nc = K.nc
    pe, act, dve, pool, sp = K.pe, K.act, K.dve, K.pool, K.sp
    P = C["P"]
    ident = C["ident"]
    w_in = C["w_in"]
    win_v = w_in.rearrange("(k p) f -> p k f", p=128)
    ag_kT = C["agk_in"]
    ag_kiT = C["agki_in"]
    ag_v = C["agv_in"]
    with ExitStack() as es:
        def sb(n, s, d):
            return es.enter_context(nc.sbuf_tensor("a_" + n, s, d))

        def ps(n, s, d=F32):
            return es.enter_context(nc.psum_tensor("a_" + n, s, d))

        uT = sb("uT", [128, 16, TOK], BF16)
        B_uT = [Buf() for _ in range(NT)]
        ptr = [ps(f"ptr{i}", [128, 1024], BF16) for i in range(2)]
        B_ptr = [Buf(), Buf()]
        pp = [ps(f"pp{i}", [128, 512]) for i in range(2)]
        B_pp = [Buf(), Buf()]
        px = [ps(f"px{i}", [128, 512]) for i in range(2)]
        B_px = [Buf(), Buf()]
        norm_T(K, C["h1"], C["mix_pre_w"], uT, B_uT, ident, ptr, B_ptr, "a1_")

        tabs = sb("tabs", [128, NT, 384], F32)
        es_t = ExitStack()

        def sbt(n, s, d):
            return es_t.enter_context(nc.sbuf_tensor("a_" + n, s, d))
        posv = sbt("posv", [128, NT], F32)
        io = sbt("io", [128, 64], F32)
        inv = sbt("inv", [128, 96], F32)
        ang = sbt("ang", [128, NT, 96], F32)
        kf = sbt("kf", [128, NT, 96], F32)
        kint = sbt("kint", [128, NT, 96], I32)
        sn = sbt("sn", [128, NT, 96], F32)
        cs = sbt("cs", [128, NT, 96], F32)
        B_t = Buf(K, dma=True)
        sp.dma(posv[:], C["posv"], B_t.dsem, writes=[B_t])
        pool.op(lambda: nc.gpsimd.iota(io[:], pattern=[[1, 64]], base=0, channel_multiplier=0,
                                       allow_small_or_imprecise_dtypes=True), writes=[B_t])
        act.op(lambda: nc.scalar.activation(out=inv[:, 0:64], in_=io[:, 0:64], func=AF.Exp,
                                            scale=-2.0 * LN_THETA / 128.0), reads=[B_t], writes=[B_t])
        act.op(lambda: nc.scalar.activation(out=inv[:, 64:96], in_=io[:, 0:32], func=AF.Exp,
                                            scale=-2.0 * LN_THETA / 64.0), reads=[B_t], writes=[B_t])
        for s in range(NT):
            dve.op(lambda: nc.vector.tensor_scalar(out=ang[:, s, :], in0=inv[:], scalar1=posv[:, s:s + 1],
                                                   scalar2=None, op0=ALU.mult), reads=[B_t], writes=[B_t])

        def V(fn):
            return dve.op(fn, reads=[B_t], writes=[B_t])
        V(lambda: nc.vector.tensor_scalar(out=kf[:], in0=ang[:], scalar1=1.0 / (2 * PI), scalar2=None, op0=ALU.mult))
        V(lambda: nc.vector.tensor_copy(kint[:], kf[:]))
        V(lambda: nc.vector.tensor_copy(kf[:], kint[:]))
        V(lambda: nc.vector.scalar_tensor_tensor(out=ang[:], in0=kf[:], scalar=-2 * PI, in1=ang[:],
                                                 op0=ALU.mult, op1=ALU.add))
        act.op(lambda: nc.scalar.activation(out=sn[:], in_=ang[:], func=AF.Sin), reads=[B_t], writes=[B_t])
        V(lambda: nc.vector.tensor_scalar(out=ang[:], in0=ang[:], scalar1=PI / 2, scalar2=None, op0=ALU.add))
        V(lambda: nc.vector.tensor_scalar(out=kf[:], in0=ang[:], scalar1=PI, scalar2=-2 * PI,
                                          op0=ALU.is_gt, op1=ALU.mult))
        V(lambda: nc.vector.tensor_tensor(out=ang[:], in0=ang[:], in1=kf[:], op=ALU.add))
        act.op(lambda: nc.scalar.activation(out=cs[:], in_=ang[:], func=AF.Sin), reads=[B_t], writes=[B_t])
        V(lambda: nc.vector.tensor_copy(tabs[:, :, 0:64], cs[:, :, 0:64]))
        V(lambda: nc.vector.tensor_copy(tabs[:, :, 64:128], cs[:, :, 0:64]))
        V(lambda: nc.vector.tensor_scalar(out=tabs[:, :, 128:192], in0=sn[:, :, 0:64], scalar1=-1.0, scalar2=None,
                                          op0=ALU.mult))
        V(lambda: nc.vector.tensor_copy(tabs[:, :, 192:256], sn[:, :, 0:64]))
        V(lambda: nc.vector.tensor_copy(tabs[:, :, 256:288], cs[:, :, 64:96]))
        V(lambda: nc.vector.tensor_copy(tabs[:, :, 288:320], cs[:, :, 64:96]))
        V(lambda: nc.vector.tensor_scalar(out=tabs[:, :, 320:352], in0=sn[:, :, 64:96], scalar1=-1.0, scalar2=None,
                                          op0=ALU.mult))
        V(lambda: nc.vector.tensor_copy(tabs[:, :, 352:384], sn[:, :, 64:96]))
        B_tabs = B_t
        K.barrier()
        es_t.close()

        wbuf = sb("wbuf", [128, 2, 16, 512], BF16)
        B_w = [Buf(K, dma=True), Buf(K, dma=True)]
        xs = sb("xs", [128, 2, 512], F32)
        B_xs = [Buf(K, dma=True), Buf(K, dma=True)]
        t1 = sb("t1", [128, 2, 512], F32)
        B_t1 = [Buf(), Buf()]
        t2 = sb("t2", [128, 2, 512], F32)
        B_t2 = [Buf(), Buf()]
        ob = sb("ob", [128, 2, 512], BF16)
        B_ob = [Buf(K, dma=True), Buf(K, dma=True)]
        of = sb("of", [128, 2, 320], F32)
        B_of = [Buf(K, dma=True), Buf(K, dma=True)]
        tst = sb("tst", [128, 2, 256], BF16)
        B_tst = [Buf(K, dma=True), Buf(K, dma=True)]
        glrT = sb("glrT", [32, TOK], BF16)
        B_glr = Buf()
        w2b = sb("w2b", [16, 512], BF16)
        negb = sb("negb", [128, 4], F32)
        B_c = Buf(K, dma=True)
        ones = sb("ones", [128, 128], F32)
        eT = sb("eT", [128, 512], F32)
        lT = sb("lT", [128, 512], F32)
        cT = eT
        B_e = Buf()
        B_l = Buf()
        B_cT = B_e
        E1 = sb("E1", [128, 512], F32)
        E2 = sb("E2", [128, 512], F32)
        B_E = [Buf()] * 3
        khT = sb("khT", [128, 512], BF16)
        B_khT = Buf()
        cnt = {"blk": 0, "pp": 0, "xs": 0, "tr": 0, "of": 0, "tst": 0, "px": 0}

        pool.dma(w2b[:], C["gla_gate_w2"], B_c.dsem, writes=[B_c])
        sp.dma(negb[:], C["gla_gate_b"].rearrange("(h p) -> p h", p=128), B_c.dsem, writes=[B_c],
               allow_slow_non_contiguous=True)
        dve.op(lambda: nc.vector.tensor_scalar(out=negb[:], in0=negb[:], scalar1=-1.0, scalar2=None, op0=ALU.mult),
               reads=[B_c], writes=[B_c])
        dve.op(lambda: nc.vector.memset(ones[:], 1.0), writes=[B_c])

        def load_w(pieces):
            slot = cnt["blk"] % 2
            cnt["blk"] += 1
            off = 0
            for (c0, w) in pieces:
                pool.dma(wbuf[:, slot, :, off:off + w], win_v[:, :, c0:c0 + w], B_w[slot].dsem, writes=[B_w[slot]])
                off += w
            return slot

        def mm_tok(slot, t, width):
            i = cnt["pp"] % 2
            cnt["pp"] += 1
            pe.pre(reads=[B_uT[t], B_w[slot]], writes=[B_pp[i]])
            for k in range(16):
                ins = nc.tensor.matmul(pp[i][:, 0:width], lhsT=uT[:, k, t * 128:(t + 1) * 128],
                                       rhs=wbuf[:, slot, k, 0:width], start=(k == 0), stop=(k == 15))
            ev = pe.done(ins)
            pe.post(ev, reads=[B_uT[t], B_w[slot]], writes=[B_pp[i]])
            return i

        def mm_feat(slot, off, m, tbi):
            t0, tn = TB[tbi]
            i = cnt["pp"] % 2
            cnt["pp"] += 1
            deps = [B_uT[t] for t in ([0, 1, 2, 3], [4, 5, 6, 7], [8])[tbi]]
            pe.pre(reads=deps + [B_w[slot]], writes=[B_pp[i]])
            for k in range(16):
                ins = nc.tensor.matmul(pp[i][0:m, 0:tn], lhsT=wbuf[:, slot, k, off:off + m],
                                       rhs=uT[:, k, t0:t0 + tn], start=(k == 0), stop=(k == 15))
            ev = pe.done(ins)
            pe.post(ev, reads=deps + [B_w[slot]], writes=[B_pp[i]])
            return i

        def evac(i, width):
            j = cnt["xs"] % 2
            cnt["xs"] += 1
            act.op(lambda: nc.scalar.copy(out=xs[:, j, 0:width], in_=pp[i][:, 0:width]),
                   reads=[B_pp[i]], writes=[B_xs[j]])
            return j

        def rope(j, c0, hs, nh, t, out_ap, B_out):
            w = nh * hs
            hh = hs // 2
            tb0 = 0 if hs == 128 else 256
            x3 = xs[:, j, c0:c0 + w].rearrange("p (h d) -> p h d", h=nh)
            cosb = tabs[:, t, tb0:tb0 + hs].unsqueeze(1).to_broadcast([128, nh, hs])
            sa = tabs[:, t, tb0 + hs:tb0 + hs + hh].unsqueeze(1).to_broadcast([128, nh, hh])
            sbb = tabs[:, t, tb0 + hs + hh:tb0 + 2 * hs].unsqueeze(1).to_broadcast([128, nh, hh])
            a3 = t1[:, j, 0:w].rearrange("p (h d) -> p h d", h=nh)
            b3 = t2[:, j, 0:w].rearrange("p (h d) -> p h d", h=nh)
            pool.op(lambda: nc.gpsimd.tensor_tensor(out=a3, in0=x3, in1=cosb, op=ALU.mult),
                    reads=[B_xs[j], B_tabs], writes=[B_t1[j]])
            dve.op(lambda: nc.vector.tensor_tensor(out=b3[:, :, 0:hh], in0=x3[:, :, hh:hs], in1=sa, op=ALU.mult),
                   reads=[B_xs[j], B_tabs], writes=[B_t2[j]])
            dve.op(lambda: nc.vector.tensor_tensor(out=b3[:, :, hh:hs], in0=x3[:, :, 0:hh], in1=sbb, op=ALU.mult),
                   reads=[B_xs[j], B_tabs], writes=[B_t2[j]])
            dve.op(lambda: nc.vector.tensor_tensor(out=out_ap, in0=t1[:, j, 0:w], in1=t2[:, j, 0:w], op=ALU.add),
                   reads=[B_t1[j], B_t2[j]], writes=[B_out])

        def transposes(j, nblk, rows, dst_fn, B_dst_fn):
            sl = cnt["tr"] % 2
            cnt["tr"] += 1
            pe.pre(reads=[B_ob[j], K.B_ident], writes=[B_ptr[sl]])
            for b in range(nblk):
                ins = nc.tensor.transpose(ptr[sl][0:rows, b * 128:(b + 1) * 128],
                                          ob[:, j, b * rows:(b + 1) * rows], ident[:])
            ev = pe.done(ins)
            pe.post(ev, reads=[B_ob[j]], writes=[B_ptr[sl]])
            return sl

        def pipelined(front, back, n=NT):
            st_ = {}
            st_[0] = front(0)
            for t in range(n):
                if t + 1 < n:
                    st_[t + 1] = front(t + 1)
                back(t, st_[t])

        for blk in range(2):
            slot = load_w([(CQ + blk * 512, 512)])

            def q_front(t, slot=slot):
                i = mm_tok(slot, t, 512)
                j = evac(i, 512)
                rope(j, 0, 128, 4, t, ob[:, j, :], B_ob[j])
                return j

            def q_back(t, j, blk=blk):
                sl = transposes(j, 4, 128, None, None)
                act.op(lambda: nc.scalar.copy(out=P["qT"][:, blk * 4:blk * 4 + 4, t * 128:(t + 1) * 128],
                                              in_=ptr[sl][:, 0:512].rearrange("p (a b) -> p a b", a=4)),
                       reads=[B_ptr[sl]], writes=[C["B_qT"]])
            pipelined(q_front, q_back)

        slot = load_w([(CK, 512)])

        def kv_front(t, slot=slot):
            i = mm_tok(slot, t, 512)
            j = evac(i, 512)
            o = cnt["of"] % 2
            cnt["of"] += 1
            rope(j, 0, 128, 2, t, of[:, o, 0:256], B_of[o])
            sp.dma(C["ko"][t * 128:(t + 1) * 128, :], of[:, o, 0:256], B_of[o].dsem, reads=[B_of[o]])
            sp.dma(C["vo"][t * 128:(t + 1) * 128, :], xs[:, j, 256:512], B_xs[j].dsem, reads=[B_xs[j]])
            pool.op(lambda: nc.gpsimd.tensor_copy(ob[:, j, 0:256], of[:, o, 0:256]), reads=[B_of[o]], writes=[B_ob[j]])
            pool.op(lambda: nc.gpsimd.tensor_copy(ob[:, j, 256:512], xs[:, j, 256:512]), reads=[B_xs[j]],
                    writes=[B_ob[j]])
            return j

        def kv_back(t, j):
            sl = transposes(j, 2, 128, None, None)
            if t < 8:
                q = cnt["tst"] % 2
                cnt["tst"] += 1
                act.op(lambda: nc.scalar.copy(out=tst[:, q, :], in_=ptr[sl][:, 0:256]), reads=[B_ptr[sl]],
                       writes=[B_tst[q]])
                for g in range(2):
                    sp.dma(ag_kT[g * 128:(g + 1) * 128, t * 64:(t + 1) * 64],
                           tst[:, q, g * 128:(g + 1) * 128].bitcast(F32),
                           B_tst[q].dsem, reads=[B_tst[q]], writes=[C["B_ag1"]])
                sp.dma(ag_v[t * 128:(t + 1) * 128, :], ob[:, j, 256:512].bitcast(F32), B_ob[j].dsem,
                       reads=[B_ob[j]], writes=[C["B_ag1"]])
            else:
                act.op(lambda: nc.scalar.copy(out=P["kT8"][:, :], in_=ptr[sl][:, 0:256]), reads=[B_ptr[sl]],
                       writes=[C["B_s8"]])
                pool.op(lambda: nc.gpsimd.tensor_copy(P["v8"][:, :], ob[:, j, 256:512]), reads=[B_ob[j]],
                        writes=[C["B_s8"]])
        pipelined(kv_front, kv_back)

        for blk in range(2):
            slot = load_w([(CQI + blk * 512, 512)])

            def qi_front(t, slot=slot):
                i = mm_tok(slot, t, 512)
                j = evac(i, 512)
                rope(j, 0, 64, 8, t, ob[:, j, :], B_ob[j])
                return j

            def qi_back(t, j, blk=blk):
                sl = transposes(j, 4, 128, None, None)
                act.op(lambda: nc.scalar.copy(out=P["qiT"][:, blk * 4:blk * 4 + 4, t * 128:(t + 1) * 128],
                                              in_=ptr[sl][:, 0:512].rearrange("p (a b) -> p a b", a=4)),
                       reads=[B_ptr[sl]], writes=[C["B_qiT"]])
            pipelined(qi_front, qi_back)

        slot = load_w([(CKI, 80)])

        def ki_front(t, slot=slot):
            i = mm_tok(slot, t, 80)
            j = evac(i, 80)
            o = cnt["of"] % 2
            cnt["of"] += 1
            rope(j, 0, 64, 1, t, of[:, o, 256:320], B_of[o])
            sp.dma(C["kio"][t * 128:(t + 1) * 128, :], of[:, o, 256:320], B_of[o].dsem, reads=[B_of[o]])
            dve.op(lambda: nc.vector.tensor_scalar(out=P["wi"][:, t, :], in0=xs[:, j, 64:80], scalar1=1.0 / 32.0,
                                                   scalar2=None, op0=ALU.mult), reads=[B_xs[j]], writes=[C["B_wi"]])
            pool.op(lambda: nc.gpsimd.tensor_copy(ob[:, j, 0:64], of[:, o, 256:320]), reads=[B_of[o]], writes=[B_ob[j]])
            pool.op(lambda: nc.gpsimd.tensor_copy(ob[:, j, 64:128], of[:, o, 256:320]), reads=[B_of[o]],
                    writes=[B_ob[j]])
            return j

        def ki_back(t, j):
            sl = transposes(j, 1, 128, None, None)
            if t < 8:
                q = cnt["tst"] % 2
                cnt["tst"] += 1
                act.op(lambda: nc.scalar.copy(out=tst[0:64, q, 0:128], in_=ptr[sl][0:64, 0:128]), reads=[B_ptr[sl]],
                       writes=[B_tst[q]])
                sp.dma(ag_kiT[:, t * 64:(t + 1) * 64], tst[0:64, q, 0:128].bitcast(F32), B_tst[q].dsem,
                       reads=[B_tst[q]], writes=[C["B_ag1"]])
            else:
                act.op(lambda: nc.scalar.copy(out=P["kiT8"][:, :], in_=ptr[sl][:, 0:128]), reads=[B_ptr[sl]],
                       writes=[C["B_s8"]])
        pipelined(ki_front, ki_back)

        for nm in ("agk", "agki", "agv"):
            all_gather(K, C[nm + "_in"], C[nm + "_out"], [C["B_ag1"]], [C["B_ag1o"]])

        for blk in range(2):
            slot = load_w([(CGV + blk * 512, 512)])
            for t in range(NT):
                i = mm_tok(slot, t, 512)
                act.op(lambda: nc.scalar.copy(out=P["gv"][:, t, blk * 512:(blk + 1) * 512], in_=pp[i][:, :]),
                       reads=[B_pp[i]], writes=[C["B_gv"]])

        slot = load_w([(CGLR, 16)])
        for tbi in range(3):
            t0, tn = TB[tbi]
            i = mm_feat(slot, 0, 16, tbi)
            act.op(lambda: nc.scalar.copy(out=glrT[0:16, t0:t0 + tn], in_=pp[i][0:16, 0:tn]), reads=[B_pp[i]],
                   writes=[B_glr])

        for h in range(4):
            slot = load_w([(CGQ + h * 128, 128), (CGK + h * 128, 128)])
            for tbi in range(3):
                t0, tn = TB[tbi]
                x = cnt["px"] % 2
                cnt["px"] += 1
                pe.op(lambda: nc.tensor.matmul(px[x][:, 0:tn], lhsT=w2b[:, h * 128:(h + 1) * 128],
                                               rhs=glrT[0:16, t0:t0 + tn], start=True, stop=True),
                      reads=[B_glr, B_c], writes=[B_px[x]])
                act.op(lambda: nc.scalar.activation(out=eT[:, 0:tn], in_=px[x][:, 0:tn], func=AF.Exp, scale=-1.0,
                                                    bias=negb[:, h:h + 1]), reads=[B_px[x], B_c], writes=[B_e])
                act.op(lambda: nc.scalar.activation(out=lT[:, 0:tn], in_=eT[:, 0:tn], func=AF.Ln, bias=1.0),
                       reads=[B_e], writes=[B_l])
                if tbi < 2:
                    for q4 in range(4):
                        dve.op(lambda: nc.vector.tensor_tensor_scan(out=cT[:, q4 * 128:(q4 + 1) * 128], data0=ones[:],
                                                                    data1=lT[:, q4 * 128:(q4 + 1) * 128], initial=0.0,
                                                                    op0=ALU.mult, op1=ALU.add),
                               reads=[B_l, B_c], writes=[B_cT])
                    act.op(lambda: nc.scalar.activation(out=E1[:, 0:tn], in_=cT[:, 0:tn], func=AF.Exp,
                                                        scale=-1.0 / 16.0), reads=[B_cT], writes=[B_E[tbi]])
                    act.op(lambda: nc.scalar.activation(out=E2[:, 0:tn], in_=cT[:, 0:tn], func=AF.Exp,
                                                        scale=1.0 / 16.0), reads=[B_cT], writes=[B_E[tbi]])
                    dve.op(lambda: nc.vector.tensor_copy(
                        P["dec"][:, h, tbi * 4:tbi * 4 + 4],
                        E1[:, 0:tn].rearrange("p (a b) -> p a b", a=4)[:, :, 127]),
                        reads=[B_E[tbi]], writes=[C["B_dec"]])
                else:
                    act.op(lambda: nc.scalar.activation(out=E1[:, 0:tn], in_=lT[:, 0:tn], func=AF.Exp,
                                                        scale=-1.0 / 16.0), reads=[B_l], writes=[B_E[tbi]])
                    dve.op(lambda: nc.vector.tensor_copy(P["dec8"][:, h, :], E1[:, 0:tn]),
                           reads=[B_E[tbi]], writes=[C["B_dec"]])
                i = mm_feat(slot, 0, 128, tbi)
                if tbi < 2:
                    dve.op(lambda: nc.vector.scalar_tensor_tensor(out=P["qgT"][:, h, t0:t0 + tn], in0=pp[i][:, 0:tn],
                                                                  scalar=SQ, in1=E1[:, 0:tn],
                                                                  op0=ALU.mult, op1=ALU.mult),
                           reads=[B_pp[i], B_E[tbi]], writes=[C["B_qgT"]])
                else:
                    dve.op(lambda: nc.vector.tensor_scalar(out=P["qgT"][:, h, t0:t0 + tn], in0=pp[i][:, 0:tn],
                                                           scalar1=SQ, scalar2=None, op0=ALU.mult),
                           reads=[B_pp[i]], writes=[C["B_qgT"]])
                i = mm_feat(slot, 128, 128, tbi)
                if tbi < 2:
                    dve.op(lambda: nc.vector.tensor_tensor(out=P["kgT"][:, h, t0:t0 + tn], in0=pp[i][:, 0:tn],
                                                           in1=E2[:, 0:tn], op=ALU.mult),
                           reads=[B_pp[i], B_E[tbi]], writes=[C["B_kgT"]])
                    dve.op(lambda: nc.vector.tensor_tensor(
                        out=khT[:, :].rearrange("p (a b) -> p a b", a=4),
                        in0=P["kgT"][:, h, t0:t0 + tn].rearrange("p (a b) -> p a b", a=4),
                        in1=P["dec"][:, h, tbi * 4:tbi * 4 + 4].unsqueeze(2).to_broadcast([128, 4, 128]),
                        op=ALU.mult), reads=[C["B_kgT"], C["B_dec"]], writes=[B_khT])
                    sl = cnt["tr"] % 2
                    cnt["tr"] += 1
                    pe.pre(reads=[B_khT, K.B_ident], writes=[B_ptr[sl]])
                    for b in range(4):
                        ins = nc.tensor.transpose(ptr[sl][:, b * 128:(b + 1) * 128], khT[:, b * 128:(b + 1) * 128],
                                                  ident[:])
                    ev = pe.done(ins)
                    pe.post(ev, reads=[B_khT], writes=[B_ptr[sl]])
                    act.op(lambda: nc.scalar.copy(out=P["khat"][:, tbi * 4:tbi * 4 + 4, h * 128:(h + 1) * 128],
                                                  in_=ptr[sl][:, 0:512].rearrange("p (a b) -> p a b", a=4)),
                           reads=[B_ptr[sl]], writes=[C["B_khat"]])
                else:
                    act.op(lambda: nc.scalar.copy(out=P["kgT"][:, h, t0:t0 + tn], in_=pp[i][:, 0:tn]),
                           reads=[B_pp[i]], writes=[C["B_kgT"]])
                    dve.op(lambda: nc.vector.tensor_copy(P["kg8f"][:, h, :], pp[i][:, 0:tn]),
                           reads=[B_pp[i]], writes=[C["B_kgT"]])
        K.end_phase()
NIT = 12
TOPK = 256
NEG = -1.0e4


def topk_threshold(K, S3, np_, junk3, st, B_S, B_junk, B_st, pw2, B_c):
    nc = K.nc
    dve = K.dve

    def V(fn, r=(), w=()):
        return dve.op(fn, reads=list(r), writes=list(w))
    V(lambda: nc.vector.tensor_scalar(out=st[0:np_, 8:8 + NIT], in0=pw2[0:np_, 0:NIT], scalar1=st[0:np_, 0:1],
                                      scalar2=None, op0=ALU.mult), r=[B_st, B_c], w=[B_st])
    V(lambda: nc.vector.tensor_scalar(out=st[0:np_, 1:2], in0=st[0:np_, 0:1], scalar1=-1.0, scalar2=None,
                                      op0=ALU.mult), r=[B_st], w=[B_st])
    V(lambda: nc.vector.memset(st[0:np_, 40:40 + NIT], 0.0), r=[B_st], w=[B_st])
    for k in range(NIT):
        V(lambda: nc.vector.tensor_tensor(out=st[0:np_, 2:3], in0=st[0:np_, 1:2], in1=st[0:np_, 8 + k:9 + k],
                                          op=ALU.add), r=[B_st], w=[B_st])
        V(lambda: nc.vector.tensor_scalar(out=junk3, in0=S3, scalar1=st[0:np_, 2:3], scalar2=0.0, op0=ALU.is_ge,
                                          op1=ALU.add, accum_out=st[0:np_, 40 + k:41 + k]),
          r=[B_S, B_st], w=[B_junk, B_st])
        V(lambda: nc.vector.tensor_scalar(out=st[0:np_, 4:5], in0=st[0:np_, 40 + k:41 + k], scalar1=TOPK - 0.5,
                                          scalar2=None, op0=ALU.is_ge), r=[B_st], w=[B_st])
        V(lambda: nc.vector.scalar_tensor_tensor(out=st[0:np_, 1:2], in0=st[0:np_, 4:5], scalar=st[0:np_, 8 + k:9 + k],
                                                 in1=st[0:np_, 1:2], op0=ALU.mult, op1=ALU.add), r=[B_st], w=[B_st])


def phase_b(K, C):
    nc = K.nc
    pe, act, dve, pool, sp = K.pe, K.act, K.dve, K.pool, K.sp
    P = C["P"]
    ident = C["ident"]
    with ExitStack() as es:
        def sb(n, s, d):
            return es.enter_context(nc.sbuf_tensor("b_" + n, s, d))

        def ps(n, s, d=F32):
            return es.enter_context(nc.psum_tensor("b_" + n, s, d))

        kT_all = sb("kT", [128, 2, 4, 1024], BF16)
        kiT2 = sb("kiT2", [128, 4, 1024], BF16)
        V1 = sb("V1", [128, 4, 8, 2, 130], BF16)
        S2 = sb("S", [128, 2, 4096], F32)
        selm = sb("selm", [128, 4096], BF16)
        selT = sb("selT", [128, 4, 8, 128], BF16)
        diagw = sb("diagw", [128, 16, 128], BF16)
        rh = sb("rh", [128, 3, 512], BF16)
        pex = sb("pex", [128, 3, 512], BF16)
        pm = sb("pm", [128, 3, 512], BF16)
        cmask = sb("cmask", [128, 512], F32)
        pw2 = sb("pw2", [128, 32], F32)
        st = sb("st", [128, 64], F32)
        rec = sb("rec", [128, 8], F32)
        psh = [ps(f"psh{i}", [128, 512]) for i in range(3)]
        psc = [ps(f"psc{i}", [128, 512]) for i in range(2)]
        ptr = [ps(f"ptr{i}", [128, 1024], BF16) for i in range(2)]
        B_kv = Buf(K, dma=True)
        B_S2, B_selm, B_selT, B_diagw, B_st, B_c, B_rec = [Buf(), Buf()], Buf(), Buf(), Buf(), Buf(), Buf(K, dma=True), Buf()
        B_rh = [Buf(), Buf(), Buf()]
        B_pex = [Buf(), Buf(), Buf()]
        B_pm = [Buf(), Buf(), Buf()]
        B_psh = [Buf(), Buf(), Buf()]
        B_psc = [Buf(), Buf()]
        B_ptr = [Buf(), Buf()]
        cnt = {"h": 0, "c": 0, "r": 0, "e": 0, "t": 0}

        agk, agki, agv = C["agk_out"], C["agki_out"], C["agv_out"]
        for r in range(4):
            for g in range(2):
                sp.dma(kT_all[:, g, r, :].bitcast(F32), agk[r * 256 + g * 128:r * 256 + (g + 1) * 128, :], B_kv.dsem,
                       reads=[C["B_ag1o"]], writes=[B_kv])
            for hf in range(2):
                sp.dma(kiT2[hf * 64:(hf + 1) * 64, r, :].bitcast(F32), agki[r * 64:(r + 1) * 64, :], B_kv.dsem,
                       reads=[C["B_ag1o"]], writes=[B_kv])
            for g in range(2):
                sp.dma(V1[:, r, :, g, 0:128],
                       agv.bitcast(BF16)[r * 1024:(r + 1) * 1024, g * 128:(g + 1) * 128].rearrange(
                           "(s p) c -> p s c", p=128),
                       B_kv.dsem, reads=[C["B_ag1o"]], writes=[B_kv])
        pool.op(lambda: nc.gpsimd.memset(V1[:, :, :, :, 128:130], 1.0), writes=[B_kv])
        sp.dma(cmask[:], C["cmask"], B_c.dsem, writes=[B_c])
        for k in range(NIT):
            dve.op(lambda: nc.vector.memset(pw2[:, k:k + 1], 2.0 ** (-k)), writes=[B_c])

        def scores(s):
            S = S2[:, s % 2, :]
            B_S = B_S2[s % 2]
            Lr = (s + 1) * 128
            qs = slice(s * 128, (s + 1) * 128)
            for h in range(16):
                dve.op(lambda: nc.vector.tensor_scalar(out=diagw[:, h, :], in0=ident[:], scalar1=P["wi"][:, s, h:h + 1],
                                                       scalar2=None, op0=ALU.mult),
                       reads=[C["B_wi"], K.B_ident], writes=[B_diagw])
            LA = 2
            units = [(r, c0, min(512, Lr - c0), h) for r in range(4) for c0 in range(0, Lr, 512) for h in range(16)]
            hi_of = {}
            ci_of = {}

            def sc_front(u):
                r, c0, cw, h = units[u]
                hi = cnt["h"] % 3
                cnt["h"] += 1
                hi_of[u] = hi
                p0 = (h % 2) * 64
                pe.op(lambda: nc.tensor.matmul(psh[hi][:, 0:cw], lhsT=P["qiT"][p0:p0 + 64, h // 2, qs],
                                               rhs=kiT2[p0:p0 + 64, r, c0:c0 + cw], start=True, stop=True),
                      reads=[C["B_qiT"], B_kv], writes=[B_psh[hi]])
                act.op(lambda: nc.scalar.activation(out=rh[:, hi, 0:cw], in_=psh[hi][:, 0:cw], func=AF.Relu),
                       reads=[B_psh[hi]], writes=[B_rh[hi]])

            def sc_back(u):
                r, c0, cw, h = units[u]
                hi = hi_of[u]
                if h == 0:
                    ci_of[(r, c0)] = cnt["c"] % 2
                    cnt["c"] += 1
                ci = ci_of[(r, c0)]
                pe.op(lambda: nc.tensor.matmul(psc[ci][:, 0:cw], lhsT=diagw[:, h, :], rhs=rh[:, hi, 0:cw],
                                               start=(h == 0), stop=(h == 15)),
                      reads=[B_diagw, B_rh[hi]], writes=[B_psc[ci]])
                if h == 15:
                    act.op(lambda: nc.scalar.copy(out=S[:, r * Lr + c0:r * Lr + c0 + cw], in_=psc[ci][:, 0:cw]),
                           reads=[B_psc[ci]], writes=[B_S])
            for u in range(min(LA, len(units))):
                sc_front(u)
            for u in range(len(units)):
                if u + LA < len(units):
                    sc_front(u + LA)
                sc_back(u)

        def rest(s):
            S = S2[:, s % 2, :]
            B_S = B_S2[s % 2]
            Lr = (s + 1) * 128
            qs = slice(s * 128, (s + 1) * 128)
            LA = 2
            S3 = S[:, 0:4 * Lr]
            dve.op(lambda: nc.vector.tensor_reduce(out=st[:, 32:33], in_=S3, axis=AX.X, op=ALU.max),
                   reads=[B_S], writes=[B_st])
            dve.op(lambda: nc.vector.tensor_reduce(out=st[:, 33:34], in_=S3, axis=AX.X, op=ALU.min),
                   reads=[B_S], writes=[B_st])
            dve.op(lambda: nc.vector.tensor_scalar(out=st[:, 33:34], in0=st[:, 33:34], scalar1=-1.0, scalar2=None,
                                                   op0=ALU.mult), reads=[B_st], writes=[B_st])
            dve.op(lambda: nc.vector.tensor_tensor(out=st[:, 0:1], in0=st[:, 32:33], in1=st[:, 33:34], op=ALU.max),
                   reads=[B_st], writes=[B_st])
            Sl = S3.rearrange("p (r l) -> p r l", r=4)[:, :, s * 128:(s + 1) * 128]
            dve.op(lambda: nc.vector.tensor_tensor(out=Sl, in0=Sl,
                                                   in1=cmask[:, :].rearrange("p (a b) -> p a b", a=4), op=ALU.add),
                   reads=[B_S, B_c], writes=[B_S])
            topk_threshold(K, S3, 128, selm[:, 0:4 * Lr], st, B_S, B_selm, B_st, pw2, B_c)
            dve.op(lambda: nc.vector.tensor_scalar(out=selm[:, 0:4 * Lr], in0=S3, scalar1=st[:, 1:2], scalar2=None,
                                                   op0=ALU.is_ge), reads=[B_S, B_st], writes=[B_selm])
            if s == 7 and "dbg2" in C:
                B_S.dsem = K.new_sem("d")
                B_st.dsem = B_S.dsem
                sp.dma(C["dbg2"][:, 0:4096], S[:], B_S.dsem, reads=[B_S])
                sp.dma(C["dbg2"][:, 4096:4136], st[:, 0:40], B_S.dsem, reads=[B_st])
            for r in range(4):
                for s0 in range(0, s + 1, 4):
                    nb = min(4, s + 1 - s0)
                    ti = cnt["t"] % 2
                    cnt["t"] += 1
                    pe.pre(reads=[B_selm, K.B_ident], writes=[B_ptr[ti]])
                    for b in range(nb):
                        ins = nc.tensor.transpose(ptr[ti][:, b * 128:(b + 1) * 128],
                                                  selm[:, r * Lr + (s0 + b) * 128:r * Lr + (s0 + b + 1) * 128], ident[:])
                    ev = pe.done(ins)
                    pe.post(ev, reads=[B_selm], writes=[B_ptr[ti]])
                    act.op(lambda: nc.scalar.copy(out=selT[:, r, s0:s0 + nb, :],
                                                  in_=ptr[ti][:, 0:nb * 128].rearrange("p (a b) -> p a b", a=nb)),
                           reads=[B_ptr[ti]], writes=[B_selT])
            tiles = [(r, s1) for r in range(4) for s1 in range(s + 1)]
            nt_ = len(tiles)
            aunits = [(g, n, r, s1) for g in range(2) for n, (r, s1) in enumerate(tiles)]
            bi_of = {}

            def at_front(u):
                g, n, r, s1 = aunits[u]
                hi = cnt["h"] % 3
                cnt["h"] += 1
                ei = cnt["e"] % 3
                cnt["e"] += 1
                bi_of[u] = ei
                pe.op(lambda: nc.tensor.matmul(psh[hi][:, :], lhsT=kT_all[:, g, r, s1 * 128:(s1 + 1) * 128],
                                               rhs=P["qT"][:, g * 4:(g + 1) * 4, qs], start=True, stop=True),
                      reads=[B_kv, C["B_qT"]], writes=[B_psh[hi]])
                act.op(lambda: nc.scalar.activation(out=pex[:, ei, :], in_=psh[hi][:, :], func=AF.Exp, scale=SQ),
                       reads=[B_psh[hi]], writes=[B_pex[ei]])
                eng = dve if u % 2 == 0 else pool
                veng = nc.vector if u % 2 == 0 else nc.gpsimd
                eng.op(lambda: veng.tensor_tensor(
                    out=pm[:, ei, :].rearrange("p (a b) -> p a b", a=4),
                    in0=pex[:, ei, :].rearrange("p (a b) -> p a b", a=4),
                    in1=selT[:, r, s1, :].unsqueeze(1).to_broadcast([128, 4, 128]), op=ALU.mult),
                    reads=[B_pex[ei], B_selT], writes=[B_pm[ei]])

            def at_back(u):
                g, n, r, s1 = aunits[u]
                ei = bi_of[u]
                pe.pre(reads=[B_pm[ei], B_kv], writes=[B_psc[0], B_psc[1]])
                for hh in range(4):
                    po = psc[hh // 3][:, (hh % 3) * 129:(hh % 3) * 129 + 129]
                    ins = nc.tensor.matmul(po, lhsT=pm[:, ei, hh * 128:(hh + 1) * 128], rhs=V1[:, r, s1, g, 0:129],
                                           start=(n == 0 and hh % 3 == 0), stop=(n == nt_ - 1),
                                           skip_group_check=True)
                ev = pe.done(ins)
                pe.post(ev, reads=[B_pm[ei], B_kv], writes=[B_psc[0], B_psc[1]])
                if n == nt_ - 1:
                    for hh in range(4):
                        po = psc[hh // 3][:, (hh % 3) * 129:(hh % 3) * 129 + 129]
                        hcol = g * 4 + hh
                        dve.op(lambda: nc.vector.reciprocal(out=rec[:, hcol:hcol + 1], in_=po[:, 128:129]),
                               reads=[B_psc[hh // 3]], writes=[B_rec])
                        dve.op(lambda: nc.vector.tensor_scalar(out=P["oat"][:, s, hcol * 128:(hcol + 1) * 128],
                                                               in0=po[:, 0:128], scalar1=rec[:, hcol:hcol + 1],
                                                               scalar2=None, op0=ALU.mult),
                               reads=[B_psc[hh // 3], B_rec], writes=[C["B_oat"]])
            for u in range(min(LA, len(aunits))):
                at_front(u)
            for u in range(len(aunits)):
                if u + LA < len(aunits):
                    at_front(u + LA)
                at_back(u)

        scores(0)
        for s in range(8):
            if s + 1 < 8:
                scores(s + 1)
            rest(s)
        K.end_phase()
def phase_bs(K, C):
    nc = K.nc
    pe, act, dve, pool, sp = K.pe, K.act, K.dve, K.pool, K.sp
    P = C["P"]
    ident = C["ident"]
    NS = 16
    LK = 2049
    with ExitStack() as es:
        def sb(n, s, d):
            return es.enter_context(nc.sbuf_tensor("s_" + n, s, d))
        ptb = sb("ptb", [128, 256], I32)
        iop = sb("iop", [128, 1], I32)
        idx = sb("idx", [128, 256], I32)
        B_idx = Buf(K, dma=True)
        sp.dma(ptb[:], C["pt"].rearrange("i p -> (i p)").rearrange("(o n) -> o n", o=1).to_broadcast([128, 256]),
               B_idx.dsem, writes=[B_idx])
        pool.op(lambda: nc.gpsimd.iota(iop[:], pattern=[[0, 1]], base=0, channel_multiplier=1), writes=[B_idx])
        pool.op(lambda: nc.gpsimd.tensor_scalar(out=idx[:], in0=ptb[:], scalar1=128, scalar2=None, op0=ALU.mult),
                reads=[B_idx], writes=[B_idx])
        pool.op(lambda: nc.gpsimd.tensor_tensor(out=idx[:], in0=idx[:], in1=iop[:].to_broadcast([128, 256]), op=ALU.add),
                reads=[B_idx], writes=[B_idx])

        wperm = sb("wperm", [128, 64], BF16)
        wTp = sb("wTp", [32, 2, 128], BF16)
        B_w = Buf()
        Ssmp = sb("Ssmp", [NS, 2176], F32)
        B_Ss = Buf(K, dma=True)
        selms = sb("selms", [NS, 2176], BF16)
        B_selms = Buf()
        selTs = sb("selTs", [128, 16, 16], BF16)
        selfs = sb("selfs", [1, 16], F32)
        B_selT = Buf()
        st = sb("st", [NS, 64], F32)
        B_st = Buf()
        pw2 = sb("pw2", [NS, 32], F32)
        B_c = Buf()
        for k in range(NIT):
            dve.op(lambda: nc.vector.memset(pw2[:, k:k + 1], 2.0 ** (-k)), writes=[B_c])

        with ExitStack() as es1:
            def sb1(n, s, d):
                return es1.enter_context(nc.sbuf_tensor("s1_" + n, s, d))

            def ps1(n, s, d=F32):
                return es1.enter_context(nc.psum_tensor("s1_" + n, s, d))
            kig = sb1("kig", [128, 2, 16, 128], BF16)
            B_kig = [Buf(K, dma=True), Buf(K, dma=True)]
            kiTs = sb1("kiTs", [128, 2, 2048], BF16)
            B_kiTs = [Buf(), Buf()]
            rh = sb1("rh", [32, 2, 2, 512], BF16)
            B_rh = [Buf(), Buf()]
            srow = sb1("srow", [1, 1, 2176], F32)
            B_srow = [Buf(K, dma=True)] * 2
            ptr = [ps1(f"ptr{i}", [128, 1024], BF16) for i in range(2)]
            B_ptr = [Buf(), Buf()]
            psh = [ps1(f"psh{i}", [128, 512]) for i in range(2)]
            pso = [ps1(f"pso{i}", [128, 512]) for i in range(2)]
            B_psh = [Buf(), Buf()]
            pss = [ps1(f"pss{i}", [128, 512]) for i in range(2)]
            B_pss = [Buf(), Buf()]
            dve.op(lambda: nc.vector.memset(wperm[:], 0.0), writes=[B_w])
            wi8 = P["wi"][:, 8, :].rearrange("p (a b) -> p a b", b=2)
            dve.op(lambda: nc.vector.tensor_copy(wperm[:, 0:8], wi8[:, :, 0]), reads=[C["B_wi"]], writes=[B_w])
            dve.op(lambda: nc.vector.tensor_copy(wperm[:, 32:40], wi8[:, :, 1]), reads=[C["B_wi"]], writes=[B_w])
            for e in range(2):
                pe.op(lambda: nc.tensor.transpose(ptr[e][0:32, 0:128], wperm[:, e * 32:(e + 1) * 32], ident[:]),
                      reads=[B_w, K.B_ident], writes=[B_ptr[e]])
                act.op(lambda: nc.scalar.copy(out=wTp[:, e, :], in_=ptr[e][0:32, 0:128]), reads=[B_ptr[e]], writes=[B_w])
            nt = 1
            nh = 0
            for i in range(NS):
                o = i % 2
                tok = 1024 + i
                pool.pre(reads=[B_idx], writes=[B_kig[o]])
                for pg in range(16):
                    ins = nc.gpsimd.indirect_dma_start(
                        out=kig[:, o, pg, 0:64], out_offset=None, in_=C["cache_kidx"],
                        in_offset=bass.IndirectOffsetOnAxis(ap=idx[:, i * 16 + pg:i * 16 + pg + 1], axis=0))
                    B_kig[o].dsem.cnt += 16
                    ins.then_inc(B_kig[o].dsem.h, 16)
                    pool.nins += 1
                K.dma_sems[B_kig[o].dsem.uid] = B_kig[o].dsem
                ev = Ev(B_kig[o].dsem, B_kig[o].dsem.cnt)
                pool.post(ev, writes=[B_kig[o]])
                dve.op(lambda: nc.vector.tensor_copy(kig[:, o, :, 64:128], kig[:, o, :, 0:64]), reads=[B_kig[o]],
                       writes=[B_kig[o]])
                for q4 in range(4):
                    x = nt % 2
                    nt += 1
                    pe.pre(reads=[B_kig[o], K.B_ident], writes=[B_ptr[x]])
                    for b in range(4):
                        ins = nc.tensor.transpose(ptr[x][:, b * 128:(b + 1) * 128], kig[:, o, q4 * 4 + b, :], ident[:])
                    ev = pe.done(ins)
                    pe.post(ev, reads=[B_kig[o]], writes=[B_ptr[x]])
                    act.op(lambda: nc.scalar.copy(out=kiTs[:, o, q4 * 512:(q4 + 1) * 512], in_=ptr[x][:, 0:512]),
                           reads=[B_ptr[x]], writes=[B_kiTs[o]])
                for c in range(5):
                    c0 = c * 512
                    cw = 512 if c < 4 else 1
                    x = nh % 2
                    nh += 1
                    if c < 4:
                        rhs_e, rhs_o = kiTs[0:64, o, c0:c0 + cw], kiTs[64:128, o, c0:c0 + cw]
                        deps = [B_kiTs[o]]
                    else:
                        rhs_e, rhs_o = P["kiT8"][0:64, i:i + 1], P["kiT8"][64:128, i:i + 1]
                        deps = [C["B_s8"]]
                    pe.pre(reads=deps + [C["B_qiT"]], writes=[B_psh[x]])
                    nc.tensor.matmul(psh[x][0:8, 0:cw], lhsT=P["qiT"][0:64, :, tok], rhs=rhs_e, start=True, stop=True)
                    ins = nc.tensor.matmul(pso[x][0:8, 0:cw], lhsT=P["qiT"][64:128, :, tok], rhs=rhs_o, start=True,
                                           stop=True)
                    ev = pe.done(ins)
                    pe.post(ev, reads=deps + [C["B_qiT"]], writes=[B_psh[x]])
                    act.op(lambda: nc.scalar.activation(out=rh[0:8, x, 0, 0:cw], in_=psh[x][0:8, 0:cw], func=AF.Relu),
                           reads=[B_psh[x]], writes=[B_rh[x]])
                    act.op(lambda: nc.scalar.activation(out=rh[0:8, x, 1, 0:cw], in_=pso[x][0:8, 0:cw], func=AF.Relu),
                           reads=[B_psh[x]], writes=[B_rh[x]])
                    pe.pre(reads=[B_rh[x], B_w], writes=[B_pss[x]])
                    nc.tensor.matmul(pss[x][0:1, 0:cw], lhsT=wTp[0:8, 0, i:i + 1], rhs=rh[0:8, x, 0, 0:cw], start=True,
                                     stop=False)
                    ins = nc.tensor.matmul(pss[x][0:1, 0:cw], lhsT=wTp[0:8, 1, i:i + 1], rhs=rh[0:8, x, 1, 0:cw],
                                           start=False, stop=True)
                    ev = pe.done(ins)
                    pe.post(ev, reads=[B_rh[x], B_w], writes=[B_pss[x]])
                    dve.op(lambda: nc.vector.tensor_copy(srow[0:1, 0, c0:c0 + cw], pss[x][0:1, 0:cw]), reads=[B_pss[x]],
                           writes=[B_srow[o]])
                sp.dma(C["sscr"][i:i + 1, 0:LK], srow[0:1, 0, 0:LK], B_srow[o].dsem, reads=[B_srow[o]],
                       writes=[C["B_sscr"]])
            K.barrier()
        sp.dma(Ssmp[:, 0:LK], C["sscr"][:, 0:LK], B_Ss.dsem, reads=[C["B_sscr"]], writes=[B_Ss])
        S3 = Ssmp[:, 0:LK]
        dve.op(lambda: nc.vector.tensor_reduce(out=st[:, 32:33], in_=S3, axis=AX.X, op=ALU.max), reads=[B_Ss],
               writes=[B_st])
        dve.op(lambda: nc.vector.tensor_reduce(out=st[:, 33:34], in_=S3, axis=AX.X, op=ALU.min), reads=[B_Ss],
               writes=[B_st])
        dve.op(lambda: nc.vector.tensor_scalar(out=st[:, 33:34], in0=st[:, 33:34], scalar1=-1.0, scalar2=None,
                                               op0=ALU.mult), reads=[B_st], writes=[B_st])
        dve.op(lambda: nc.vector.tensor_tensor(out=st[:, 0:1], in0=st[:, 32:33], in1=st[:, 33:34], op=ALU.max),
               reads=[B_st], writes=[B_st])
        topk_threshold(K, S3, NS, selms[:, 0:LK], st, B_Ss, B_selms, B_st, pw2, B_c)
        dve.op(lambda: nc.vector.tensor_scalar(out=selms[:, 0:LK], in0=S3, scalar1=st[:, 1:2], scalar2=None,
                                               op0=ALU.is_ge), reads=[B_Ss, B_st], writes=[B_selms])

        with ExitStack() as es2:
            def sb2(n, s, d):
                return es2.enter_context(nc.sbuf_tensor("s2_" + n, s, d))

            def ps2(n, s, d=F32):
                return es2.enter_context(nc.psum_tensor("s2_" + n, s, d))
            Kg = sb2("Kg", [128, 2, 16, 256], BF16)
            B_Kg = [Buf(K, dma=True), Buf(K, dma=True)]
            Vc = sb2("Vc", [128, 1, 16, 256], BF16)
            B_Vc = [Buf(K, dma=True)] * 2
            Vg = sb2("Vg", [128, 2, 16, 2, 130], BF16)
            B_Vg = [Buf(), Buf()]
            kTs = sb2("kTs", [128, 2, 2, 2048], BF16)
            B_kTs = [Buf(), Buf()]
            vself = sb2("vself", [1, 16, 2, 130], BF16)
            B_vs = Buf(K, dma=True)
            pTs = sb2("pTs", [128, 2, 128], BF16)
            B_pTs = [Buf(), Buf()]
            pms = sb2("pms", [128, 2, 128], BF16)
            B_pms = [Buf(), Buf()]
            pself = sb2("pself", [1, 2, 8], BF16)
            pselfm = sb2("pselfm", [1, 2, 8], BF16)
            B_pself = [Buf(), Buf()]
            osm = sb2("osm", [4, 2, 2, 128], F32)
            B_osm = [Buf(K, dma=True), Buf(K, dma=True)]
            rec = sb2("rec", [4, 4], F32)
            B_rec = Buf()
            ptr = [ps2(f"ptr{i}", [128, 1024], BF16) for i in range(2)]
            B_ptr = [Buf(), Buf()]
            pl = [ps2(f"pl{i}", [128, 512]) for i in range(2)]
            B_pl = [Buf(), Buf()]
            psf = ps2("psf", [128, 512])
            B_psf = Buf()
            pos = [ps2(f"pos{i}", [128, 512]) for i in range(2)]
            B_pos = [Buf(), Buf()]
            pe.pre(reads=[B_selms, K.B_ident], writes=[B_ptr[0]])
            for pg in range(16):
                ins = nc.tensor.transpose(ptr[0][:, pg * 16:(pg + 1) * 16], selms[0:NS, pg * 128:(pg + 1) * 128],
                                          ident[0:NS, 0:NS])
            ev = pe.done(ins)
            pe.post(ev, reads=[B_selms], writes=[B_ptr[0]])
            act.op(lambda: nc.scalar.copy(out=selTs[:].rearrange("p a b -> p (a b)"), in_=ptr[0][:, 0:256]),
                   reads=[B_ptr[0]], writes=[B_selT])
            pe.op(lambda: nc.tensor.transpose(ptr[1][0:1, 0:NS], selms[0:NS, 2048:2049], ident[0:NS, 0:NS]),
                  reads=[B_selms, K.B_ident], writes=[B_ptr[1]])
            act.op(lambda: nc.scalar.copy(out=selfs[0:1, :], in_=ptr[1][0:1, 0:NS]), reads=[B_ptr[1]], writes=[B_selT])
            pool.op(lambda: nc.gpsimd.memset(vself[:], 1.0), writes=[B_vs])
            pool.dma(vself[0:1, :, :, 0:128], C["vo"][1024:1040, :].rearrange("(o i) (g d) -> o i g d", o=1, g=2),
                     B_vs.dsem, writes=[B_vs])
            for o in range(2):
                pool.op(lambda: nc.gpsimd.memset(Vg[:, o, :, :, 128:130], 1.0), writes=[B_Vg[o]])
            nt = 0
            for i in range(NS):
                o = i % 2
                tok = 1024 + i
                for (dst, Bd, srcc, oo) in ((Kg, B_Kg, C["cache_k"], o), (Vc, B_Vc, C["cache_v"], 0)):
                    pool.pre(reads=[B_idx], writes=[Bd[o]])
                    for pg in range(16):
                        ins = nc.gpsimd.indirect_dma_start(
                            out=dst[:, oo, pg, :], out_offset=None, in_=srcc,
                            in_offset=bass.IndirectOffsetOnAxis(ap=idx[:, i * 16 + pg:i * 16 + pg + 1], axis=0))
                        Bd[o].dsem.cnt += 16
                        ins.then_inc(Bd[o].dsem.h, 16)
                        pool.nins += 1
                    K.dma_sems[Bd[o].dsem.uid] = Bd[o].dsem
                    ev = Ev(Bd[o].dsem, Bd[o].dsem.cnt)
                    pool.post(ev, writes=[Bd[o]])
                act.op(lambda: nc.scalar.copy(out=Vg[:, o, :, :, 0:128],
                                              in_=Vc[:, 0, :, :].rearrange("p a (g d) -> p a g d", g=2)),
                       reads=[B_Vc[o]], writes=[B_Vg[o]])
                for pg4 in range(8):
                    x = nt % 2
                    nt += 1
                    pe.pre(reads=[B_Kg[o], K.B_ident], writes=[B_ptr[x]])
                    for b in range(4):
                        pg, g = (pg4 * 4 + b) // 2, (pg4 * 4 + b) % 2
                        ins = nc.tensor.transpose(ptr[x][:, b * 128:(b + 1) * 128], Kg[:, o, pg, g * 128:(g + 1) * 128],
                                                  ident[:])
                    ev = pe.done(ins)
                    pe.post(ev, reads=[B_Kg[o]], writes=[B_ptr[x]])
                    dstv = kTs[:, o, :, pg4 * 256:(pg4 + 1) * 256].rearrange("p g (a l) -> p a g l", a=2)
                    srcv = ptr[x][:, 0:512].rearrange("p (a g l) -> p a g l", a=2, g=2)
                    eng, veng = (act, None) if pg4 % 2 == 0 else (dve, None)
                    if pg4 % 2 == 0:
                        act.op(lambda: nc.scalar.copy(out=dstv, in_=srcv), reads=[B_ptr[x]], writes=[B_kTs[o]])
                    else:
                        dve.op(lambda: nc.vector.tensor_copy(dstv, srcv), reads=[B_ptr[x]], writes=[B_kTs[o]])
                pe.pre(reads=[B_kTs[o], C["B_qT"]], writes=[B_pl[o]])
                for pg in range(16):
                    for g in range(2):
                        ins = nc.tensor.matmul(pl[o][:, (pg * 2 + g) * 4:(pg * 2 + g) * 4 + 4],
                                               lhsT=kTs[:, o, g, pg * 128:(pg + 1) * 128],
                                               rhs=P["qT"][:, g * 4:(g + 1) * 4, tok], start=True, stop=True,
                                               skip_group_check=True)
                ev = pe.done(ins)
                pe.post(ev, reads=[B_kTs[o], C["B_qT"]], writes=[B_pl[o]])
                pe.pre(reads=[C["B_s8"], C["B_qT"]], writes=[B_psf])
                for g in range(2):
                    ins = nc.tensor.matmul(psf[0:1, g * 4:(g + 1) * 4], lhsT=P["kT8"][:, g * 128 + i:g * 128 + i + 1],
                                           rhs=P["qT"][:, g * 4:(g + 1) * 4, tok], start=True, stop=True,
                                           skip_group_check=True)
                ev = pe.done(ins)
                pe.post(ev, reads=[C["B_s8"], C["B_qT"]], writes=[B_psf])
                act.op(lambda: nc.scalar.activation(out=pTs[:, o, :], in_=pl[o][:, 0:128], func=AF.Exp, scale=SQ),
                       reads=[B_pl[o]], writes=[B_pTs[o]])
                act.op(lambda: nc.scalar.activation(out=pself[0:1, o, :], in_=psf[0:1, 0:8], func=AF.Exp, scale=SQ),
                       reads=[B_psf], writes=[B_pself[o]])
                dve.op(lambda: nc.vector.tensor_tensor(
                    out=pms[:, o, :].rearrange("p (a b) -> p a b", a=16),
                    in0=pTs[:, o, :].rearrange("p (a b) -> p a b", a=16),
                    in1=selTs[:, :, i].unsqueeze(2).to_broadcast([128, 16, 8]), op=ALU.mult),
                    reads=[B_pTs[o], B_selT], writes=[B_pms[o]])
                dve.op(lambda: nc.vector.tensor_scalar(out=pselfm[0:1, o, :], in0=pself[0:1, o, :],
                                                       scalar1=selfs[0:1, i:i + 1], scalar2=None, op0=ALU.mult),
                       reads=[B_pself[o], B_selT], writes=[B_pself[o]])
                for g in range(2):
                    pe.pre(reads=[B_pms[o], B_Vg[o], B_pself[o], B_vs], writes=[B_pos[g]])
                    for pg in range(16):
                        nc.tensor.matmul(pos[g][0:4, 0:129], lhsT=pms[:, o, (pg * 2 + g) * 4:(pg * 2 + g) * 4 + 4],
                                         rhs=Vg[:, o, pg, g, 0:129], start=(pg == 0), stop=False)
                    ins = nc.tensor.matmul(pos[g][0:4, 0:129], lhsT=pselfm[0:1, o, g * 4:(g + 1) * 4],
                                           rhs=vself[0:1, i, g, 0:129], start=False, stop=True)
                    ev = pe.done(ins)
                    pe.post(ev, reads=[B_pms[o], B_Vg[o], B_pself[o], B_vs], writes=[B_pos[g]])
                    dve.op(lambda: nc.vector.reciprocal(out=rec[0:4, g:g + 1], in_=pos[g][0:4, 128:129]),
                           reads=[B_pos[g]], writes=[B_rec])
                    dve.op(lambda: nc.vector.tensor_scalar(out=osm[0:4, o, g, :], in0=pos[g][0:4, 0:128],
                                                           scalar1=rec[0:4, g:g + 1], scalar2=None, op0=ALU.mult),
                           reads=[B_pos[g], B_rec], writes=[B_osm[o]])
                    sp.dma(C["oscr"][i, g * 512:(g + 1) * 512].rearrange("(h d) -> h d", h=4), osm[0:4, o, g, :],
                           B_osm[o].dsem, reads=[B_osm[o]], writes=[C["B_oscr"]])
            K.barrier()
        pool.dma(P["oat"][0:NS, 8, :], C["oscr"][:, :], C["B_oat"].dsem, reads=[C["B_oscr"]], writes=[C["B_oat"]])
        K.end_phase()
def phase_g1(K, C):
    nc = K.nc
    pe, act, dve, pool, sp = K.pe, K.act, K.dve, K.pool, K.sp
    P = C["P"]
    with ExitStack() as es:
        def sb(n, s, d):
            return es.enter_context(nc.sbuf_tensor("g1_" + n, s, d))

        def ps(n, s, d=F32):
            return es.enter_context(nc.psum_tensor("g1_" + n, s, d))
        triu = sb("triu", [128, 128], F32)
        B_tri = Buf()
        sst = sb("sst", [128, 2, 4, 256], F32)
        B_sst = [Buf(K, dma=True), Buf(K, dma=True)]
        pa = [ps(f"pa{i}", [128, 512]) for i in range(2)]
        B_pa = [Buf(), Buf()]
        pl = [ps(f"pl{i}", [128, 512]) for i in range(2)]
        B_pl = [Buf(), Buf()]
        pool.op(lambda: nc.gpsimd.memset(triu[:], 1.0), writes=[B_tri])
        pool.op(lambda: nc.gpsimd.affine_select(out=triu[:], in_=triu[:], pattern=[[1, 128]], compare_op=ALU.is_ge,
                                                fill=0.0, base=0, channel_multiplier=-1),
                reads=[B_tri], writes=[B_tri])
        sp.dma(C["dec_in"], P["dec"][:].rearrange("p h t -> p (h t)"), C["B_dec"].dsem, reads=[C["B_dec"]],
               writes=[C["B_decin"]])
        all_gather(K, C["dec_in"], C["dec_out"], [C["B_decin"]], [C["B_deco"]])
        n = 0
        for t in range(8):
            ts = slice(t * 128, (t + 1) * 128)
            o = t % 2
            for h in range(4):
                i = n % 2
                n += 1
                pe.op(lambda: nc.tensor.matmul(pa[i][:, 0:128], lhsT=P["kgT"][:, h, ts], rhs=P["qgT"][:, h, ts],
                                               start=True, stop=True),
                      reads=[C["B_kgT"], C["B_qgT"]], writes=[B_pa[i]])
                dve.op(lambda: nc.vector.tensor_tensor(out=P["AT"][:, t, h, :], in0=pa[i][:, 0:128], in1=triu[:],
                                                       op=ALU.mult), reads=[B_pa[i], B_tri], writes=[C["B_AT"]])
                pe.op(lambda: nc.tensor.matmul(pl[i][:, 0:256], lhsT=P["khat"][:, t, h * 128:(h + 1) * 128],
                                               rhs=P["gv"][:, t, h * 256:(h + 1) * 256], start=True, stop=True),
                      reads=[C["B_khat"], C["B_gv"]], writes=[B_pl[i]])
                act.op(lambda: nc.scalar.copy(out=sst[:, o, h, :], in_=pl[i][:, 0:256]), reads=[B_pl[i]],
                       writes=[B_sst[o]])
            sp.dma(C["gst_in"][t].rearrange("(h p) v -> p h v", p=128), sst[:, o], B_sst[o].dsem, reads=[B_sst[o]],
                   writes=[C["B_gin"][t]])
            all_gather(K, C["gst_in"][t], C["gst_out"][t], [C["B_gin"][t]], [C["B_gout"][t]])
        K.end_phase()


def phase_g2(K, C):
    nc = K.nc
    pe, act, dve, pool, sp = K.pe, K.act, K.dve, K.pool, K.sp
    P = C["P"]
    ident = C["ident"]
    with ExitStack() as es:
        def sb(n, s, d):
            return es.enter_context(nc.sbuf_tensor("g2_" + n, s, d))

        def ps(n, s, d=F32):
            return es.enter_context(nc.psum_tensor("g2_" + n, s, d))
        Sg = sb("Sg", [128, 2, 4, 4, 256], F32)
        B_Sg = [Buf(K, dma=True), Buf(K, dma=True)]
        dg = sb("dg", [128, 4, 32], F32)
        B_dg = Buf(K, dma=True)
        oh = sb("oh", [128, 4], F32)
        gnw = sb("gnw", [128, 256], F32)
        B_c = Buf(K, dma=True)
        Srun = sb("Srun", [128, 4, 256], F32)
        B_run = Buf(K, dma=True)
        Sin = sb("Sin", [128, 4, 256], F32)
        B_in = Buf()
        Sinb = sb("Sinb", [128, 4, 256], BF16)
        B_inb = Buf()
        st = sb("st", [128, 16], F32)
        B_st = Buf()
        junk = sb("junk", [128, 256], BF16)
        B_junk = Buf()
        po = [ps(f"po{i}", [128, 512]) for i in range(2)]
        B_po = [Buf(), Buf()]

        sp.dma(oh[:], C["onehot"], B_c.dsem, writes=[B_c])
        sp.dma(gnw[:], C["gla_norm_w"].rearrange("(o d) -> o d", o=1).to_broadcast([128, 256]), B_c.dsem, writes=[B_c])
        sp.dma(dg[:], C["dec_out"].rearrange("(r p) c -> p r c", p=128), B_dg.dsem, reads=[C["B_deco"]], writes=[B_dg])
        dve.op(lambda: nc.vector.memset(Srun[:], 0.0), writes=[B_run])

        def head_norm(pz, B_pz, np_, out_ap, B_out):
            dve.op(lambda: nc.vector.memset(st[0:np_, 0:1], 0.0), writes=[B_st])
            act.op(lambda: nc.scalar.activation(out=junk[0:np_, :], in_=pz, func=AF.Square,
                                                accum_out=st[0:np_, 0:1]), reads=[B_pz, B_st], writes=[B_junk, B_st])
            act.op(lambda: nc.scalar.activation(out=st[0:np_, 1:2], in_=st[0:np_, 0:1], func=AF.Sqrt,
                                                scale=1.0 / 256.0, bias=EPS), reads=[B_st], writes=[B_st])
            dve.op(lambda: nc.vector.reciprocal(out=st[0:np_, 1:2], in_=st[0:np_, 1:2]), reads=[B_st], writes=[B_st])
            dve.op(lambda: nc.vector.scalar_tensor_tensor(out=out_ap, in0=pz, scalar=st[0:np_, 1:2],
                                                          in1=gnw[0:np_, :], op0=ALU.mult, op1=ALU.mult),
                   reads=[B_pz, B_st, B_c], writes=[B_out])

        n = 0
        for s in range(8):
            ts = slice(s * 128, (s + 1) * 128)
            if s == 0:
                sp.dma(Sg[:, 0], C["gst_out"][0].rearrange("(r h p) v -> p r h v", p=128, h=4), B_Sg[0].dsem,
                       reads=[C["B_gout"][0]], writes=[B_Sg[0]])
            if s + 1 < 8:
                sp.dma(Sg[:, (s + 1) % 2], C["gst_out"][s + 1].rearrange("(r h p) v -> p r h v", p=128, h=4),
                       B_Sg[(s + 1) % 2].dsem, reads=[C["B_gout"][s + 1]], writes=[B_Sg[(s + 1) % 2]])
            dve.op(lambda: nc.vector.memset(Sin[:], 0.0), reads=[], writes=[B_in])
            for r in range(4):
                dve.op(lambda: nc.vector.scalar_tensor_tensor(out=Sin[:], in0=Srun[:], scalar=oh[:, r:r + 1],
                                                              in1=Sin[:], op0=ALU.mult, op1=ALU.add),
                       reads=[B_run, B_c, B_in], writes=[B_in])
                for h in range(4):
                    dve.op(lambda: nc.vector.scalar_tensor_tensor(out=Srun[:, h, :], in0=Srun[:, h, :],
                                                                  scalar=dg[:, r, h * 8 + s:h * 8 + s + 1],
                                                                  in1=Sg[:, s % 2, r, h, :], op0=ALU.mult, op1=ALU.add),
                           reads=[B_run, B_dg, B_Sg[s % 2]], writes=[B_run])
            act.op(lambda: nc.scalar.copy(out=Sinb[:], in_=Sin[:]), reads=[B_in], writes=[B_inb])
            for h in range(4):
                i = n % 2
                n += 1
                pe.pre(reads=[C["B_AT"], C["B_gv"], C["B_qgT"], B_inb], writes=[B_po[i]])
                nc.tensor.matmul(po[i][:, 0:256], lhsT=P["AT"][:, s, h, :], rhs=P["gv"][:, s, h * 256:(h + 1) * 256],
                                 start=True, stop=False)
                ins = nc.tensor.matmul(po[i][:, 0:256], lhsT=P["qgT"][:, h, ts], rhs=Sinb[:, h, :],
                                       start=False, stop=True)
                ev = pe.done(ins)
                pe.post(ev, reads=[C["B_AT"], C["B_gv"], C["B_qgT"], B_inb], writes=[B_po[i]])
                head_norm(po[i][:, 0:256], B_po[i], 128, P["og"][:, s, h * 256:(h + 1) * 256], C["B_og"])
        sp.dma(C["gla_p"].rearrange("(h p) v -> p h v", p=128), Srun[:], B_run.dsem, reads=[B_run])

        S0 = sb("S0", [128, 2, 4, 256], F32)
        B_S0 = [Buf(K, dma=True), Buf(K, dma=True)]
        Sn = sb("Sn", [128, 2, 4, 256], F32)
        B_Sn = [Buf(K, dma=True), Buf(K, dma=True)]
        Snb = sb("Snb", [128, 2, 4, 256], BF16)
        B_Snb = [Buf(), Buf()]
        tmp = sb("tmp", [128, 2, 256], F32)
        B_tmp = [Buf(), Buf()]
        Bsel = sb("Bsel", [128, 16, 128], BF16)
        I16 = sb("I16", [128, 16, 16], BF16)
        Qpad = sb("Qpad", [128, 4, 16, 16], BF16)
        B_q = Buf()
        pb = [ps(f"pb{i}", [128, 512]) for i in range(2)]
        B_pb = [Buf(), Buf()]
        pso = [ps(f"pso{i}", [128, 512]) for i in range(2)]
        B_pso = Buf()
        dve.op(lambda: nc.vector.tensor_copy(Bsel[:], ident[:, 0:16].unsqueeze(2).to_broadcast([128, 16, 128])),
               reads=[K.B_ident], writes=[B_q])
        dve.op(lambda: nc.vector.memset(I16[:], 0.0), writes=[B_q])
        for i in range(16):
            dve.op(lambda: nc.vector.memset(I16[:, i, i:i + 1], 1.0), writes=[B_q])
        for h in range(4):
            dve.op(lambda: nc.vector.tensor_tensor(out=Qpad[:, h], in0=P["qgT"][:, h, 1024:1040].unsqueeze(2).to_broadcast(
                [128, 16, 16]), in1=I16[:], op=ALU.mult), reads=[C["B_qgT"], B_q], writes=[B_q])
        stin = C["state_in"].rearrange("(i h p) v -> i p h v", p=128, h=4)
        stout = C["gla_s"].rearrange("(i h p) v -> i p h v", p=128, h=4)
        pe.pre(writes=[B_pso])
        for i in range(16):
            o = i % 2
            sp.dma(S0[:, o], stin[i], B_S0[o].dsem, writes=[B_S0[o]])
            for h in range(4):
                x = n % 2
                n += 1
                pe.op(lambda: nc.tensor.matmul(pb[x][:, 0:256], lhsT=Bsel[:, i, :], rhs=P["gv"][:, 8, h * 256:(h + 1) * 256],
                                               start=True, stop=True), reads=[B_q, C["B_gv"]], writes=[B_pb[x]])
                dve.op(lambda: nc.vector.tensor_scalar(out=tmp[:, x, :], in0=pb[x][:, 0:256],
                                                       scalar1=P["kg8f"][:, h, i:i + 1], scalar2=None, op0=ALU.mult),
                       reads=[B_pb[x], C["B_kgT"]], writes=[B_tmp[x]])
                dve.op(lambda: nc.vector.scalar_tensor_tensor(out=Sn[:, o, h, :], in0=S0[:, o, h, :],
                                                              scalar=P["dec8"][:, h, i:i + 1], in1=tmp[:, x, :],
                                                              op0=ALU.mult, op1=ALU.add),
                       reads=[B_S0[o], B_tmp[x], C["B_dec"]], writes=[B_Sn[o]])
            sp.dma(stout[i], Sn[:, o], B_Sn[o].dsem, reads=[B_Sn[o]])
            act.op(lambda: nc.scalar.copy(out=Snb[:, o], in_=Sn[:, o]), reads=[B_Sn[o]], writes=[B_Snb[o]])
            pe.pre(reads=[B_Snb[o], B_q])
            for h in range(4):
                ins = nc.tensor.matmul(pso[h // 2][0:16, (h % 2) * 256:(h % 2) * 256 + 256], lhsT=Qpad[:, h, i, :],
                                       rhs=Snb[:, o, h, :], start=(i == 0 and h % 2 == 0), stop=(i == 15),
                                       skip_group_check=True)
            ev = pe.done(ins)
            pe.post(ev, reads=[B_Snb[o], B_q], writes=[B_pso])
        for h in range(4):
            head_norm(pso[h // 2][0:16, (h % 2) * 256:(h % 2) * 256 + 256], B_pso, 16,
                      P["og"][0:16, 8, h * 256:(h + 1) * 256], C["B_og"])
        K.end_phase()
def phase_c(K, C):
    nc = K.nc
    pe, act, dve, pool, sp = K.pe, K.act, K.dve, K.pool, K.sp
    ident = C["ident"]
    win_v = C["w_in"].rearrange("(k p) f -> p k f", p=128)
    wpa_v = C["w_proj_attn"].rearrange("(k p) f -> p k f", p=128)
    wpg_v = C["w_proj_gla"].rearrange("(k p) f -> p k f", p=128)
    wo_v = C["w_out"].rearrange("(k p) f -> p k f", p=128)
    with ExitStack() as esm:
        def sbm(n, s, d):
            return esm.enter_context(nc.sbuf_tensor("c_" + n, s, d))
        mT = sbm("mT", [128, 16, TOK], BF16)
        B_mT = [Buf() for _ in range(NT)]
        with ExitStack() as esg:
            merged = esg.enter_context(nc.sbuf_tensor("c_merged", [128, NT, D], BF16))
            B_mg = [Buf() for _ in range(NT)]
            with ExitStack() as es:
                def sb(n, s, d):
                    return es.enter_context(nc.sbuf_tensor("c_" + n, s, d))

                def ps(n, s, d=F32):
                    return es.enter_context(nc.psum_tensor("c_" + n, s, d))
                uT = sb("uT", [128, 16, TOK], BF16)
                B_uT = [Buf() for _ in range(NT)]
                oatT = sb("oatT", [128, 8, TOK], BF16)
                ogT = sb("ogT", [128, 8, TOK], BF16)
                B_oatT = [Buf() for _ in range(NT)]
                B_ogT = [Buf() for _ in range(NT)]
                ptr = [ps(f"ptr{i}", [128, 1024], BF16) for i in range(2)]
                B_ptr = [Buf(), Buf()]
                norm_T(K, C["h1"], C["mix_pre_w"], uT, B_uT, ident, ptr, B_ptr, "c1_")
                pp = [ps(f"pp{i}", [128, 512]) for i in range(4)]
                B_pp = [Buf() for _ in range(4)]
                wg = sb("wg", [128, 2, 16, 512], BF16)
                B_wg = [Buf(K, dma=True), Buf(K, dma=True)]
                wp = sb("wp", [128, 2, 8, 512], BF16)
                B_wp = [Buf(K, dma=True), Buf(K, dma=True)]
                ost = sb("ost", [128, 1, 1024], BF16)
                B_ost = [Buf(K, dma=True)] * 2
                gst = sb("gst", [128, 1, 1024], BF16)
                B_gst = [Buf(K, dma=True)] * 2
                sg = sb("sg", [128, 4, 512], BF16)
                B_sg = [Buf() for _ in range(4)]
                tm = sb("tm", [128, 2, 512], F32)
                B_tm = [Buf(), Buf()]
                npp = [0]
                ntr = [0]

                def tr8(src_slot_ap, B_src, dstT, B_dst, t):
                    for half in range(2):
                        x = ntr[0] % 2
                        ntr[0] += 1
                        pe.pre(reads=[B_src, K.B_ident], writes=[B_ptr[x]])
                        for b in range(4):
                            k = half * 4 + b
                            ins = nc.tensor.transpose(ptr[x][:, b * 128:(b + 1) * 128],
                                                      src_slot_ap[:, k * 128:(k + 1) * 128], ident[:])
                        ev = pe.done(ins)
                        pe.post(ev, reads=[B_src], writes=[B_ptr[x]])
                        act.op(lambda: nc.scalar.copy(out=dstT[:, half * 4:half * 4 + 4, t * 128:(t + 1) * 128],
                                                      in_=ptr[x][:, 0:512].rearrange("p (a b) -> p a b", a=4)),
                               reads=[B_ptr[x]], writes=[B_dst[t]])

                oat_d = C["oat_d"].bitcast(BF16)
                og_d = C["og_d"].bitcast(BF16)
                for t in range(NT):
                    o = t % 2
                    sp.dma(ost[:, 0, :], oat_d[t * 128:(t + 1) * 128, :], B_ost[o].dsem, writes=[B_ost[o]])
                    tr8(ost[:, 0, :], B_ost[o], oatT, B_oatT, t)
                for blk in range(2):
                    pool.dma(wg[:, blk, :, :], win_v[:, :, CGR + blk * 512:CGR + (blk + 1) * 512], B_wg[blk].dsem,
                             writes=[B_wg[blk]])
                for t in range(NT):
                    o = t % 2
                    sp.dma(gst[:, 0, :], og_d[t * 128:(t + 1) * 128, :], B_gst[o].dsem, writes=[B_gst[o]])
                    for blk in range(2):
                        i = npp[0] % 4
                        npp[0] += 1
                        pe.pre(reads=[B_uT[t], B_wg[blk]], writes=[B_pp[i]])
                        for k in range(16):
                            ins = nc.tensor.matmul(pp[i][:, :], lhsT=uT[:, k, t * 128:(t + 1) * 128], rhs=wg[:, blk, k, :],
                                                   start=(k == 0), stop=(k == 15))
                        ev = pe.done(ins)
                        pe.post(ev, reads=[B_uT[t], B_wg[blk]], writes=[B_pp[i]])
                        act.op(lambda: nc.scalar.activation(out=sg[:, i, :], in_=pp[i][:, :], func=AF.Silu),
                               reads=[B_pp[i]], writes=[B_sg[i]])
                        dve.op(lambda: nc.vector.tensor_tensor(out=gst[:, 0, blk * 512:(blk + 1) * 512],
                                                               in0=gst[:, 0, blk * 512:(blk + 1) * 512], in1=sg[:, i, :],
                                                               op=ALU.mult), reads=[B_sg[i], B_gst[o]], writes=[B_gst[o]])
                    tr8(gst[:, 0, :], B_gst[o], ogT, B_ogT, t)
                B_wgb = [[Buf(K, dma=True), Buf(K, dma=True)] for _ in range(2)]
                B_wpb = [[Buf(K, dma=True), Buf(K, dma=True)] for _ in range(2)]
                NB = D // 256

                def load_blk(nb):
                    b = nb % 2
                    bs_ = slice(b * 256, (b + 1) * 256)
                    c0 = nb * 256
                    pool.dma(wg[:, 0, :, bs_], win_v[:, :, CGA + c0:CGA + c0 + 256], B_wgb[0][b].dsem,
                             writes=[B_wgb[0][b], B_wg[0]])
                    pool.dma(wg[:, 1, :, bs_], win_v[:, :, CGG + c0:CGG + c0 + 256], B_wgb[1][b].dsem,
                             writes=[B_wgb[1][b], B_wg[1]])
                    pool.dma(wp[:, 0, :, bs_], wpa_v[:, :, c0:c0 + 256], B_wpb[0][b].dsem, writes=[B_wpb[0][b]])
                    pool.dma(wp[:, 1, :, bs_], wpg_v[:, :, c0:c0 + 256], B_wpb[1][b].dsem, writes=[B_wpb[1][b]])
                load_blk(0)
                for nb in range(NB):
                    b = nb % 2
                    bs_ = slice(b * 256, (b + 1) * 256)
                    cs = slice(nb * 256, (nb + 1) * 256)
                    if nb + 1 < NB:
                        load_blk(nb + 1)
                    for t in range(NT):
                        ts = slice(t * 128, (t + 1) * 128)
                        ids = []
                        for which in range(4):
                            i = npp[0] % 4
                            npp[0] += 1
                            ids.append(i)
                            if which < 2:
                                srcT, Bs, w, Bw, nk = uT, B_uT, wg[:, which], B_wgb[which][b], 16
                            elif which == 2:
                                srcT, Bs, w, Bw, nk = oatT, B_oatT, wp[:, 0], B_wpb[0][b], 8
                            else:
                                srcT, Bs, w, Bw, nk = ogT, B_ogT, wp[:, 1], B_wpb[1][b], 8
                            pe.pre(reads=[Bs[t], Bw], writes=[B_pp[i]])
                            for k in range(nk):
                                ins = nc.tensor.matmul(pp[i][:, 0:256], lhsT=srcT[:, k, ts], rhs=w[:, k, bs_],
                                                       start=(k == 0), stop=(k == nk - 1))
                            ev = pe.done(ins)
                            pe.post(ev, reads=[Bs[t], Bw], writes=[B_pp[i]])
                            if which < 2:
                                act.op(lambda: nc.scalar.activation(out=sg[:, i, 0:256], in_=pp[i][:, 0:256],
                                                                    func=AF.Sigmoid),
                                       reads=[B_pp[i]], writes=[B_sg[i]])
                        ia, ig, ipa, ipg = ids
                        x = t % 2
                        dve.op(lambda: nc.vector.tensor_tensor(out=tm[:, x, 0:256], in0=sg[:, ia, 0:256],
                                                               in1=pp[ipa][:, 0:256], op=ALU.mult),
                               reads=[B_sg[ia], B_pp[ipa]], writes=[B_tm[x]])
                        dve.op(lambda: nc.vector.tensor_tensor(out=sg[:, ig, 0:256], in0=sg[:, ig, 0:256],
                                                               in1=pp[ipg][:, 0:256], op=ALU.mult),
                               reads=[B_sg[ig], B_pp[ipg]], writes=[B_sg[ig]])
                        pool.op(lambda: nc.gpsimd.tensor_tensor(out=merged[:, t, cs], in0=tm[:, x, 0:256],
                                                                in1=sg[:, ig, 0:256], op=ALU.add),
                                reads=[B_tm[x], B_sg[ig]], writes=[B_mg[t]])
                K.barrier()
            with ExitStack() as es:
                ptr = [es.enter_context(nc.psum_tensor(f"c2_ptr{i}", [128, 1024], BF16)) for i in range(2)]
                B_ptr = [Buf(), Buf()]
                n = 0
                for t in range(NT):
                    for q4 in range(4):
                        x = n % 2
                        n += 1
                        pe.pre(reads=[B_mg[t], K.B_ident], writes=[B_ptr[x]])
                        for b in range(4):
                            k = q4 * 4 + b
                            ins = nc.tensor.transpose(ptr[x][:, b * 128:(b + 1) * 128], merged[:, t, k * 128:(k + 1) * 128],
                                                      ident[:])
                        ev = pe.done(ins)
                        pe.post(ev, reads=[B_mg[t]], writes=[B_ptr[x]])
                        if q4 % 2 == 0:
                            act.op(lambda: nc.scalar.copy(out=mT[:, q4 * 4:q4 * 4 + 4, t * 128:(t + 1) * 128],
                                                          in_=ptr[x][:, 0:512].rearrange("p (a b) -> p a b", a=4)),
                                   reads=[B_ptr[x]], writes=[B_mT[t]])
                        else:
                            dve.op(lambda: nc.vector.tensor_copy(mT[:, q4 * 4:q4 * 4 + 4, t * 128:(t + 1) * 128],
                                                                 ptr[x][:, 0:512].rearrange("p (a b) -> p a b", a=4)),
                                   reads=[B_ptr[x]], writes=[B_mT[t]])
                K.barrier()
        with ExitStack() as es:
            def sb(n, s, d):
                return es.enter_context(nc.sbuf_tensor("c3_" + n, s, d))
            wo = sb("wo", [128, 16, D], BF16)
            B_wo = Buf(K, dma=True)
            wbc = sb("wbc", [128, D], F32)
            hst = sb("hst", [128, 2, D], F32)
            B_hst = [Buf(K, dma=True), Buf(K, dma=True)]
            ot = sb("ot", [128, 2, D], F32)
            B_ot = [Buf(K, dma=True), Buf(K, dma=True)]
            junk = sb("junk", [128, 512], BF16)
            B_junk = Buf()
            st = sb("st", [128, 8 * NT], F32)
            B_st = Buf()
            po = [es.enter_context(nc.psum_tensor(f"c3_po{i}", [128, 512])) for i in range(8)]
            B_po = [Buf() for _ in range(8)]
            for nb in range(4):
                pool.dma(wo[:, :, nb * 512:(nb + 1) * 512], wo_v[:, :, nb * 512:(nb + 1) * 512], B_wo.dsem, writes=[B_wo])
            sp.dma(wbc[:], C["mix_post_w"].rearrange("(o d) -> o d", o=1).to_broadcast([128, D]), B_wo.dsem,
                   writes=[B_wo])
            dve.op(lambda: nc.vector.memset(st[:], 0.0), writes=[B_st])
            for t in range(NT):
                o = t % 2
                ts = slice(t * 128, (t + 1) * 128)
                sp.dma(hst[:, o, :], C["h1"][ts, :], B_hst[o].dsem, writes=[B_hst[o]])
                for nb in range(4):
                    i = o * 4 + nb
                    pe.pre(reads=[B_mT[t], B_wo], writes=[B_po[i]])
                    for k in range(16):
                        ins = nc.tensor.matmul(po[i][:, :], lhsT=mT[:, k, ts], rhs=wo[:, k, nb * 512:(nb + 1) * 512],
                                               start=(k == 0), stop=(k == 15))
                    ev = pe.done(ins)
                    pe.post(ev, reads=[B_mT[t], B_wo], writes=[B_po[i]])
                    act.op(lambda: nc.scalar.activation(out=junk[:], in_=po[i][:, :], func=AF.Square,
                                                        accum_out=st[:, t * 8 + nb:t * 8 + nb + 1]),
                           reads=[B_po[i], B_st], writes=[B_junk, B_st])
                c = t * 8
                dve.op(lambda: nc.vector.tensor_reduce(out=st[:, c + 4:c + 5], in_=st[:, c:c + 4], axis=AX.X, op=ALU.add),
                       reads=[B_st], writes=[B_st])
                act.op(lambda: nc.scalar.activation(out=st[:, c + 5:c + 6], in_=st[:, c + 4:c + 5], func=AF.Sqrt,
                                                    scale=1.0 / D, bias=EPS), reads=[B_st], writes=[B_st])
                dve.op(lambda: nc.vector.reciprocal(out=st[:, c + 5:c + 6], in_=st[:, c + 5:c + 6]), reads=[B_st],
                       writes=[B_st])
                for nb in range(4):
                    i = o * 4 + nb
                    cs = slice(nb * 512, (nb + 1) * 512)
                    dve.op(lambda: nc.vector.scalar_tensor_tensor(out=ot[:, o, cs], in0=po[i][:, :],
                                                                  scalar=st[:, c + 5:c + 6], in1=wbc[:, cs],
                                                                  op0=ALU.mult, op1=ALU.mult),
                           reads=[B_po[i], B_st, B_wo], writes=[B_ot[o]])
                pool.op(lambda: nc.gpsimd.tensor_tensor(out=ot[:, o, :], in0=ot[:, o, :], in1=hst[:, o, :], op=ALU.add),
                        reads=[B_hst[o], B_ot[o]], writes=[B_ot[o]])
                sp.dma(C["h2"][ts, :], ot[:, o, :], B_ot[o].dsem, reads=[B_ot[o]])
            K.barrier()


WNAMES = [("ffn1_pre_w", [D]), ("ffn1_w_gate", [D, DFF]), ("ffn1_w_up", [D, DFF]), ("ffn1_w_down", [DFF, D]),
          ("ffn1_post_w", [D]), ("mix_pre_w", [D]), ("w_in", [D, DIN]), ("gla_gate_w2", [16, 512]),
          ("gla_gate_b", [512]), ("gla_norm_w", [256]), ("w_proj_attn", [1024, D]), ("w_proj_gla", [1024, D]),
          ("w_out", [D, D]), ("mix_post_w", [D]), ("ffn2_pre_w", [D]), ("ffn2_w_gate", [D, DFF]),
          ("ffn2_w_up", [D, DFF]), ("ffn2_w_down", [DFF, D]), ("ffn2_post_w", [D])]
NPOOL_ROWS = 2560 * 128


def build(stage=99, debug=False):
    K = Kern()
    nc = K.nc
    C = {}
    full = stage >= 4
    x = K.dram("x", [TOK, D], F32, "ExternalInput").ap()
    C["posv"] = K.dram("posv", [128, NT], F32, "ExternalInput").ap()
    K.used = WNAMES[:5] if stage == 1 else (WNAMES[:9] if stage in (2, 3) else WNAMES)
    for name, shape in K.used:
        C[name] = K.dram(name, shape, F32, "ExternalInput").ap()
    y = K.dram("y", [TOK, D], F32, "ExternalOutput").ap()
    C["ko"] = K.dram("ko", [TOK, 256], F32, "ExternalOutput").ap()
    C["vo"] = K.dram("vo", [TOK, 256], F32, "ExternalOutput").ap()
    C["kio"] = K.dram("kio", [TOK, 64], F32, "ExternalOutput").ap()
    dk = "ExternalOutput" if debug else "Internal"
    C["h1"] = K.dram("h1s", [TOK, D], F32, dk).ap()
    C["h2"] = K.dram("h2s", [TOK, D], F32, dk).ap()
    C["cmask"] = K.dram("cmask", [128, 512], F32, "ExternalInput").ap()
    if stage == 3:
        C["dbg"] = K.dram("dbg", [TOK, 1024], F32, "ExternalOutput").ap()
        C["dbg2"] = K.dram("dbg2", [128, 4136], F32, "ExternalOutput").ap()
    for nm, r, c in (("agk", 256, 512), ("agki", 64, 512), ("agv", 1024, 128), ("dec", 128, 32)):
        C[nm + "_in"] = K.dram(nm + "_in", [r, c], F32).ap()
        C[nm + "_out"] = K.dram(nm + "_out", [4 * r, c], F32).ap()
    if full:
        C["onehot"] = K.dram("onehot", [128, 4], F32, "ExternalInput").ap()
        C["pt"] = K.dram("pt", [16, 16], I32, "ExternalInput").ap()
        C["state_in"] = K.dram("state_in", [16 * 512, 256], F32, "ExternalInput").ap()
        C["cache_k"] = K.dram("cache_k", [NPOOL_ROWS, 256], F32, "ExternalInput").ap()
        C["cache_v"] = K.dram("cache_v", [NPOOL_ROWS, 256], F32, "ExternalInput").ap()
        C["cache_kidx"] = K.dram("cache_kidx", [NPOOL_ROWS, 64], F32, "ExternalInput").ap()
        C["gla_p"] = K.dram("gla_p", [512, 256], F32, "ExternalOutput").ap()
        C["gla_s"] = K.dram("gla_s", [16 * 512, 256], F32, "ExternalOutput").ap()
        C["gst_in"] = [K.dram(f"gst_in{t}", [512, 256], F32).ap() for t in range(8)]
        C["gst_out"] = [K.dram(f"gst_out{t}", [2048, 256], F32).ap() for t in range(8)]
        C["sscr"] = K.dram("sscr", [16, 2176], F32).ap()
        C["oscr"] = K.dram("oscr", [16, 1024], F32).ap()
        C["oat_d"] = K.dram("oat_d", [TOK, 512], F32, dk).ap()
        C["og_d"] = K.dram("og_d", [TOK, 512], F32, dk).ap()

    with ExitStack() as es0:
        ident = es0.enter_context(nc.sbuf_tensor("ident", [128, 128], BF16))
        C["ident"] = ident
        K.B_ident = Buf()
        K.pool.op(lambda: nc.gpsimd.memset(ident[:], 1.0), writes=[K.B_ident])
        K.pool.op(lambda: nc.gpsimd.affine_select(out=ident[:], in_=ident[:], pattern=[[-1, 128]],
                                                  compare_op=ALU.is_equal, fill=0.0, base=0, channel_multiplier=1),
                  reads=[K.B_ident], writes=[K.B_ident])
        K.barrier()

        ffn_phase(K, x, (y if stage == 1 else C["h1"]), C["ffn1_pre_w"], C["ffn1_w_gate"], C["ffn1_w_up"],
                  C["ffn1_w_down"], C["ffn1_post_w"], ident)
        if stage >= 2:
            with ExitStack() as es:
                def sb(n, s, d):
                    return es.enter_context(nc.sbuf_tensor(n, s, d))
                P = {}
                P["qT"] = sb("p_qT", [128, 8, TOK], BF16)
                P["qiT"] = sb("p_qiT", [128, 8, TOK], BF16)
                P["wi"] = sb("p_wi", [128, NT, 16], F32)
                P["qgT"] = sb("p_qgT", [128, 4, TOK], BF16)
                P["gv"] = sb("p_gv", [128, NT, 1024], BF16)
                P["dec"] = sb("p_dec", [128, 4, 8], F32)
                P["dec8"] = sb("p_dec8", [128, 4, 128], F32)
                P["kg8f"] = sb("p_kg8f", [128, 4, 128], F32)
                P["kT8"] = sb("p_kT8", [128, 256], BF16)
                P["v8"] = sb("p_v8", [128, 256], BF16)
                P["kiT8"] = sb("p_kiT8", [128, 128], BF16)
                if full:
                    P["AT"] = sb("p_AT", [128, 8, 4, 128], BF16)
                C["P"] = P
                for n in ("B_qT", "B_qiT", "B_wi", "B_qgT", "B_kgT", "B_khat", "B_gv", "B_s8", "B_AT", "B_og",
                          "B_ag1", "B_ag1o", "B_decin", "B_deco", "B_sscr", "B_oscr"):
                    C[n] = Buf()
                C["B_dec"] = Buf(K, dma=True, persist=True)
                C["B_gin"] = [Buf() for _ in range(8)]
                C["B_gout"] = [Buf() for _ in range(8)]
                with ExitStack() as esk:
                    P["kgT"] = esk.enter_context(nc.sbuf_tensor("p_kgT", [128, 4, TOK], BF16))
                    P["khat"] = esk.enter_context(nc.sbuf_tensor("p_khat", [128, NT, 512], BF16))
                    phase_a(K, C)
                    if full:
                        phase_g1(K, C)
                if stage >= 3:
                    P["oat"] = sb("p_oat", [128, NT, 1024], BF16)
                    C["B_oat"] = Buf(K, dma=True, persist=True)
                    K.dve.op(lambda: nc.vector.memset(P["oat"][:, 8, :], 0.0), writes=[C["B_oat"]])
                    phase_b(K, C)
                if stage == 3:
                    K.pool.dma(C["dbg"].rearrange("(t p) c -> p t c", p=128), P["oat"][:], C["B_oat"].dsem,
                               reads=[C["B_oat"]])
                if full:
                    phase_bs(K, C)
                    K.sp.dma(C["oat_d"].bitcast(BF16).rearrange("(t p) c -> p t c", p=128), P["oat"][:],
                             C["B_oat"].dsem, reads=[C["B_oat"]])
                    P["og"] = sb("p_og", [128, NT, 1024], BF16)
                    C["B_og"] = Buf(K, dma=True, persist=True)
                    K.dve.op(lambda: nc.vector.memset(P["og"][:, 8, :], 0.0), writes=[C["B_og"]])
                    phase_g2(K, C)
                    K.sp.dma(C["og_d"].bitcast(BF16).rearrange("(t p) c -> p t c", p=128), P["og"][:],
                             C["B_og"].dsem, reads=[C["B_og"]])
                K.barrier()
            if full:
                phase_c(K, C)
                ffn_phase(K, C["h2"], y, C["ffn2_pre_w"], C["ffn2_w_gate"], C["ffn2_w_up"], C["ffn2_w_down"],
                          C["ffn2_post_w"], ident, tag="f2")
    K.barrier()
    K.es.close()
    return K


def core_rows(x_prompt, x_sample, c):
    b, j = c // 4, c % 4
    rows = [x_prompt[b, (4 * s + j) * 128:(4 * s + j + 1) * 128] for s in range(8)]
    pad = np.zeros((128, x_sample.shape[-1]), np.float32)
    pad[:16] = x_sample[16 * c:16 * c + 16, 0]
    rows.append(pad)
    return np.ascontiguousarray(np.concatenate(rows, 0))


def make_in_maps(inputs, used=WNAMES, full=True):
    in_maps = []
    shared = {k: np.ascontiguousarray(inputs[k], dtype=np.float32) for k, _ in used}
    if full:
        shared["cache_k"] = np.ascontiguousarray(inputs["cache_k"]).reshape(NPOOL_ROWS, 256)
        shared["cache_v"] = np.ascontiguousarray(inputs["cache_v"]).reshape(NPOOL_ROWS, 256)
        shared["cache_kidx"] = np.ascontiguousarray(inputs["cache_kidx"]).reshape(NPOOL_ROWS, 64)
    for c in range(NCORES):
        j = c % 4
        m = {"x": core_rows(inputs["x_prompt"], inputs["x_sample"], c)}
        posv = np.zeros((128, NT), np.float32)
        for s in range(8):
            posv[:, s] = (4 * s + j) * 128 + np.arange(128)
        posv[:, 8] = 2048.0
        m["posv"] = posv
        cm = np.zeros((128, 4, 128), np.float32)
        for r in range(4):
            if r > j:
                cm[:, r, :] = -1.0e4
            elif r == j:
                cm[:, r, :] = np.where(np.arange(128)[None, :] > np.arange(128)[:, None], -1.0e4, 0.0)
        m["cmask"] = cm.reshape(128, 512)
        if full:
            oh = np.zeros((128, 4), np.float32)
            oh[:, j] = 1.0
            m["onehot"] = oh
            m["pt"] = np.ascontiguousarray(inputs["page_table"][16 * c:16 * c + 16], dtype=np.int32)
            m["state_in"] = np.ascontiguousarray(inputs["state_gla"][16 * c:16 * c + 16], dtype=np.float32).reshape(
                16 * 512, 256)
        m.update(shared)
        in_maps.append(m)
    return in_maps


def assemble(results):
    y_p = np.zeros((2, 4096, D), np.float32)
    y_s = np.zeros((128, 1, D), np.float32)
    k_p = np.zeros((2, 4096, 2, 128), np.float32)
    v_p = np.zeros((2, 4096, 2, 128), np.float32)
    ki_p = np.zeros((2, 4096, 64), np.float32)
    k_s = np.zeros((128, 1, 2, 128), np.float32)
    v_s = np.zeros((128, 1, 2, 128), np.float32)
    ki_s = np.zeros((128, 1, 64), np.float32)
    gla_p = np.zeros((2, 4, 128, 256), np.float32)
    gla_s = np.zeros((128, 4, 128, 256), np.float32)
    for c, r in enumerate(results):
        b, j = c // 4, c % 4
        for s in range(8):
            sl = slice((4 * s + j) * 128, (4 * s + j + 1) * 128)
            rs = slice(s * 128, (s + 1) * 128)
            y_p[b, sl] = r["y"][rs]
            k_p[b, sl] = r["ko"][rs].reshape(128, 2, 128)
            v_p[b, sl] = r["vo"][rs].reshape(128, 2, 128)
            ki_p[b, sl] = r["kio"][rs]
        ss = slice(16 * c, 16 * c + 16)
        y_s[ss, 0] = r["y"][1024:1040]
        k_s[ss, 0] = r["ko"][1024:1040].reshape(16, 2, 128)
        v_s[ss, 0] = r["vo"][1024:1040].reshape(16, 2, 128)
        ki_s[ss, 0] = r["kio"][1024:1040]
        gla_s[ss] = r["gla_s"].reshape(16, 4, 128, 256)
        if j == 0:
            gla_p[b] = r["gla_p"].reshape(4, 128, 256)
    return (y_p, y_s, k_p, v_p, ki_p, gla_p, k_s, v_s, ki_s, gla_s)


def kernel(**inputs):
    K = build()
    in_maps = make_in_maps(inputs, K.used, True)
    res = run_bass_kernel_spmd(K.nc, in_maps, core_ids=list(range(NCORES)))
    return assemble(res.results)
```
